# Optimizing a Trainium2 kernel written in Bass

```python
import math
import jax, jax.numpy as jnp
from jax import lax
import numpy as np

D_MODEL = 1024
BATCH = 8
SEQ = 4096
DEPTH = 2

CHUNK = 64
N_MIXERS = 2
N_SSD_LAYERS = (DEPTH + 1) // 2
N_MLA_LAYERS = DEPTH // 2

SSD_EXPAND = 2
SSD_D_INNER = SSD_EXPAND * D_MODEL
SSD_HEAD_DIM = 64
SSD_N_HEADS = SSD_D_INNER // SSD_HEAD_DIM
SSD_N_GROUPS = 8
SSD_HEADS_PER_GROUP = SSD_N_HEADS // SSD_N_GROUPS
SSD_D_STATE = 128
SSD_CONV_W = 4
SSD_BC_DIM = SSD_N_GROUPS * SSD_D_STATE
SSD_CONV_DIM = SSD_D_INNER + 2 * SSD_BC_DIM
SSD_IN_DIM = SSD_D_INNER + SSD_CONV_DIM + SSD_N_HEADS

MLA_N_HEADS = 16
MLA_Q_RANK = 384
MLA_KV_RANK = 256
MLA_NOPE = 64
MLA_ROPE = 32
MLA_V = 64
MLA_DOWN_DIM = MLA_Q_RANK + MLA_KV_RANK + MLA_ROPE
ROPE_THETA = 10000.0
Q_BLOCK = 128

N_EXPERTS = 16
N_EXPERT_GROUPS = 4
EXPERTS_PER_GROUP = N_EXPERTS // N_EXPERT_GROUPS
TOP_K = 2
D_FF_EXPERT = 512

DEEPNORM_ALPHA = (2.0 * DEPTH) ** 0.25
DEEPNORM_BETA = (8.0 * DEPTH) ** -0.25
LN_EPS = 1e-5
RMS_EPS = 1e-6

kernel_name = "hybrid_ssd_mla_grouped_moe_deepnorm"

F32 = jnp.float32


def layer_norm(x, g, b):
    xf = x.astype(F32)
    mu = jnp.mean(xf, -1, keepdims=True)
    var = jnp.mean(jnp.square(xf - mu), -1, keepdims=True)
    return ((xf - mu) * lax.rsqrt(var + LN_EPS) * g.astype(F32) + b.astype(F32)).astype(x.dtype)


def rms_norm(x, w):
    xf = x.astype(F32)
    y = xf * lax.rsqrt(jnp.mean(xf * xf, -1, keepdims=True) + RMS_EPS)
    return (y * w.astype(F32)).astype(x.dtype)


def causal_depthwise_conv(x, w, b):
    c = x.shape[-1]
    y = lax.conv_general_dilated(x, w[:, None, :], window_strides=(1,),
                                 padding=[(SSD_CONV_W - 1, 0)],
                                 dimension_numbers=("NWC", "WIO", "NWC"),
                                 feature_group_count=c)
    return y + b


def ssd_chunked_scan(xdt, a, b, c):
    bsz, s = xdt.shape[:2]
    nc = s // CHUNK

    def to_chunks(t):
        return jnp.moveaxis(t.astype(F32).reshape(bsz, nc, CHUNK, *t.shape[2:]), 1, 0)

    xs = (to_chunks(xdt), to_chunks(a), to_chunks(b), to_chunks(c))
    causal = jnp.tril(jnp.ones((CHUNK, CHUNK), bool))[None, :, :, None, None]

    def step(state, inp):
        x_c, a_c, b_c, c_c = inp
        a_cum = jnp.cumsum(a_c, axis=1)
        seg = a_cum[:, :, None] - a_cum[:, None, :]
        decay = jnp.exp(jnp.where(causal, seg, -jnp.inf))
        scores = jnp.einsum("blgn,bsgn->blsg", c_c, b_c)
        y_diag = jnp.einsum("blsg,blsgr,bsgrp->blgrp", scores, decay, x_c)
        y_off = jnp.einsum("blgn,bgrpn,blgr->blgrp", c_c, state, jnp.exp(a_cum))
        to_end = jnp.exp(a_cum[:, -1:] - a_cum)
        new_state = (state * jnp.exp(a_cum[:, -1])[..., None, None]
                     + jnp.einsum("blgn,blgr,blgrp->bgrpn", b_c, to_end, x_c))
        return new_state, y_diag + y_off

    state0 = jnp.zeros((bsz, SSD_N_GROUPS, SSD_HEADS_PER_GROUP, SSD_HEAD_DIM, SSD_D_STATE), F32)
    _, ys = lax.scan(step, state0, xs)
    return jnp.moveaxis(ys, 0, 1).reshape(xdt.shape)


def ssd_mixer(x, w_in, conv_w, conv_b, dt_bias, a_log, d_skip, norm_w, w_out):
    bsz, s, _ = x.shape
    g, r, p, n = SSD_N_GROUPS, SSD_HEADS_PER_GROUP, SSD_HEAD_DIM, SSD_D_STATE
    proj = x @ w_in
    z, xbc, dt = jnp.split(proj, [SSD_D_INNER, SSD_D_INNER + SSD_CONV_DIM], axis=-1)
    xbc = jax.nn.silu(causal_depthwise_conv(xbc, conv_w, conv_b))
    xs, bm, cm = jnp.split(xbc, [SSD_D_INNER, SSD_D_INNER + SSD_BC_DIM], axis=-1)
    xs = xs.astype(F32).reshape(bsz, s, g, r, p)
    bm = bm.reshape(bsz, s, g, n)
    cm = cm.reshape(bsz, s, g, n)
    dt = jax.nn.softplus(dt.astype(F32) + dt_bias.astype(F32)).reshape(bsz, s, g, r)
    a = -jnp.exp(a_log.astype(F32)).reshape(g, r)
    y = ssd_chunked_scan(xs * dt[..., None], dt * a, bm, cm)
    y = y + xs * d_skip.astype(F32).reshape(g, r, 1)
    y = (y.reshape(bsz, s, SSD_D_INNER) * jax.nn.silu(z.astype(F32))).reshape(bsz, s, g, -1)
    y = y * lax.rsqrt(jnp.mean(y * y, -1, keepdims=True) + RMS_EPS)
    y = y.reshape(bsz, s, SSD_D_INNER) * norm_w.astype(F32)
    return y.astype(x.dtype) @ w_out


def rope_tables(s):
    inv = ROPE_THETA ** (-jnp.arange(0, MLA_ROPE, 2, dtype=F32) / MLA_ROPE)
    ang = jnp.arange(s, dtype=F32)[:, None] * inv[None, :]
    return jnp.cos(ang), jnp.sin(ang)


def apply_rope(t, cos, sin):
    tf = t.astype(F32)
    t1, t2 = jnp.split(tf, 2, axis=-1)
    extra = t.ndim - 3
    cos = cos.reshape(cos.shape[0], *([1] * extra), cos.shape[1])
    sin = sin.reshape(sin.shape[0], *([1] * extra), sin.shape[1])
    return jnp.concatenate([t1 * cos - t2 * sin, t1 * sin + t2 * cos], -1).astype(t.dtype)


def mla_mixer(x, w_down, q_norm_w, w_uq, kv_norm_w, w_ukv, w_out):
    bsz, s, _ = x.shape
    h = MLA_N_HEADS
    down = x @ w_down
    c_q, c_kv, k_rope = jnp.split(down, [MLA_Q_RANK, MLA_Q_RANK + MLA_KV_RANK], axis=-1)
    q = (rms_norm(c_q, q_norm_w) @ w_uq).reshape(bsz, s, h, MLA_NOPE + MLA_ROPE)
    kv = (rms_norm(c_kv, kv_norm_w) @ w_ukv).reshape(bsz, s, h, MLA_NOPE + MLA_V)
    q_nope, q_rope = jnp.split(q, [MLA_NOPE], axis=-1)
    k_nope, v = jnp.split(kv, [MLA_NOPE], axis=-1)
    cos, sin = rope_tables(s)
    q_rope = apply_rope(q_rope, cos, sin)
    k_rope = apply_rope(k_rope, cos, sin)
    scale = 1.0 / math.sqrt(MLA_NOPE + MLA_ROPE)
    nblk = s // Q_BLOCK
    qn_blocks = jnp.moveaxis(q_nope.reshape(bsz, nblk, Q_BLOCK, h, MLA_NOPE), 1, 0)
    qr_blocks = jnp.moveaxis(q_rope.reshape(bsz, nblk, Q_BLOCK, h, MLA_ROPE), 1, 0)
    k_chunk = jnp.arange(s) // CHUNK

    def attend(args):
        qn, qr, blk = args
        q_chunk = (blk * Q_BLOCK + jnp.arange(Q_BLOCK)) // CHUNK
        mask = k_chunk[None, :] <= q_chunk[:, None]
        scores = (jnp.einsum("bqhd,bkhd->bhqk", qn, k_nope, preferred_element_type=F32)
                  + jnp.einsum("bqhr,bkr->bhqk", qr, k_rope, preferred_element_type=F32)) * scale
        probs = jax.nn.softmax(jnp.where(mask, scores, -jnp.inf), axis=-1)
        return jnp.einsum("bhqk,bkhd->bqhd", probs.astype(v.dtype), v)

    out = lax.map(attend, (qn_blocks, qr_blocks, jnp.arange(nblk)))
    out = jnp.moveaxis(out, 0, 1).reshape(bsz, s, h * MLA_V)
    return out @ w_out


def grouped_moe(x, router_w, router_bias, w_gate, w_up, w_down):
    bsz, s, d = x.shape
    t = x.reshape(-1, d)
    affinity = jax.nn.sigmoid(jnp.dot(t, router_w, preferred_element_type=F32))
    sel = (affinity + router_bias.astype(F32)).reshape(-1, N_EXPERT_GROUPS, EXPERTS_PER_GROUP)
    group_score = lax.top_k(sel, TOP_K)[0].sum(-1)
    best_group = jnp.argmax(group_score, axis=-1)
    in_group = jnp.take_along_axis(sel, best_group[:, None, None], axis=1)[:, 0]
    _, local_idx = lax.top_k(in_group, TOP_K)
    expert_idx = best_group[:, None] * EXPERTS_PER_GROUP + local_idx
    w_sel = jnp.take_along_axis(affinity, expert_idx, axis=-1)
    w_sel = w_sel / jnp.sum(w_sel, -1, keepdims=True)
    gates = jnp.sum(jax.nn.one_hot(expert_idx, N_EXPERTS, dtype=F32) * w_sel[..., None], axis=1)
    out = jnp.zeros((t.shape[0], d), F32)
    for e in range(N_EXPERTS):
        hdn = jax.nn.silu(t @ w_gate[e]) * (t @ w_up[e])
        out = out + gates[:, e:e + 1] * (hdn @ w_down[e])
    return out.astype(x.dtype).reshape(bsz, s, d)


def setup_inputs(seed: int = 0) -> dict:
    key = jax.random.key(seed)
    ks = jax.random.split(key, 32)
    nrm = jax.random.normal
    Ls, Lm = N_SSD_LAYERS, N_MLA_LAYERS
    dt0 = jnp.exp(jax.random.uniform(ks[4], (Ls, SSD_N_HEADS), minval=math.log(1e-3), maxval=math.log(1e-1)))
    return {
        "x": nrm(ks[0], (BATCH, SEQ, D_MODEL), F32),
        "ssd_w_in": nrm(ks[1], (Ls, D_MODEL, SSD_IN_DIM), F32) * D_MODEL ** -0.5,
        "ssd_conv_w": nrm(ks[2], (Ls, SSD_CONV_W, SSD_CONV_DIM), F32) * SSD_CONV_W ** -0.5,
        "ssd_conv_b": 0.01 * nrm(ks[3], (Ls, SSD_CONV_DIM), F32),
        "ssd_dt_bias": dt0 + jnp.log(-jnp.expm1(-dt0)),
        "ssd_a_log": jnp.log(jax.random.uniform(ks[5], (Ls, SSD_N_HEADS), minval=1.0, maxval=16.0)),
        "ssd_d": 1.0 + 0.1 * nrm(ks[6], (Ls, SSD_N_HEADS), F32),
        "ssd_norm_w": 1.0 + 0.05 * nrm(ks[7], (Ls, SSD_D_INNER), F32),
        "ssd_w_out": nrm(ks[8], (Ls, SSD_D_INNER, D_MODEL), F32) * SSD_D_INNER ** -0.5 * DEEPNORM_BETA,
        "mla_w_down": nrm(ks[9], (Lm, D_MODEL, MLA_DOWN_DIM), F32) * D_MODEL ** -0.5,
        "mla_q_norm": 1.0 + 0.05 * nrm(ks[10], (Lm, MLA_Q_RANK), F32),
        "mla_w_uq": nrm(ks[11], (Lm, MLA_Q_RANK, MLA_N_HEADS * (MLA_NOPE + MLA_ROPE)), F32) * MLA_Q_RANK ** -0.5,
        "mla_kv_norm": 1.0 + 0.05 * nrm(ks[12], (Lm, MLA_KV_RANK), F32),
        "mla_w_ukv": nrm(ks[13], (Lm, MLA_KV_RANK, MLA_N_HEADS * (MLA_NOPE + MLA_V)), F32) * MLA_KV_RANK ** -0.5,
        "mla_w_out": nrm(ks[14], (Lm, MLA_N_HEADS * MLA_V, D_MODEL), F32) * (MLA_N_HEADS * MLA_V) ** -0.5 * DEEPNORM_BETA,
        "router_w": nrm(ks[15], (D_MODEL, N_EXPERTS), F32) * D_MODEL ** -0.5,
        "router_bias": 0.01 * nrm(ks[16], (N_EXPERTS,), F32),
        "moe_w_gate": nrm(ks[17], (DEPTH, N_EXPERTS, D_MODEL, D_FF_EXPERT), F32) * D_MODEL ** -0.5,
        "moe_w_up": nrm(ks[18], (DEPTH, N_EXPERTS, D_MODEL, D_FF_EXPERT), F32) * D_MODEL ** -0.5,
        "moe_w_down": nrm(ks[19], (DEPTH, N_EXPERTS, D_FF_EXPERT, D_MODEL), F32) * D_FF_EXPERT ** -0.5 * DEEPNORM_BETA,
        "ln_mix_g": 1.0 + 0.05 * nrm(ks[20], (DEPTH, D_MODEL), F32),
        "ln_mix_b": 0.01 * nrm(ks[21], (DEPTH, D_MODEL), F32),
        "ln_ffn_g": 1.0 + 0.05 * nrm(ks[22], (DEPTH, D_MODEL), F32),
        "ln_ffn_b": 0.01 * nrm(ks[23], (DEPTH, D_MODEL), F32),
    }


def reference(x, ssd_w_in, ssd_conv_w, ssd_conv_b, ssd_dt_bias, ssd_a_log, ssd_d, ssd_norm_w, ssd_w_out,
              mla_w_down, mla_q_norm, mla_w_uq, mla_kv_norm, mla_w_ukv, mla_w_out,
              router_w, router_bias, moe_w_gate, moe_w_up, moe_w_down,
              ln_mix_g, ln_mix_b, ln_ffn_g, ln_ffn_b):
    h = x
    for i in range(DEPTH):
        j = i // N_MIXERS
        if i % N_MIXERS == 0:
            m = ssd_mixer(h, ssd_w_in[j], ssd_conv_w[j], ssd_conv_b[j], ssd_dt_bias[j], ssd_a_log[j],
                          ssd_d[j], ssd_norm_w[j], ssd_w_out[j])
        else:
            m = mla_mixer(h, mla_w_down[j], mla_q_norm[j], mla_w_uq[j], mla_kv_norm[j], mla_w_ukv[j], mla_w_out[j])
        h = layer_norm(DEEPNORM_ALPHA * h + m, ln_mix_g[i], ln_mix_b[i])
        f = grouped_moe(h, router_w, router_bias, moe_w_gate[i], moe_w_up[i], moe_w_down[i])
        h = layer_norm(DEEPNORM_ALPHA * h + f, ln_ffn_g[i], ln_ffn_b[i])
    return h
```

```python
import contextlib
import math
import numpy as np
import ml_dtypes
import concourse.bass as bass
import concourse.mybir as mybir
from concourse.bass_utils import run_bass_kernel_spmd

F32 = mybir.dt.float32
BF16 = mybir.dt.bfloat16
AF = mybir.ActivationFunctionType
ALU = mybir.AluOpType
AX = mybir.AxisListType

KEEP_WARM = 0
N_DMA_SEMS = {"sp": 12, "pool": 8, "act": 4}

S = 4096
D = 1024
NT = S // 128
ALPHA = (2.0 * 2) ** 0.25
LN_EPS = 1e-5
RMS_EPS = 1e-6
NH_SSD = 32
E = 16
DFF = 512
MLA_H = 16


class Buf:
    __slots__ = ("name", "gen_w", "gen_r", "prev_w", "prev_r")

    def __init__(self, name=""):
        self.name = name
        self.gen_w = []
        self.gen_r = []
        self.prev_w = []
        self.prev_r = []


class Op:
    __slots__ = ("eng", "fn", "deps", "is_dma", "idx", "needed", "sig", "dma_i")

    def __init__(self, eng, fn, is_dma, idx):
        self.eng = eng
        self.fn = fn
        self.deps = []
        self.is_dma = is_dma
        self.idx = idx
        self.needed = False
        self.sig = None
        self.dma_i = None


class Prog:
    def __init__(self, nc):
        self.nc = nc
        self.ops = []
        self.by_eng = {e: [] for e in ("pe", "act", "dve", "pool", "sp")}
        self.dma_count = {q: 0 for q in N_DMA_SEMS}
        self.fence_deps = []
        self.fence_seen = set()

    def fence(self):
        deps = []
        for e, lst in self.by_eng.items():
            last_c = None
            dmas = []
            for o in reversed(lst):
                if o.is_dma:
                    if len(dmas) < N_DMA_SEMS[e]:
                        dmas.append(o)
                elif last_c is None:
                    last_c = o
                if last_c is not None and (e not in N_DMA_SEMS or len(dmas) >= N_DMA_SEMS[e]):
                    break
            if last_c is not None:
                deps.append(last_c)
            deps.extend(dmas)
        self.fence_deps = deps
        self.fence_seen = set()

    def add(self, eng, fn, reads=(), writes=(), dma=False, partial=False):
        op = Op(eng, fn, dma, len(self.ops))
        deps = {}
        if self.fence_deps and eng not in self.fence_seen:
            self.fence_seen.add(eng)
            for o in self.fence_deps:
                deps[o.idx] = o

        def dep(o):
            if o is op:
                return
            if (not o.is_dma) and (not dma) and o.eng == eng and eng == "pe":
                return
            deps[o.idx] = o

        for b in reads:
            for w in b.gen_w:
                dep(w)
        for b in writes:
            if b.gen_r or not partial or not b.gen_w:
                for r in b.gen_r:
                    dep(r)
                for w in b.gen_w:
                    dep(w)
                b.prev_w, b.prev_r = b.gen_w, b.gen_r
                b.gen_w, b.gen_r = [op], []
            else:
                for r in b.prev_r:
                    dep(r)
                for w in b.prev_w:
                    dep(w)
                b.gen_w.append(op)
        for b in reads:
            if b in writes:
                continue
            if dma:
                b.gen_r.append(op)
            else:
                b.gen_r = [r for r in b.gen_r if r.is_dma or r.eng != eng]
                b.gen_r.append(op)
        for b in writes:
            if not dma and len(b.gen_w) > 1:
                b.gen_w = [w for w in b.gen_w if w.is_dma or w.eng != eng or w is op]
        op.deps = list(deps.values())
        for d in op.deps:
            d.needed = True
        self.ops.append(op)
        self.by_eng[eng].append(op)
        if dma:
            op.dma_i = self.dma_count[eng]
            self.dma_count[eng] += 1
        return op

    def emit(self, st, final_wait_ops=()):
        nc = self.nc
        esem = {e: st.enter_context(nc.semaphore("s_" + e)) for e in self.by_eng}
        dsem = {
            q: [st.enter_context(nc.semaphore(f"d_{q}{i}")) for i in range(n)]
            for q, n in N_DMA_SEMS.items()
        }
        cnt = {e: 0 for e in self.by_eng}
        for op in self.ops:
            if op.is_dma:
                n = N_DMA_SEMS[op.eng]
                j = op.dma_i % n
                op.sig = (dsem[op.eng][j], 16 * (op.dma_i // n + 1))
            elif op.needed:
                cnt[op.eng] += 1
                op.sig = (esem[op.eng], cnt[op.eng])
        self.final_counts = dict(cnt)
        block = st.enter_context(nc.Block())

        def run_engine(ename, eng):
            known = {}

            def wait(sig):
                sem, val = sig
                if known.get(sem.num, 0) >= val:
                    return
                eng.wait_ge(sem, val)
                known[sem.num] = val

            for op in self.by_eng[ename]:
                for d in op.deps:
                    wait(d.sig)
                if op.is_dma:
                    sem, val = op.sig
                    if val > 16:
                        wait((sem, val - 16))
                    ins = op.fn(eng)
                    ins.then_inc(sem, 16)
                else:
                    ins = op.fn(eng)
                    if op.sig is not None:
                        ins.then_inc(op.sig[0], 1)
            if ename == "sp":
                for op in final_wait_ops:
                    wait(op.sig)

        @block.tensor
        def _(e):
            run_engine("pe", e)

        @block.scalar
        def _(e):
            run_engine("act", e)

        @block.vector
        def _(e):
            run_engine("dve", e)

        @block.gpsimd
        def _(e):
            run_engine("pool", e)

        @block.sync
        def _(e):
            run_engine("sp", e)


class TT:
    def __init__(self, t, name):
        self.t = t
        self.b = Buf(name)

    def __getitem__(self, k):
        return self.t[k]


def _bufs(xs):
    out = []
    for x in xs:
        if x is None:
            continue
        out.append(x.b if isinstance(x, TT) else x)
    return out


class K:
    def __init__(self, nc, st):
        self.nc = nc
        self.st = st
        self.P = Prog(nc)
        self.out_ops = []

    def sb(self, name, shape, dt):
        return TT(self.st.enter_context(self.nc.sbuf_tensor(name, shape, dt)), name)

    def ps(self, name, shape, dt):
        return TT(self.st.enter_context(self.nc.psum_tensor(name, shape, dt)), name)

    def dram(self, name, shape, dt, kind="Internal"):
        return self.nc.dram_tensor(name, shape, dt, kind=kind).ap()

    def op(self, eng, fn, r=(), w=(), partial=False):
        return self.P.add(eng, fn, _bufs(r), _bufs(w), dma=False, partial=partial)

    def dma(self, q, out, in_, r=(), w=(), partial=True):
        return self.P.add(q, lambda e: e.dma_start(out=out, in_=in_), _bufs(r), _bufs(w), dma=True, partial=partial)

    def mm(self, out, lhsT, rhs, start, stop, r=(), w=()):
        return self.P.add("pe", lambda e: e.matmul(out, lhsT=lhsT, rhs=rhs, start=start, stop=stop),
                          _bufs(r), _bufs(w), partial=True)

    def tr(self, out, in_, ident, r=(), w=()):
        return self.P.add("pe", lambda e: e.transpose(out=out, in_=in_, identity=ident),
                          _bufs(r), _bufs(w), partial=True)

    def act(self, out, in_, func, r=(), w=(), partial=False, **kw):
        return self.P.add("act", lambda e: e.activation(out=out, in_=in_, func=func, **kw),
                          _bufs(r), _bufs(w), partial=partial)

    def v(self, eng, name, r=(), w=(), partial=False, **kw):
        return self.P.add(eng, lambda e: getattr(e, name)(**kw), _bufs(r), _bufs(w), partial=partial)


def bc(ap, dims):
    a = list(ap.ap)
    return bass.AP(ap.tensor, ap.offset, [list(a[0])] + [list(d) for d in dims])


def layer_norm_tile(k, r, g_bc, b_bc, out, tmp, eps_t, tag, eng2="dve"):
    stats, mv, lnv, rstd = tmp
    for i in range(2):
        k.v("dve", "bn_stats", r=[r], w=[stats], partial=True, out=stats[:, i * 6:(i + 1) * 6], in_=r[:, i * 512:(i + 1) * 512])
    k.v("dve", "bn_aggr", r=[stats], w=[mv], out=mv[:, 0:2], in_=stats[:, 0:12])
    k.act(lnv[:, 0:1], mv[:, 1:2], AF.Ln, r=[mv, eps_t], w=[lnv], bias=eps_t[:, 0:1])
    k.act(rstd[:, 0:1], lnv[:, 0:1], AF.Exp, r=[lnv], w=[rstd], scale=-0.5)
    k.v("dve", "tensor_scalar", r=[r, mv, rstd], w=[r], out=r[:, :], in0=r[:, :], scalar1=mv[:, 0:1], scalar2=rstd[:, 0:1],
        op0=ALU.subtract, op1=ALU.mult)
    k.v(eng2, "tensor_tensor", r=[r, g_bc], w=[r], out=r[:, :], in0=r[:, :], in1=g_bc[:, :], op=ALU.mult)
    k.v(eng2, "tensor_tensor", r=[r, b_bc], w=[out], out=out[:, :], in0=r[:, :], in1=b_bc[:, :], op=ALU.add)


def build_program(stop_after=None, dbg=False):
    nc = bass.Bass("TRN2", target_bir_lowering=False)
    st = contextlib.ExitStack()
    k = K(nc, st)

    def din(name, shape, dt=F32):
        return nc.dram_tensor(name, list(shape), dt, kind="ExternalInput").ap()

    x_d = din("x", [S, D])
    ssd_w_in = din("ssd_w_in", [D, 6176])
    ssd_conv_w = din("ssd_conv_w", [4, 4096])
    ssd_conv_b = din("ssd_conv_b", [4096])
    ssd_dt_bias = din("ssd_dt_bias", [32])
    ssd_a_log = din("ssd_a_log", [32])
    ssd_d = din("ssd_d", [32])
    ssd_norm_w = din("ssd_norm_w", [2048])
    ssd_w_out = din("ssd_w_out", [2048, D])
    mla_w_down = din("mla_w_down", [D, 672])
    mla_w_kr = din("mla_w_kr", [D, 192])
    mla_q_norm = din("mla_q_norm", [384])
    mla_w_uq = din("mla_w_uq", [384, 1536])
    mla_w_uq_sw = din("mla_w_uq_sw", [384, 1536])
    mla_kv_norm = din("mla_kv_norm", [256])
    mla_w_kn = din("mla_w_kn", [256, 1024])
    mla_w_v = din("mla_w_v", [256, 1024])
    mla_w_out = din("mla_w_out", [D, D])
    router_w = din("router_w", [D, E])
    router_bias = din("router_bias", [E])
    moe_w_gate = din("moe_w_gate", [2 * E * 128 * 2, 2048])
    moe_w_up = din("moe_w_up", [2 * E * 128 * 2, 2048])
    moe_w_down = din("moe_w_down", [2 * E * 128 * 2, 2048])
    ln_mix_g = din("ln_mix_g", [2, D])
    ln_mix_b = din("ln_mix_b", [2, D])
    ln_ffn_g = din("ln_ffn_g", [2, D])
    ln_ffn_b = din("ln_ffn_b", [2, D])
    c_ident = din("c_ident", [128, 128])
    c_rmat = din("c_rmat", [128, 128])
    c_lmat = din("c_lmat", [128, 128])
    c_rope = din("c_rope", [2, 128, S])
    c_amask = din("c_amask", [128, 4, 512])
    c_umat = din("c_umat", [128, 128])
    c_misc = din("c_misc", [128, 48])
    out_d = nc.dram_tensor("out", [S, D], F32, kind="ExternalOutput").ap()

    h1_d = k.dram("h1_d", [S, D], F32, kind="ExternalOutput" if dbg else "Internal")
    h2_d = k.dram("h2_d", [S, D], F32, kind="ExternalOutput" if dbg else "Internal")
    h3_d = k.dram("h3_d", [S, D], F32, kind="ExternalOutput" if dbg else "Internal")
    xs_d = k.dram("xs_d", [S, 2048], BF16)
    bt_d = k.dram("bt_d", [S, 1024], BF16)
    bT_d = k.dram("bT_d", [1024, S], BF16)
    cT_d = k.dram("cT_d", [1024, S], BF16)
    z_d = k.dram("z_d", [S, 2048], BF16)
    dt_d = k.dram("dt_d", [S, 32], F32)
    a_d = k.dram("a_d", [S, 32], F32)
    attnT_d = k.dram("attnT_d", [D, S], BF16)
    NSLOT = 16384
    xsl_d = k.dram("xsl_d", [NSLOT, D], BF16)
    ysl_d = k.dram("ysl_d", [NSLOT, D], F32)
    B_xsl = Buf("xsl"); B_ysl = Buf("ysl")
    zt = k.sb("zero_t", [128, D], BF16)
    k.v("pool", "memset", w=[zt], ap=zt[:, :], constant=0.0)
    xsl_z = xsl_d.rearrange("(a p) d -> a p d", p=128)
    for a_ in range(NSLOT // 128):
        k.dma("sp", xsl_z[a_], zt[:, :], r=[zt], w=[B_xsl])
    B_h = [[Buf(f"h{i}_{t}") for t in range(NT)] for i in range(4)]
    B_xs = Buf("xs_d"); B_bt = Buf("bt_d"); B_bT = Buf("bT_d"); B_cT = Buf("cT_d")
    B_z = Buf("z_d"); B_dt = Buf("dt_d"); B_a = Buf("a_d"); B_attn = Buf("attn_d")
    B_out = Buf("out")

    ident_f = k.sb("ident_f", [128, 128], F32)
    ident_b = k.sb("ident_b", [128, 128], BF16)
    rmat_f = k.sb("rmat_f", [128, 128], F32)
    rmat_b = k.sb("rmat_b", [128, 128], BF16)
    lmat_f = k.sb("lmat_f", [128, 128], F32)
    lmat_b = k.sb("lmat_b", [128, 128], BF16)
    ones_f = k.sb("ones_f", [128, 128], F32)
    ones_b = k.sb("ones_b", [128, 128], BF16)
    eps_ln = k.sb("eps_ln", [128, 1], F32)
    eps_rms = k.sb("eps_rms", [128, 1], F32)
    k.dma("sp", ident_f[:, :], c_ident, w=[ident_f])
    k.dma("sp", rmat_f[:, :], c_rmat, w=[rmat_f])
    k.dma("sp", lmat_f[:, :], c_lmat, w=[lmat_f])
    k.v("dve", "tensor_copy", r=[ident_f], w=[ident_b], out=ident_b[:, :], in_=ident_f[:, :])
    k.v("dve", "tensor_copy", r=[rmat_f], w=[rmat_b], out=rmat_b[:, :], in_=rmat_f[:, :])
    k.v("dve", "tensor_copy", r=[lmat_f], w=[lmat_b], out=lmat_b[:, :], in_=lmat_f[:, :])
    k.v("dve", "memset", w=[ones_f], ap=ones_f[:, :], constant=1.0)
    k.v("dve", "memset", w=[ones_b], ap=ones_b[:, :], constant=1.0)
    k.v("dve", "memset", w=[eps_ln], ap=eps_ln[:, :], constant=LN_EPS)
    k.v("dve", "memset", w=[eps_rms], ap=eps_rms[:, :], constant=RMS_EPS)

    DBK = [k.ps(f"psd{i}", [128, 1024], F32) for i in range(2)]
    PS = []
    for i in range(4):
        v_ = TTview(DBK[i // 2], DBK[i // 2].t[:, (i % 2) * 512:(i % 2 + 1) * 512])
        v_.b = Buf(f"ps{i}")
        PS.append(v_)
    PS += [k.ps(f"ps{i}", [128, 512], F32) for i in range(4, 8)]

    rw_g = k.sb("rw_g", [128, 8, E], F32)
    k.dma("sp", rw_g[:, :, :], router_w.rearrange("(kc p) e -> p kc e", p=128), w=[rw_g])
    lg_all = [k.sb("lg_all0", [128, NT, E], F32), None]
    lg_fused = {}
    ctx = dict(locals())
    if stop_after is not None and stop_after.startswith("moeonly"):
        h1_in = din("h1_in", [S, D])
        ctx["moe_stage"] = stop_after.split(":")[1]
        ctx["dbg_outs"] = {}
        for nm, shp, dt_ in (("d_pos1", [128, NT], mybir.dt.int32), ("d_pos2", [128, NT], mybir.dt.int32),
                             ("d_widx", [128, 2, 32], mybir.dt.int32), ("d_g1", [128, NT], F32), ("d_g2", [128, NT], F32)):
            ctx["dbg_outs"][nm] = nc.dram_tensor(nm, shp, dt_, kind="ExternalOutput").ap()
        moe_layer(k, ctx, 0, h1_in, [Buf() for _ in range(NT)], h2_d, B_h[1], False)
        return finish(k, nc, st)
    ssd_layer(k, ctx)
    if stop_after == "ssd":
        return finish(k, nc, st)
    moe_layer(k, ctx, 0, h1_d, B_h[0], h2_d, B_h[1], False)
    if stop_after == "moe0":
        return finish(k, nc, st)
    mla_layer(k, ctx)
    if stop_after == "mla":
        return finish(k, nc, st)
    moe_layer(k, ctx, 1, h3_d, B_h[2], out_d, None, True)
    return finish(k, nc, st)


def route_tile(k, c, layer, tt, src, hT_, tr_banks, lg_bank):
    ident_f, rw, lg = c["ident_f"], c["rw_g"], c["lg_all"][layer]
    for half in range(2):
        pb = tr_banks[half]
        for j in range(4):
            kc = half * 4 + j
            k.tr(pb[:, j * 128:(j + 1) * 128], src[:, kc * 128:(kc + 1) * 128], ident_f[:, :], r=[src, ident_f], w=[pb])
        k.act(hT_[:, half * 4:half * 4 + 4, :], pb[:, :].rearrange("p (j c) -> p j c", j=4), AF.Copy, r=[pb], w=[hT_], partial=True)
    for kc in range(8):
        k.mm(lg_bank[:, 0:E], hT_[:, kc, :], rw[:, kc, :], kc == 0, kc == 7, r=[hT_, rw], w=[lg_bank])
    k.act(lg[:, tt, :], lg_bank[:, 0:E], AF.Exp, r=[lg_bank], w=[lg], partial=True, scale=-1.0)
    c["lg_fused"][layer] = True


def load_ln(k, g_src, b_src, layer):
    t = k.sb(f"lnp_{k.P.dma_count['sp']}", [128, 2, D], F32)
    k.dma("sp", t[:, 0, :], g_src[layer].partition_broadcast(128), w=[t])
    k.dma("sp", t[:, 1, :], b_src[layer].partition_broadcast(128), w=[t])
    return TTview(t, t.t[:, 0, :]), TTview(t, t.t[:, 1, :])


class TTview:
    def __init__(self, parent, ap):
        self.t = ap
        self.b = parent.b

    def __getitem__(self, k):
        return self.t[k]


def finish(k, nc, st):
    k.P.emit(st, final_wait_ops=k.out_ops)
    st.close()
    return nc


def _bufs(xs):
    out = []
    for x in xs:
        if x is None:
            continue
        out.append(getattr(x, "b", x))
    return out


def ssd_layer(k, c):
    nc = k.nc
    PS = c["PS"]
    x_d = c["x_d"]
    ident_b, rmat_f, rmat_b, lmat_f, lmat_b, ones_f, ones_b = (c[n] for n in
        ("ident_b", "rmat_f", "rmat_b", "lmat_f", "lmat_b", "ones_f", "ones_b"))
    xs_d, bt_d, bT_d, cT_d, z_d, dt_d, a_d = (c[n] for n in ("xs_d", "bt_d", "bT_d", "cT_d", "z_d", "dt_d", "a_d"))
    B_xs, B_bt, B_bT, B_cT, B_z, B_dt, B_a = (c[n] for n in ("B_xs", "B_bt", "B_bT", "B_cT", "B_z", "B_dt", "B_a"))
    st_outer = k.st
    st = contextlib.ExitStack()
    k.st = st

    xT = k.sb("xT", [128, 8, S], BF16)
    xT_b = [Buf(f"xT{t}") for t in range(NT)]
    st1a = contextlib.ExitStack()
    k.st = st1a
    xbf = [k.sb(f"xbf{i}", [128, D], BF16) for i in range(3)]
    for tt in range(NT):
        xb = xbf[tt % 3]
        k.dma("pool", xb[:, :], x_d[tt * 128:(tt + 1) * 128, :], w=[xb], partial=False)
        pb = PS[tt % 2]
        pv = pb[:, :].bitcast(BF16)
        for kc in range(8):
            k.tr(pv[:, kc * 128:(kc + 1) * 128], xb[:, kc * 128:(kc + 1) * 128], ident_b[:, :], r=[xb, ident_b], w=[pb])
        dst = xT.t[:, :, tt * 128:(tt + 1) * 128]
        src = pv.rearrange("p (k t) -> p k t", k=8)
        if tt % 2:
            k.act(dst, src, AF.Copy, r=[pb], w=[xT_b[tt]])
        else:
            k.v("dve", "tensor_copy", r=[pb], w=[xT_b[tt]], out=dst, in_=src)

    w_in_v = c["ssd_w_in"].rearrange("(kc p) c -> p kc c", p=128)
    wz = k.sb("wz", [128, 8, 2048], BF16)
    wz_b = [Buf(f"wz{i}") for i in range(4)]
    for cb in range(4):
        k.dma("pool", wz[:, :, cb * 512:(cb + 1) * 512], w_in_v[:, :, cb * 512:(cb + 1) * 512], w=[wz_b[cb]])
    wdt = k.sb("wdt", [128, 8, 32], BF16)
    k.dma("pool", wdt[:, :, :], w_in_v[:, :, 6144:6176], w=[wdt])
    dtraw = k.sb("dtraw", [128, NT, 32], F32)
    zs = [k.sb(f"zs{i}", [128, 2048], BF16) for i in range(2)]
    n_ps = 0
    for tt in range(NT):
        zt = zs[tt % 2]
        for cb in range(4):
            pb = PS[6 + n_ps % 2]
            n_ps += 1
            for kc in range(8):
                k.mm(pb[:, :], xT.t[:, kc, tt * 128:(tt + 1) * 128], wz[:, kc, cb * 512:(cb + 1) * 512], kc == 0, kc == 7,
                     r=[xT_b[tt], wz_b[cb]], w=[pb])
            k.act(zt[:, cb * 512:(cb + 1) * 512], pb[:, :], AF.Silu, r=[pb], w=[zt], partial=True)
        k.dma("sp", z_d[tt * 128:(tt + 1) * 128, :], zt[:, :], r=[zt], w=[B_z])
        pb = PS[6 + n_ps % 2]
        n_ps += 1
        for kc in range(8):
            k.mm(pb[:, 0:32], xT.t[:, kc, tt * 128:(tt + 1) * 128], wdt[:, kc, :], kc == 0, kc == 7, r=[xT_b[tt], wdt], w=[pb])
        k.v("dve", "tensor_copy", r=[pb], w=[dtraw], partial=True, out=dtraw[:, tt, :], in_=pb[:, 0:32])

    sm32 = k.sb("sm32", [128, 3, 32], F32)
    k.dma("sp", sm32[:, 0, :], c["ssd_dt_bias"].partition_broadcast(128), w=[sm32])
    k.dma("sp", sm32[:, 1, :], c["ssd_a_log"].partition_broadcast(128), w=[sm32])
    e1 = k.sb("e1", [128, NT, 32], F32)
    dtv = k.sb("dtv", [128, NT, 32], F32)
    av = k.sb("av", [128, NT, 32], F32)
    k.v("dve", "tensor_tensor", r=[dtraw, sm32], w=[dtraw], out=dtraw[:, :, :], in0=dtraw[:, :, :],
        in1=bc(sm32[:, 0, :], [[0, NT], [1, 32]]), op=ALU.add)
    k.act(e1[:, :, :], dtraw[:, :, :], AF.Exp, r=[dtraw], w=[e1])
    k.act(dtv[:, :, :], e1[:, :, :], AF.Ln, r=[e1], w=[dtv], bias=1.0)
    k.act(sm32[:, 2, :], sm32[:, 1, :], AF.Exp, r=[sm32], w=[sm32])
    k.v("dve", "scalar_tensor_tensor", r=[dtv, sm32], w=[av], out=av[:, :, :], in0=dtv[:, :, :], scalar=-1.0,
        in1=bc(sm32[:, 2, :], [[0, NT], [1, 32]]), op0=ALU.mult, op1=ALU.mult)
    k.dma("sp", dt_d.rearrange("(t p) h -> p t h", p=128), dtv[:, :, :], r=[dtv], w=[B_dt])
    k.dma("sp", a_d.rearrange("(t p) h -> p t h", p=128), av[:, :, :], r=[av], w=[B_a])

    st1a.close()
    k.P.fence()
    k.st = st
    convw = k.sb("convw", [128, 128], F32)
    convb = k.sb("convb", [128, 32], F32)
    k.dma("sp", convw[:, :], c["ssd_conv_w"], w=[convw])
    k.dma("sp", convb[:, :], c["ssd_conv_b"], w=[convb])
    diagw = k.sb("diagw", [128, 128, 128], BF16)
    k.v("dve", "tensor_tensor", r=[ident_b, convw], w=[diagw], out=diagw[:, :, :], in0=bc(ident_b[:, :], [[0, 128], [1, 128]]),
        in1=bc(convw[:, :], [[1, 128], [0, 128]]), op=ALU.mult)
    wch = [k.sb(f"wch{i}", [128, 8, 128], BF16) for i in range(3)]
    xpre = [k.sb(f"xpre{i}", [128, 3 + S], BF16) for i in range(2)]
    xact = [k.sb(f"xact{i}", [128, S], BF16) for i in range(2)]
    stg = [k.sb(f"stg{i}", [128, 8, 128], BF16) for i in range(3)]
    for xp in xpre:
        k.v("dve", "memset", w=[xp], ap=xp[:, 0:3], constant=0.0)
    xs_v = xs_d.rearrange("(t p) c -> p t c", p=128)
    bt_v = bt_d.rearrange("(t p) c -> p t c", p=128)
    n_ps = 0
    n_cv = 0
    n_tr = 0

    def conv_block(cc, tb, xp, xa):
        nonlocal n_cv
        cv = PS[6 + n_cv % 2]
        n_cv += 1
        for tap in range(4):
            k.mm(cv[:, :], diagw[:, cc * 4 + tap, :], xp[:, tb * 512 + tap:tb * 512 + tap + 512], tap == 0, tap == 3, r=[diagw, xp], w=[cv])
        k.act(xa[:, tb * 512:(tb + 1) * 512], cv[:, :], AF.Silu, r=[cv, convb], w=[xa], partial=True, bias=convb[:, cc:cc + 1])

    for cc in range(32):
        wc = wch[cc % 3]
        k.dma("pool", wc[:, :, :], w_in_v[:, :, 2048 + cc * 128:2048 + (cc + 1) * 128], w=[wc], partial=False)
        xp = xpre[cc % 2]
        xa = xact[cc % 2]
        for tb in range(8):
            pb = PS[2 + n_ps % 2]
            n_ps += 1
            for kc in range(8):
                k.mm(pb[:, :], wc[:, kc, :], xT.t[:, kc, tb * 512:(tb + 1) * 512], kc == 0, kc == 7,
                     r=[wc] + xT_b[4 * tb:4 * tb + 4], w=[pb])
            if tb > 0:
                conv_block(cc, tb - 1, xp, xa)
            k.act(xp[:, 3 + tb * 512:3 + (tb + 1) * 512], pb[:, :], AF.Copy, r=[pb], w=[xp], partial=True)
        conv_block(cc, 7, xp, xa)
        if cc < 24:
            dv, Bd, col = (xs_v, B_xs, cc * 128) if cc < 16 else (bt_v, B_bt, (cc - 16) * 128)
            for g8 in range(4):
                pb = PS[4 + n_tr % 2]
                sg = stg[n_tr % 3]
                n_tr += 1
                pv = pb[:, :].bitcast(BF16)
                for j in range(8):
                    t = g8 * 8 + j
                    k.tr(pv[:, j * 128:(j + 1) * 128], xa[:, t * 128:(t + 1) * 128], ident_b[:, :], r=[xa, ident_b], w=[pb])
                k.v("dve", "tensor_copy", r=[pb], w=[sg], out=sg[:, :, :], in_=pv.rearrange("p (j c) -> p j c", j=8))
                k.dma("sp", dv[:, g8 * 8:(g8 + 1) * 8, col:col + 128], sg[:, :, :], r=[sg], w=[Bd])
        if 16 <= cc < 24:
            k.dma("sp", bT_d[(cc - 16) * 128:(cc - 15) * 128, :], xa[:, :], r=[xa], w=[B_bT])
        if cc >= 24:
            k.dma("sp", cT_d[(cc - 24) * 128:(cc - 23) * 128, :], xa[:, :], r=[xa], w=[B_cT])
    st.close()
    k.P.fence()

    st = contextlib.ExitStack()
    k.st = st
    h1_d = c["h1_d"]
    B_h1 = c["B_h"][0]
    eps_ln, eps_rms = c["eps_ln"], c["eps_rms"]
    wout = k.sb("wout", [128, 16, D], BF16)
    k.dma("pool", wout[:, :, :], c["ssd_w_out"].rearrange("(kc p) d -> p kc d", p=128), w=[wout], partial=False)
    normw = k.sb("normw", [128, 2048], F32)
    k.dma("sp", normw[:, :], c["ssd_norm_w"].partition_broadcast(128), w=[normw])
    dsk = k.sb("dsk", [128, 32], F32)
    k.dma("sp", dsk[:, :], c["ssd_d"].partition_broadcast(128), w=[dsk])
    diagD = k.sb("diagD", [128, 32, 128], BF16)
    identb3 = bc(ident_b[:, :], [[0, 32], [1, 128]])
    k.v("dve", "tensor_tensor", r=[ident_b, dsk], w=[diagD], out=diagD[:, :, :], in0=identb3,
        in1=bc(dsk[:, :], [[1, 32], [0, 128]]), op=ALU.mult)
    state = k.sb("state", [128, 2048], F32)
    state_bf = k.sb("state_bf", [128, 2048], BF16)
    st_b = [Buf(f"st{g}") for g in range(8)]
    stbf_b = [Buf(f"stbf{g}") for g in range(8)]
    k.v("dve", "memset", w=st_b, ap=state[:, :], constant=0.0)
    k.v("dve", "memset", w=stbf_b, ap=state_bf[:, :], constant=0.0)

    NB = 3
    xs_t = [k.sb(f"xs_t{i}", [128, 2048], BF16) for i in range(NB)]
    bt_t = [k.sb(f"bt_t{i}", [128, 1024], BF16) for i in range(NB)]
    bT_t = [k.sb(f"bT_t{i}", [128, 8, 128], BF16) for i in range(NB)]
    cT_t = [k.sb(f"cT_t{i}", [128, 8, 128], BF16) for i in range(NB)]
    a_t = [k.sb(f"a_t{i}", [128, 32], F32) for i in range(NB)]
    dt_t = [k.sb(f"dt_t{i}", [128, 32], F32) for i in range(NB)]
    z_t = [k.sb(f"z_t{i}", [128, 2048], BF16) for i in range(2)]
    x_t = [k.sb(f"x_t{i}", [128, D], F32) for i in range(2)]
    exps2 = [k.sb(f"exps{i}", [128, 96], F32) for i in range(2)]
    a_bf2 = [k.sb(f"a_bf{i}", [128, 32], BF16) for i in range(2)]
    dtte2 = [k.sb(f"dtte{i}", [128, 32], F32) for i in range(2)]
    aR2 = [k.sb(f"aR{i}", [128, 32, 128], BF16) for i in range(2)]
    dec = [k.sb(f"dec{i}", [128, 8, 128], BF16) for i in range(1)] * 2
    smk = [k.sb(f"smk{i}", [128, 2, 128], BF16) for i in range(2)]
    MT = [k.sb(f"MT{i}", [128, 8, 128], BF16) for i in range(2)]
    xdt2 = [k.sb(f"xdt{i}", [128, 2048], BF16) for i in range(2)]
    xw2 = [k.sb(f"xw{i}", [128, 2048], BF16) for i in range(2)]
    tmpq = [k.sb(f"tmpq{i}", [128, 512], F32) for i in range(1)] * 2
    stq = [k.sb(f"stq{i}", [128, 512], F32) for i in range(1)] * 2
    ycomb2 = [k.sb(f"ycomb{i}", [128, 2048], F32) for i in range(2)]
    ssq = k.sb("ssq", [128, 8], F32)
    junk = k.sb("junk", [128, 256], F32)
    lnv8 = k.sb("lnv8", [128, 8], F32)
    rstd8 = k.sb("rstd8", [128, 8], F32)
    yn = k.sb("yn", [128, 2048], BF16)
    ynT = k.sb("ynT", [128, 16, 128], BF16)
    res = k.sb("res", [128, D], F32)
    hout = [k.sb(f"hout{i}", [128, D], F32) for i in range(1)] * 2
    lntmp = (k.sb("ln_stats", [128, 12], F32), k.sb("ln_mv", [128, 2], F32), k.sb("ln_lnv", [128, 1], F32),
             k.sb("ln_rstd", [128, 1], F32))
    bT_v = bT_d.rearrange("(g n) t -> n g t", n=128)
    cT_v = cT_d.rearrange("(g n) t -> n g t", n=128)
    g_bc, b_bc = load_ln(k, c["ln_mix_g"], c["ln_mix_b"], 0)
    MISC, SEG0, SEG1, YB, YOB, STB, OPB, TRB = PS

    pend_route = []
    hT_r = k.sb("s2hTr", [128, 8, 128], F32)

    def loads(tt):
        i = tt % NB
        sl = slice(tt * 128, (tt + 1) * 128)
        k.dma("sp", a_t[i][:, :], a_d[sl, :], r=[B_a], w=[a_t[i]], partial=False)
        k.dma("sp", dt_t[i][:, :], dt_d[sl, :], r=[B_dt], w=[dt_t[i]], partial=False)
        k.dma("sp", xs_t[i][:, :], xs_d[sl, :], r=[B_xs], w=[xs_t[i]], partial=False)
        k.dma("sp", bT_t[i][:, :, :], bT_v[:, :, sl], r=[B_bT], w=[bT_t[i]], partial=False)
        k.dma("sp", cT_t[i][:, :, :], cT_v[:, :, sl], r=[B_cT], w=[cT_t[i]], partial=False)
        k.dma("sp", bt_t[i][:, :], bt_d[sl, :], r=[B_bt], w=[bt_t[i]], partial=False)

    def loads_zx(tt):
        sl = slice(tt * 128, (tt + 1) * 128)
        k.dma("sp", z_t[tt % 2][:, :], z_d[sl, :], r=[B_z], w=[z_t[tt % 2]], partial=False)
        k.dma("sp", x_t[tt % 2][:, :], x_d[sl, :], w=[x_t[tt % 2]], partial=False)

    def prologue(tt):
        i = tt % NB
        j = tt % 2
        xs_, a_, dt_ = xs_t[i], a_t[i], dt_t[i]
        exps, a_bf, dtte, aR, xdt, xw = exps2[j], a_bf2[j], dtte2[j], aR2[j], xdt2[j], xw2[j]
        k.mm(MISC[:, 0:32], rmat_f[:, :], a_[:, :], True, True, r=[rmat_f, a_], w=[MISC])
        k.mm(MISC[:, 32:64], lmat_f[:, :], a_[:, :], True, True, r=[lmat_f, a_], w=[MISC])
        k.mm(MISC[:, 64:96], ones_f[:, :], a_[:, :], True, True, r=[ones_f, a_], w=[MISC])
        k.act(exps[:, :], MISC[:, 0:96], AF.Exp, r=[MISC], w=[exps])
        k.v("dve", "tensor_tensor", r=[dt_, exps], w=[dtte], out=dtte[:, :], in0=dt_[:, :], in1=exps[:, 32:64], op=ALU.mult)
        k.v("dve", "tensor_copy", r=[a_], w=[a_bf], out=a_bf[:, :], in_=a_[:, :])
        k.v("dve", "tensor_tensor", r=[a_bf, rmat_b], w=[aR], out=aR[:, :, :], in0=bc(a_bf[:, :], [[1, 32], [0, 128]]),
            in1=bc(rmat_b[:, :], [[0, 32], [1, 128]]), op=ALU.mult)
        xs3 = xs_[:, :].rearrange("p (h d) -> p h d", h=32)
        k.v("pool", "tensor_tensor", r=[xs_, dt_], w=[xdt], out=xdt[:, :].rearrange("p (h d) -> p h d", h=32), in0=xs3,
            in1=bc(dt_[:, :], [[1, 32], [0, 64]]), op=ALU.mult)
        k.v("pool", "tensor_tensor", r=[xs_, dtte], w=[xw], out=xw[:, :].rearrange("p (h d) -> p h d", h=32), in0=xs3,
            in1=bc(dtte[:, :], [[1, 32], [0, 64]]), op=ALU.mult)

    def quarter(tt, q):
        i = tt % NB
        j = tt % 2
        xs_, bt_, bT_, cT_ = xs_t[i], bt_t[i], bT_t[i], cT_t[i]
        exps, aR, xdt, xw, ycomb = exps2[j], aR2[j], xdt2[j], xw2[j], ycomb2[j]
        SEG = (SEG0, SEG1)
        for half in range(2):
            k.mm(SEG[half][:, :], lmat_b[:, :], aR[:, q * 8 + half * 4:q * 8 + half * 4 + 4, :], True, True,
                 r=[lmat_b, aR], w=[SEG[half]])
        for gi in range(2):
            g = q * 2 + gi
            k.mm(MISC[:, 128 + gi * 128:256 + gi * 128], bT_[:, g, :], cT_[:, g, :], True, True, r=[bT_, cT_], w=[MISC])
        dq = dec[q % 2]
        for half in range(2):
            k.act(dq[:, half * 4:half * 4 + 4, :], SEG[half][:, :].rearrange("p (h l) -> p h l", h=4), AF.Exp,
                  r=[SEG[half]], w=[dq], partial=True)
        sq_ = smk[q % 2]
        k.v("dve", "tensor_tensor", r=[MISC, rmat_f], w=[sq_], out=sq_[:, :, :],
            in0=MISC[:, 128:384].rearrange("p (g l) -> p g l", g=2), in1=bc(rmat_f[:, :], [[0, 2], [1, 128]]), op=ALU.mult)
        mq = MT[q % 2]
        k.v("dve", "tensor_tensor", r=[dq, sq_], w=[mq], out=mq[:, :, :].rearrange("p (g r) l -> p g r l", g=2),
            in0=dq[:, :, :].rearrange("p (g r) l -> p g r l", g=2),
            in1=bc(sq_[:, :, :], [[128, 2], [0, 4], [1, 128]]), op=ALU.mult)
        for gi in range(2):
            g = q * 2 + gi
            k.mm(YOB[:, gi * 256:(gi + 1) * 256], cT_[:, g, :], state_bf[:, g * 256:(g + 1) * 256], True, True,
                 r=[cT_, stbf_b[g]], w=[YOB])
        for gi in range(2):
            g = q * 2 + gi
            k.mm(STB[:, gi * 256:(gi + 1) * 256], bt_[:, g * 128:(g + 1) * 128], xw[:, g * 256:(g + 1) * 256], True, True,
                 r=[bt_, xw], w=[STB])
        for hq in range(8):
            h = q * 8 + hq
            k.mm(YB[:, hq * 64:(hq + 1) * 64], diagD[:, h, :], xs_[:, h * 64:(h + 1) * 64], True, False,
                 r=[diagD, xs_], w=[YB])
            k.mm(YB[:, hq * 64:(hq + 1) * 64], mq[:, hq, :], xdt[:, h * 64:(h + 1) * 64], False, True,
                 r=[mq, xdt], w=[YB])
        sq2 = stq[q % 2]
        sl = slice(q * 512, (q + 1) * 512)
        gb = [st_b[q * 2], st_b[q * 2 + 1]]
        k.v("dve", "tensor_tensor", r=gb + [exps], w=[sq2], out=sq2[:, :].rearrange("p (h d) -> p h d", h=8),
            in0=state[:, sl].rearrange("p (h d) -> p h d", h=8), in1=bc(exps[:, 64 + q * 8:64 + q * 8 + 8], [[1, 8], [0, 64]]),
            op=ALU.mult)
        k.v("dve", "tensor_tensor", r=[STB, sq2], w=gb, out=state[:, sl], in0=STB[:, :], in1=sq2[:, :], op=ALU.add)
        k.act(state_bf[:, sl], state[:, sl], AF.Copy, r=gb, w=[stbf_b[q * 2], stbf_b[q * 2 + 1]])
        tq = tmpq[q % 2]
        k.v("dve", "tensor_tensor", r=[YOB, exps], w=[tq], out=tq[:, :].rearrange("p (h d) -> p h d", h=8),
            in0=YOB[:, :].rearrange("p (h d) -> p h d", h=8), in1=bc(exps[:, q * 8:q * 8 + 8], [[1, 8], [0, 64]]), op=ALU.mult)
        k.v("dve", "tensor_tensor", r=[YB, tq], w=[ycomb], partial=True, out=ycomb[:, q * 512:(q + 1) * 512], in0=YB[:, :],
            in1=tq[:, :], op=ALU.add)

    def epi(tt, part):
        i = tt % NB
        ycomb = ycomb2[tt % 2]
        z_, x_ = z_t[tt % 2], x_t[tt % 2]
        if part == 0:
            k.v("dve", "tensor_tensor", r=[ycomb, z_], w=[ycomb], out=ycomb[:, :], in0=ycomb[:, :], in1=z_[:, :], op=ALU.mult)
            for g in range(8):
                k.act(junk[:, :], ycomb[:, g * 256:(g + 1) * 256], AF.Square, r=[ycomb], w=[junk, ssq], partial=True,
                      accum_out=ssq[:, g:g + 1])
            k.act(lnv8[:, :], ssq[:, :], AF.Ln, r=[ssq, eps_rms], w=[lnv8], scale=1.0 / 256.0, bias=eps_rms[:, 0:1])
            k.act(rstd8[:, :], lnv8[:, :], AF.Exp, r=[lnv8], w=[rstd8], scale=-0.5)
        elif part == 1:
            k.v("pool", "tensor_tensor", r=[ycomb, rstd8], w=[ycomb], out=ycomb[:, :].rearrange("p (g d) -> p g d", g=8),
                in0=ycomb[:, :].rearrange("p (g d) -> p g d", g=8), in1=bc(rstd8[:, :], [[1, 8], [0, 256]]), op=ALU.mult)
            k.v("pool", "tensor_tensor", r=[ycomb, normw], w=[yn], out=yn[:, :], in0=ycomb[:, :], in1=normw[:, :], op=ALU.mult)
            for g8 in range(2):
                pv = TRB[:, :].bitcast(BF16)
                for j in range(8):
                    kc = g8 * 8 + j
                    k.tr(pv[:, j * 128:(j + 1) * 128], yn[:, kc * 128:(kc + 1) * 128], ident_b[:, :], r=[yn, ident_b], w=[TRB])
                k.act(ynT[:, g8 * 8:(g8 + 1) * 8, :], pv.rearrange("p (j c) -> p j c", j=8), AF.Copy, r=[TRB], w=[ynT], partial=True)
        elif part == 2:
            for dh in range(2):
                for kc in range(16):
                    k.mm(OPB[:, :], ynT[:, kc, :], wout[:, kc, dh * 512:(dh + 1) * 512], kc == 0, kc == 15, r=[ynT, wout], w=[OPB])
                k.v("dve", "scalar_tensor_tensor", r=[x_, OPB], w=[res], partial=True, out=res[:, dh * 512:(dh + 1) * 512],
                    in0=x_[:, dh * 512:(dh + 1) * 512], scalar=ALPHA, in1=OPB[:, :], op0=ALU.mult, op1=ALU.add)
        else:
            ho = hout[tt % 2]
            if pend_route:
                route_tile(k, c, 0, pend_route.pop(0), ho, hT_r, (TRB, TRB), OPB)
            layer_norm_tile(k, res, g_bc, b_bc, ho, lntmp, eps_ln, "s", eng2="pool")
            op = k.dma("sp", h1_d[tt * 128:(tt + 1) * 128, :], ho[:, :], r=[ho], w=[B_h1[tt]])
            k.out_ops.append(op)
            pend_route.append(tt)

    loads(0)
    loads(1)
    prologue(0)
    for tt in range(NT):
        for q in range(4):
            quarter(tt, q)
            if tt > 0:
                epi(tt - 1, q)
            if q == 1 and tt + 1 < NT:
                prologue(tt + 1)
        if tt + 2 < NT:
            loads(tt + 2)
        loads_zx(tt)
    for part in range(4):
        epi(NT - 1, part)
    route_tile(k, c, 0, pend_route.pop(0), hout[0], hT_r, (TRB, TRB), OPB)
    st.close()
    k.P.fence()
    k.st = st_outer


def moe_layer_dense(k, c, layer, hin_d, B_hin, hout_d, B_hout, is_final):
    PS = c["PS"]
    ident_f = c["ident_f"]
    eps_ln = c["eps_ln"]
    st_outer = k.st
    st = contextlib.ExitStack()
    k.st = st
    L = f"m{layer}"
    TH = 16
    xT = k.sb(L + "xT", [128, 8, TH * 128], BF16)
    xT_b = [Buf(f"{L}xT{t}") for t in range(TH)]
    acc = k.sb(L + "acc", [128, TH, D], F32)
    acc_b = [Buf(f"{L}acc{t}") for t in range(TH)]
    rw = k.sb(L + "rw", [128, 8, E], F32)
    k.dma("sp", rw[:, :, :], c["router_w"].rearrange("(kc p) e -> p kc e", p=128), w=[rw])
    rb = k.sb(L + "rb", [128, E], F32)
    k.dma("sp", rb[:, :], c["router_bias"].partition_broadcast(128), w=[rb])
    g_bc, b_bc = load_ln(k, c["ln_ffn_g"], c["ln_ffn_b"], layer)
    hin = [k.sb(f"{L}hin{i}", [128, D], F32) for i in range(2)]
    hTf = k.sb(L + "hTf", [128, 8, 128], F32)
    lg = k.sb(L + "lg", [128, TH, E], F32)
    aff = k.sb(L + "aff", [128, TH, E], F32)
    sel = k.sb(L + "sel", [128, TH, E], F32)
    p6 = k.sb(L + "p6", [128, TH * 4, 6], F32)
    gs = k.sb(L + "gs", [128, TH * 4], F32)
    gmax = k.sb(L + "gmax", [128, TH], F32)
    gm = k.sb(L + "gm", [128, TH * 4], F32)
    pen = k.sb(L + "pen", [128, TH * 4], F32)
    selm = k.sb(L + "selm", [128, TH, E], F32)
    m1 = k.sb(L + "m1", [128, TH], F32)
    mk1 = k.sb(L + "mk1", [128, TH, E], F32)
    selm2 = k.sb(L + "selm2", [128, TH, E], F32)
    mk2 = k.sb(L + "mk2", [128, TH, E], F32)
    wsum = k.sb(L + "wsum", [128, TH], F32)
    gates = k.sb(L + "gates", [128, TH, E], F32)
    wg = [k.sb(f"{L}wg{i}", [128, 8, DFF], BF16) for i in range(2)]
    wu = [k.sb(f"{L}wu{i}", [128, 8, DFF], BF16) for i in range(2)]
    wd = [k.sb(f"{L}wd{i}", [128, 4, D], BF16) for i in range(2)]
    hT = [k.sb(f"{L}hT{i}", [128, 4, 512], BF16) for i in range(2)]
    sg = [k.sb(f"{L}sg{i}", [128, 512], BF16) for i in range(2)]
    res = [k.sb(f"{L}res{i}", [128, D], F32) for i in range(2)]
    lntmp = (k.sb(L + "ln_stats", [128, 12], F32), k.sb(L + "ln_mv", [128, 2], F32), k.sb(L + "ln_lnv", [128, 1], F32),
             k.sb(L + "ln_rstd", [128, 1], F32))
    wgv = c["moe_w_gate"][layer].rearrange("e (kc p) f -> e p kc f", p=128)
    wuv = c["moe_w_up"][layer].rearrange("e (kc p) f -> e p kc f", p=128)
    wdv = c["moe_w_down"][layer].rearrange("e (fc p) d -> e p fc d", p=128)
    n_w = 0
    n_g = 0
    n_d = 0
    n_h = 0
    for hf in range(2):
        for t in range(TH):
            tt = hf * TH + t
            hi = hin[t % 2]
            k.dma("sp", hi[:, :], hin_d[tt * 128:(tt + 1) * 128, :], r=[B_hin[tt]], w=[hi], partial=False)
            for half in range(2):
                pb = PS[half]
                for j in range(4):
                    kc = half * 4 + j
                    k.tr(pb[:, j * 128:(j + 1) * 128], hi[:, kc * 128:(kc + 1) * 128], ident_f[:, :], r=[hi, ident_f], w=[pb])
                k.act(hTf[:, half * 4:half * 4 + 4, :], pb[:, :].rearrange("p (j c) -> p j c", j=4), AF.Copy, r=[pb], w=[hTf],
                      partial=True)
            k.v("dve", "tensor_copy", r=[hTf], w=[xT_b[t]], out=xT.t[:, :, t * 128:(t + 1) * 128], in_=hTf[:, :, :])
            pl = PS[2]
            for kc in range(8):
                k.mm(pl[:, 0:E], hTf[:, kc, :], rw[:, kc, :], kc == 0, kc == 7, r=[hTf, rw], w=[pl])
            k.act(lg[:, t, :], pl[:, 0:E], AF.Exp, r=[pl], w=[lg], partial=True, scale=-1.0)
        V = lambda name, **kw: k.v("dve", name, **kw)
        V("tensor_scalar", r=[lg], w=[lg], out=lg[:, :, :], in0=lg[:, :, :], scalar1=1.0, scalar2=None, op0=ALU.add)
        V("reciprocal", r=[lg], w=[aff], out=aff[:, :, :], in_=lg[:, :, :])
        V("tensor_tensor", r=[aff, rb], w=[sel], out=sel[:, :, :], in0=aff[:, :, :], in1=bc(rb[:, :], [[0, TH], [1, E]]), op=ALU.add)
        s4 = sel[:, :, :].rearrange("p t (g i) -> p (t g) i", g=4)
        V("tensor_tensor", r=[sel], w=[p6], partial=True, out=p6[:, :, 0:3], in0=s4[:, :, 0:3], in1=s4[:, :, 1:4], op=ALU.add)
        V("tensor_tensor", r=[sel], w=[p6], partial=True, out=p6[:, :, 3:5], in0=s4[:, :, 0:2], in1=s4[:, :, 2:4], op=ALU.add)
        V("tensor_tensor", r=[sel], w=[p6], partial=True, out=p6[:, :, 5:6], in0=s4[:, :, 0:1], in1=s4[:, :, 3:4], op=ALU.add)
        V("tensor_reduce", r=[p6], w=[gs], out=gs[:, :], in_=p6[:, :, :], axis=AX.X, op=ALU.max)
        V("tensor_reduce", r=[gs], w=[gmax], out=gmax[:, :], in_=gs[:, :].rearrange("p (t g) -> p t g", g=4), axis=AX.X, op=ALU.max)
        V("tensor_tensor", r=[gs, gmax], w=[gm], out=gm[:, :].rearrange("p (t g) -> p t g", g=4),
          in0=gs[:, :].rearrange("p (t g) -> p t g", g=4), in1=bc(gmax[:, :], [[1, TH], [0, 4]]), op=ALU.is_ge)
        V("tensor_scalar", r=[gm], w=[pen], out=pen[:, :], in0=gm[:, :], scalar1=-1.0, scalar2=1.0e4, op0=ALU.add, op1=ALU.mult)
        sm4 = selm[:, :, :].rearrange("p t (g i) -> p (t g) i", g=4)
        V("tensor_tensor", r=[sel, gm], w=[selm], out=sm4, in0=s4, in1=bc(gm[:, :], [[1, TH * 4], [0, 4]]), op=ALU.mult)
        V("tensor_tensor", r=[selm, pen], w=[selm], out=sm4, in0=sm4, in1=bc(pen[:, :], [[1, TH * 4], [0, 4]]), op=ALU.add)
        V("tensor_reduce", r=[selm], w=[m1], out=m1[:, :], in_=selm[:, :, :], axis=AX.X, op=ALU.max)
        V("tensor_tensor", r=[selm, m1], w=[mk1], out=mk1[:, :, :], in0=selm[:, :, :], in1=bc(m1[:, :], [[1, TH], [0, E]]), op=ALU.is_ge)
        V("scalar_tensor_tensor", r=[mk1, selm], w=[selm2], out=selm2[:, :, :], in0=mk1[:, :, :], scalar=-1.0e4, in1=selm[:, :, :],
          op0=ALU.mult, op1=ALU.add)
        V("tensor_reduce", r=[selm2], w=[m1], out=m1[:, :], in_=selm2[:, :, :], axis=AX.X, op=ALU.max)
        V("tensor_tensor", r=[selm2, m1], w=[mk2], out=mk2[:, :, :], in0=selm2[:, :, :], in1=bc(m1[:, :], [[1, TH], [0, E]]), op=ALU.is_ge)
        V("tensor_tensor", r=[mk1, mk2], w=[mk1], out=mk1[:, :, :], in0=mk1[:, :, :], in1=mk2[:, :, :], op=ALU.add)
        V("tensor_tensor", r=[mk1, aff], w=[mk1], out=mk1[:, :, :], in0=mk1[:, :, :], in1=aff[:, :, :], op=ALU.mult)
        V("tensor_reduce", r=[mk1], w=[wsum], out=wsum[:, :], in_=mk1[:, :, :], axis=AX.X, op=ALU.add)
        V("reciprocal", r=[wsum], w=[wsum], out=wsum[:, :], in_=wsum[:, :])
        V("tensor_tensor", r=[mk1, wsum], w=[gates], out=gates[:, :, :], in0=mk1[:, :, :], in1=bc(wsum[:, :], [[1, TH], [0, E]]), op=ALU.mult)
        for e in range(E):
            i = n_w % 2
            n_w += 1
            k.dma("pool", wg[i][:, :, :], wgv[e], w=[wg[i]], partial=False)
            k.dma("pool", wu[i][:, :, :], wuv[e], w=[wu[i]], partial=False)
            k.dma("pool", wd[i][:, :, :], wdv[e], w=[wd[i]], partial=False)
            for tb in range(4):
                hTt = hT[n_h % 2]
                n_h += 1
                xr = xT_b[tb * 4:tb * 4 + 4]
                for fc in range(4):
                    G = PS[n_g % 2]
                    U = PS[2 + n_g % 2]
                    s_ = sg[n_g % 2]
                    n_g += 1
                    for kc in range(8):
                        k.mm(G[:, :], wg[i][:, kc, fc * 128:(fc + 1) * 128], xT.t[:, kc, tb * 512:(tb + 1) * 512], kc == 0, kc == 7,
                             r=[wg[i]] + xr, w=[G])
                    for kc in range(8):
                        k.mm(U[:, :], wu[i][:, kc, fc * 128:(fc + 1) * 128], xT.t[:, kc, tb * 512:(tb + 1) * 512], kc == 0, kc == 7,
                             r=[wu[i]] + xr, w=[U])
                    k.act(s_[:, :], G[:, :], AF.Silu, r=[G], w=[s_])
                    k.v("dve", "tensor_tensor", r=[s_, U], w=[hTt], partial=True, out=hTt[:, fc, :], in0=s_[:, :], in1=U[:, :], op=ALU.mult)
                for tl in range(4):
                    t = tb * 4 + tl
                    for dh in range(2):
                        Dp = PS[4 + n_d % 4]
                        n_d += 1
                        for fc in range(4):
                            k.mm(Dp[:, :], hTt[:, fc, tl * 128:(tl + 1) * 128], wd[i][:, fc, dh * 512:(dh + 1) * 512], fc == 0, fc == 3,
                                 r=[hTt, wd[i]], w=[Dp])
                        gsc = gates[:, t, e:e + 1]
                        if e == 0:
                            k.v("dve", "tensor_scalar", r=[Dp, gates], w=[acc_b[t]], partial=True, out=acc[:, t, dh * 512:(dh + 1) * 512],
                                in0=Dp[:, :], scalar1=gsc, scalar2=None, op0=ALU.mult)
                        else:
                            k.v("dve", "scalar_tensor_tensor", r=[Dp, gates, acc_b[t]], w=[acc_b[t]], partial=True,
                                out=acc[:, t, dh * 512:(dh + 1) * 512], in0=Dp[:, :], scalar=gsc, in1=acc[:, t, dh * 512:(dh + 1) * 512],
                                op0=ALU.mult, op1=ALU.add)
        for t in range(TH):
            tt = hf * TH + t
            hi = hin[t % 2]
            k.dma("sp", hi[:, :], hin_d[tt * 128:(tt + 1) * 128, :], r=[B_hin[tt]], w=[hi], partial=False)
            r_ = res[t % 2]
            k.v("dve", "scalar_tensor_tensor", r=[hi, acc_b[t]], w=[r_], out=r_[:, :], in0=hi[:, :], scalar=ALPHA, in1=acc[:, t, :],
                op0=ALU.mult, op1=ALU.add)
            layer_norm_tile(k, r_, g_bc, b_bc, r_, lntmp, eps_ln, L)
            op = k.dma("sp", hout_d[tt * 128:(tt + 1) * 128, :], r_[:, :], r=[r_], w=[B_hout[tt]] if B_hout is not None else [])
            k.out_ops.append(op)
    st.close()
    k.P.fence()
    k.st = st_outer


def mla_layer(k, c):
    PS = c["PS"]
    ident_b, ones_f, eps_ln, eps_rms = c["ident_b"], c["ones_f"], c["eps_ln"], c["eps_rms"]
    DBK = c["DBK"]
    h2_d, h3_d, attnT_d = c["h2_d"], c["h3_d"], c["attnT_d"]
    B_h2, B_h3, B_attn = c["B_h"][1], c["B_h"][2], c["B_attn"]
    st_outer = k.st
    if c["lg_all"][1] is None:
        c["lg_all"][1] = k.sb("lg_all1", [128, NT, E], F32)
    stA = contextlib.ExitStack()
    k.st = stA
    cT = k.sb("cT", [128, 5, S], BF16)
    cT_b = [Buf(f"cT{t}") for t in range(NT)]
    krT = k.sb("krT", [128, S], BF16)
    krT_b = [Buf(f"krT{t}") for t in range(8)]
    st = contextlib.ExitStack()
    k.st = st
    wdn = k.sb("wdn", [128, 8, 640], BF16)
    k.dma("pool", wdn[:, :, :], c["mla_w_down"].rearrange("(kc p) n -> p kc n", p=128)[:, :, 0:640], w=[wdn], partial=False)
    wkr = k.sb("wkr", [128, 8, 192], BF16)
    k.dma("pool", wkr[:, :, :], c["mla_w_kr"].rearrange("(kc p) n -> p kc n", p=128), w=[wkr], partial=False)
    qkn = k.sb("qkn", [128, 640], F32)
    k.dma("sp", qkn[:, 0:384], c["mla_q_norm"].partition_broadcast(128), w=[qkn])
    k.dma("sp", qkn[:, 384:640], c["mla_kv_norm"].partition_broadcast(128), w=[qkn])
    hin = [k.sb(f"a1hin{i}", [128, D], F32) for i in range(2)]
    hbf = [k.sb(f"a1hbf{i}", [128, D], BF16) for i in range(2)]
    hT = [k.sb(f"a1hT{i}", [128, 8, 512], BF16) for i in range(2)]
    cq = k.sb("a1cq", [128, 640], F32)
    cqn = k.sb("a1cqn", [128, 640], BF16)
    junk = k.sb("a1junk", [128, 384], F32)
    ss2 = k.sb("a1ss2", [128, 2], F32)
    ln2 = k.sb("a1ln2", [128, 2], F32)
    rs2 = k.sb("a1rs2", [128, 2], F32)
    rope = [k.sb(f"a1rope{i}", [128, 2, 512], F32) for i in range(2)]
    rt1 = k.sb("a1rt1", [128, 512], F32)
    rt2 = k.sb("a1rt2", [128, 512], F32)
    rope_v = c["c_rope"]
    cq2 = [cq, k.sb("a1cq_b", [128, 640], F32)]
    hT_bufs_all = {}

    def a1_front(tt):
        tb, tl = tt // 4, tt % 4
        hTb = hT[tb % 2]
        if tl == 0:
            hT_bufs_all[tb] = [Buf(f"hTb{tb}_{i}") for i in range(4)]
        hT_bufs = hT_bufs_all[tb]
        hi = hin[tt % 2]
        hb = hbf[tt % 2]
        k.dma("sp", hi[:, :], h2_d[tt * 128:(tt + 1) * 128, :], r=[B_h2[tt]], w=[hi], partial=False)
        k.v("dve", "tensor_copy", r=[hi], w=[hb], out=hb[:, :], in_=hi[:, :])
        pb = PS[tt % 2]
        pv = pb[:, :].bitcast(BF16)
        for kc in range(8):
            k.tr(pv[:, kc * 128:(kc + 1) * 128], hb[:, kc * 128:(kc + 1) * 128], ident_b[:, :], r=[hb, ident_b], w=[pb])
        k.act(hTb[:, :, tl * 128:(tl + 1) * 128], pv.rearrange("p (k t) -> p k t", k=8), AF.Copy, r=[pb, hT_bufs[tl]], w=[hT_bufs[tl]])
        P0, P1 = PS[2 + tt % 2], PS[4 + tt % 2]
        for kc in range(8):
            k.mm(P0[:, :], hTb[:, kc, tl * 128:(tl + 1) * 128], wdn[:, kc, 0:512], kc == 0, kc == 7, r=[hT_bufs[tl], wdn], w=[P0])
        for kc in range(8):
            k.mm(P1[:, 0:128], hTb[:, kc, tl * 128:(tl + 1) * 128], wdn[:, kc, 512:640], kc == 0, kc == 7, r=[hT_bufs[tl], wdn], w=[P1])
        cq_ = cq2[tt % 2]
        k.act(cq_[:, 0:512], P0[:, :], AF.Copy, r=[P0], w=[cq_], partial=True)
        k.act(cq_[:, 512:640], P1[:, 0:128], AF.Copy, r=[P1], w=[cq_], partial=True)

    def a1_back(tt):
        cq_ = cq2[tt % 2]
        k.act(junk[:, 0:384], cq_[:, 0:384], AF.Square, r=[cq_], w=[junk, ss2], partial=True, accum_out=ss2[:, 0:1])
        k.act(junk[:, 0:256], cq_[:, 384:640], AF.Square, r=[cq_], w=[junk, ss2], partial=True, accum_out=ss2[:, 1:2])
        k.act(ln2[:, 0:1], ss2[:, 0:1], AF.Ln, r=[ss2, eps_rms], w=[ln2], partial=True, scale=1.0 / 384.0, bias=eps_rms[:, 0:1])
        k.act(ln2[:, 1:2], ss2[:, 1:2], AF.Ln, r=[ss2, eps_rms], w=[ln2], partial=True, scale=1.0 / 256.0, bias=eps_rms[:, 0:1])
        k.act(rs2[:, :], ln2[:, :], AF.Exp, r=[ln2], w=[rs2], scale=-0.5)
        k.v("dve", "scalar_tensor_tensor", r=[cq_, rs2, qkn], w=[cqn], partial=True, out=cqn[:, 0:384], in0=cq_[:, 0:384], scalar=rs2[:, 0:1],
            in1=qkn[:, 0:384], op0=ALU.mult, op1=ALU.mult)
        k.v("dve", "scalar_tensor_tensor", r=[cq_, rs2, qkn], w=[cqn], partial=True, out=cqn[:, 384:640], in0=cq_[:, 384:640], scalar=rs2[:, 1:2],
            in1=qkn[:, 384:640], op0=ALU.mult, op1=ALU.mult)
        pb2 = PS[tt % 2]
        pv2 = pb2[:, :].bitcast(BF16)
        for j in range(5):
            k.tr(pv2[:, j * 128:(j + 1) * 128], cqn[:, j * 128:(j + 1) * 128], ident_b[:, :], r=[cqn, ident_b], w=[pb2])
        k.act(cT.t[:, :, tt * 128:(tt + 1) * 128], pv2[:, 0:640].rearrange("p (j t) -> p j t", j=5), AF.Copy, r=[pb2], w=[cT_b[tt]])

    def a1_krope(tb):
        hTb = hT[tb % 2]
        hT_bufs = hT_bufs_all[tb]
        rp = rope[tb % 2]
        k.dma("sp", rp[:, 0, :], rope_v[0][:, tb * 512:(tb + 1) * 512], w=[rp])
        k.dma("sp", rp[:, 1, :], rope_v[1][:, tb * 512:(tb + 1) * 512], w=[rp])
        KA, KB = PS[6], PS[7]
        for kc in range(8):
            k.mm(KA[0:96, :], wkr[:, kc, 0:96], hTb[:, kc, :], kc == 0, kc == 7, r=hT_bufs + [wkr], w=[KA])
        for kc in range(8):
            k.mm(KB[0:96, :], wkr[:, kc, 96:192], hTb[:, kc, :], kc == 0, kc == 7, r=hT_bufs + [wkr], w=[KB])
        k.v("dve", "tensor_tensor", r=[KA, rp], w=[rt1], out=rt1[64:96, :], in0=KA[64:96, :], in1=rp[64:96, 0, :], op=ALU.mult)
        k.v("dve", "tensor_tensor", r=[KB, rp], w=[rt2], out=rt2[64:96, :], in0=KB[64:96, :], in1=rp[64:96, 1, :], op=ALU.mult)
        k.v("dve", "tensor_tensor", r=[rt1, rt2], w=[krT_b[tb]], out=krT[64:96, tb * 512:(tb + 1) * 512], in0=rt1[64:96, :], in1=rt2[64:96, :],
            op=ALU.add)

    a1_front(0)
    for tt in range(NT):
        if tt + 1 < NT:
            a1_front(tt + 1)
        a1_back(tt)
        if tt % 4 == 3:
            a1_krope(tt // 4)
    st.close()
    k.P.fence()
    st = contextlib.ExitStack()
    k.st = st
    HG = 4
    qT = k.sb("qT", [128, HG, S], BF16)
    kT = k.sb("kT", [128, HG, S], BF16)
    Vt = k.sb("Vt", [128, NT, HG, 65], BF16)
    qT_b = [[Buf(f"qT{h}_{b}") for b in range(8)] for h in range(HG)]
    kT_b = [[Buf(f"kT{h}_{b}") for b in range(8)] for h in range(HG)]
    V_b = [Buf(f"V{t}") for t in range(NT)]
    k.v("dve", "memset", w=V_b, ap=Vt[:, :, :, :], constant=1.0)
    wq = [k.sb(f"wq{i}", [128, 3, HG * 96], BF16) for i in range(2)]
    wqs = [k.sb(f"wqs{i}", [128, 3, HG * 96], BF16) for i in range(2)]
    wkn = [k.sb(f"wkn{i}", [128, 2, HG * 64], BF16) for i in range(2)]
    wv = [k.sb(f"wv{i}", [128, 2, HG * 64], BF16) for i in range(2)]
    amask_f = k.sb("amask_f", [128, 4, 512], F32)
    amask = k.sb("amask", [128, 4, 512], BF16)
    k.dma("sp", amask_f[:, :, :], c["c_amask"], w=[amask_f], partial=False)
    k.v("dve", "tensor_copy", r=[amask_f], w=[amask], out=amask[:, :, :], in_=amask_f[:, :, :])
    rope = [k.sb(f"a2rope{i}", [128, 2, 512], F32) for i in range(2)]
    rt1s = [k.sb(f"a2rt1{i}", [128, 512], F32) for i in range(2)]
    rt2s = [k.sb(f"a2rt2{i}", [128, 512], F32) for i in range(2)]
    pt = [k.sb(f"pt{i}", [128, 2, 512], BF16) for i in range(3)]
    rinv = k.sb("rinv", [128, 512], F32)
    osb = k.sb("osb", [128, 512], F32)
    at = [k.sb(f"at{i}", [128, 512], BF16) for i in range(2)]
    DB = [k.ps_pair(i) for i in range(4)] if False else None
    wq_v = c["mla_w_uq"].rearrange("(kc p) n -> p kc n", p=128)
    wqs_v = c["mla_w_uq_sw"].rearrange("(kc p) n -> p kc n", p=128)
    wkn_v = c["mla_w_kn"].rearrange("(kc p) n -> p kc n", p=128)
    wv_v = c["mla_w_v"].rearrange("(kc p) n -> p kc n", p=128)
    SCALE = 1.0 / math.sqrt(96.0)
    n_st = 0
    n_ot = 0
    n_at = 0
    for hg in range(MLA_H // HG):
        i = hg % 2
        k.dma("pool", wq[i][:, :, :], wq_v[:, :, hg * HG * 96:(hg + 1) * HG * 96], w=[wq[i]], partial=False)
        k.dma("pool", wqs[i][:, :, :], wqs_v[:, :, hg * HG * 96:(hg + 1) * HG * 96], w=[wqs[i]], partial=False)
        k.dma("pool", wkn[i][:, :, :], wkn_v[:, :, hg * HG * 64:(hg + 1) * HG * 64], w=[wkn[i]], partial=False)
        k.dma("pool", wv[i][:, :, :], wv_v[:, :, hg * HG * 64:(hg + 1) * HG * 64], w=[wv[i]], partial=False)
        for tb in range(8):
            blk = slice(tb * 512, (tb + 1) * 512)
            cr = cT_b[tb * 4:tb * 4 + 4]
            rp = rope[tb % 2]
            k.dma("sp", rp[:, 0, :], rope_v[0][:, blk], w=[rp])
            k.dma("sp", rp[:, 1, :], rope_v[1][:, blk], w=[rp])
            for h in range(HG):
                n_pj = (tb * HG + h) % 2
                QA, QB = PS[n_pj * 2], PS[n_pj * 2 + 1]
                for kc in range(3):
                    k.mm(QA[0:96, :], wq[i][:, kc, h * 96:(h + 1) * 96], cT.t[:, kc, blk], kc == 0, kc == 2, r=[wq[i]] + cr, w=[QA])
                for kc in range(3):
                    k.mm(QB[0:96, :], wqs[i][:, kc, h * 96:(h + 1) * 96], cT.t[:, kc, blk], kc == 0, kc == 2, r=[wqs[i]] + cr, w=[QB])
                KN = PS[6 + n_pj]
                for kc in range(2):
                    k.mm(KN[0:64, :], wkn[i][:, kc, h * 64:(h + 1) * 64], cT.t[:, 3 + kc, blk], kc == 0, kc == 1, r=[wkn[i]] + cr, w=[KN])
                qb = qT_b[h][tb]
                rt1, rt2 = rt1s[n_pj], rt2s[n_pj]
                k.act(qT.t[0:64, h, blk], QA[0:64, :], AF.Copy, r=[QA], w=[qb], partial=True)
                k.v("dve", "tensor_tensor", r=[QA, rp], w=[rt1], out=rt1[64:96, :], in0=QA[64:96, :], in1=rp[64:96, 0, :], op=ALU.mult)
                k.v("dve", "tensor_tensor", r=[QB, rp], w=[rt2], out=rt2[64:96, :], in0=QB[64:96, :], in1=rp[64:96, 1, :], op=ALU.mult)
                k.v("dve", "tensor_tensor", r=[rt1, rt2], w=[qb], partial=True, out=qT.t[64:96, h, blk], in0=rt1[64:96, :], in1=rt2[64:96, :],
                    op=ALU.add)
                kb = kT_b[h][tb]
                k.act(kT.t[0:64, h, blk], KN[0:64, :], AF.Copy, r=[KN], w=[kb], partial=True)
                k.v("pool", "tensor_copy", r=[krT_b[tb]], w=[kb], partial=True, out=kT.t[64:96, h, blk], in_=krT[64:96, blk])
            for tl in range(4):
                tt = tb * 4 + tl
                VP = PS[4 + tl % 2]
                for kc in range(2):
                    k.mm(VP[:, 0:HG * 64], cT.t[:, 3 + kc, tt * 128:(tt + 1) * 128], wv[i][:, kc, :], kc == 0, kc == 1, r=[wv[i], cT_b[tt]], w=[VP])
                k.act(Vt.t[:, tt, :, 0:64], VP[:, 0:HG * 64].rearrange("p (h d) -> p h d", h=HG), AF.Copy, r=[VP], w=[V_b[tt]])
        pairs = []
        for h in range(HG):
            for Qb in range(8):
                nk = 4 * Qb + 4
                for jp in range(nk // 2):
                    pairs.append((h, Qb, jp, nk))
        OTs = {}

        def emit_qk(p):
            nonlocal n_st, n_ot
            h, Qb, jp, nk = p
            if jp == 0:
                OTs[(h, Qb)] = PS[4 + n_ot % 2]
                n_ot += 1
            j0 = jp * 2
            diag = j0 >= 4 * Qb
            q0 = (j0 - 4 * Qb) * 128 if diag else 0
            qs0 = Qb * 512
            SA, SB = PS[(n_st % 2) * 2], PS[(n_st % 2) * 2 + 1]
            ptt = pt[n_st % 3]
            n_st += 1
            for u, SP_ in enumerate((SA, SB)):
                j = j0 + u
                k.mm(SP_[:, q0:512], kT.t[0:96, h, j * 128:(j + 1) * 128], qT.t[0:96, h, qs0 + q0:qs0 + 512], True, True,
                     r=[kT_b[h][j // 4], qT_b[h][Qb]], w=[SP_])
            return (p, q0, diag, SA, SB, ptt)

        def emit_exp(st_):
            p, q0, diag, SA, SB, ptt = st_
            h, Qb, jp, nk = p
            dbk = DBK[0] if SA is PS[0] else DBK[1]
            k.act(ptt[:, :, q0:512], dbk[:, :].rearrange("p (u q) -> p u q", u=2)[:, :, q0:512], AF.Exp, r=[SA, SB], w=[ptt],
                  scale=SCALE)
            if diag:
                jj = jp * 2 - 4 * Qb
                k.v("dve", "tensor_tensor", r=[ptt, amask], w=[ptt], out=ptt[:, :, q0:512], in0=ptt[:, :, q0:512],
                    in1=amask[:, jj:jj + 2, q0:512], op=ALU.mult)

        def emit_pv(st_):
            p, q0, diag, SA, SB, ptt = st_
            h, Qb, jp, nk = p
            OT = OTs[(h, Qb)]
            for u in range(2):
                j = jp * 2 + u
                k.mm(OT[0:65, q0:512], Vt.t[:, j, h, 0:65], ptt[:, u, q0:512], j == 0, j == nk - 1, r=[V_b[j], ptt], w=[OT])
            if KEEP_WARM:
                k.mm(PS[7][:, 0:KEEP_WARM], ident_b[:, :], amask[:, 0, 0:KEEP_WARM], True, True, r=[ident_b, amask], w=[PS[7]])

        def emit_norm(p):
            nonlocal n_at
            h, Qb, jp, nk = p
            habs = hg * HG + h
            qs0 = Qb * 512
            OT = OTs[(h, Qb)]
            k.v("dve", "reciprocal", r=[OT], w=[rinv], out=rinv[64:65, :], in_=OT[64:65, :])
            BC = PS[6]
            k.mm(BC[0:64, :], ones_f[64:65, 0:64], rinv[64:65, :], True, True, r=[ones_f, rinv], w=[BC])
            k.act(osb[0:64, :], OT[0:64, :], AF.Copy, r=[OT], w=[osb])
            a_ = at[n_at % 2]
            n_at += 1
            k.v("dve", "tensor_tensor", r=[osb, BC], w=[a_], out=a_[0:64, :], in0=osb[0:64, :], in1=BC[0:64, :], op=ALU.mult)
            k.dma("sp", attnT_d[habs * 64:(habs + 1) * 64, qs0:qs0 + 512], a_[0:64, :], r=[a_], w=[B_attn])

        prev = None
        pend_norm = None
        for p in pairs:
            cur = emit_qk(p)
            if prev is not None:
                emit_pv(prev)
                if pend_norm is not None:
                    emit_norm(pend_norm)
                    pend_norm = None
                pp = prev[0]
                if pp[2] == pp[3] // 2 - 1:
                    pend_norm = pp
            emit_exp(cur)
            prev = cur
        emit_pv(prev)
        if pend_norm is not None:
            emit_norm(pend_norm)
        emit_norm(prev[0])
    st.close()
    stA.close()
    k.P.fence()
    st = contextlib.ExitStack()
    k.st = st
    wo = k.sb("wo", [128, 8, D], BF16)
    k.dma("pool", wo[:, :, :], c["mla_w_out"].rearrange("(kc p) d -> p kc d", p=128), w=[wo], partial=False)
    g_bc, b_bc = load_ln(k, c["ln_mix_g"], c["ln_mix_b"], 1)
    NB3 = 4
    aT = [k.sb(f"aT{i}", [128, 8, 128], BF16) for i in range(NB3)]
    hin = [k.sb(f"a3hin{i}", [128, D], F32) for i in range(NB3)]
    res = [k.sb(f"a3res{i}", [128, D], F32) for i in range(2)]
    lntmp = (k.sb("a3ln_stats", [128, 12], F32), k.sb("a3ln_mv", [128, 2], F32), k.sb("a3ln_lnv", [128, 1], F32),
             k.sb("a3ln_rstd", [128, 1], F32))
    at_v = attnT_d.rearrange("(kc p) t -> p kc t", p=128)
    hT_r3 = [k.sb(f"a3hTr{i}", [128, 8, 128], F32) for i in range(2)]

    def a3_issue(tt):
        k.dma("sp", aT[tt % NB3][:, :, :], at_v[:, :, tt * 128:(tt + 1) * 128], r=[B_attn], w=[aT[tt % NB3]], partial=False)
        k.dma("sp", hin[tt % NB3][:, :], h2_d[tt * 128:(tt + 1) * 128, :], r=[B_h2[tt]], w=[hin[tt % NB3]], partial=False)

    a3_issue(0)
    a3_issue(1)
    for tt in range(NT):
        if tt + 2 < NT:
            a3_issue(tt + 2)
        a_ = aT[tt % NB3]
        hi = hin[tt % NB3]
        r_ = res[tt % 2]
        for dh in range(2):
            OP = PS[4 + dh]
            for kc in range(8):
                k.mm(OP[:, :], a_[:, kc, :], wo[:, kc, dh * 512:(dh + 1) * 512], kc == 0, kc == 7, r=[a_, wo], w=[OP])
            k.v("dve", "scalar_tensor_tensor", r=[hi, OP], w=[r_], partial=True, out=r_[:, dh * 512:(dh + 1) * 512],
                in0=hi[:, dh * 512:(dh + 1) * 512], scalar=ALPHA, in1=OP[:, :], op0=ALU.mult, op1=ALU.add)
        if tt > 0:
            route_tile(k, c, 1, tt - 1, res[(tt - 1) % 2], hT_r3[(tt - 1) % 2], (PS[0], PS[1]), PS[6 + (tt - 1) % 2])
        layer_norm_tile(k, r_, g_bc, b_bc, r_, lntmp, eps_ln, "a3", eng2="pool")
        op = k.dma("sp", h3_d[tt * 128:(tt + 1) * 128, :], r_[:, :], r=[r_], w=[B_h3[tt]])
        k.out_ops.append(op)
    route_tile(k, c, 1, NT - 1, res[(NT - 1) % 2], hT_r3[(NT - 1) % 2], (PS[0], PS[1]), PS[6 + (NT - 1) % 2])
    st.close()
    k.P.fence()
    k.st = st_outer


I32 = mybir.dt.int32
IOA = bass.IndirectOffsetOnAxis
N_ITEMS = 32


def moe_layer(k, c, layer, hin_d, B_hin, hout_d, B_hout, is_final):
    PS = c["PS"]
    ident_f, ident_b, ones_f, eps_ln = c["ident_f"], c["ident_b"], c["ones_f"], c["eps_ln"]
    xsl_d, ysl_d, B_xsl, B_ysl = c["xsl_d"], c["ysl_d"], c["B_xsl"], c["B_ysl"]
    st_outer = k.st
    stR = contextlib.ExitStack()
    k.st = stR
    L = f"s{layer}"
    TH = NT
    g1 = k.sb(L + "g1", [128, TH], F32)
    g2 = k.sb(L + "g2", [128, TH], F32)
    pos1_i = k.sb(L + "pos1i", [128, TH], I32)
    pos2_i = k.sb(L + "pos2i", [128, TH], I32)
    widx = k.sb(L + "widx", [128, 2, N_ITEMS], I32)
    wg = [k.sb(f"{L}wg{i}", [128, 8, DFF], BF16) for i in range(2)]
    wu = [k.sb(f"{L}wu{i}", [128, 8, DFF], BF16) for i in range(2)]
    wd = [k.sb(f"{L}wd{i}", [128, 4, D], BF16) for i in range(2)]
    wsrc = (c["moe_w_gate"], c["moe_w_up"], c["moe_w_down"])

    def item_loads_w(i):
        b = i % 2
        for src, dst in zip(wsrc, (wg[b], wu[b], wd[b])):
            nh = dst.t.shape[1] // 2
            for hh in range(2):
                def gw(e, src=src, dst=dst, hh=hh, nh=nh, i=i):
                    if getattr(k, "bc_val", None) is None:
                        r_ = e.alloc_register("moe_bc")
                        e.reg_mov(r_, 2 * E * 128 * 2 - 1)
                        k.bc_val = e.snap(r_)
                    return e.indirect_dma_start(
                        out=dst[:, hh * nh:(hh + 1) * nh, :].rearrange("p a b -> p (a b)"), out_offset=None, in_=src,
                        in_offset=IOA(ap=widx[:, hh, i:i + 1], axis=0), bounds_check=k.bc_val, oob_is_err=False)
                k.P.add("pool", gw, _bufs([widx]), _bufs([dst]), dma=True, partial=(hh == 1))
    st = contextlib.ExitStack()
    k.st = st
    rw = k.sb(L + "rw", [128, 8, E], F32)
    k.dma("sp", rw[:, :, :], c["router_w"].rearrange("(kc p) e -> p kc e", p=128), w=[rw])
    rb = k.sb(L + "rb", [128, E], F32)
    k.dma("sp", rb[:, :], c["router_bias"].partition_broadcast(128), w=[rb])
    umat = k.sb(L + "umat", [128, 128], F32)
    k.dma("sp", umat[:, :], c["c_umat"], w=[umat])
    misc = k.sb(L + "misc", [128, 48], F32)
    k.dma("sp", misc[:, :], c["c_misc"], w=[misc])
    hin = [k.sb(f"{L}hin{i}", [128, D], F32) for i in range(3)]
    hTf = [k.sb(f"{L}hTf{i}", [128, 8, 128], F32) for i in range(2)]
    fused = bool(c.get("lg_fused", {}).get(layer))
    lg = c["lg_all"][layer] if fused else k.sb(L + "lg", [128, TH, E], F32)
    for tt in range(0 if fused else TH):
        hi = hin[tt % 3]
        hT_ = hTf[tt % 2]
        k.dma("sp", hi[:, :], hin_d[tt * 128:(tt + 1) * 128, :], r=[B_hin[tt]], w=[hi], partial=False)
        for half in range(2):
            pb = PS[(tt % 2) * 2 + half]
            for j in range(4):
                kc = half * 4 + j
                k.tr(pb[:, j * 128:(j + 1) * 128], hi[:, kc * 128:(kc + 1) * 128], ident_f[:, :], r=[hi, ident_f], w=[pb])
            if half == 0:
                k.act(hT_[:, 0:4, :], pb[:, :].rearrange("p (j c) -> p j c", j=4), AF.Copy, r=[pb], w=[hT_], partial=True)
            else:
                k.v("dve", "tensor_copy", r=[pb], w=[hT_], partial=True, out=hT_[:, 4:8, :], in_=pb[:, :].rearrange("p (j c) -> p j c", j=4))
        pl = PS[4 + tt % 2]
        for kc in range(8):
            k.mm(pl[:, 0:E], hT_[:, kc, :], rw[:, kc, :], kc == 0, kc == 7, r=[hT_, rw], w=[pl])
        k.act(lg[:, tt, :], pl[:, 0:E], AF.Exp, r=[pl], w=[lg], partial=True, scale=-1.0)
    T3 = lambda nm: k.sb(L + nm, [128, TH, E], F32)
    aff, sel, selm, mk1, selm2, mk2, w12, posall = (T3(n) for n in ("aff", "sel", "selm", "mk1", "selm2", "mk2", "w12", "posall"))
    p6 = k.sb(L + "p6", [128, TH * 4, 6], F32)
    gs = k.sb(L + "gs", [128, TH * 4], F32)
    gmax = k.sb(L + "gmax", [128, TH], F32)
    gm = k.sb(L + "gm", [128, TH * 4], F32)
    pen = k.sb(L + "pen", [128, TH * 4], F32)
    m1 = k.sb(L + "m1", [128, TH], F32)
    wsum = k.sb(L + "wsum", [128, TH], F32)
    V = lambda name, **kw: k.v("dve", name, **kw)
    V("tensor_scalar", r=[lg], w=[lg], out=lg[:, :, :], in0=lg[:, :, :], scalar1=1.0, scalar2=None, op0=ALU.add)
    V("reciprocal", r=[lg], w=[aff], out=aff[:, :, :], in_=lg[:, :, :])
    V("tensor_tensor", r=[aff, rb], w=[sel], out=sel[:, :, :], in0=aff[:, :, :], in1=bc(rb[:, :], [[0, TH], [1, E]]), op=ALU.add)
    s4 = sel[:, :, :].rearrange("p t (g i) -> p (t g) i", g=4)
    V("tensor_tensor", r=[sel], w=[p6], partial=True, out=p6[:, :, 0:3], in0=s4[:, :, 0:3], in1=s4[:, :, 1:4], op=ALU.add)
    V("tensor_tensor", r=[sel], w=[p6], partial=True, out=p6[:, :, 3:5], in0=s4[:, :, 0:2], in1=s4[:, :, 2:4], op=ALU.add)
    V("tensor_tensor", r=[sel], w=[p6], partial=True, out=p6[:, :, 5:6], in0=s4[:, :, 0:1], in1=s4[:, :, 3:4], op=ALU.add)
    V("tensor_reduce", r=[p6], w=[gs], out=gs[:, :], in_=p6[:, :, :], axis=AX.X, op=ALU.max)
    V("tensor_reduce", r=[gs], w=[gmax], out=gmax[:, :], in_=gs[:, :].rearrange("p (t g) -> p t g", g=4), axis=AX.X, op=ALU.max)
    V("tensor_tensor", r=[gs, gmax], w=[gm], out=gm[:, :].rearrange("p (t g) -> p t g", g=4),
      in0=gs[:, :].rearrange("p (t g) -> p t g", g=4), in1=bc(gmax[:, :], [[1, TH], [0, 4]]), op=ALU.is_ge)
    V("tensor_scalar", r=[gm], w=[pen], out=pen[:, :], in0=gm[:, :], scalar1=-1.0, scalar2=1.0e4, op0=ALU.add, op1=ALU.mult)
    sm4 = selm[:, :, :].rearrange("p t (g i) -> p (t g) i", g=4)
    V("tensor_tensor", r=[sel, gm], w=[selm], out=sm4, in0=s4, in1=bc(gm[:, :], [[1, TH * 4], [0, 4]]), op=ALU.mult)
    V("tensor_tensor", r=[selm, pen], w=[selm], out=sm4, in0=sm4, in1=bc(pen[:, :], [[1, TH * 4], [0, 4]]), op=ALU.add)
    V("tensor_reduce", r=[selm], w=[m1], out=m1[:, :], in_=selm[:, :, :], axis=AX.X, op=ALU.max)
    V("tensor_tensor", r=[selm, m1], w=[mk1], out=mk1[:, :, :], in0=selm[:, :, :], in1=bc(m1[:, :], [[1, TH], [0, E]]), op=ALU.is_ge)
    V("scalar_tensor_tensor", r=[mk1, selm], w=[selm2], out=selm2[:, :, :], in0=mk1[:, :, :], scalar=-1.0e4, in1=selm[:, :, :],
      op0=ALU.mult, op1=ALU.add)
    V("tensor_reduce", r=[selm2], w=[m1], out=m1[:, :], in_=selm2[:, :, :], axis=AX.X, op=ALU.max)
    V("tensor_tensor", r=[selm2, m1], w=[mk2], out=mk2[:, :, :], in0=selm2[:, :, :], in1=bc(m1[:, :], [[1, TH], [0, E]]), op=ALU.is_ge)
    V("tensor_tensor", r=[mk1, aff], w=[w12], out=w12[:, :, :], in0=mk1[:, :, :], in1=aff[:, :, :], op=ALU.mult)
    V("tensor_reduce", r=[w12], w=[g1], out=g1[:, :], in_=w12[:, :, :], axis=AX.X, op=ALU.add)
    V("tensor_tensor", r=[mk2, aff], w=[w12], out=w12[:, :, :], in0=mk2[:, :, :], in1=aff[:, :, :], op=ALU.mult)
    V("tensor_reduce", r=[w12], w=[g2], out=g2[:, :], in_=w12[:, :, :], axis=AX.X, op=ALU.add)
    V("tensor_tensor", r=[g1, g2], w=[wsum], out=wsum[:, :], in0=g1[:, :], in1=g2[:, :], op=ALU.add)
    V("reciprocal", r=[wsum], w=[wsum], out=wsum[:, :], in_=wsum[:, :])
    V("tensor_tensor", r=[g1, wsum], w=[g1], out=g1[:, :], in0=g1[:, :], in1=wsum[:, :], op=ALU.mult)
    V("tensor_tensor", r=[g2, wsum], w=[g2], out=g2[:, :], in0=g2[:, :], in1=wsum[:, :], op=ALU.mult)
    mk = w12
    V("tensor_tensor", r=[mk1, mk2], w=[mk], out=mk[:, :, :], in0=mk1[:, :, :], in1=mk2[:, :, :], op=ALU.add)
    PRE, TOT = PS[6], PS[7]
    mk2d = mk[:, :, :].rearrange("p t e -> p (t e)")
    k.mm(PRE[:, :], umat[:, :], mk2d, True, True, r=[umat, mk], w=[PRE])
    k.mm(TOT[:, :], ones_f[:, :], mk2d, True, True, r=[ones_f, mk], w=[TOT])
    tot = sel
    base = selm
    V("tensor_copy", r=[TOT], w=[tot], out=tot[:, :, :], in_=TOT[:, :].rearrange("p (t e) -> p t e", e=E))
    base_b = [Buf(f"{L}base{t}") for t in range(TH)]
    V("memset", w=[base_b[0]], ap=base[:, 0, :], constant=0.0)
    for t in range(1, TH):
        V("tensor_tensor", r=[base_b[t - 1], tot], w=[base_b[t]], out=base[:, t, :], in0=base[:, t - 1, :], in1=tot[:, t - 1, :], op=ALU.add)
    sm16 = k.sb(L + "sm16", [128, 6, E], F32)
    cmp8 = k.sb(L + "cmp8", [128, E, 8], F32)
    V("tensor_tensor", r=[base_b[TH - 1], tot], w=[sm16], out=sm16[:, 0, :], in0=base[:, TH - 1, :], in1=tot[:, TH - 1, :], op=ALU.add)
    V("tensor_tensor", r=[sm16, misc], w=[cmp8], out=cmp8[:, :, :], in0=bc(sm16[:, 0, :], [[1, E], [0, 8]]),
      in1=bc(misc[:, 0:8], [[0, E], [1, 8]]), op=ALU.is_gt)
    V("tensor_reduce", r=[cmp8], w=[sm16], out=sm16[:, 1, :], in_=cmp8[:, :, :], axis=AX.X, op=ALU.add)
    V("tensor_scalar", r=[sm16], w=[sm16], out=sm16[:, 2, :], in0=sm16[:, 1, :], scalar1=512.0, scalar2=None, op0=ALU.mult)
    V("memset", w=[sm16], ap=sm16[:, 3, 0:1], constant=0.0)
    for e in range(1, E):
        V("tensor_tensor", r=[sm16], w=[sm16], out=sm16[:, 3, e:e + 1], in0=sm16[:, 3, e - 1:e], in1=sm16[:, 2, e - 1:e], op=ALU.add)
    V("tensor_tensor", r=[sm16], w=[sm16], out=sm16[:, 4, :], in0=sm16[:, 3, :], in1=sm16[:, 2, :], op=ALU.add)
    V("tensor_tensor", r=[PRE] + base_b, w=[posall], out=posall[:, :, :], in0=PRE[:, :].rearrange("p (t e) -> p t e", e=E), in1=base[:, :, :],
      op=ALU.add)
    V("tensor_tensor", r=[posall, sm16], w=[posall], out=posall[:, :, :], in0=posall[:, :, :], in1=bc(sm16[:, 3, :], [[0, TH], [1, E]]),
      op=ALU.add)
    posf = k.sb(L + "posf", [128, 2, TH], F32)
    V("tensor_tensor", r=[mk1, posall], w=[mk1], out=mk1[:, :, :], in0=mk1[:, :, :], in1=posall[:, :, :], op=ALU.mult)
    V("tensor_reduce", r=[mk1], w=[posf], out=posf[:, 0, :], in_=mk1[:, :, :], axis=AX.X, op=ALU.add)
    V("tensor_tensor", r=[mk2, posall], w=[mk2], out=mk2[:, :, :], in0=mk2[:, :, :], in1=posall[:, :, :], op=ALU.mult)
    V("tensor_reduce", r=[mk2], w=[posf], out=posf[:, 1, :], in_=mk2[:, :, :], axis=AX.X, op=ALU.add)
    V("tensor_copy", r=[posf], w=[pos1_i], out=pos1_i[:, :], in_=posf[:, 0, :])
    V("tensor_copy", r=[posf], w=[pos2_i], out=pos2_i[:, :], in_=posf[:, 1, :])
    icmp = k.sb(L + "icmp", [128, N_ITEMS, E], F32)
    ei = k.sb(L + "ei", [128, N_ITEMS], F32)
    V("tensor_tensor", r=[sm16, misc], w=[icmp], out=icmp[:, :, :], in0=bc(sm16[:, 4, :], [[0, N_ITEMS], [1, E]]),
      in1=bc(misc[:, 8:40], [[1, N_ITEMS], [0, E]]), op=ALU.is_le)
    V("tensor_reduce", r=[icmp], w=[ei], out=ei[:, :], in_=icmp[:, :, :], axis=AX.X, op=ALU.add)
    oob = k.sb(L + "oob", [128, N_ITEMS], F32)
    V("tensor_scalar", r=[ei], w=[oob], out=oob[:, :], in0=ei[:, :], scalar1=float(E), scalar2=4.0e6, op0=ALU.is_ge, op1=ALU.mult)
    V("tensor_scalar", r=[ei], w=[ei], out=ei[:, :], in0=ei[:, :], scalar1=15.0, scalar2=float(layer * E), op0=ALU.min, op1=ALU.add)
    V("tensor_scalar", r=[ei], w=[ei], out=ei[:, :], in0=ei[:, :], scalar1=256.0, scalar2=None, op0=ALU.mult)
    V("tensor_tensor", r=[ei, oob], w=[ei], out=ei[:, :], in0=ei[:, :], in1=oob[:, :], op=ALU.add)
    V("scalar_tensor_tensor", r=[misc, ei], w=[ei], out=ei[:, :], in0=bc(misc[:, 40:41], [[0, N_ITEMS]]), scalar=2.0, in1=ei[:, :],
      op0=ALU.mult, op1=ALU.add)
    V("tensor_copy", r=[ei], w=[widx], partial=True, out=widx[:, 0, :], in_=ei[:, :])
    V("tensor_scalar", r=[ei], w=[ei], out=ei[:, :], in0=ei[:, :], scalar1=1.0, scalar2=None, op0=ALU.add)
    V("tensor_copy", r=[ei], w=[widx], partial=True, out=widx[:, 1, :], in_=ei[:, :])
    stage = c.get("moe_stage", "C")
    if "dbg_outs" in c:
        do = c["dbg_outs"]
        for nm, t_ in (("d_pos1", pos1_i), ("d_pos2", pos2_i), ("d_g1", g1), ("d_g2", g2)):
            k.out_ops.append(k.dma("sp", do[nm], t_[:, :], r=[t_]))
        k.out_ops.append(k.dma("sp", do["d_widx"], widx[:, :, :], r=[widx]))
    if stage == "P":
        st.close(); stR.close(); k.P.fence(); k.st = st_outer
        return
    xb = [k.sb(f"{L}xb{i}", [128, D], BF16) for i in range(3)]
    item_loads_w(0)
    item_loads_w(1)
    for tt in range(TH):
        x_ = xb[tt % 3]
        hi = hin[tt % 3]
        k.dma("sp", hi[:, :], hin_d[tt * 128:(tt + 1) * 128, :], r=[B_hin[tt]], w=[hi], partial=False)
        k.act(x_[:, :], hi[:, :], AF.Copy, r=[hi], w=[x_])
        for pi_ in (pos1_i, pos2_i):
            k.P.add("pool", (lambda e, x_=x_, pi_=pi_, tt=tt: e.indirect_dma_start(
                out=xsl_d, out_offset=IOA(ap=pi_[:, tt:tt + 1], axis=0), in_=x_[:, :], in_offset=None)),
                _bufs([x_, pi_]), [B_xsl], dma=True, partial=True)
    st.close()
    k.P.fence()
    if stage == "S":
        stR.close(); k.st = st_outer
        return
    st = contextlib.ExitStack()
    k.st = st
    xi = [k.sb(f"{L}xi{i}", [128, 4, D], BF16) for i in range(2)]
    xTi = [k.sb(f"{L}xTi{i}", [128, 8, 512], BF16) for i in range(2)]
    hT = [k.sb(f"{L}hT{i}", [128, 4, 512], BF16) for i in range(2)]
    sg = [k.sb(f"{L}sg{i}", [128, 512], BF16) for i in range(2)]
    yi = [k.sb(f"{L}yi{i}", [128, 4, D], F32) for i in range(2)]
    xsl_v = xsl_d.rearrange("(i s p) d -> i p s d", p=128, s=4)
    ysl_v = ysl_d.rearrange("(i s p) d -> i p s d", p=128, s=4)
    n_g = 0
    n_d = 0

    def item_loads(i, with_w=True):
        b = i % 2
        if with_w:
            item_loads_w(i)
        k.dma("sp", xi[b][:, :, :], xsl_v[i], r=[B_xsl], w=[xi[b]], partial=False)

    item_loads(0, with_w=False)
    for i in range(N_ITEMS):
        b = i % 2
        if i + 1 < N_ITEMS:
            item_loads(i + 1, with_w=(i + 1 >= 2))
        xT_ = xTi[b]
        for s_ in range(4):
            pb = PS[6 + s_ % 2]
            pv = pb[:, :].bitcast(BF16)
            for kc in range(8):
                k.tr(pv[:, kc * 128:(kc + 1) * 128], xi[b][:, s_, kc * 128:(kc + 1) * 128], ident_b[:, :], r=[xi[b], ident_b], w=[pb])
            if s_ % 2:
                k.act(xT_[:, :, s_ * 128:(s_ + 1) * 128], pv.rearrange("p (k t) -> p k t", k=8), AF.Copy, r=[pb], w=[xT_], partial=True)
            else:
                k.v("dve", "tensor_copy", r=[pb], w=[xT_], partial=True, out=xT_[:, :, s_ * 128:(s_ + 1) * 128],
                    in_=pv.rearrange("p (k t) -> p k t", k=8))
        hTt = hT[b]
        for fc in range(4):
            G = PS[n_g % 2]
            U = PS[2 + n_g % 2]
            s2 = sg[n_g % 2]
            n_g += 1
            for kc in range(8):
                k.mm(G[:, :], wg[b][:, kc, fc * 128:(fc + 1) * 128], xT_[:, kc, :], kc == 0, kc == 7, r=[wg[b], xT_], w=[G])
            for kc in range(8):
                k.mm(U[:, :], wu[b][:, kc, fc * 128:(fc + 1) * 128], xT_[:, kc, :], kc == 0, kc == 7, r=[wu[b], xT_], w=[U])
            k.act(s2[:, :], G[:, :], AF.Silu, r=[G], w=[s2])
            k.v("dve", "tensor_tensor", r=[s2, U], w=[hTt], partial=True, out=hTt[:, fc, :], in0=s2[:, :], in1=U[:, :], op=ALU.mult)
        y_ = yi[b]
        for tl in range(4):
            for dh in range(2):
                Dp = PS[4 + n_d % 2]
                n_d += 1
                for fc in range(4):
                    k.mm(Dp[:, :], hTt[:, fc, tl * 128:(tl + 1) * 128], wd[b][:, fc, dh * 512:(dh + 1) * 512], fc == 0, fc == 3,
                         r=[hTt, wd[b]], w=[Dp])
                if n_d % 2:
                    k.act(y_[:, tl, dh * 512:(dh + 1) * 512], Dp[:, :], AF.Copy, r=[Dp], w=[y_], partial=True)
                else:
                    k.v("dve", "tensor_copy", r=[Dp], w=[y_], partial=True, out=y_[:, tl, dh * 512:(dh + 1) * 512], in_=Dp[:, :])
        k.dma("sp", ysl_v[i], y_[:, :, :], r=[y_], w=[B_ysl])
    st.close()
    k.P.fence()
    if stage == "X":
        stR.close(); k.st = st_outer
        return
    st = contextlib.ExitStack()
    k.st = st
    g_bc, b_bc = load_ln(k, c["ln_ffn_g"], c["ln_ffn_b"], layer)
    NBC = 4
    hin = [k.sb(f"{L}chin{i}", [128, D], F32) for i in range(NBC)]
    y1 = [k.sb(f"{L}y1{i}", [128, D], F32) for i in range(NBC)]
    y2 = [k.sb(f"{L}y2{i}", [128, D], F32) for i in range(NBC)]
    res = [k.sb(f"{L}res{i}", [128, D], F32) for i in range(2)]
    lntmp = (k.sb(L + "ln_stats", [128, 12], F32), k.sb(L + "ln_mv", [128, 2], F32), k.sb(L + "ln_lnv", [128, 1], F32),
             k.sb(L + "ln_rstd", [128, 1], F32))

    def c_issue(tt):
        b = tt % NBC
        k.dma("sp", hin[b][:, :], hin_d[tt * 128:(tt + 1) * 128, :], r=[B_hin[tt]], w=[hin[b]], partial=False)
        for y_, pi_ in ((y1[b], pos1_i), (y2[b], pos2_i)):
            k.P.add("pool", (lambda e, y_=y_, pi_=pi_, tt=tt: e.indirect_dma_start(
                out=y_[:, :], out_offset=None, in_=ysl_d, in_offset=IOA(ap=pi_[:, tt:tt + 1], axis=0))),
                _bufs([pi_]) + [B_ysl], _bufs([y_]), dma=True, partial=False)

    c_issue(0)
    c_issue(1)
    for tt in range(TH):
        b = tt % NBC
        if tt + 2 < TH:
            c_issue(tt + 2)
        hi = hin[b]
        r_ = res[tt % 2]
        k.v("dve", "tensor_scalar", r=[y1[b], g1], w=[y1[b]], out=y1[b][:, :], in0=y1[b][:, :], scalar1=g1[:, tt:tt + 1], scalar2=None, op0=ALU.mult)
        k.v("dve", "scalar_tensor_tensor", r=[y2[b], g2, y1[b]], w=[y1[b]], out=y1[b][:, :], in0=y2[b][:, :], scalar=g2[:, tt:tt + 1], in1=y1[b][:, :],
            op0=ALU.mult, op1=ALU.add)
        k.v("dve", "scalar_tensor_tensor", r=[hi, y1[b]], w=[r_], out=r_[:, :], in0=hi[:, :], scalar=ALPHA, in1=y1[b][:, :],
            op0=ALU.mult, op1=ALU.add)
        layer_norm_tile(k, r_, g_bc, b_bc, r_, lntmp, eps_ln, L, eng2="dve")
        op = k.dma("sp", hout_d[tt * 128:(tt + 1) * 128, :], r_[:, :], r=[r_], w=[B_hout[tt]] if B_hout is not None else [])
        k.out_ops.append(op)
    st.close()
    stR.close()
    k.P.fence()
    k.st = st_outer


def _consts():
    j = np.arange(128)
    rmat = (j[:, None] <= j[None, :]).astype(np.float32)
    lmat = (j[:, None] > j[None, :]).astype(np.float32)
    inv = (10000.0 ** (-np.arange(0, 32, 2, dtype=np.float32) / 32)).astype(np.float32)
    ang = np.arange(S, dtype=np.float32)[:, None] * inv[None, :]
    cos, sin = np.cos(ang).astype(np.float32), np.sin(ang).astype(np.float32)
    rope = np.zeros((2, 128, S), np.float32)
    rope[0, 64:80] = cos.T
    rope[0, 80:96] = cos.T
    rope[1, 64:80] = -sin.T
    rope[1, 80:96] = sin.T
    kk = np.arange(128)[:, None, None]
    jj = np.arange(4)[None, :, None]
    qq = np.arange(512)[None, None, :]
    amask = ((qq // 64) >= ((jj * 128 + kk) // 64)).astype(np.float32)
    umat = (j[:, None] < j[None, :]).astype(np.float32)
    misc = np.zeros((128, 48), np.float32)
    misc[:, 0:8] = 512.0 * np.arange(8)[None, :]
    misc[:, 8:40] = 512.0 * np.arange(32)[None, :]
    misc[:, 40] = np.arange(128)
    return {"c_ident": np.eye(128, dtype=np.float32), "c_rmat": rmat, "c_lmat": lmat, "c_rope": rope,
            "c_amask": np.ascontiguousarray(amask), "c_umat": umat, "c_misc": misc}


def prep_shared(inp):
    f = lambda a: np.ascontiguousarray(np.asarray(a, dtype=np.float32))
    sh = {}
    sh["ssd_w_in"] = f(inp["ssd_w_in"][0])
    cw = np.asarray(inp["ssd_conv_w"][0], np.float32)
    sh["ssd_conv_w"] = f(cw.reshape(4, 32, 128).transpose(2, 1, 0).reshape(128, 128))
    sh["ssd_conv_b"] = f(np.asarray(inp["ssd_conv_b"][0], np.float32).reshape(32, 128).T)
    for n in ("ssd_dt_bias", "ssd_a_log", "ssd_d", "ssd_norm_w", "ssd_w_out", "mla_w_down", "mla_q_norm", "mla_w_uq",
              "mla_kv_norm", "mla_w_ukv", "mla_w_out"):
        sh[n] = f(inp[n][0])
    wd = sh["mla_w_down"]
    sh["mla_w_kr"] = f(np.concatenate([wd[:, 0:64], wd[:, 640:672], wd[:, 0:64], wd[:, 656:672], wd[:, 640:656]], axis=1))
    wkv = sh.pop("mla_w_ukv").reshape(256, 16, 128)
    sh["mla_w_kn"] = f(wkv[:, :, 0:64].reshape(256, 1024))
    sh["mla_w_v"] = f(wkv[:, :, 64:128].reshape(256, 1024))
    wq = sh["mla_w_uq"].reshape(384, 16, 96)
    sh["mla_w_uq_sw"] = f(np.concatenate([wq[:, :, 0:64], wq[:, :, 80:96], wq[:, :, 64:80]], axis=2).reshape(384, 1536))
    for n in ("router_w", "router_bias", "ln_mix_g", "ln_mix_b", "ln_ffn_g", "ln_ffn_b"):
        sh[n] = f(inp[n])
    for n in ("moe_w_gate", "moe_w_up"):
        w = np.asarray(inp[n], np.float32).reshape(2, E, 8, 128, DFF)
        sh[n] = f(w.transpose(0, 1, 3, 2, 4).reshape(2 * E * 128 * 2, 2048))
    w = np.asarray(inp["moe_w_down"], np.float32).reshape(2, E, 4, 128, D)
    sh["moe_w_down"] = f(w.transpose(0, 1, 3, 2, 4).reshape(2 * E * 128 * 2, 2048))
    sh.update(_consts())
    return sh


_NC_CACHE = {}


def kernel(**inputs):
    sh = prep_shared(inputs)
    x = np.asarray(inputs["x"], np.float32)
    if "nc" not in _NC_CACHE:
        _NC_CACHE["nc"] = build_program()
    nc = _NC_CACHE["nc"]
    in_maps = []
    for b in range(8):
        m = dict(sh)
        m["x"] = np.ascontiguousarray(x[b])
        in_maps.append(m)
    res = run_bass_kernel_spmd(nc, in_maps, core_ids=list(range(8)))
    return np.stack([np.asarray(r["out"], np.float32) for r in res.results], axis=0)
```

```python
import contextlib
import math
import numpy as np
import ml_dtypes
import concourse.bass as bass
import concourse.mybir as mybir
from concourse.bass_utils import run_bass_kernel_spmd

F32 = mybir.dt.float32
BF16 = mybir.dt.bfloat16
AF = mybir.ActivationFunctionType
ALU = mybir.AluOpType
AX = mybir.AxisListType

KEEP_WARM = 0
N_DMA_SEMS = {"sp": 12, "pool": 8, "act": 4}

S = 4096
D = 1024
NT = S // 128
ALPHA = (2.0 * 2) ** 0.25
LN_EPS = 1e-5
RMS_EPS = 1e-6
NH_SSD = 32
E = 16
DFF = 512
MLA_H = 16


class Buf:
    __slots__ = ("name", "gen_w", "gen_r", "prev_w", "prev_r")

    def __init__(self, name=""):
        self.name = name
        self.gen_w = []
        self.gen_r = []
        self.prev_w = []
        self.prev_r = []


class Op:
    __slots__ = ("eng", "fn", "deps", "is_dma", "idx", "needed", "sig", "dma_i")

    def __init__(self, eng, fn, is_dma, idx):
        self.eng = eng
        self.fn = fn
        self.deps = []
        self.is_dma = is_dma
        self.idx = idx
        self.needed = False
        self.sig = None
        self.dma_i = None


class Prog:
    def __init__(self, nc):
        self.nc = nc
        self.ops = []
        self.by_eng = {e: [] for e in ("pe", "act", "dve", "pool", "sp")}
        self.dma_count = {q: 0 for q in N_DMA_SEMS}
        self.fence_deps = []
        self.fence_seen = set()

    def fence(self):
        deps = []
        for e, lst in self.by_eng.items():
            last_c = None
            dmas = []
            for o in reversed(lst):
                if o.is_dma:
                    if len(dmas) < N_DMA_SEMS[e]:
                        dmas.append(o)
                elif last_c is None:
                    last_c = o
                if last_c is not None and (e not in N_DMA_SEMS or len(dmas) >= N_DMA_SEMS[e]):
                    break
            if last_c is not None:
                deps.append(last_c)
            deps.extend(dmas)
        self.fence_deps = deps
        self.fence_seen = set()

    def add(self, eng, fn, reads=(), writes=(), dma=False, partial=False):
        op = Op(eng, fn, dma, len(self.ops))
        deps = {}
        if self.fence_deps and eng not in self.fence_seen:
            self.fence_seen.add(eng)
            for o in self.fence_deps:
                deps[o.idx] = o

        def dep(o):
            if o is op:
                return
            if (not o.is_dma) and (not dma) and o.eng == eng and eng == "pe":
                return
            deps[o.idx] = o

        for b in reads:
            for w in b.gen_w:
                dep(w)
        for b in writes:
            if b.gen_r or not partial or not b.gen_w:
                for r in b.gen_r:
                    dep(r)
                for w in b.gen_w:
                    dep(w)
                b.prev_w, b.prev_r = b.gen_w, b.gen_r
                b.gen_w, b.gen_r = [op], []
            else:
                for r in b.prev_r:
                    dep(r)
                for w in b.prev_w:
                    dep(w)
                b.gen_w.append(op)
        for b in reads:
            if b in writes:
                continue
            if dma:
                b.gen_r.append(op)
            else:
                b.gen_r = [r for r in b.gen_r if r.is_dma or r.eng != eng]
                b.gen_r.append(op)
        for b in writes:
            if not dma and len(b.gen_w) > 1:
                b.gen_w = [w for w in b.gen_w if w.is_dma or w.eng != eng or w is op]
        op.deps = list(deps.values())
        for d in op.deps:
            d.needed = True
        self.ops.append(op)
        self.by_eng[eng].append(op)
        if dma:
            op.dma_i = self.dma_count[eng]
            self.dma_count[eng] += 1
        return op

    def emit(self, st, final_wait_ops=()):
        nc = self.nc
        esem = {e: st.enter_context(nc.semaphore("s_" + e)) for e in self.by_eng}
        dsem = {
            q: [st.enter_context(nc.semaphore(f"d_{q}{i}")) for i in range(n)]
            for q, n in N_DMA_SEMS.items()
        }
        cnt = {e: 0 for e in self.by_eng}
        for op in self.ops:
            if op.is_dma:
                n = N_DMA_SEMS[op.eng]
                j = op.dma_i % n
                op.sig = (dsem[op.eng][j], 16 * (op.dma_i // n + 1))
            elif op.needed:
                cnt[op.eng] += 1
                op.sig = (esem[op.eng], cnt[op.eng])
        self.final_counts = dict(cnt)
        block = st.enter_context(nc.Block())

        def run_engine(ename, eng):
            known = {}

            def wait(sig):
                sem, val = sig
                if known.get(sem.num, 0) >= val:
                    return
                eng.wait_ge(sem, val)
                known[sem.num] = val

            for op in self.by_eng[ename]:
                for d in op.deps:
                    wait(d.sig)
                if op.is_dma:
                    sem, val = op.sig
                    if val > 16:
                        wait((sem, val - 16))
                    ins = op.fn(eng)
                    ins.then_inc(sem, 16)
                else:
                    ins = op.fn(eng)
                    if op.sig is not None:
                        ins.then_inc(op.sig[0], 1)
            if ename == "sp":
                for op in final_wait_ops:
                    wait(op.sig)

        @block.tensor
        def _(e):
            run_engine("pe", e)

        @block.scalar
        def _(e):
            run_engine("act", e)

        @block.vector
        def _(e):
            run_engine("dve", e)

        @block.gpsimd
        def _(e):
            run_engine("pool", e)

        @block.sync
        def _(e):
            run_engine("sp", e)


class TT:
    def __init__(self, t, name):
        self.t = t
        self.b = Buf(name)

    def __getitem__(self, k):
        return self.t[k]


def _bufs(xs):
    out = []
    for x in xs:
        if x is None:
            continue
        out.append(x.b if isinstance(x, TT) else x)
    return out


class K:
    def __init__(self, nc, st):
        self.nc = nc
        self.st = st
        self.P = Prog(nc)
        self.out_ops = []

    def sb(self, name, shape, dt):
        return TT(self.st.enter_context(self.nc.sbuf_tensor(name, shape, dt)), name)

    def ps(self, name, shape, dt):
        return TT(self.st.enter_context(self.nc.psum_tensor(name, shape, dt)), name)

    def dram(self, name, shape, dt, kind="Internal"):
        return self.nc.dram_tensor(name, shape, dt, kind=kind).ap()

    def op(self, eng, fn, r=(), w=(), partial=False):
        return self.P.add(eng, fn, _bufs(r), _bufs(w), dma=False, partial=partial)

    def dma(self, q, out, in_, r=(), w=(), partial=True):
        return self.P.add(q, lambda e: e.dma_start(out=out, in_=in_), _bufs(r), _bufs(w), dma=True, partial=partial)

    def mm(self, out, lhsT, rhs, start, stop, r=(), w=()):
        return self.P.add("pe", lambda e: e.matmul(out, lhsT=lhsT, rhs=rhs, start=start, stop=stop),
                          _bufs(r), _bufs(w), partial=True)

    def tr(self, out, in_, ident, r=(), w=()):
        return self.P.add("pe", lambda e: e.transpose(out=out, in_=in_, identity=ident),
                          _bufs(r), _bufs(w), partial=True)

    def act(self, out, in_, func, r=(), w=(), partial=False, **kw):
        return self.P.add("act", lambda e: e.activation(out=out, in_=in_, func=func, **kw),
                          _bufs(r), _bufs(w), partial=partial)

    def v(self, eng, name, r=(), w=(), partial=False, **kw):
        return self.P.add(eng, lambda e: getattr(e, name)(**kw), _bufs(r), _bufs(w), partial=partial)


def bc(ap, dims):
    a = list(ap.ap)
    return bass.AP(ap.tensor, ap.offset, [list(a[0])] + [list(d) for d in dims])


def layer_norm_tile(k, r, g_bc, b_bc, out, tmp, eps_t, tag, eng2="dve"):
    stats, mv, lnv, rstd = tmp
    for i in range(2):
        k.v("dve", "bn_stats", r=[r], w=[stats], partial=True, out=stats[:, i * 6:(i + 1) * 6], in_=r[:, i * 512:(i + 1) * 512])
    k.v("dve", "bn_aggr", r=[stats], w=[mv], out=mv[:, 0:2], in_=stats[:, 0:12])
    k.act(lnv[:, 0:1], mv[:, 1:2], AF.Ln, r=[mv, eps_t], w=[lnv], bias=eps_t[:, 0:1])
    k.act(rstd[:, 0:1], lnv[:, 0:1], AF.Exp, r=[lnv], w=[rstd], scale=-0.5)
    k.v("dve", "tensor_scalar", r=[r, mv, rstd], w=[r], out=r[:, :], in0=r[:, :], scalar1=mv[:, 0:1], scalar2=rstd[:, 0:1],
        op0=ALU.subtract, op1=ALU.mult)
    k.v(eng2, "tensor_tensor", r=[r, g_bc], w=[r], out=r[:, :], in0=r[:, :], in1=g_bc[:, :], op=ALU.mult)
    k.v(eng2, "tensor_tensor", r=[r, b_bc], w=[out], out=out[:, :], in0=r[:, :], in1=b_bc[:, :], op=ALU.add)


def build_program(stop_after=None, dbg=False):
    nc = bass.Bass("TRN2", target_bir_lowering=False)
    st = contextlib.ExitStack()
    k = K(nc, st)

    def din(name, shape, dt=F32):
        return nc.dram_tensor(name, list(shape), dt, kind="ExternalInput").ap()

    x_d = din("x", [S, D])
    ssd_w_in = din("ssd_w_in", [D, 6176])
    ssd_conv_w = din("ssd_conv_w", [4, 4096])
    ssd_conv_b = din("ssd_conv_b", [4096])
    ssd_dt_bias = din("ssd_dt_bias", [32])
    ssd_a_log = din("ssd_a_log", [32])
    ssd_d = din("ssd_d", [32])
    ssd_norm_w = din("ssd_norm_w", [2048])
    ssd_w_out = din("ssd_w_out", [2048, D])
    mla_w_down = din("mla_w_down", [D, 672])
    mla_w_kr = din("mla_w_kr", [D, 192])
    mla_q_norm = din("mla_q_norm", [384])
    mla_w_uq = din("mla_w_uq", [384, 1536])
    mla_w_uq_sw = din("mla_w_uq_sw", [384, 1536])
    mla_kv_norm = din("mla_kv_norm", [256])
    mla_w_kn = din("mla_w_kn", [256, 1024])
    mla_w_v = din("mla_w_v", [256, 1024])
    mla_w_out = din("mla_w_out", [D, D])
    router_w = din("router_w", [D, E])
    router_bias = din("router_bias", [E])
    moe_w_gate = din("moe_w_gate", [2 * E * 128 * 2, 2048])
    moe_w_up = din("moe_w_up", [2 * E * 128 * 2, 2048])
    moe_w_down = din("moe_w_down", [2 * E * 128 * 2, 2048])
    ln_mix_g = din("ln_mix_g", [2, D])
    ln_mix_b = din("ln_mix_b", [2, D])
    ln_ffn_g = din("ln_ffn_g", [2, D])
    ln_ffn_b = din("ln_ffn_b", [2, D])
    c_ident = din("c_ident", [128, 128])
    c_rmat = din("c_rmat", [128, 128])
    c_lmat = din("c_lmat", [128, 128])
    c_rope = din("c_rope", [2, 128, S])
    c_amask = din("c_amask", [128, 4, 512])
    c_umat = din("c_umat", [128, 128])
    c_misc = din("c_misc", [128, 48])
    out_d = nc.dram_tensor("out", [S, D], F32, kind="ExternalOutput").ap()

    h1_d = k.dram("h1_d", [S, D], F32, kind="ExternalOutput" if dbg else "Internal")
    h2_d = k.dram("h2_d", [S, D], F32, kind="ExternalOutput" if dbg else "Internal")
    h3_d = k.dram("h3_d", [S, D], F32, kind="ExternalOutput" if dbg else "Internal")
    xs_d = k.dram("xs_d", [S, 2048], BF16)
    bt_d = k.dram("bt_d", [S, 1024], BF16)
    bT_d = k.dram("bT_d", [1024, S], BF16)
    cT_d = k.dram("cT_d", [1024, S], BF16)
    z_d = k.dram("z_d", [S, 2048], BF16)
    dt_d = k.dram("dt_d", [S, 32], F32)
    a_d = k.dram("a_d", [S, 32], F32)
    attnT_d = k.dram("attnT_d", [D, S], BF16)
    NSLOT = 16384
    xsl_d = k.dram("xsl_d", [NSLOT, D], BF16)
    ysl_d = k.dram("ysl_d", [NSLOT, D], F32)
    B_xsl = Buf("xsl"); B_ysl = Buf("ysl")
    zt = k.sb("zero_t", [128, D], BF16)
    k.v("pool", "memset", w=[zt], ap=zt[:, :], constant=0.0)
    xsl_z = xsl_d.rearrange("(a p) d -> a p d", p=128)
    for a_ in range(NSLOT // 128):
        k.dma("sp", xsl_z[a_], zt[:, :], r=[zt], w=[B_xsl])
    B_h = [[Buf(f"h{i}_{t}") for t in range(NT)] for i in range(4)]
    B_xs = Buf("xs_d"); B_bt = Buf("bt_d"); B_bT = Buf("bT_d"); B_cT = Buf("cT_d")
    B_z = Buf("z_d"); B_dt = Buf("dt_d"); B_a = Buf("a_d"); B_attn = Buf("attn_d")
    B_out = Buf("out")

    ident_f = k.sb("ident_f", [128, 128], F32)
    ident_b = k.sb("ident_b", [128, 128], BF16)
    rmat_f = k.sb("rmat_f", [128, 128], F32)
    rmat_b = k.sb("rmat_b", [128, 128], BF16)
    lmat_f = k.sb("lmat_f", [128, 128], F32)
    lmat_b = k.sb("lmat_b", [128, 128], BF16)
    ones_f = k.sb("ones_f", [128, 128], F32)
    ones_b = k.sb("ones_b", [128, 128], BF16)
    eps_ln = k.sb("eps_ln", [128, 1], F32)
    eps_rms = k.sb("eps_rms", [128, 1], F32)
    k.dma("sp", ident_f[:, :], c_ident, w=[ident_f])
    k.dma("sp", rmat_f[:, :], c_rmat, w=[rmat_f])
    k.dma("sp", lmat_f[:, :], c_lmat, w=[lmat_f])
    k.v("dve", "tensor_copy", r=[ident_f], w=[ident_b], out=ident_b[:, :], in_=ident_f[:, :])
    k.v("dve", "tensor_copy", r=[rmat_f], w=[rmat_b], out=rmat_b[:, :], in_=rmat_f[:, :])
    k.v("dve", "tensor_copy", r=[lmat_f], w=[lmat_b], out=lmat_b[:, :], in_=lmat_f[:, :])
    k.v("dve", "memset", w=[ones_f], ap=ones_f[:, :], constant=1.0)
    k.v("dve", "memset", w=[ones_b], ap=ones_b[:, :], constant=1.0)
    k.v("dve", "memset", w=[eps_ln], ap=eps_ln[:, :], constant=LN_EPS)
    k.v("dve", "memset", w=[eps_rms], ap=eps_rms[:, :], constant=RMS_EPS)

    DBK = [k.ps(f"psd{i}", [128, 1024], F32) for i in range(2)]
    PS = []
    for i in range(4):
        v_ = TTview(DBK[i // 2], DBK[i // 2].t[:, (i % 2) * 512:(i % 2 + 1) * 512])
        v_.b = Buf(f"ps{i}")
        PS.append(v_)
    PS += [k.ps(f"ps{i}", [128, 512], F32) for i in range(4, 8)]

    rw_g = k.sb("rw_g", [128, 8, E], F32)
    k.dma("sp", rw_g[:, :, :], router_w.rearrange("(kc p) e -> p kc e", p=128), w=[rw_g])
    lg_all = [None, None]
    lg_fused = {}
    ctx = dict(locals())
    if stop_after is not None and stop_after.startswith("moeonly"):
        h1_in = din("h1_in", [S, D])
        ctx["moe_stage"] = stop_after.split(":")[1]
        ctx["dbg_outs"] = {}
        for nm, shp, dt_ in (("d_pos1", [128, NT], mybir.dt.int32), ("d_pos2", [128, NT], mybir.dt.int32),
                             ("d_widx", [128, 2, 32], mybir.dt.int32), ("d_g1", [128, NT], F32), ("d_g2", [128, NT], F32)):
            ctx["dbg_outs"][nm] = nc.dram_tensor(nm, shp, dt_, kind="ExternalOutput").ap()
        moe_layer(k, ctx, 0, h1_in, [Buf() for _ in range(NT)], h2_d, B_h[1], False)
        return finish(k, nc, st)
    ssd_layer(k, ctx)
    if stop_after == "ssd":
        return finish(k, nc, st)
    moe_layer(k, ctx, 0, h1_d, B_h[0], h2_d, B_h[1], False)
    if stop_after == "moe0":
        return finish(k, nc, st)
    mla_layer(k, ctx)
    if stop_after == "mla":
        return finish(k, nc, st)
    moe_layer(k, ctx, 1, h3_d, B_h[2], out_d, None, True)
    return finish(k, nc, st)


def route_step(k, c, layer, tt, src, hT_, tr_banks, lg_bank, step):
    ident_f, rw, lg = c["ident_f"], c["rw_g"], c["lg_all"][layer]
    if step < 2:
        half = step
        pb = tr_banks[half]
        for j in range(4):
            kc = half * 4 + j
            k.tr(pb[:, j * 128:(j + 1) * 128], src[:, kc * 128:(kc + 1) * 128], ident_f[:, :], r=[src, ident_f], w=[pb])
        k.act(hT_[:, half * 4:half * 4 + 4, :], pb[:, :].rearrange("p (j c) -> p j c", j=4), AF.Copy, r=[pb], w=[hT_], partial=True)
    else:
        for kc in range(8):
            k.mm(lg_bank[:, 0:E], hT_[:, kc, :], rw[:, kc, :], kc == 0, kc == 7, r=[hT_, rw], w=[lg_bank])
        k.act(lg[:, tt, :], lg_bank[:, 0:E], AF.Exp, r=[lg_bank], w=[lg], partial=True, scale=-1.0)
        c["lg_fused"][layer] = True


def route_tile(k, c, layer, tt, src, hT_, tr_banks, lg_bank):
    for step in range(3):
        route_step(k, c, layer, tt, src, hT_, tr_banks, lg_bank, step)


def load_ln(k, g_src, b_src, layer):
    t = k.sb(f"lnp_{k.P.dma_count['sp']}", [128, 2, D], F32)
    k.dma("sp", t[:, 0, :], g_src[layer].partition_broadcast(128), w=[t])
    k.dma("sp", t[:, 1, :], b_src[layer].partition_broadcast(128), w=[t])
    return TTview(t, t.t[:, 0, :]), TTview(t, t.t[:, 1, :])


class TTview:
    def __init__(self, parent, ap):
        self.t = ap
        self.b = parent.b

    def __getitem__(self, k):
        return self.t[k]


def finish(k, nc, st):
    k.P.emit(st, final_wait_ops=k.out_ops)
    st.close()
    return nc


def _bufs(xs):
    out = []
    for x in xs:
        if x is None:
            continue
        out.append(getattr(x, "b", x))
    return out


def ssd_layer(k, c):
    nc = k.nc
    PS = c["PS"]
    x_d = c["x_d"]
    ident_b, rmat_f, rmat_b, lmat_f, lmat_b, ones_f, ones_b = (c[n] for n in
        ("ident_b", "rmat_f", "rmat_b", "lmat_f", "lmat_b", "ones_f", "ones_b"))
    xs_d, bt_d, bT_d, cT_d, z_d, dt_d, a_d = (c[n] for n in ("xs_d", "bt_d", "bT_d", "cT_d", "z_d", "dt_d", "a_d"))
    B_xs, B_bt, B_bT, B_cT, B_z, B_dt, B_a = (c[n] for n in ("B_xs", "B_bt", "B_bT", "B_cT", "B_z", "B_dt", "B_a"))
    st_outer = k.st
    st = contextlib.ExitStack()
    k.st = st

    xT = k.sb("xT", [128, 8, S], BF16)
    xT_b = [Buf(f"xT{t}") for t in range(NT)]
    st1a = contextlib.ExitStack()
    k.st = st1a
    xbf = [k.sb(f"xbf{i}", [128, D], BF16) for i in range(3)]
    for tt in range(NT):
        xb = xbf[tt % 3]
        k.dma("pool", xb[:, :], x_d[tt * 128:(tt + 1) * 128, :], w=[xb], partial=False)
        pb = PS[tt % 2]
        pv = pb[:, :].bitcast(BF16)
        for kc in range(8):
            k.tr(pv[:, kc * 128:(kc + 1) * 128], xb[:, kc * 128:(kc + 1) * 128], ident_b[:, :], r=[xb, ident_b], w=[pb])
        dst = xT.t[:, :, tt * 128:(tt + 1) * 128]
        src = pv.rearrange("p (k t) -> p k t", k=8)
        if tt % 2:
            k.act(dst, src, AF.Copy, r=[pb], w=[xT_b[tt]])
        else:
            k.v("dve", "tensor_copy", r=[pb], w=[xT_b[tt]], out=dst, in_=src)

    w_in_v = c["ssd_w_in"].rearrange("(kc p) c -> p kc c", p=128)
    wz = k.sb("wz", [128, 8, 2048], BF16)
    wz_b = [Buf(f"wz{i}") for i in range(4)]
    for cb in range(4):
        k.dma("pool", wz[:, :, cb * 512:(cb + 1) * 512], w_in_v[:, :, cb * 512:(cb + 1) * 512], w=[wz_b[cb]])
    wdt = k.sb("wdt", [128, 8, 32], BF16)
    k.dma("pool", wdt[:, :, :], w_in_v[:, :, 6144:6176], w=[wdt])
    dtraw = k.sb("dtraw", [128, NT, 32], F32)
    zs = [k.sb(f"zs{i}", [128, 2048], BF16) for i in range(2)]
    n_ps = 0
    for tt in range(NT):
        zt = zs[tt % 2]
        for cb in range(4):
            pb = PS[6 + n_ps % 2]
            n_ps += 1
            for kc in range(8):
                k.mm(pb[:, :], xT.t[:, kc, tt * 128:(tt + 1) * 128], wz[:, kc, cb * 512:(cb + 1) * 512], kc == 0, kc == 7,
                     r=[xT_b[tt], wz_b[cb]], w=[pb])
            k.act(zt[:, cb * 512:(cb + 1) * 512], pb[:, :], AF.Silu, r=[pb], w=[zt], partial=True)
        k.dma("sp", z_d[tt * 128:(tt + 1) * 128, :], zt[:, :], r=[zt], w=[B_z])
        pb = PS[6 + n_ps % 2]
        n_ps += 1
        for kc in range(8):
            k.mm(pb[:, 0:32], xT.t[:, kc, tt * 128:(tt + 1) * 128], wdt[:, kc, :], kc == 0, kc == 7, r=[xT_b[tt], wdt], w=[pb])
        k.v("dve", "tensor_copy", r=[pb], w=[dtraw], partial=True, out=dtraw[:, tt, :], in_=pb[:, 0:32])

    sm32 = k.sb("sm32", [128, 3, 32], F32)
    k.dma("sp", sm32[:, 0, :], c["ssd_dt_bias"].partition_broadcast(128), w=[sm32])
    k.dma("sp", sm32[:, 1, :], c["ssd_a_log"].partition_broadcast(128), w=[sm32])
    e1 = k.sb("e1", [128, NT, 32], F32)
    dtv = k.sb("dtv", [128, NT, 32], F32)
    av = k.sb("av", [128, NT, 32], F32)
    k.v("dve", "tensor_tensor", r=[dtraw, sm32], w=[dtraw], out=dtraw[:, :, :], in0=dtraw[:, :, :],
        in1=bc(sm32[:, 0, :], [[0, NT], [1, 32]]), op=ALU.add)
    k.act(e1[:, :, :], dtraw[:, :, :], AF.Exp, r=[dtraw], w=[e1])
    k.act(dtv[:, :, :], e1[:, :, :], AF.Ln, r=[e1], w=[dtv], bias=1.0)
    k.act(sm32[:, 2, :], sm32[:, 1, :], AF.Exp, r=[sm32], w=[sm32])
    k.v("dve", "scalar_tensor_tensor", r=[dtv, sm32], w=[av], out=av[:, :, :], in0=dtv[:, :, :], scalar=-1.0,
        in1=bc(sm32[:, 2, :], [[0, NT], [1, 32]]), op0=ALU.mult, op1=ALU.mult)
    k.dma("sp", dt_d.rearrange("(t p) h -> p t h", p=128), dtv[:, :, :], r=[dtv], w=[B_dt])
    k.dma("sp", a_d.rearrange("(t p) h -> p t h", p=128), av[:, :, :], r=[av], w=[B_a])

    st1a.close()
    k.P.fence()
    k.st = st
    convw = k.sb("convw", [128, 128], F32)
    convb = k.sb("convb", [128, 32], F32)
    k.dma("sp", convw[:, :], c["ssd_conv_w"], w=[convw])
    k.dma("sp", convb[:, :], c["ssd_conv_b"], w=[convb])
    diagw = k.sb("diagw", [128, 128, 128], BF16)
    k.v("dve", "tensor_tensor", r=[ident_b, convw], w=[diagw], out=diagw[:, :, :], in0=bc(ident_b[:, :], [[0, 128], [1, 128]]),
        in1=bc(convw[:, :], [[1, 128], [0, 128]]), op=ALU.mult)
    wch = [k.sb(f"wch{i}", [128, 8, 128], BF16) for i in range(3)]
    xpre = [k.sb(f"xpre{i}", [128, 3 + S], BF16) for i in range(2)]
    xact = [k.sb(f"xact{i}", [128, S], BF16) for i in range(2)]
    stg = [k.sb(f"stg{i}", [128, 8, 128], BF16) for i in range(3)]
    for xp in xpre:
        k.v("dve", "memset", w=[xp], ap=xp[:, 0:3], constant=0.0)
    xs_v = xs_d.rearrange("(t p) c -> p t c", p=128)
    bt_v = bt_d.rearrange("(t p) c -> p t c", p=128)
    n_ps = 0
    n_cv = 0
    n_tr = 0

    def conv_block(cc, tb, xp, xa):
        nonlocal n_cv
        cv = PS[6 + n_cv % 2]
        n_cv += 1
        for tap in range(4):
            k.mm(cv[:, :], diagw[:, cc * 4 + tap, :], xp[:, tb * 512 + tap:tb * 512 + tap + 512], tap == 0, tap == 3, r=[diagw, xp], w=[cv])
        k.act(xa[:, tb * 512:(tb + 1) * 512], cv[:, :], AF.Silu, r=[cv, convb], w=[xa], partial=True, bias=convb[:, cc:cc + 1])

    for cc in range(32):
        wc = wch[cc % 3]
        k.dma("pool", wc[:, :, :], w_in_v[:, :, 2048 + cc * 128:2048 + (cc + 1) * 128], w=[wc], partial=False)
        xp = xpre[cc % 2]
        xa = xact[cc % 2]
        for tb in range(8):
            pb = PS[2 + n_ps % 2]
            n_ps += 1
            for kc in range(8):
                k.mm(pb[:, :], wc[:, kc, :], xT.t[:, kc, tb * 512:(tb + 1) * 512], kc == 0, kc == 7,
                     r=[wc] + xT_b[4 * tb:4 * tb + 4], w=[pb])
            if tb > 0:
                conv_block(cc, tb - 1, xp, xa)
            k.act(xp[:, 3 + tb * 512:3 + (tb + 1) * 512], pb[:, :], AF.Copy, r=[pb], w=[xp], partial=True)
        conv_block(cc, 7, xp, xa)
        if cc < 24:
            dv, Bd, col = (xs_v, B_xs, cc * 128) if cc < 16 else (bt_v, B_bt, (cc - 16) * 128)
            for g8 in range(4):
                pb = PS[4 + n_tr % 2]
                sg = stg[n_tr % 3]
                n_tr += 1
                pv = pb[:, :].bitcast(BF16)
                for j in range(8):
                    t = g8 * 8 + j
                    k.tr(pv[:, j * 128:(j + 1) * 128], xa[:, t * 128:(t + 1) * 128], ident_b[:, :], r=[xa, ident_b], w=[pb])
                k.v("dve", "tensor_copy", r=[pb], w=[sg], out=sg[:, :, :], in_=pv.rearrange("p (j c) -> p j c", j=8))
                k.dma("sp", dv[:, g8 * 8:(g8 + 1) * 8, col:col + 128], sg[:, :, :], r=[sg], w=[Bd])
        if 16 <= cc < 24:
            k.dma("sp", bT_d[(cc - 16) * 128:(cc - 15) * 128, :], xa[:, :], r=[xa], w=[B_bT])
        if cc >= 24:
            k.dma("sp", cT_d[(cc - 24) * 128:(cc - 23) * 128, :], xa[:, :], r=[xa], w=[B_cT])
    st.close()
    k.P.fence()

    st = contextlib.ExitStack()
    k.st = st
    h1_d = c["h1_d"]
    B_h1 = c["B_h"][0]
    eps_ln, eps_rms = c["eps_ln"], c["eps_rms"]
    wout = k.sb("wout", [128, 16, D], BF16)
    k.dma("pool", wout[:, :, :], c["ssd_w_out"].rearrange("(kc p) d -> p kc d", p=128), w=[wout], partial=False)
    normw = k.sb("normw", [128, 2048], F32)
    k.dma("sp", normw[:, :], c["ssd_norm_w"].partition_broadcast(128), w=[normw])
    dsk = k.sb("dsk", [128, 32], F32)
    k.dma("sp", dsk[:, :], c["ssd_d"].partition_broadcast(128), w=[dsk])
    diagD = k.sb("diagD", [128, 32, 128], BF16)
    identb3 = bc(ident_b[:, :], [[0, 32], [1, 128]])
    k.v("dve", "tensor_tensor", r=[ident_b, dsk], w=[diagD], out=diagD[:, :, :], in0=identb3,
        in1=bc(dsk[:, :], [[1, 32], [0, 128]]), op=ALU.mult)
    state = k.sb("state", [128, 2048], F32)
    state_bf = k.sb("state_bf", [128, 2048], BF16)
    st_b = [Buf(f"st{g}") for g in range(8)]
    stbf_b = [Buf(f"stbf{g}") for g in range(8)]
    k.v("dve", "memset", w=st_b, ap=state[:, :], constant=0.0)
    k.v("dve", "memset", w=stbf_b, ap=state_bf[:, :], constant=0.0)

    NB = 3
    xs_t = [k.sb(f"xs_t{i}", [128, 2048], BF16) for i in range(NB)]
    bt_t = [k.sb(f"bt_t{i}", [128, 1024], BF16) for i in range(NB)]
    bT_t = [k.sb(f"bT_t{i}", [128, 8, 128], BF16) for i in range(NB)]
    cT_t = [k.sb(f"cT_t{i}", [128, 8, 128], BF16) for i in range(NB)]
    a_t = [k.sb(f"a_t{i}", [128, 32], F32) for i in range(NB)]
    dt_t = [k.sb(f"dt_t{i}", [128, 32], F32) for i in range(NB)]
    z_t = [k.sb(f"z_t{i}", [128, 2048], BF16) for i in range(NB)]
    x_t = [k.sb(f"x_t{i}", [128, D], F32) for i in range(NB)]
    exps2 = [k.sb(f"exps{i}", [128, 96], F32) for i in range(2)]
    a_bf2 = [k.sb(f"a_bf{i}", [128, 32], BF16) for i in range(2)]
    dtte2 = [k.sb(f"dtte{i}", [128, 32], F32) for i in range(2)]
    aR2 = [k.sb(f"aR{i}", [128, 32, 128], BF16) for i in range(2)]
    dec = [k.sb(f"dec{i}", [128, 8, 128], BF16) for i in range(2)]
    smk = [k.sb(f"smk{i}", [128, 2, 128], BF16) for i in range(2)]
    MT = [k.sb(f"MT{i}", [128, 8, 128], BF16) for i in range(2)]
    xdt2 = [k.sb(f"xdt{i}", [128, 2048], BF16) for i in range(2)]
    xw2 = [k.sb(f"xw{i}", [128, 2048], BF16) for i in range(2)]
    tmpq = [k.sb(f"tmpq{i}", [128, 512], F32) for i in range(1)] * 2
    stq = [k.sb(f"stq{i}", [128, 512], F32) for i in range(1)] * 2
    ycomb2 = [k.sb(f"ycomb{i}", [128, 2048], F32) for i in range(2)]
    ssq = k.sb("ssq", [128, 8], F32)
    junk = k.sb("junk", [128, 256], F32)
    lnv8 = k.sb("lnv8", [128, 8], F32)
    rstd8 = k.sb("rstd8", [128, 8], F32)
    yn = k.sb("yn", [128, 2048], BF16)
    ynT = k.sb("ynT", [128, 16, 128], BF16)
    res = k.sb("res", [128, D], F32)
    hout = [k.sb(f"hout{i}", [128, D], F32) for i in range(1)] * 2
    lntmp = (k.sb("ln_stats", [128, 12], F32), k.sb("ln_mv", [128, 2], F32), k.sb("ln_lnv", [128, 1], F32),
             k.sb("ln_rstd", [128, 1], F32))
    bT_v = bT_d.rearrange("(g n) t -> n g t", n=128)
    cT_v = cT_d.rearrange("(g n) t -> n g t", n=128)
    g_bc, b_bc = load_ln(k, c["ln_mix_g"], c["ln_mix_b"], 0)
    MISC, SEG0, SEG1, YB, YOB, STB, OPB, TRB = PS

    hT_r = None

    def loads(tt):
        i = tt % NB
        sl = slice(tt * 128, (tt + 1) * 128)
        k.dma("sp", a_t[i][:, :], a_d[sl, :], r=[B_a], w=[a_t[i]], partial=False)
        k.dma("sp", dt_t[i][:, :], dt_d[sl, :], r=[B_dt], w=[dt_t[i]], partial=False)
        k.dma("sp", xs_t[i][:, :], xs_d[sl, :], r=[B_xs], w=[xs_t[i]], partial=False)
        k.dma("sp", bT_t[i][:, :, :], bT_v[:, :, sl], r=[B_bT], w=[bT_t[i]], partial=False)
        k.dma("sp", cT_t[i][:, :, :], cT_v[:, :, sl], r=[B_cT], w=[cT_t[i]], partial=False)
        k.dma("sp", bt_t[i][:, :], bt_d[sl, :], r=[B_bt], w=[bt_t[i]], partial=False)
        k.dma("sp", z_t[i][:, :], z_d[sl, :], r=[B_z], w=[z_t[i]], partial=False)
        k.dma("sp", x_t[i][:, :], x_d[sl, :], w=[x_t[i]], partial=False)

    def prologue(tt):
        i = tt % NB
        j = tt % 2
        xs_, a_, dt_ = xs_t[i], a_t[i], dt_t[i]
        exps, a_bf, dtte, aR, xdt, xw = exps2[j], a_bf2[j], dtte2[j], aR2[j], xdt2[j], xw2[j]
        k.mm(MISC[:, 0:32], rmat_f[:, :], a_[:, :], True, True, r=[rmat_f, a_], w=[MISC])
        k.mm(MISC[:, 32:64], lmat_f[:, :], a_[:, :], True, True, r=[lmat_f, a_], w=[MISC])
        k.mm(MISC[:, 64:96], ones_f[:, :], a_[:, :], True, True, r=[ones_f, a_], w=[MISC])
        k.act(exps[:, :], MISC[:, 0:96], AF.Exp, r=[MISC], w=[exps])
        k.v("dve", "tensor_tensor", r=[dt_, exps], w=[dtte], out=dtte[:, :], in0=dt_[:, :], in1=exps[:, 32:64], op=ALU.mult)
        k.v("dve", "tensor_copy", r=[a_], w=[a_bf], out=a_bf[:, :], in_=a_[:, :])
        k.v("dve", "tensor_tensor", r=[a_bf, rmat_b], w=[aR], out=aR[:, :, :], in0=bc(a_bf[:, :], [[1, 32], [0, 128]]),
            in1=bc(rmat_b[:, :], [[0, 32], [1, 128]]), op=ALU.mult)
        xs3 = xs_[:, :].rearrange("p (h d) -> p h d", h=32)
        k.v("pool", "tensor_tensor", r=[xs_, dt_], w=[xdt], out=xdt[:, :].rearrange("p (h d) -> p h d", h=32), in0=xs3,
            in1=bc(dt_[:, :], [[1, 32], [0, 64]]), op=ALU.mult)
        k.v("pool", "tensor_tensor", r=[xs_, dtte], w=[xw], out=xw[:, :].rearrange("p (h d) -> p h d", h=32), in0=xs3,
            in1=bc(dtte[:, :], [[1, 32], [0, 64]]), op=ALU.mult)

    def quarter(tt, q):
        i = tt % NB
        j = tt % 2
        xs_, bt_, bT_, cT_ = xs_t[i], bt_t[i], bT_t[i], cT_t[i]
        exps, aR, xdt, xw, ycomb = exps2[j], aR2[j], xdt2[j], xw2[j], ycomb2[j]
        SEG = (SEG0, SEG1)
        for half in range(2):
            k.mm(SEG[half][:, :], lmat_b[:, :], aR[:, q * 8 + half * 4:q * 8 + half * 4 + 4, :], True, True,
                 r=[lmat_b, aR], w=[SEG[half]])
        for gi in range(2):
            g = q * 2 + gi
            k.mm(MISC[:, 128 + gi * 128:256 + gi * 128], bT_[:, g, :], cT_[:, g, :], True, True, r=[bT_, cT_], w=[MISC])
        dq = dec[q % 2]
        for half in range(2):
            k.act(dq[:, half * 4:half * 4 + 4, :], SEG[half][:, :].rearrange("p (h l) -> p h l", h=4), AF.Exp,
                  r=[SEG[half]], w=[dq], partial=True)
        sq_ = smk[q % 2]
        k.v("dve", "tensor_tensor", r=[MISC, rmat_f], w=[sq_], out=sq_[:, :, :],
            in0=MISC[:, 128:384].rearrange("p (g l) -> p g l", g=2), in1=bc(rmat_f[:, :], [[0, 2], [1, 128]]), op=ALU.mult)
        mq = MT[q % 2]
        k.v("dve", "tensor_tensor", r=[dq, sq_], w=[mq], out=mq[:, :, :].rearrange("p (g r) l -> p g r l", g=2),
            in0=dq[:, :, :].rearrange("p (g r) l -> p g r l", g=2),
            in1=bc(sq_[:, :, :], [[128, 2], [0, 4], [1, 128]]), op=ALU.mult)
        for gi in range(2):
            g = q * 2 + gi
            k.mm(YOB[:, gi * 256:(gi + 1) * 256], cT_[:, g, :], state_bf[:, g * 256:(g + 1) * 256], True, True,
                 r=[cT_, stbf_b[g]], w=[YOB])
        for gi in range(2):
            g = q * 2 + gi
            k.mm(STB[:, gi * 256:(gi + 1) * 256], bt_[:, g * 128:(g + 1) * 128], xw[:, g * 256:(g + 1) * 256], True, True,
                 r=[bt_, xw], w=[STB])
        for hq in range(8):
            h = q * 8 + hq
            k.mm(YB[:, hq * 64:(hq + 1) * 64], diagD[:, h, :], xs_[:, h * 64:(h + 1) * 64], True, False,
                 r=[diagD, xs_], w=[YB])
            k.mm(YB[:, hq * 64:(hq + 1) * 64], mq[:, hq, :], xdt[:, h * 64:(h + 1) * 64], False, True,
                 r=[mq, xdt], w=[YB])
        sq2 = stq[q % 2]
        sl = slice(q * 512, (q + 1) * 512)
        gb = [st_b[q * 2], st_b[q * 2 + 1]]
        k.v("dve", "tensor_tensor", r=gb + [exps], w=[sq2], out=sq2[:, :].rearrange("p (h d) -> p h d", h=8),
            in0=state[:, sl].rearrange("p (h d) -> p h d", h=8), in1=bc(exps[:, 64 + q * 8:64 + q * 8 + 8], [[1, 8], [0, 64]]),
            op=ALU.mult)
        k.v("dve", "tensor_tensor", r=[STB, sq2], w=gb, out=state[:, sl], in0=STB[:, :], in1=sq2[:, :], op=ALU.add)
        k.act(state_bf[:, sl], state[:, sl], AF.Copy, r=gb, w=[stbf_b[q * 2], stbf_b[q * 2 + 1]])
        tq = tmpq[q % 2]
        k.v("dve", "tensor_tensor", r=[YOB, exps], w=[tq], out=tq[:, :].rearrange("p (h d) -> p h d", h=8),
            in0=YOB[:, :].rearrange("p (h d) -> p h d", h=8), in1=bc(exps[:, q * 8:q * 8 + 8], [[1, 8], [0, 64]]), op=ALU.mult)
        k.v("dve", "tensor_tensor", r=[YB, tq], w=[ycomb], partial=True, out=ycomb[:, q * 512:(q + 1) * 512], in0=YB[:, :],
            in1=tq[:, :], op=ALU.add)

    def epi(tt, part):
        i = tt % NB
        ycomb = ycomb2[tt % 2]
        z_, x_ = z_t[i], x_t[i]
        if part == 0:
            k.v("dve", "tensor_tensor", r=[ycomb, z_], w=[ycomb], out=ycomb[:, :], in0=ycomb[:, :], in1=z_[:, :], op=ALU.mult)
            for g in range(8):
                k.act(junk[:, :], ycomb[:, g * 256:(g + 1) * 256], AF.Square, r=[ycomb], w=[junk, ssq], partial=True,
                      accum_out=ssq[:, g:g + 1])
            k.act(lnv8[:, :], ssq[:, :], AF.Ln, r=[ssq, eps_rms], w=[lnv8], scale=1.0 / 256.0, bias=eps_rms[:, 0:1])
            k.act(rstd8[:, :], lnv8[:, :], AF.Exp, r=[lnv8], w=[rstd8], scale=-0.5)
        elif part == 1:
            k.v("pool", "tensor_tensor", r=[ycomb, rstd8], w=[ycomb], out=ycomb[:, :].rearrange("p (g d) -> p g d", g=8),
                in0=ycomb[:, :].rearrange("p (g d) -> p g d", g=8), in1=bc(rstd8[:, :], [[1, 8], [0, 256]]), op=ALU.mult)
            k.v("pool", "tensor_tensor", r=[ycomb, normw], w=[yn], out=yn[:, :], in0=ycomb[:, :], in1=normw[:, :], op=ALU.mult)
            for g8 in range(2):
                pv = TRB[:, :].bitcast(BF16)
                for j in range(8):
                    kc = g8 * 8 + j
                    k.tr(pv[:, j * 128:(j + 1) * 128], yn[:, kc * 128:(kc + 1) * 128], ident_b[:, :], r=[yn, ident_b], w=[TRB])
                k.act(ynT[:, g8 * 8:(g8 + 1) * 8, :], pv.rearrange("p (j c) -> p j c", j=8), AF.Copy, r=[TRB], w=[ynT], partial=True)
        elif part == 2:
            for dh in range(2):
                for kc in range(16):
                    k.mm(OPB[:, :], ynT[:, kc, :], wout[:, kc, dh * 512:(dh + 1) * 512], kc == 0, kc == 15, r=[ynT, wout], w=[OPB])
                k.v("dve", "scalar_tensor_tensor", r=[x_, OPB], w=[res], partial=True, out=res[:, dh * 512:(dh + 1) * 512],
                    in0=x_[:, dh * 512:(dh + 1) * 512], scalar=ALPHA, in1=OPB[:, :], op0=ALU.mult, op1=ALU.add)
        else:
            ho = hout[tt % 2]
            layer_norm_tile(k, res, g_bc, b_bc, ho, lntmp, eps_ln, "s", eng2="pool")
            op = k.dma("sp", h1_d[tt * 128:(tt + 1) * 128, :], ho[:, :], r=[ho], w=[B_h1[tt]])
            k.out_ops.append(op)

    loads(0)
    loads(1)
    prologue(0)
    def rstep(tile, step):
        route_step(k, c, 0, tile, hout[0], hT_r, (TRB, TRB), OPB, step)

    FUSE_L0 = False

    for tt in range(NT):
        for q in range(4):
            quarter(tt, q)
            if FUSE_L0 and tt >= 2 and q == 3:
                rstep(tt - 2, 2)
            if tt > 0:
                epi(tt - 1, q)
            if FUSE_L0 and tt >= 2 and q == 0:
                rstep(tt - 2, 0)
            if FUSE_L0 and tt >= 2 and q == 2:
                rstep(tt - 2, 1)
            if q == 1 and tt + 1 < NT:
                prologue(tt + 1)
        if tt + 2 < NT:
            loads(tt + 2)
    epi(NT - 1, 0)
    if FUSE_L0:
        rstep(NT - 2, 0)
    epi(NT - 1, 1)
    epi(NT - 1, 2)
    if FUSE_L0:
        rstep(NT - 2, 1)
        rstep(NT - 2, 2)
    epi(NT - 1, 3)
    if FUSE_L0:
        for step in range(3):
            rstep(NT - 1, step)
    st.close()
    k.P.fence()
    k.st = st_outer


def moe_layer_dense(k, c, layer, hin_d, B_hin, hout_d, B_hout, is_final):
    PS = c["PS"]
    ident_f = c["ident_f"]
    eps_ln = c["eps_ln"]
    st_outer = k.st
    st = contextlib.ExitStack()
    k.st = st
    L = f"m{layer}"
    TH = 16
    xT = k.sb(L + "xT", [128, 8, TH * 128], BF16)
    xT_b = [Buf(f"{L}xT{t}") for t in range(TH)]
    acc = k.sb(L + "acc", [128, TH, D], F32)
    acc_b = [Buf(f"{L}acc{t}") for t in range(TH)]
    rw = k.sb(L + "rw", [128, 8, E], F32)
    k.dma("sp", rw[:, :, :], c["router_w"].rearrange("(kc p) e -> p kc e", p=128), w=[rw])
    rb = k.sb(L + "rb", [128, E], F32)
    k.dma("sp", rb[:, :], c["router_bias"].partition_broadcast(128), w=[rb])
    g_bc, b_bc = load_ln(k, c["ln_ffn_g"], c["ln_ffn_b"], layer)
    hin = [k.sb(f"{L}hin{i}", [128, D], F32) for i in range(2)]
    hTf = k.sb(L + "hTf", [128, 8, 128], F32)
    lg = k.sb(L + "lg", [128, TH, E], F32)
    aff = k.sb(L + "aff", [128, TH, E], F32)
    sel = k.sb(L + "sel", [128, TH, E], F32)
    p6 = k.sb(L + "p6", [128, TH * 4, 6], F32)
    gs = k.sb(L + "gs", [128, TH * 4], F32)
    gmax = k.sb(L + "gmax", [128, TH], F32)
    gm = k.sb(L + "gm", [128, TH * 4], F32)
    pen = k.sb(L + "pen", [128, TH * 4], F32)
    selm = k.sb(L + "selm", [128, TH, E], F32)
    m1 = k.sb(L + "m1", [128, TH], F32)
    mk1 = k.sb(L + "mk1", [128, TH, E], F32)
    selm2 = k.sb(L + "selm2", [128, TH, E], F32)
    mk2 = k.sb(L + "mk2", [128, TH, E], F32)
    wsum = k.sb(L + "wsum", [128, TH], F32)
    gates = k.sb(L + "gates", [128, TH, E], F32)
    wg = [k.sb(f"{L}wg{i}", [128, 8, DFF], BF16) for i in range(2)]
    wu = [k.sb(f"{L}wu{i}", [128, 8, DFF], BF16) for i in range(2)]
    wd = [k.sb(f"{L}wd{i}", [128, 4, D], BF16) for i in range(2)]
    hT = [k.sb(f"{L}hT{i}", [128, 4, 512], BF16) for i in range(2)]
    sg = [k.sb(f"{L}sg{i}", [128, 512], BF16) for i in range(2)]
    res = [k.sb(f"{L}res{i}", [128, D], F32) for i in range(2)]
    lntmp = (k.sb(L + "ln_stats", [128, 12], F32), k.sb(L + "ln_mv", [128, 2], F32), k.sb(L + "ln_lnv", [128, 1], F32),
             k.sb(L + "ln_rstd", [128, 1], F32))
    wgv = c["moe_w_gate"][layer].rearrange("e (kc p) f -> e p kc f", p=128)
    wuv = c["moe_w_up"][layer].rearrange("e (kc p) f -> e p kc f", p=128)
    wdv = c["moe_w_down"][layer].rearrange("e (fc p) d -> e p fc d", p=128)
    n_w = 0
    n_g = 0
    n_d = 0
    n_h = 0
    for hf in range(2):
        for t in range(TH):
            tt = hf * TH + t
            hi = hin[t % 2]
            k.dma("sp", hi[:, :], hin_d[tt * 128:(tt + 1) * 128, :], r=[B_hin[tt]], w=[hi], partial=False)
            for half in range(2):
                pb = PS[half]
                for j in range(4):
                    kc = half * 4 + j
                    k.tr(pb[:, j * 128:(j + 1) * 128], hi[:, kc * 128:(kc + 1) * 128], ident_f[:, :], r=[hi, ident_f], w=[pb])
                k.act(hTf[:, half * 4:half * 4 + 4, :], pb[:, :].rearrange("p (j c) -> p j c", j=4), AF.Copy, r=[pb], w=[hTf],
                      partial=True)
            k.v("dve", "tensor_copy", r=[hTf], w=[xT_b[t]], out=xT.t[:, :, t * 128:(t + 1) * 128], in_=hTf[:, :, :])
            pl = PS[2]
            for kc in range(8):
                k.mm(pl[:, 0:E], hTf[:, kc, :], rw[:, kc, :], kc == 0, kc == 7, r=[hTf, rw], w=[pl])
            k.act(lg[:, t, :], pl[:, 0:E], AF.Exp, r=[pl], w=[lg], partial=True, scale=-1.0)
        V = lambda name, **kw: k.v("dve", name, **kw)
        V("tensor_scalar", r=[lg], w=[lg], out=lg[:, :, :], in0=lg[:, :, :], scalar1=1.0, scalar2=None, op0=ALU.add)
        V("reciprocal", r=[lg], w=[aff], out=aff[:, :, :], in_=lg[:, :, :])
        V("tensor_tensor", r=[aff, rb], w=[sel], out=sel[:, :, :], in0=aff[:, :, :], in1=bc(rb[:, :], [[0, TH], [1, E]]), op=ALU.add)
        s4 = sel[:, :, :].rearrange("p t (g i) -> p (t g) i", g=4)
        V("tensor_tensor", r=[sel], w=[p6], partial=True, out=p6[:, :, 0:3], in0=s4[:, :, 0:3], in1=s4[:, :, 1:4], op=ALU.add)
        V("tensor_tensor", r=[sel], w=[p6], partial=True, out=p6[:, :, 3:5], in0=s4[:, :, 0:2], in1=s4[:, :, 2:4], op=ALU.add)
        V("tensor_tensor", r=[sel], w=[p6], partial=True, out=p6[:, :, 5:6], in0=s4[:, :, 0:1], in1=s4[:, :, 3:4], op=ALU.add)
        V("tensor_reduce", r=[p6], w=[gs], out=gs[:, :], in_=p6[:, :, :], axis=AX.X, op=ALU.max)
        V("tensor_reduce", r=[gs], w=[gmax], out=gmax[:, :], in_=gs[:, :].rearrange("p (t g) -> p t g", g=4), axis=AX.X, op=ALU.max)
        V("tensor_tensor", r=[gs, gmax], w=[gm], out=gm[:, :].rearrange("p (t g) -> p t g", g=4),
          in0=gs[:, :].rearrange("p (t g) -> p t g", g=4), in1=bc(gmax[:, :], [[1, TH], [0, 4]]), op=ALU.is_ge)
        V("tensor_scalar", r=[gm], w=[pen], out=pen[:, :], in0=gm[:, :], scalar1=-1.0, scalar2=1.0e4, op0=ALU.add, op1=ALU.mult)
        sm4 = selm[:, :, :].rearrange("p t (g i) -> p (t g) i", g=4)
        V("tensor_tensor", r=[sel, gm], w=[selm], out=sm4, in0=s4, in1=bc(gm[:, :], [[1, TH * 4], [0, 4]]), op=ALU.mult)
        V("tensor_tensor", r=[selm, pen], w=[selm], out=sm4, in0=sm4, in1=bc(pen[:, :], [[1, TH * 4], [0, 4]]), op=ALU.add)
        V("tensor_reduce", r=[selm], w=[m1], out=m1[:, :], in_=selm[:, :, :], axis=AX.X, op=ALU.max)
        V("tensor_tensor", r=[selm, m1], w=[mk1], out=mk1[:, :, :], in0=selm[:, :, :], in1=bc(m1[:, :], [[1, TH], [0, E]]), op=ALU.is_ge)
        V("scalar_tensor_tensor", r=[mk1, selm], w=[selm2], out=selm2[:, :, :], in0=mk1[:, :, :], scalar=-1.0e4, in1=selm[:, :, :],
          op0=ALU.mult, op1=ALU.add)
        V("tensor_reduce", r=[selm2], w=[m1], out=m1[:, :], in_=selm2[:, :, :], axis=AX.X, op=ALU.max)
        V("tensor_tensor", r=[selm2, m1], w=[mk2], out=mk2[:, :, :], in0=selm2[:, :, :], in1=bc(m1[:, :], [[1, TH], [0, E]]), op=ALU.is_ge)
        V("tensor_tensor", r=[mk1, mk2], w=[mk1], out=mk1[:, :, :], in0=mk1[:, :, :], in1=mk2[:, :, :], op=ALU.add)
        V("tensor_tensor", r=[mk1, aff], w=[mk1], out=mk1[:, :, :], in0=mk1[:, :, :], in1=aff[:, :, :], op=ALU.mult)
        V("tensor_reduce", r=[mk1], w=[wsum], out=wsum[:, :], in_=mk1[:, :, :], axis=AX.X, op=ALU.add)
        V("reciprocal", r=[wsum], w=[wsum], out=wsum[:, :], in_=wsum[:, :])
        V("tensor_tensor", r=[mk1, wsum], w=[gates], out=gates[:, :, :], in0=mk1[:, :, :], in1=bc(wsum[:, :], [[1, TH], [0, E]]), op=ALU.mult)
        for e in range(E):
            i = n_w % 2
            n_w += 1
            k.dma("pool", wg[i][:, :, :], wgv[e], w=[wg[i]], partial=False)
            k.dma("pool", wu[i][:, :, :], wuv[e], w=[wu[i]], partial=False)
            k.dma("pool", wd[i][:, :, :], wdv[e], w=[wd[i]], partial=False)
            for tb in range(4):
                hTt = hT[n_h % 2]
                n_h += 1
                xr = xT_b[tb * 4:tb * 4 + 4]
                for fc in range(4):
                    G = PS[n_g % 2]
                    U = PS[2 + n_g % 2]
                    s_ = sg[n_g % 2]
                    n_g += 1
                    for kc in range(8):
                        k.mm(G[:, :], wg[i][:, kc, fc * 128:(fc + 1) * 128], xT.t[:, kc, tb * 512:(tb + 1) * 512], kc == 0, kc == 7,
                             r=[wg[i]] + xr, w=[G])
                    for kc in range(8):
                        k.mm(U[:, :], wu[i][:, kc, fc * 128:(fc + 1) * 128], xT.t[:, kc, tb * 512:(tb + 1) * 512], kc == 0, kc == 7,
                             r=[wu[i]] + xr, w=[U])
                    k.act(s_[:, :], G[:, :], AF.Silu, r=[G], w=[s_])
                    k.v("dve", "tensor_tensor", r=[s_, U], w=[hTt], partial=True, out=hTt[:, fc, :], in0=s_[:, :], in1=U[:, :], op=ALU.mult)
                for tl in range(4):
                    t = tb * 4 + tl
                    for dh in range(2):
                        Dp = PS[4 + n_d % 4]
                        n_d += 1
                        for fc in range(4):
                            k.mm(Dp[:, :], hTt[:, fc, tl * 128:(tl + 1) * 128], wd[i][:, fc, dh * 512:(dh + 1) * 512], fc == 0, fc == 3,
                                 r=[hTt, wd[i]], w=[Dp])
                        gsc = gates[:, t, e:e + 1]
                        if e == 0:
                            k.v("dve", "tensor_scalar", r=[Dp, gates], w=[acc_b[t]], partial=True, out=acc[:, t, dh * 512:(dh + 1) * 512],
                                in0=Dp[:, :], scalar1=gsc, scalar2=None, op0=ALU.mult)
                        else:
                            k.v("dve", "scalar_tensor_tensor", r=[Dp, gates, acc_b[t]], w=[acc_b[t]], partial=True,
                                out=acc[:, t, dh * 512:(dh + 1) * 512], in0=Dp[:, :], scalar=gsc, in1=acc[:, t, dh * 512:(dh + 1) * 512],
                                op0=ALU.mult, op1=ALU.add)
        for t in range(TH):
            tt = hf * TH + t
            hi = hin[t % 2]
            k.dma("sp", hi[:, :], hin_d[tt * 128:(tt + 1) * 128, :], r=[B_hin[tt]], w=[hi], partial=False)
            r_ = res[t % 2]
            k.v("dve", "scalar_tensor_tensor", r=[hi, acc_b[t]], w=[r_], out=r_[:, :], in0=hi[:, :], scalar=ALPHA, in1=acc[:, t, :],
                op0=ALU.mult, op1=ALU.add)
            layer_norm_tile(k, r_, g_bc, b_bc, r_, lntmp, eps_ln, L)
            op = k.dma("sp", hout_d[tt * 128:(tt + 1) * 128, :], r_[:, :], r=[r_], w=[B_hout[tt]] if B_hout is not None else [])
            k.out_ops.append(op)
    st.close()
    k.P.fence()
    k.st = st_outer


def mla_layer(k, c):
    PS = c["PS"]
    ident_b, ones_f, eps_ln, eps_rms = c["ident_b"], c["ones_f"], c["eps_ln"], c["eps_rms"]
    DBK = c["DBK"]
    h2_d, h3_d, attnT_d = c["h2_d"], c["h3_d"], c["attnT_d"]
    B_h2, B_h3, B_attn = c["B_h"][1], c["B_h"][2], c["B_attn"]
    st_outer = k.st
    if c["lg_all"][1] is None:
        c["lg_all"][1] = k.sb("lg_all1", [128, NT, E], F32)
    stA = contextlib.ExitStack()
    k.st = stA
    cT = k.sb("cT", [128, 5, S], BF16)
    cT_b = [Buf(f"cT{t}") for t in range(NT)]
    krT = k.sb("krT", [128, S], BF16)
    krT_b = [Buf(f"krT{t}") for t in range(8)]
    st = contextlib.ExitStack()
    k.st = st
    wdn = k.sb("wdn", [128, 8, 640], BF16)
    k.dma("pool", wdn[:, :, :], c["mla_w_down"].rearrange("(kc p) n -> p kc n", p=128)[:, :, 0:640], w=[wdn], partial=False)
    wkr = k.sb("wkr", [128, 8, 192], BF16)
    k.dma("pool", wkr[:, :, :], c["mla_w_kr"].rearrange("(kc p) n -> p kc n", p=128), w=[wkr], partial=False)
    qkn = k.sb("qkn", [128, 640], F32)
    k.dma("sp", qkn[:, 0:384], c["mla_q_norm"].partition_broadcast(128), w=[qkn])
    k.dma("sp", qkn[:, 384:640], c["mla_kv_norm"].partition_broadcast(128), w=[qkn])
    hin = [k.sb(f"a1hin{i}", [128, D], F32) for i in range(2)]
    hbf = [k.sb(f"a1hbf{i}", [128, D], BF16) for i in range(2)]
    hT = [k.sb(f"a1hT{i}", [128, 8, 512], BF16) for i in range(2)]
    cq = k.sb("a1cq", [128, 640], F32)
    cqn = k.sb("a1cqn", [128, 640], BF16)
    junk = k.sb("a1junk", [128, 384], F32)
    ss2 = k.sb("a1ss2", [128, 2], F32)
    ln2 = k.sb("a1ln2", [128, 2], F32)
    rs2 = k.sb("a1rs2", [128, 2], F32)
    rope = [k.sb(f"a1rope{i}", [128, 2, 512], F32) for i in range(2)]
    rt1 = k.sb("a1rt1", [128, 512], F32)
    rt2 = k.sb("a1rt2", [128, 512], F32)
    rope_v = c["c_rope"]
    cq2 = [cq, k.sb("a1cq_b", [128, 640], F32)]
    hT_bufs_all = {}

    def a1_front(tt):
        tb, tl = tt // 4, tt % 4
        hTb = hT[tb % 2]
        if tl == 0:
            hT_bufs_all[tb] = [Buf(f"hTb{tb}_{i}") for i in range(4)]
        hT_bufs = hT_bufs_all[tb]
        hi = hin[tt % 2]
        hb = hbf[tt % 2]
        k.dma("sp", hi[:, :], h2_d[tt * 128:(tt + 1) * 128, :], r=[B_h2[tt]], w=[hi], partial=False)
        k.v("dve", "tensor_copy", r=[hi], w=[hb], out=hb[:, :], in_=hi[:, :])
        pb = PS[tt % 2]
        pv = pb[:, :].bitcast(BF16)
        for kc in range(8):
            k.tr(pv[:, kc * 128:(kc + 1) * 128], hb[:, kc * 128:(kc + 1) * 128], ident_b[:, :], r=[hb, ident_b], w=[pb])
        k.act(hTb[:, :, tl * 128:(tl + 1) * 128], pv.rearrange("p (k t) -> p k t", k=8), AF.Copy, r=[pb, hT_bufs[tl]], w=[hT_bufs[tl]])
        P0, P1 = PS[2 + tt % 2], PS[4 + tt % 2]
        for kc in range(8):
            k.mm(P0[:, :], hTb[:, kc, tl * 128:(tl + 1) * 128], wdn[:, kc, 0:512], kc == 0, kc == 7, r=[hT_bufs[tl], wdn], w=[P0])
        for kc in range(8):
            k.mm(P1[:, 0:128], hTb[:, kc, tl * 128:(tl + 1) * 128], wdn[:, kc, 512:640], kc == 0, kc == 7, r=[hT_bufs[tl], wdn], w=[P1])
        cq_ = cq2[tt % 2]
        k.act(cq_[:, 0:512], P0[:, :], AF.Copy, r=[P0], w=[cq_], partial=True)
        k.act(cq_[:, 512:640], P1[:, 0:128], AF.Copy, r=[P1], w=[cq_], partial=True)

    def a1_back(tt):
        cq_ = cq2[tt % 2]
        k.act(junk[:, 0:384], cq_[:, 0:384], AF.Square, r=[cq_], w=[junk, ss2], partial=True, accum_out=ss2[:, 0:1])
        k.act(junk[:, 0:256], cq_[:, 384:640], AF.Square, r=[cq_], w=[junk, ss2], partial=True, accum_out=ss2[:, 1:2])
        k.act(ln2[:, 0:1], ss2[:, 0:1], AF.Ln, r=[ss2, eps_rms], w=[ln2], partial=True, scale=1.0 / 384.0, bias=eps_rms[:, 0:1])
        k.act(ln2[:, 1:2], ss2[:, 1:2], AF.Ln, r=[ss2, eps_rms], w=[ln2], partial=True, scale=1.0 / 256.0, bias=eps_rms[:, 0:1])
        k.act(rs2[:, :], ln2[:, :], AF.Exp, r=[ln2], w=[rs2], scale=-0.5)
        k.v("dve", "scalar_tensor_tensor", r=[cq_, rs2, qkn], w=[cqn], partial=True, out=cqn[:, 0:384], in0=cq_[:, 0:384], scalar=rs2[:, 0:1],
            in1=qkn[:, 0:384], op0=ALU.mult, op1=ALU.mult)
        k.v("dve", "scalar_tensor_tensor", r=[cq_, rs2, qkn], w=[cqn], partial=True, out=cqn[:, 384:640], in0=cq_[:, 384:640], scalar=rs2[:, 1:2],
            in1=qkn[:, 384:640], op0=ALU.mult, op1=ALU.mult)
        pb2 = PS[tt % 2]
        pv2 = pb2[:, :].bitcast(BF16)
        for j in range(5):
            k.tr(pv2[:, j * 128:(j + 1) * 128], cqn[:, j * 128:(j + 1) * 128], ident_b[:, :], r=[cqn, ident_b], w=[pb2])
        k.act(cT.t[:, :, tt * 128:(tt + 1) * 128], pv2[:, 0:640].rearrange("p (j t) -> p j t", j=5), AF.Copy, r=[pb2], w=[cT_b[tt]])

    def a1_krope(tb):
        hTb = hT[tb % 2]
        hT_bufs = hT_bufs_all[tb]
        rp = rope[tb % 2]
        k.dma("sp", rp[:, 0, :], rope_v[0][:, tb * 512:(tb + 1) * 512], w=[rp])
        k.dma("sp", rp[:, 1, :], rope_v[1][:, tb * 512:(tb + 1) * 512], w=[rp])
        KA, KB = PS[6], PS[7]
        for kc in range(8):
            k.mm(KA[0:96, :], wkr[:, kc, 0:96], hTb[:, kc, :], kc == 0, kc == 7, r=hT_bufs + [wkr], w=[KA])
        for kc in range(8):
            k.mm(KB[0:96, :], wkr[:, kc, 96:192], hTb[:, kc, :], kc == 0, kc == 7, r=hT_bufs + [wkr], w=[KB])
        k.v("dve", "tensor_tensor", r=[KA, rp], w=[rt1], out=rt1[64:96, :], in0=KA[64:96, :], in1=rp[64:96, 0, :], op=ALU.mult)
        k.v("dve", "tensor_tensor", r=[KB, rp], w=[rt2], out=rt2[64:96, :], in0=KB[64:96, :], in1=rp[64:96, 1, :], op=ALU.mult)
        k.v("dve", "tensor_tensor", r=[rt1, rt2], w=[krT_b[tb]], out=krT[64:96, tb * 512:(tb + 1) * 512], in0=rt1[64:96, :], in1=rt2[64:96, :],
            op=ALU.add)

    a1_front(0)
    for tt in range(NT):
        if tt + 1 < NT:
            a1_front(tt + 1)
        a1_back(tt)
        if tt % 4 == 3:
            a1_krope(tt // 4)
    st.close()
    k.P.fence()
    st = contextlib.ExitStack()
    k.st = st
    HG = 4
    qT = k.sb("qT", [128, HG, S], BF16)
    kT = k.sb("kT", [128, HG, S], BF16)
    Vt = k.sb("Vt", [128, NT, HG, 65], BF16)
    qT_b = [[Buf(f"qT{h}_{b}") for b in range(8)] for h in range(HG)]
    kT_b = [[Buf(f"kT{h}_{b}") for b in range(8)] for h in range(HG)]
    V_b = [Buf(f"V{t}") for t in range(NT)]
    k.v("dve", "memset", w=V_b, ap=Vt[:, :, :, :], constant=1.0)
    wq = [k.sb(f"wq{i}", [128, 3, HG * 96], BF16) for i in range(2)]
    wqs = [k.sb(f"wqs{i}", [128, 3, HG * 96], BF16) for i in range(2)]
    wkn = [k.sb(f"wkn{i}", [128, 2, HG * 64], BF16) for i in range(2)]
    wv = [k.sb(f"wv{i}", [128, 2, HG * 64], BF16) for i in range(2)]
    amask_f = k.sb("amask_f", [128, 4, 512], F32)
    amask = k.sb("amask", [128, 4, 512], BF16)
    k.dma("sp", amask_f[:, :, :], c["c_amask"], w=[amask_f], partial=False)
    k.v("dve", "tensor_copy", r=[amask_f], w=[amask], out=amask[:, :, :], in_=amask_f[:, :, :])
    rope = [k.sb(f"a2rope{i}", [128, 2, 512], F32) for i in range(2)]
    rt1s = [k.sb(f"a2rt1{i}", [128, 512], F32) for i in range(2)]
    rt2s = [k.sb(f"a2rt2{i}", [128, 512], F32) for i in range(2)]
    pt = [k.sb(f"pt{i}", [128, 2, 512], BF16) for i in range(3)]
    rinv = k.sb("rinv", [128, 512], F32)
    osb = k.sb("osb", [128, 512], F32)
    at = [k.sb(f"at{i}", [128, 512], BF16) for i in range(2)]
    DB = [k.ps_pair(i) for i in range(4)] if False else None
    wq_v = c["mla_w_uq"].rearrange("(kc p) n -> p kc n", p=128)
    wqs_v = c["mla_w_uq_sw"].rearrange("(kc p) n -> p kc n", p=128)
    wkn_v = c["mla_w_kn"].rearrange("(kc p) n -> p kc n", p=128)
    wv_v = c["mla_w_v"].rearrange("(kc p) n -> p kc n", p=128)
    SCALE = 1.0 / math.sqrt(96.0)
    n_st = 0
    n_ot = 0
    n_at = 0
    for hg in range(MLA_H // HG):
        i = hg % 2
        k.dma("pool", wq[i][:, :, :], wq_v[:, :, hg * HG * 96:(hg + 1) * HG * 96], w=[wq[i]], partial=False)
        k.dma("pool", wqs[i][:, :, :], wqs_v[:, :, hg * HG * 96:(hg + 1) * HG * 96], w=[wqs[i]], partial=False)
        k.dma("pool", wkn[i][:, :, :], wkn_v[:, :, hg * HG * 64:(hg + 1) * HG * 64], w=[wkn[i]], partial=False)
        k.dma("pool", wv[i][:, :, :], wv_v[:, :, hg * HG * 64:(hg + 1) * HG * 64], w=[wv[i]], partial=False)
        for tb in range(8):
            blk = slice(tb * 512, (tb + 1) * 512)
            cr = cT_b[tb * 4:tb * 4 + 4]
            rp = rope[tb % 2]
            k.dma("sp", rp[:, 0, :], rope_v[0][:, blk], w=[rp])
            k.dma("sp", rp[:, 1, :], rope_v[1][:, blk], w=[rp])
            for h in range(HG):
                n_pj = (tb * HG + h) % 2
                QA, QB = PS[n_pj * 2], PS[n_pj * 2 + 1]
                for kc in range(3):
                    k.mm(QA[0:96, :], wq[i][:, kc, h * 96:(h + 1) * 96], cT.t[:, kc, blk], kc == 0, kc == 2, r=[wq[i]] + cr, w=[QA])
                for kc in range(3):
                    k.mm(QB[0:96, :], wqs[i][:, kc, h * 96:(h + 1) * 96], cT.t[:, kc, blk], kc == 0, kc == 2, r=[wqs[i]] + cr, w=[QB])
                KN = PS[6 + n_pj]
                for kc in range(2):
                    k.mm(KN[0:64, :], wkn[i][:, kc, h * 64:(h + 1) * 64], cT.t[:, 3 + kc, blk], kc == 0, kc == 1, r=[wkn[i]] + cr, w=[KN])
                qb = qT_b[h][tb]
                rt1, rt2 = rt1s[n_pj], rt2s[n_pj]
                k.act(qT.t[0:64, h, blk], QA[0:64, :], AF.Copy, r=[QA], w=[qb], partial=True)
                k.v("dve", "tensor_tensor", r=[QA, rp], w=[rt1], out=rt1[64:96, :], in0=QA[64:96, :], in1=rp[64:96, 0, :], op=ALU.mult)
                k.v("dve", "tensor_tensor", r=[QB, rp], w=[rt2], out=rt2[64:96, :], in0=QB[64:96, :], in1=rp[64:96, 1, :], op=ALU.mult)
                k.v("dve", "tensor_tensor", r=[rt1, rt2], w=[qb], partial=True, out=qT.t[64:96, h, blk], in0=rt1[64:96, :], in1=rt2[64:96, :],
                    op=ALU.add)
                kb = kT_b[h][tb]
                k.act(kT.t[0:64, h, blk], KN[0:64, :], AF.Copy, r=[KN], w=[kb], partial=True)
                k.v("pool", "tensor_copy", r=[krT_b[tb]], w=[kb], partial=True, out=kT.t[64:96, h, blk], in_=krT[64:96, blk])
            for tl in range(4):
                tt = tb * 4 + tl
                VP = PS[4 + tl % 2]
                for kc in range(2):
                    k.mm(VP[:, 0:HG * 64], cT.t[:, 3 + kc, tt * 128:(tt + 1) * 128], wv[i][:, kc, :], kc == 0, kc == 1, r=[wv[i], cT_b[tt]], w=[VP])
                k.act(Vt.t[:, tt, :, 0:64], VP[:, 0:HG * 64].rearrange("p (h d) -> p h d", h=HG), AF.Copy, r=[VP], w=[V_b[tt]])
        pairs = []
        for h in range(HG):
            for Qb in range(8):
                nk = 4 * Qb + 4
                for jp in range(nk // 2):
                    pairs.append((h, Qb, jp, nk))
        OTs = {}

        def emit_qk(p):
            nonlocal n_st, n_ot
            h, Qb, jp, nk = p
            if jp == 0:
                OTs[(h, Qb)] = PS[4 + n_ot % 2]
                n_ot += 1
            j0 = jp * 2
            diag = j0 >= 4 * Qb
            q0 = (j0 - 4 * Qb) * 128 if diag else 0
            qs0 = Qb * 512
            SA, SB = PS[(n_st % 2) * 2], PS[(n_st % 2) * 2 + 1]
            ptt = pt[n_st % 3]
            n_st += 1
            for u, SP_ in enumerate((SA, SB)):
                j = j0 + u
                k.mm(SP_[:, q0:512], kT.t[0:96, h, j * 128:(j + 1) * 128], qT.t[0:96, h, qs0 + q0:qs0 + 512], True, True,
                     r=[kT_b[h][j // 4], qT_b[h][Qb]], w=[SP_])
            return (p, q0, diag, SA, SB, ptt)

        def emit_exp(st_):
            p, q0, diag, SA, SB, ptt = st_
            h, Qb, jp, nk = p
            dbk = DBK[0] if SA is PS[0] else DBK[1]
            k.act(ptt[:, :, q0:512], dbk[:, :].rearrange("p (u q) -> p u q", u=2)[:, :, q0:512], AF.Exp, r=[SA, SB], w=[ptt],
                  scale=SCALE)
            if diag:
                jj = jp * 2 - 4 * Qb
                k.v("dve", "tensor_tensor", r=[ptt, amask], w=[ptt], out=ptt[:, :, q0:512], in0=ptt[:, :, q0:512],
                    in1=amask[:, jj:jj + 2, q0:512], op=ALU.mult)

        def emit_pv(st_):
            p, q0, diag, SA, SB, ptt = st_
            h, Qb, jp, nk = p
            OT = OTs[(h, Qb)]
            for u in range(2):
                j = jp * 2 + u
                k.mm(OT[0:65, q0:512], Vt.t[:, j, h, 0:65], ptt[:, u, q0:512], j == 0, j == nk - 1, r=[V_b[j], ptt], w=[OT])
            if KEEP_WARM:
                k.mm(PS[7][:, 0:KEEP_WARM], ident_b[:, :], amask[:, 0, 0:KEEP_WARM], True, True, r=[ident_b, amask], w=[PS[7]])

        def emit_norm(p):
            nonlocal n_at
            h, Qb, jp, nk = p
            habs = hg * HG + h
            qs0 = Qb * 512
            OT = OTs[(h, Qb)]
            k.v("dve", "reciprocal", r=[OT], w=[rinv], out=rinv[64:65, :], in_=OT[64:65, :])
            BC = PS[6]
            k.mm(BC[0:64, :], ones_f[64:65, 0:64], rinv[64:65, :], True, True, r=[ones_f, rinv], w=[BC])
            k.act(osb[0:64, :], OT[0:64, :], AF.Copy, r=[OT], w=[osb])
            a_ = at[n_at % 2]
            n_at += 1
            k.v("dve", "tensor_tensor", r=[osb, BC], w=[a_], out=a_[0:64, :], in0=osb[0:64, :], in1=BC[0:64, :], op=ALU.mult)
            k.dma("sp", attnT_d[habs * 64:(habs + 1) * 64, qs0:qs0 + 512], a_[0:64, :], r=[a_], w=[B_attn])

        prev = None
        pend_norm = None
        for p in pairs:
            cur = emit_qk(p)
            if prev is not None:
                emit_pv(prev)
                if pend_norm is not None:
                    emit_norm(pend_norm)
                    pend_norm = None
                pp = prev[0]
                if pp[2] == pp[3] // 2 - 1:
                    pend_norm = pp
            emit_exp(cur)
            prev = cur
        emit_pv(prev)
        if pend_norm is not None:
            emit_norm(pend_norm)
        emit_norm(prev[0])
    st.close()
    stA.close()
    k.P.fence()
    st = contextlib.ExitStack()
    k.st = st
    wo = k.sb("wo", [128, 8, D], BF16)
    k.dma("pool", wo[:, :, :], c["mla_w_out"].rearrange("(kc p) d -> p kc d", p=128), w=[wo], partial=False)
    g_bc, b_bc = load_ln(k, c["ln_mix_g"], c["ln_mix_b"], 1)
    NB3 = 4
    aT = [k.sb(f"aT{i}", [128, 8, 128], BF16) for i in range(NB3)]
    hin = [k.sb(f"a3hin{i}", [128, D], F32) for i in range(NB3)]
    res = [k.sb(f"a3res{i}", [128, D], F32) for i in range(2)]
    lntmp = (k.sb("a3ln_stats", [128, 12], F32), k.sb("a3ln_mv", [128, 2], F32), k.sb("a3ln_lnv", [128, 1], F32),
             k.sb("a3ln_rstd", [128, 1], F32))
    at_v = attnT_d.rearrange("(kc p) t -> p kc t", p=128)
    hT_r3 = [k.sb(f"a3hTr{i}", [128, 8, 128], F32) for i in range(2)]

    def a3_issue(tt):
        k.dma("sp", aT[tt % NB3][:, :, :], at_v[:, :, tt * 128:(tt + 1) * 128], r=[B_attn], w=[aT[tt % NB3]], partial=False)
        k.dma("sp", hin[tt % NB3][:, :], h2_d[tt * 128:(tt + 1) * 128, :], r=[B_h2[tt]], w=[hin[tt % NB3]], partial=False)

    a3_issue(0)
    a3_issue(1)
    for tt in range(NT):
        if tt + 2 < NT:
            a3_issue(tt + 2)
        a_ = aT[tt % NB3]
        hi = hin[tt % NB3]
        r_ = res[tt % 2]
        for dh in range(2):
            OP = PS[4 + dh]
            for kc in range(8):
                k.mm(OP[:, :], a_[:, kc, :], wo[:, kc, dh * 512:(dh + 1) * 512], kc == 0, kc == 7, r=[a_, wo], w=[OP])
            k.v("dve", "scalar_tensor_tensor", r=[hi, OP], w=[r_], partial=True, out=r_[:, dh * 512:(dh + 1) * 512],
                in0=hi[:, dh * 512:(dh + 1) * 512], scalar=ALPHA, in1=OP[:, :], op0=ALU.mult, op1=ALU.add)
        if tt > 0:
            route_tile(k, c, 1, tt - 1, res[(tt - 1) % 2], hT_r3[(tt - 1) % 2], (PS[0], PS[1]), PS[6 + (tt - 1) % 2])
        layer_norm_tile(k, r_, g_bc, b_bc, r_, lntmp, eps_ln, "a3", eng2="pool")
        op = k.dma("sp", h3_d[tt * 128:(tt + 1) * 128, :], r_[:, :], r=[r_], w=[B_h3[tt]])
        k.out_ops.append(op)
    route_tile(k, c, 1, NT - 1, res[(NT - 1) % 2], hT_r3[(NT - 1) % 2], (PS[0], PS[1]), PS[6 + (NT - 1) % 2])
    st.close()
    k.P.fence()
    k.st = st_outer


I32 = mybir.dt.int32
IOA = bass.IndirectOffsetOnAxis
N_ITEMS = 32


def moe_layer(k, c, layer, hin_d, B_hin, hout_d, B_hout, is_final):
    PS = c["PS"]
    ident_f, ident_b, ones_f, eps_ln = c["ident_f"], c["ident_b"], c["ones_f"], c["eps_ln"]
    xsl_d, ysl_d, B_xsl, B_ysl = c["xsl_d"], c["ysl_d"], c["B_xsl"], c["B_ysl"]
    st_outer = k.st
    stR = contextlib.ExitStack()
    k.st = stR
    L = f"s{layer}"
    TH = NT
    g1 = k.sb(L + "g1", [128, TH], F32)
    g2 = k.sb(L + "g2", [128, TH], F32)
    pos1_i = k.sb(L + "pos1i", [128, TH], I32)
    pos2_i = k.sb(L + "pos2i", [128, TH], I32)
    widx = k.sb(L + "widx", [128, 2, N_ITEMS], I32)
    wg = [k.sb(f"{L}wg{i}", [128, 8, DFF], BF16) for i in range(2)]
    wu = [k.sb(f"{L}wu{i}", [128, 8, DFF], BF16) for i in range(2)]
    wd = [k.sb(f"{L}wd{i}", [128, 4, D], BF16) for i in range(2)]
    wsrc = (c["moe_w_gate"], c["moe_w_up"], c["moe_w_down"])

    def item_loads_w(i):
        b = i % 2
        for src, dst in zip(wsrc, (wg[b], wu[b], wd[b])):
            nh = dst.t.shape[1] // 2
            for hh in range(2):
                def gw(e, src=src, dst=dst, hh=hh, nh=nh, i=i):
                    if getattr(k, "bc_val", None) is None:
                        r_ = e.alloc_register("moe_bc")
                        e.reg_mov(r_, 2 * E * 128 * 2 - 1)
                        k.bc_val = e.snap(r_)
                    return e.indirect_dma_start(
                        out=dst[:, hh * nh:(hh + 1) * nh, :].rearrange("p a b -> p (a b)"), out_offset=None, in_=src,
                        in_offset=IOA(ap=widx[:, hh, i:i + 1], axis=0), bounds_check=k.bc_val, oob_is_err=False)
                k.P.add("pool", gw, _bufs([widx]), _bufs([dst]), dma=True, partial=(hh == 1))
    st = contextlib.ExitStack()
    k.st = st
    rw = k.sb(L + "rw", [128, 8, E], F32)
    k.dma("sp", rw[:, :, :], c["router_w"].rearrange("(kc p) e -> p kc e", p=128), w=[rw])
    rb = k.sb(L + "rb", [128, E], F32)
    k.dma("sp", rb[:, :], c["router_bias"].partition_broadcast(128), w=[rb])
    umat = k.sb(L + "umat", [128, 128], F32)
    k.dma("sp", umat[:, :], c["c_umat"], w=[umat])
    misc = k.sb(L + "misc", [128, 48], F32)
    k.dma("sp", misc[:, :], c["c_misc"], w=[misc])
    hin = [k.sb(f"{L}hin{i}", [128, D], F32) for i in range(3)]
    hTf = [k.sb(f"{L}hTf{i}", [128, 8, 128], F32) for i in range(2)]
    fused = bool(c.get("lg_fused", {}).get(layer))
    lg = c["lg_all"][layer] if fused else k.sb(L + "lg", [128, TH, E], F32)
    for tt in range(0 if fused else TH):
        hi = hin[tt % 3]
        hT_ = hTf[tt % 2]
        k.dma("sp", hi[:, :], hin_d[tt * 128:(tt + 1) * 128, :], r=[B_hin[tt]], w=[hi], partial=False)
        for half in range(2):
            pb = PS[(tt % 2) * 2 + half]
            for j in range(4):
                kc = half * 4 + j
                k.tr(pb[:, j * 128:(j + 1) * 128], hi[:, kc * 128:(kc + 1) * 128], ident_f[:, :], r=[hi, ident_f], w=[pb])
            if half == 0:
                k.act(hT_[:, 0:4, :], pb[:, :].rearrange("p (j c) -> p j c", j=4), AF.Copy, r=[pb], w=[hT_], partial=True)
            else:
                k.v("dve", "tensor_copy", r=[pb], w=[hT_], partial=True, out=hT_[:, 4:8, :], in_=pb[:, :].rearrange("p (j c) -> p j c", j=4))
        pl = PS[4 + tt % 2]
        for kc in range(8):
            k.mm(pl[:, 0:E], hT_[:, kc, :], rw[:, kc, :], kc == 0, kc == 7, r=[hT_, rw], w=[pl])
        k.act(lg[:, tt, :], pl[:, 0:E], AF.Exp, r=[pl], w=[lg], partial=True, scale=-1.0)
    T3 = lambda nm: k.sb(L + nm, [128, TH, E], F32)
    aff, sel, selm, mk1, selm2, mk2, w12, posall = (T3(n) for n in ("aff", "sel", "selm", "mk1", "selm2", "mk2", "w12", "posall"))
    p6 = k.sb(L + "p6", [128, TH * 4, 6], F32)
    gs = k.sb(L + "gs", [128, TH * 4], F32)
    gmax = k.sb(L + "gmax", [128, TH], F32)
    gm = k.sb(L + "gm", [128, TH * 4], F32)
    pen = k.sb(L + "pen", [128, TH * 4], F32)
    m1 = k.sb(L + "m1", [128, TH], F32)
    wsum = k.sb(L + "wsum", [128, TH], F32)
    V = lambda name, **kw: k.v("dve", name, **kw)
    V("tensor_scalar", r=[lg], w=[lg], out=lg[:, :, :], in0=lg[:, :, :], scalar1=1.0, scalar2=None, op0=ALU.add)
    V("reciprocal", r=[lg], w=[aff], out=aff[:, :, :], in_=lg[:, :, :])
    V("tensor_tensor", r=[aff, rb], w=[sel], out=sel[:, :, :], in0=aff[:, :, :], in1=bc(rb[:, :], [[0, TH], [1, E]]), op=ALU.add)
    s4 = sel[:, :, :].rearrange("p t (g i) -> p (t g) i", g=4)
    V("tensor_tensor", r=[sel], w=[p6], partial=True, out=p6[:, :, 0:3], in0=s4[:, :, 0:3], in1=s4[:, :, 1:4], op=ALU.add)
    V("tensor_tensor", r=[sel], w=[p6], partial=True, out=p6[:, :, 3:5], in0=s4[:, :, 0:2], in1=s4[:, :, 2:4], op=ALU.add)
    V("tensor_tensor", r=[sel], w=[p6], partial=True, out=p6[:, :, 5:6], in0=s4[:, :, 0:1], in1=s4[:, :, 3:4], op=ALU.add)
    V("tensor_reduce", r=[p6], w=[gs], out=gs[:, :], in_=p6[:, :, :], axis=AX.X, op=ALU.max)
    V("tensor_reduce", r=[gs], w=[gmax], out=gmax[:, :], in_=gs[:, :].rearrange("p (t g) -> p t g", g=4), axis=AX.X, op=ALU.max)
    V("tensor_tensor", r=[gs, gmax], w=[gm], out=gm[:, :].rearrange("p (t g) -> p t g", g=4),
      in0=gs[:, :].rearrange("p (t g) -> p t g", g=4), in1=bc(gmax[:, :], [[1, TH], [0, 4]]), op=ALU.is_ge)
    V("tensor_scalar", r=[gm], w=[pen], out=pen[:, :], in0=gm[:, :], scalar1=-1.0, scalar2=1.0e4, op0=ALU.add, op1=ALU.mult)
    sm4 = selm[:, :, :].rearrange("p t (g i) -> p (t g) i", g=4)
    V("tensor_tensor", r=[sel, gm], w=[selm], out=sm4, in0=s4, in1=bc(gm[:, :], [[1, TH * 4], [0, 4]]), op=ALU.mult)
    V("tensor_tensor", r=[selm, pen], w=[selm], out=sm4, in0=sm4, in1=bc(pen[:, :], [[1, TH * 4], [0, 4]]), op=ALU.add)
    V("tensor_reduce", r=[selm], w=[m1], out=m1[:, :], in_=selm[:, :, :], axis=AX.X, op=ALU.max)
    V("tensor_tensor", r=[selm, m1], w=[mk1], out=mk1[:, :, :], in0=selm[:, :, :], in1=bc(m1[:, :], [[1, TH], [0, E]]), op=ALU.is_ge)
    V("scalar_tensor_tensor", r=[mk1, selm], w=[selm2], out=selm2[:, :, :], in0=mk1[:, :, :], scalar=-1.0e4, in1=selm[:, :, :],
      op0=ALU.mult, op1=ALU.add)
    V("tensor_reduce", r=[selm2], w=[m1], out=m1[:, :], in_=selm2[:, :, :], axis=AX.X, op=ALU.max)
    V("tensor_tensor", r=[selm2, m1], w=[mk2], out=mk2[:, :, :], in0=selm2[:, :, :], in1=bc(m1[:, :], [[1, TH], [0, E]]), op=ALU.is_ge)
    V("tensor_tensor", r=[mk1, aff], w=[w12], out=w12[:, :, :], in0=mk1[:, :, :], in1=aff[:, :, :], op=ALU.mult)
    V("tensor_reduce", r=[w12], w=[g1], out=g1[:, :], in_=w12[:, :, :], axis=AX.X, op=ALU.add)
    V("tensor_tensor", r=[mk2, aff], w=[w12], out=w12[:, :, :], in0=mk2[:, :, :], in1=aff[:, :, :], op=ALU.mult)
    V("tensor_reduce", r=[w12], w=[g2], out=g2[:, :], in_=w12[:, :, :], axis=AX.X, op=ALU.add)
    V("tensor_tensor", r=[g1, g2], w=[wsum], out=wsum[:, :], in0=g1[:, :], in1=g2[:, :], op=ALU.add)
    V("reciprocal", r=[wsum], w=[wsum], out=wsum[:, :], in_=wsum[:, :])
    V("tensor_tensor", r=[g1, wsum], w=[g1], out=g1[:, :], in0=g1[:, :], in1=wsum[:, :], op=ALU.mult)
    V("tensor_tensor", r=[g2, wsum], w=[g2], out=g2[:, :], in0=g2[:, :], in1=wsum[:, :], op=ALU.mult)
    mk = w12
    V("tensor_tensor", r=[mk1, mk2], w=[mk], out=mk[:, :, :], in0=mk1[:, :, :], in1=mk2[:, :, :], op=ALU.add)
    PRE, TOT = PS[6], PS[7]
    mk2d = mk[:, :, :].rearrange("p t e -> p (t e)")
    k.mm(PRE[:, :], umat[:, :], mk2d, True, True, r=[umat, mk], w=[PRE])
    k.mm(TOT[:, :], ones_f[:, :], mk2d, True, True, r=[ones_f, mk], w=[TOT])
    tot = sel
    base = selm
    V("tensor_copy", r=[TOT], w=[tot], out=tot[:, :, :], in_=TOT[:, :].rearrange("p (t e) -> p t e", e=E))
    base_b = [Buf(f"{L}base{t}") for t in range(TH)]
    V("memset", w=[base_b[0]], ap=base[:, 0, :], constant=0.0)
    for t in range(1, TH):
        V("tensor_tensor", r=[base_b[t - 1], tot], w=[base_b[t]], out=base[:, t, :], in0=base[:, t - 1, :], in1=tot[:, t - 1, :], op=ALU.add)
    sm16 = k.sb(L + "sm16", [128, 6, E], F32)
    cmp8 = k.sb(L + "cmp8", [128, E, 8], F32)
    V("tensor_tensor", r=[base_b[TH - 1], tot], w=[sm16], out=sm16[:, 0, :], in0=base[:, TH - 1, :], in1=tot[:, TH - 1, :], op=ALU.add)
    V("tensor_tensor", r=[sm16, misc], w=[cmp8], out=cmp8[:, :, :], in0=bc(sm16[:, 0, :], [[1, E], [0, 8]]),
      in1=bc(misc[:, 0:8], [[0, E], [1, 8]]), op=ALU.is_gt)
    V("tensor_reduce", r=[cmp8], w=[sm16], out=sm16[:, 1, :], in_=cmp8[:, :, :], axis=AX.X, op=ALU.add)
    V("tensor_scalar", r=[sm16], w=[sm16], out=sm16[:, 2, :], in0=sm16[:, 1, :], scalar1=512.0, scalar2=None, op0=ALU.mult)
    V("memset", w=[sm16], ap=sm16[:, 3, 0:1], constant=0.0)
    for e in range(1, E):
        V("tensor_tensor", r=[sm16], w=[sm16], out=sm16[:, 3, e:e + 1], in0=sm16[:, 3, e - 1:e], in1=sm16[:, 2, e - 1:e], op=ALU.add)
    V("tensor_tensor", r=[sm16], w=[sm16], out=sm16[:, 4, :], in0=sm16[:, 3, :], in1=sm16[:, 2, :], op=ALU.add)
    V("tensor_tensor", r=[PRE] + base_b, w=[posall], out=posall[:, :, :], in0=PRE[:, :].rearrange("p (t e) -> p t e", e=E), in1=base[:, :, :],
      op=ALU.add)
    V("tensor_tensor", r=[posall, sm16], w=[posall], out=posall[:, :, :], in0=posall[:, :, :], in1=bc(sm16[:, 3, :], [[0, TH], [1, E]]),
      op=ALU.add)
    posf = k.sb(L + "posf", [128, 2, TH], F32)
    V("tensor_tensor", r=[mk1, posall], w=[mk1], out=mk1[:, :, :], in0=mk1[:, :, :], in1=posall[:, :, :], op=ALU.mult)
    V("tensor_reduce", r=[mk1], w=[posf], out=posf[:, 0, :], in_=mk1[:, :, :], axis=AX.X, op=ALU.add)
    V("tensor_tensor", r=[mk2, posall], w=[mk2], out=mk2[:, :, :], in0=mk2[:, :, :], in1=posall[:, :, :], op=ALU.mult)
    V("tensor_reduce", r=[mk2], w=[posf], out=posf[:, 1, :], in_=mk2[:, :, :], axis=AX.X, op=ALU.add)
    V("tensor_copy", r=[posf], w=[pos1_i], out=pos1_i[:, :], in_=posf[:, 0, :])
    V("tensor_copy", r=[posf], w=[pos2_i], out=pos2_i[:, :], in_=posf[:, 1, :])
    icmp = k.sb(L + "icmp", [128, N_ITEMS, E], F32)
    ei = k.sb(L + "ei", [128, N_ITEMS], F32)
    V("tensor_tensor", r=[sm16, misc], w=[icmp], out=icmp[:, :, :], in0=bc(sm16[:, 4, :], [[0, N_ITEMS], [1, E]]),
      in1=bc(misc[:, 8:40], [[1, N_ITEMS], [0, E]]), op=ALU.is_le)
    V("tensor_reduce", r=[icmp], w=[ei], out=ei[:, :], in_=icmp[:, :, :], axis=AX.X, op=ALU.add)
    oob = k.sb(L + "oob", [128, N_ITEMS], F32)
    V("tensor_scalar", r=[ei], w=[oob], out=oob[:, :], in0=ei[:, :], scalar1=float(E), scalar2=4.0e6, op0=ALU.is_ge, op1=ALU.mult)
    V("tensor_scalar", r=[ei], w=[ei], out=ei[:, :], in0=ei[:, :], scalar1=15.0, scalar2=float(layer * E), op0=ALU.min, op1=ALU.add)
    V("tensor_scalar", r=[ei], w=[ei], out=ei[:, :], in0=ei[:, :], scalar1=256.0, scalar2=None, op0=ALU.mult)
    V("tensor_tensor", r=[ei, oob], w=[ei], out=ei[:, :], in0=ei[:, :], in1=oob[:, :], op=ALU.add)
    V("scalar_tensor_tensor", r=[misc, ei], w=[ei], out=ei[:, :], in0=bc(misc[:, 40:41], [[0, N_ITEMS]]), scalar=2.0, in1=ei[:, :],
      op0=ALU.mult, op1=ALU.add)
    V("tensor_copy", r=[ei], w=[widx], partial=True, out=widx[:, 0, :], in_=ei[:, :])
    V("tensor_scalar", r=[ei], w=[ei], out=ei[:, :], in0=ei[:, :], scalar1=1.0, scalar2=None, op0=ALU.add)
    V("tensor_copy", r=[ei], w=[widx], partial=True, out=widx[:, 1, :], in_=ei[:, :])
    stage = c.get("moe_stage", "C")
    if "dbg_outs" in c:
        do = c["dbg_outs"]
        for nm, t_ in (("d_pos1", pos1_i), ("d_pos2", pos2_i), ("d_g1", g1), ("d_g2", g2)):
            k.out_ops.append(k.dma("sp", do[nm], t_[:, :], r=[t_]))
        k.out_ops.append(k.dma("sp", do["d_widx"], widx[:, :, :], r=[widx]))
    if stage == "P":
        st.close(); stR.close(); k.P.fence(); k.st = st_outer
        return
    xb = [k.sb(f"{L}xb{i}", [128, D], BF16) for i in range(3)]
    item_loads_w(0)
    item_loads_w(1)
    for tt in range(TH):
        x_ = xb[tt % 3]
        hi = hin[tt % 3]
        k.dma("sp", hi[:, :], hin_d[tt * 128:(tt + 1) * 128, :], r=[B_hin[tt]], w=[hi], partial=False)
        k.act(x_[:, :], hi[:, :], AF.Copy, r=[hi], w=[x_])
        for pi_ in (pos1_i, pos2_i):
            k.P.add("pool", (lambda e, x_=x_, pi_=pi_, tt=tt: e.indirect_dma_start(
                out=xsl_d, out_offset=IOA(ap=pi_[:, tt:tt + 1], axis=0), in_=x_[:, :], in_offset=None)),
                _bufs([x_, pi_]), [B_xsl], dma=True, partial=True)
    st.close()
    k.P.fence()
    if stage == "S":
        stR.close(); k.st = st_outer
        return
    st = contextlib.ExitStack()
    k.st = st
    xi = [k.sb(f"{L}xi{i}", [128, 4, D], BF16) for i in range(2)]
    xTi = [k.sb(f"{L}xTi{i}", [128, 8, 512], BF16) for i in range(2)]
    hT = [k.sb(f"{L}hT{i}", [128, 4, 512], BF16) for i in range(2)]
    sg = [k.sb(f"{L}sg{i}", [128, 512], BF16) for i in range(2)]
    yi = [k.sb(f"{L}yi{i}", [128, 4, D], F32) for i in range(2)]
    xsl_v = xsl_d.rearrange("(i s p) d -> i p s d", p=128, s=4)
    ysl_v = ysl_d.rearrange("(i s p) d -> i p s d", p=128, s=4)
    n_g = 0
    n_d = 0

    def item_loads(i, with_w=True):
        b = i % 2
        if with_w:
            item_loads_w(i)
        k.dma("sp", xi[b][:, :, :], xsl_v[i], r=[B_xsl], w=[xi[b]], partial=False)

    item_loads(0, with_w=False)
    for i in range(N_ITEMS):
        b = i % 2
        if i + 1 < N_ITEMS:
            item_loads(i + 1, with_w=(i + 1 >= 2))
        xT_ = xTi[b]
        for s_ in range(4):
            pb = PS[6 + s_ % 2]
            pv = pb[:, :].bitcast(BF16)
            for kc in range(8):
                k.tr(pv[:, kc * 128:(kc + 1) * 128], xi[b][:, s_, kc * 128:(kc + 1) * 128], ident_b[:, :], r=[xi[b], ident_b], w=[pb])
            if s_ % 2:
                k.act(xT_[:, :, s_ * 128:(s_ + 1) * 128], pv.rearrange("p (k t) -> p k t", k=8), AF.Copy, r=[pb], w=[xT_], partial=True)
            else:
                k.v("dve", "tensor_copy", r=[pb], w=[xT_], partial=True, out=xT_[:, :, s_ * 128:(s_ + 1) * 128],
                    in_=pv.rearrange("p (k t) -> p k t", k=8))
        hTt = hT[b]
        for fc in range(4):
            G = PS[n_g % 2]
            U = PS[2 + n_g % 2]
            s2 = sg[n_g % 2]
            n_g += 1
            for kc in range(8):
                k.mm(G[:, :], wg[b][:, kc, fc * 128:(fc + 1) * 128], xT_[:, kc, :], kc == 0, kc == 7, r=[wg[b], xT_], w=[G])
            for kc in range(8):
                k.mm(U[:, :], wu[b][:, kc, fc * 128:(fc + 1) * 128], xT_[:, kc, :], kc == 0, kc == 7, r=[wu[b], xT_], w=[U])
            k.act(s2[:, :], G[:, :], AF.Silu, r=[G], w=[s2])
            k.v("dve", "tensor_tensor", r=[s2, U], w=[hTt], partial=True, out=hTt[:, fc, :], in0=s2[:, :], in1=U[:, :], op=ALU.mult)
        y_ = yi[b]
        for tl in range(4):
            for dh in range(2):
                Dp = PS[4 + n_d % 2]
                n_d += 1
                for fc in range(4):
                    k.mm(Dp[:, :], hTt[:, fc, tl * 128:(tl + 1) * 128], wd[b][:, fc, dh * 512:(dh + 1) * 512], fc == 0, fc == 3,
                         r=[hTt, wd[b]], w=[Dp])
                if n_d % 2:
                    k.act(y_[:, tl, dh * 512:(dh + 1) * 512], Dp[:, :], AF.Copy, r=[Dp], w=[y_], partial=True)
                else:
                    k.v("dve", "tensor_copy", r=[Dp], w=[y_], partial=True, out=y_[:, tl, dh * 512:(dh + 1) * 512], in_=Dp[:, :])
        k.dma("sp", ysl_v[i], y_[:, :, :], r=[y_], w=[B_ysl])
    st.close()
    k.P.fence()
    if stage == "X":
        stR.close(); k.st = st_outer
        return
    st = contextlib.ExitStack()
    k.st = st
    g_bc, b_bc = load_ln(k, c["ln_ffn_g"], c["ln_ffn_b"], layer)
    NBC = 4
    hin = [k.sb(f"{L}chin{i}", [128, D], F32) for i in range(NBC)]
    y1 = [k.sb(f"{L}y1{i}", [128, D], F32) for i in range(NBC)]
    y2 = [k.sb(f"{L}y2{i}", [128, D], F32) for i in range(NBC)]
    res = [k.sb(f"{L}res{i}", [128, D], F32) for i in range(2)]
    lntmp = (k.sb(L + "ln_stats", [128, 12], F32), k.sb(L + "ln_mv", [128, 2], F32), k.sb(L + "ln_lnv", [128, 1], F32),
             k.sb(L + "ln_rstd", [128, 1], F32))

    def c_issue(tt):
        b = tt % NBC
        k.dma("sp", hin[b][:, :], hin_d[tt * 128:(tt + 1) * 128, :], r=[B_hin[tt]], w=[hin[b]], partial=False)
        for y_, pi_ in ((y1[b], pos1_i), (y2[b], pos2_i)):
            k.P.add("pool", (lambda e, y_=y_, pi_=pi_, tt=tt: e.indirect_dma_start(
                out=y_[:, :], out_offset=None, in_=ysl_d, in_offset=IOA(ap=pi_[:, tt:tt + 1], axis=0))),
                _bufs([pi_]) + [B_ysl], _bufs([y_]), dma=True, partial=False)

    c_issue(0)
    c_issue(1)
    for tt in range(TH):
        b = tt % NBC
        if tt + 2 < TH:
            c_issue(tt + 2)
        hi = hin[b]
        r_ = res[tt % 2]
        k.v("dve", "tensor_scalar", r=[y1[b], g1], w=[y1[b]], out=y1[b][:, :], in0=y1[b][:, :], scalar1=g1[:, tt:tt + 1], scalar2=None, op0=ALU.mult)
        k.v("dve", "scalar_tensor_tensor", r=[y2[b], g2, y1[b]], w=[y1[b]], out=y1[b][:, :], in0=y2[b][:, :], scalar=g2[:, tt:tt + 1], in1=y1[b][:, :],
            op0=ALU.mult, op1=ALU.add)
        k.v("dve", "scalar_tensor_tensor", r=[hi, y1[b]], w=[r_], out=r_[:, :], in0=hi[:, :], scalar=ALPHA, in1=y1[b][:, :],
            op0=ALU.mult, op1=ALU.add)
        layer_norm_tile(k, r_, g_bc, b_bc, r_, lntmp, eps_ln, L, eng2="dve")
        op = k.dma("sp", hout_d[tt * 128:(tt + 1) * 128, :], r_[:, :], r=[r_], w=[B_hout[tt]] if B_hout is not None else [])
        k.out_ops.append(op)
    st.close()
    stR.close()
    k.P.fence()
    k.st = st_outer


def _consts():
    j = np.arange(128)
    rmat = (j[:, None] <= j[None, :]).astype(np.float32)
    lmat = (j[:, None] > j[None, :]).astype(np.float32)
    inv = (10000.0 ** (-np.arange(0, 32, 2, dtype=np.float32) / 32)).astype(np.float32)
    ang = np.arange(S, dtype=np.float32)[:, None] * inv[None, :]
    cos, sin = np.cos(ang).astype(np.float32), np.sin(ang).astype(np.float32)
    rope = np.zeros((2, 128, S), np.float32)
    rope[0, 64:80] = cos.T
    rope[0, 80:96] = cos.T
    rope[1, 64:80] = -sin.T
    rope[1, 80:96] = sin.T
    kk = np.arange(128)[:, None, None]
    jj = np.arange(4)[None, :, None]
    qq = np.arange(512)[None, None, :]
    amask = ((qq // 64) >= ((jj * 128 + kk) // 64)).astype(np.float32)
    umat = (j[:, None] < j[None, :]).astype(np.float32)
    misc = np.zeros((128, 48), np.float32)
    misc[:, 0:8] = 512.0 * np.arange(8)[None, :]
    misc[:, 8:40] = 512.0 * np.arange(32)[None, :]
    misc[:, 40] = np.arange(128)
    return {"c_ident": np.eye(128, dtype=np.float32), "c_rmat": rmat, "c_lmat": lmat, "c_rope": rope,
            "c_amask": np.ascontiguousarray(amask), "c_umat": umat, "c_misc": misc}


def prep_shared(inp):
    f = lambda a: np.ascontiguousarray(np.asarray(a, dtype=np.float32))
    sh = {}
    sh["ssd_w_in"] = f(inp["ssd_w_in"][0])
    cw = np.asarray(inp["ssd_conv_w"][0], np.float32)
    sh["ssd_conv_w"] = f(cw.reshape(4, 32, 128).transpose(2, 1, 0).reshape(128, 128))
    sh["ssd_conv_b"] = f(np.asarray(inp["ssd_conv_b"][0], np.float32).reshape(32, 128).T)
    for n in ("ssd_dt_bias", "ssd_a_log", "ssd_d", "ssd_norm_w", "ssd_w_out", "mla_w_down", "mla_q_norm", "mla_w_uq",
              "mla_kv_norm", "mla_w_ukv", "mla_w_out"):
        sh[n] = f(inp[n][0])
    wd = sh["mla_w_down"]
    sh["mla_w_kr"] = f(np.concatenate([wd[:, 0:64], wd[:, 640:672], wd[:, 0:64], wd[:, 656:672], wd[:, 640:656]], axis=1))
    wkv = sh.pop("mla_w_ukv").reshape(256, 16, 128)
    sh["mla_w_kn"] = f(wkv[:, :, 0:64].reshape(256, 1024))
    sh["mla_w_v"] = f(wkv[:, :, 64:128].reshape(256, 1024))
    wq = sh["mla_w_uq"].reshape(384, 16, 96)
    sh["mla_w_uq_sw"] = f(np.concatenate([wq[:, :, 0:64], wq[:, :, 80:96], wq[:, :, 64:80]], axis=2).reshape(384, 1536))
    for n in ("router_w", "router_bias", "ln_mix_g", "ln_mix_b", "ln_ffn_g", "ln_ffn_b"):
        sh[n] = f(inp[n])
    for n in ("moe_w_gate", "moe_w_up"):
        w = np.asarray(inp[n], np.float32).reshape(2, E, 8, 128, DFF)
        sh[n] = f(w.transpose(0, 1, 3, 2, 4).reshape(2 * E * 128 * 2, 2048))
    w = np.asarray(inp["moe_w_down"], np.float32).reshape(2, E, 4, 128, D)
    sh["moe_w_down"] = f(w.transpose(0, 1, 3, 2, 4).reshape(2 * E * 128 * 2, 2048))
    sh.update(_consts())
    return sh


_NC_CACHE = {}


def kernel(**inputs):
    sh = prep_shared(inputs)
    x = np.asarray(inputs["x"], np.float32)
    if "nc" not in _NC_CACHE:
        _NC_CACHE["nc"] = build_program()
    nc = _NC_CACHE["nc"]
    in_maps = []
    for b in range(8):
        m = dict(sh)
        m["x"] = np.ascontiguousarray(x[b])
        in_maps.append(m)
    res = run_bass_kernel_spmd(nc, in_maps, core_ids=list(range(8)))
    return np.stack([np.asarray(r["out"], np.float32) for r in res.results], axis=0)
```

```python
import contextlib
import math
import numpy as np
import ml_dtypes
import concourse.bass as bass
import concourse.mybir as mybir
from concourse.bass_utils import run_bass_kernel_spmd

F32 = mybir.dt.float32
BF16 = mybir.dt.bfloat16
AF = mybir.ActivationFunctionType
ALU = mybir.AluOpType
AX = mybir.AxisListType

KEEP_WARM = 0
N_DMA_SEMS = {"sp": 12, "pool": 8, "act": 4}

S = 4096
D = 1024
NT = S // 128
ALPHA = (2.0 * 2) ** 0.25
LN_EPS = 1e-5
RMS_EPS = 1e-6
NH_SSD = 32
E = 16
DFF = 512
MLA_H = 16


class Buf:
    __slots__ = ("name", "gen_w", "gen_r", "prev_w", "prev_r")

    def __init__(self, name=""):
        self.name = name
        self.gen_w = []
        self.gen_r = []
        self.prev_w = []
        self.prev_r = []


class Op:
    __slots__ = ("eng", "fn", "deps", "is_dma", "idx", "needed", "sig", "dma_i")

    def __init__(self, eng, fn, is_dma, idx):
        self.eng = eng
        self.fn = fn
        self.deps = []
        self.is_dma = is_dma
        self.idx = idx
        self.needed = False
        self.sig = None
        self.dma_i = None


class Prog:
    def __init__(self, nc):
        self.nc = nc
        self.ops = []
        self.by_eng = {e: [] for e in ("pe", "act", "dve", "pool", "sp")}
        self.dma_count = {q: 0 for q in N_DMA_SEMS}
        self.fence_deps = []
        self.fence_seen = set()

    def fence(self):
        deps = []
        for e, lst in self.by_eng.items():
            last_c = None
            dmas = []
            for o in reversed(lst):
                if o.is_dma:
                    if len(dmas) < N_DMA_SEMS[e]:
                        dmas.append(o)
                elif last_c is None:
                    last_c = o
                if last_c is not None and (e not in N_DMA_SEMS or len(dmas) >= N_DMA_SEMS[e]):
                    break
            if last_c is not None:
                deps.append(last_c)
            deps.extend(dmas)
        self.fence_deps = deps
        self.fence_seen = set()

    def add(self, eng, fn, reads=(), writes=(), dma=False, partial=False):
        op = Op(eng, fn, dma, len(self.ops))
        deps = {}
        if self.fence_deps and eng not in self.fence_seen:
            self.fence_seen.add(eng)
            for o in self.fence_deps:
                deps[o.idx] = o

        def dep(o):
            if o is op:
                return
            if (not o.is_dma) and (not dma) and o.eng == eng and eng == "pe":
                return
            deps[o.idx] = o

        for b in reads:
            for w in b.gen_w:
                dep(w)
        for b in writes:
            if b.gen_r or not partial or not b.gen_w:
                for r in b.gen_r:
                    dep(r)
                for w in b.gen_w:
                    dep(w)
                b.prev_w, b.prev_r = b.gen_w, b.gen_r
                b.gen_w, b.gen_r = [op], []
            else:
                for r in b.prev_r:
                    dep(r)
                for w in b.prev_w:
                    dep(w)
                b.gen_w.append(op)
        for b in reads:
            if b in writes:
                continue
            if dma:
                b.gen_r.append(op)
            else:
                b.gen_r = [r for r in b.gen_r if r.is_dma or r.eng != eng]
                b.gen_r.append(op)
        for b in writes:
            if not dma and len(b.gen_w) > 1:
                b.gen_w = [w for w in b.gen_w if w.is_dma or w.eng != eng or w is op]
        op.deps = list(deps.values())
        for d in op.deps:
            d.needed = True
        self.ops.append(op)
        self.by_eng[eng].append(op)
        if dma:
            op.dma_i = self.dma_count[eng]
            self.dma_count[eng] += 1
        return op

    def emit(self, st, final_wait_ops=()):
        nc = self.nc
        esem = {e: st.enter_context(nc.semaphore("s_" + e)) for e in self.by_eng}
        dsem = {
            q: [st.enter_context(nc.semaphore(f"d_{q}{i}")) for i in range(n)]
            for q, n in N_DMA_SEMS.items()
        }
        cnt = {e: 0 for e in self.by_eng}
        for op in self.ops:
            if op.is_dma:
                n = N_DMA_SEMS[op.eng]
                j = op.dma_i % n
                op.sig = (dsem[op.eng][j], 16 * (op.dma_i // n + 1))
            elif op.needed:
                cnt[op.eng] += 1
                op.sig = (esem[op.eng], cnt[op.eng])
        self.final_counts = dict(cnt)
        block = st.enter_context(nc.Block())

        def run_engine(ename, eng):
            known = {}

            def wait(sig):
                sem, val = sig
                if known.get(sem.num, 0) >= val:
                    return
                eng.wait_ge(sem, val)
                known[sem.num] = val

            for op in self.by_eng[ename]:
                for d in op.deps:
                    wait(d.sig)
                if op.is_dma:
                    sem, val = op.sig
                    if val > 16:
                        wait((sem, val - 16))
                    ins = op.fn(eng)
                    ins.then_inc(sem, 16)
                else:
                    ins = op.fn(eng)
                    if op.sig is not None:
                        ins.then_inc(op.sig[0], 1)
            if ename == "sp":
                for op in final_wait_ops:
                    wait(op.sig)

        @block.tensor
        def _(e):
            run_engine("pe", e)

        @block.scalar
        def _(e):
            run_engine("act", e)

        @block.vector
        def _(e):
            run_engine("dve", e)

        @block.gpsimd
        def _(e):
            run_engine("pool", e)

        @block.sync
        def _(e):
            run_engine("sp", e)


class TT:
    def __init__(self, t, name):
        self.t = t
        self.b = Buf(name)

    def __getitem__(self, k):
        return self.t[k]


def _bufs(xs):
    out = []
    for x in xs:
        if x is None:
            continue
        out.append(x.b if isinstance(x, TT) else x)
    return out


class K:
    def __init__(self, nc, st):
        self.nc = nc
        self.st = st
        self.P = Prog(nc)
        self.out_ops = []

    def sb(self, name, shape, dt):
        return TT(self.st.enter_context(self.nc.sbuf_tensor(name, shape, dt)), name)

    def ps(self, name, shape, dt):
        return TT(self.st.enter_context(self.nc.psum_tensor(name, shape, dt)), name)

    def dram(self, name, shape, dt, kind="Internal"):
        return self.nc.dram_tensor(name, shape, dt, kind=kind).ap()

    def op(self, eng, fn, r=(), w=(), partial=False):
        return self.P.add(eng, fn, _bufs(r), _bufs(w), dma=False, partial=partial)

    def dma(self, q, out, in_, r=(), w=(), partial=True):
        return self.P.add(q, lambda e: e.dma_start(out=out, in_=in_), _bufs(r), _bufs(w), dma=True, partial=partial)

    def mm(self, out, lhsT, rhs, start, stop, r=(), w=()):
        return self.P.add("pe", lambda e: e.matmul(out, lhsT=lhsT, rhs=rhs, start=start, stop=stop),
                          _bufs(r), _bufs(w), partial=True)

    def tr(self, out, in_, ident, r=(), w=()):
        return self.P.add("pe", lambda e: e.transpose(out=out, in_=in_, identity=ident),
                          _bufs(r), _bufs(w), partial=True)

    def act(self, out, in_, func, r=(), w=(), partial=False, **kw):
        return self.P.add("act", lambda e: e.activation(out=out, in_=in_, func=func, **kw),
                          _bufs(r), _bufs(w), partial=partial)

    def v(self, eng, name, r=(), w=(), partial=False, **kw):
        return self.P.add(eng, lambda e: getattr(e, name)(**kw), _bufs(r), _bufs(w), partial=partial)


def bc(ap, dims):
    a = list(ap.ap)
    return bass.AP(ap.tensor, ap.offset, [list(a[0])] + [list(d) for d in dims])


def layer_norm_tile(k, r, g_bc, b_bc, out, tmp, eps_t, tag, eng2="dve"):
    stats, mv, lnv, rstd = tmp
    for i in range(2):
        k.v("dve", "bn_stats", r=[r], w=[stats], partial=True, out=stats[:, i * 6:(i + 1) * 6], in_=r[:, i * 512:(i + 1) * 512])
    k.v("dve", "bn_aggr", r=[stats], w=[mv], out=mv[:, 0:2], in_=stats[:, 0:12])
    k.act(lnv[:, 0:1], mv[:, 1:2], AF.Ln, r=[mv, eps_t], w=[lnv], bias=eps_t[:, 0:1])
    k.act(rstd[:, 0:1], lnv[:, 0:1], AF.Exp, r=[lnv], w=[rstd], scale=-0.5)
    k.v("dve", "tensor_scalar", r=[r, mv, rstd], w=[r], out=r[:, :], in0=r[:, :], scalar1=mv[:, 0:1], scalar2=rstd[:, 0:1],
        op0=ALU.subtract, op1=ALU.mult)
    k.v(eng2, "tensor_tensor", r=[r, g_bc], w=[r], out=r[:, :], in0=r[:, :], in1=g_bc[:, :], op=ALU.mult)
    k.v(eng2, "tensor_tensor", r=[r, b_bc], w=[out], out=out[:, :], in0=r[:, :], in1=b_bc[:, :], op=ALU.add)


def build_program(stop_after=None, dbg=False):
    nc = bass.Bass("TRN2", target_bir_lowering=False)
    st = contextlib.ExitStack()
    k = K(nc, st)

    def din(name, shape, dt=F32):
        return nc.dram_tensor(name, list(shape), dt, kind="ExternalInput").ap()

    x_d = din("x", [S, D])
    ssd_w_in = din("ssd_w_in", [D, 6176])
    ssd_conv_w = din("ssd_conv_w", [4, 4096])
    ssd_conv_b = din("ssd_conv_b", [4096])
    ssd_dt_bias = din("ssd_dt_bias", [32])
    ssd_a_log = din("ssd_a_log", [32])
    ssd_d = din("ssd_d", [32])
    ssd_norm_w = din("ssd_norm_w", [2048])
    ssd_w_out = din("ssd_w_out", [2048, D])
    mla_w_down = din("mla_w_down", [D, 672])
    mla_w_kr = din("mla_w_kr", [D, 192])
    mla_q_norm = din("mla_q_norm", [384])
    mla_w_uq = din("mla_w_uq", [384, 1536])
    mla_w_uq_sw = din("mla_w_uq_sw", [384, 1536])
    mla_kv_norm = din("mla_kv_norm", [256])
    mla_w_kn = din("mla_w_kn", [256, 1024])
    mla_w_v = din("mla_w_v", [256, 1024])
    mla_w_out = din("mla_w_out", [D, D])
    router_w = din("router_w", [D, E])
    router_bias = din("router_bias", [E])
    moe_w_gate = din("moe_w_gate", [2 * E * 128 * 2, 2048])
    moe_w_up = din("moe_w_up", [2 * E * 128 * 2, 2048])
    moe_w_down = din("moe_w_down", [2 * E * 128 * 2, 2048])
    ln_mix_g = din("ln_mix_g", [2, D])
    ln_mix_b = din("ln_mix_b", [2, D])
    ln_ffn_g = din("ln_ffn_g", [2, D])
    ln_ffn_b = din("ln_ffn_b", [2, D])
    c_ident = din("c_ident", [128, 128])
    c_rmat = din("c_rmat", [128, 128])
    c_lmat = din("c_lmat", [128, 128])
    c_rope = din("c_rope", [2, 128, S])
    c_amask = din("c_amask", [128, 4, 512])
    c_umat = din("c_umat", [128, 128])
    c_misc = din("c_misc", [128, 48])
    out_d = nc.dram_tensor("out", [S, D], F32, kind="ExternalOutput").ap()

    h1_d = k.dram("h1_d", [S, D], F32, kind="ExternalOutput" if dbg else "Internal")
    h2_d = k.dram("h2_d", [S, D], F32, kind="ExternalOutput" if dbg else "Internal")
    h3_d = k.dram("h3_d", [S, D], F32, kind="ExternalOutput" if dbg else "Internal")
    xs_d = k.dram("xs_d", [S, 2048], BF16)
    bt_d = k.dram("bt_d", [S, 1024], BF16)
    bT_d = k.dram("bT_d", [1024, S], BF16)
    cT_d = k.dram("cT_d", [1024, S], BF16)
    z_d = k.dram("z_d", [S, 2048], BF16)
    dt_d = k.dram("dt_d", [S, 32], F32)
    a_d = k.dram("a_d", [S, 32], F32)
    attnT_d = k.dram("attnT_d", [D, S], BF16)
    rn_d = k.dram("rn_d", [128, 512], F32)
    NSLOT = 16384
    xsl_d = k.dram("xsl_d", [NSLOT, D], BF16)
    ysl_d = k.dram("ysl_d", [NSLOT, D], F32)
    B_xsl = Buf("xsl"); B_ysl = Buf("ysl")
    zt = k.sb("zero_t", [128, D], BF16)
    k.v("pool", "memset", w=[zt], ap=zt[:, :], constant=0.0)
    xsl_z = xsl_d.rearrange("(a p) d -> a p d", p=128)
    for a_ in range(NSLOT // 128):
        k.dma("sp", xsl_z[a_], zt[:, :], r=[zt], w=[B_xsl])
    B_h = [[Buf(f"h{i}_{t}") for t in range(NT)] for i in range(4)]
    B_xs = Buf("xs_d"); B_bt = Buf("bt_d"); B_bT = Buf("bT_d"); B_cT = Buf("cT_d")
    B_z = Buf("z_d"); B_dt = Buf("dt_d"); B_a = Buf("a_d"); B_attn = Buf("attn_d")
    B_out = Buf("out")

    ident_f = k.sb("ident_f", [128, 128], F32)
    ident_b = k.sb("ident_b", [128, 128], BF16)
    rmat_f = k.sb("rmat_f", [128, 128], F32)
    rmat_b = k.sb("rmat_b", [128, 128], BF16)
    lmat_f = k.sb("lmat_f", [128, 128], F32)
    lmat_b = k.sb("lmat_b", [128, 128], BF16)
    ones_f = k.sb("ones_f", [128, 128], F32)
    ones_b = k.sb("ones_b", [128, 128], BF16)
    eps_ln = k.sb("eps_ln", [128, 1], F32)
    eps_rms = k.sb("eps_rms", [128, 1], F32)
    k.dma("sp", ident_f[:, :], c_ident, w=[ident_f])
    k.dma("sp", rmat_f[:, :], c_rmat, w=[rmat_f])
    k.dma("sp", lmat_f[:, :], c_lmat, w=[lmat_f])
    k.v("dve", "tensor_copy", r=[ident_f], w=[ident_b], out=ident_b[:, :], in_=ident_f[:, :])
    k.v("dve", "tensor_copy", r=[rmat_f], w=[rmat_b], out=rmat_b[:, :], in_=rmat_f[:, :])
    k.v("dve", "tensor_copy", r=[lmat_f], w=[lmat_b], out=lmat_b[:, :], in_=lmat_f[:, :])
    k.v("dve", "memset", w=[ones_f], ap=ones_f[:, :], constant=1.0)
    k.v("dve", "memset", w=[ones_b], ap=ones_b[:, :], constant=1.0)
    k.v("dve", "memset", w=[eps_ln], ap=eps_ln[:, :], constant=LN_EPS)
    k.v("dve", "memset", w=[eps_rms], ap=eps_rms[:, :], constant=RMS_EPS)

    DBK = [k.ps(f"psd{i}", [128, 1024], F32) for i in range(2)]
    PS = []
    for i in range(4):
        v_ = TTview(DBK[i // 2], DBK[i // 2].t[:, (i % 2) * 512:(i % 2 + 1) * 512])
        v_.b = Buf(f"ps{i}")
        PS.append(v_)
    PS += [k.ps(f"ps{i}", [128, 512], F32) for i in range(4, 6)]
    DBK.append(k.ps("psd2", [128, 1024], F32))
    for i in range(2):
        v_ = TTview(DBK[2], DBK[2].t[:, i * 512:(i + 1) * 512])
        v_.b = Buf(f"ps{6 + i}")
        PS.append(v_)

    rw_g = k.sb("rw_g", [128, 8, E], F32)
    k.dma("sp", rw_g[:, :, :], router_w.rearrange("(kc p) e -> p kc e", p=128), w=[rw_g])
    lg_all = [None, None]
    lg_fused = {}
    ctx = dict(locals())
    if stop_after is not None and stop_after.startswith("moeonly"):
        h1_in = din("h1_in", [S, D])
        ctx["moe_stage"] = stop_after.split(":")[1]
        ctx["dbg_outs"] = {}
        for nm, shp, dt_ in (("d_pos1", [128, NT], mybir.dt.int32), ("d_pos2", [128, NT], mybir.dt.int32),
                             ("d_widx", [128, 2, 32], mybir.dt.int32), ("d_g1", [128, NT], F32), ("d_g2", [128, NT], F32)):
            ctx["dbg_outs"][nm] = nc.dram_tensor(nm, shp, dt_, kind="ExternalOutput").ap()
        moe_layer(k, ctx, 0, h1_in, [Buf() for _ in range(NT)], h2_d, B_h[1], False)
        return finish(k, nc, st)
    ssd_layer(k, ctx)
    if stop_after == "ssd":
        return finish(k, nc, st)
    moe_layer(k, ctx, 0, h1_d, B_h[0], h2_d, B_h[1], False)
    if stop_after == "moe0":
        return finish(k, nc, st)
    mla_layer(k, ctx)
    if stop_after == "mla":
        return finish(k, nc, st)
    moe_layer(k, ctx, 1, h3_d, B_h[2], out_d, None, True)
    return finish(k, nc, st)


def route_step(k, c, layer, tt, src, hT_, tr_banks, lg_bank, step):
    ident_f, rw, lg = c["ident_f"], c["rw_g"], c["lg_all"][layer]
    if step < 2:
        half = step
        pb = tr_banks[half]
        for j in range(4):
            kc = half * 4 + j
            k.tr(pb[:, j * 128:(j + 1) * 128], src[:, kc * 128:(kc + 1) * 128], ident_f[:, :], r=[src, ident_f], w=[pb])
        k.act(hT_[:, half * 4:half * 4 + 4, :], pb[:, :].rearrange("p (j c) -> p j c", j=4), AF.Copy, r=[pb], w=[hT_], partial=True)
    else:
        for kc in range(8):
            k.mm(lg_bank[:, 0:E], hT_[:, kc, :], rw[:, kc, :], kc == 0, kc == 7, r=[hT_, rw], w=[lg_bank])
        k.act(lg[:, tt, :], lg_bank[:, 0:E], AF.Exp, r=[lg_bank], w=[lg], partial=True, scale=-1.0)
        c["lg_fused"][layer] = True


def route_tile(k, c, layer, tt, src, hT_, tr_banks, lg_bank):
    for step in range(3):
        route_step(k, c, layer, tt, src, hT_, tr_banks, lg_bank, step)


def load_ln(k, g_src, b_src, layer):
    t = k.sb(f"lnp_{k.P.dma_count['sp']}", [128, 2, D], F32)
    k.dma("sp", t[:, 0, :], g_src[layer].partition_broadcast(128), w=[t])
    k.dma("sp", t[:, 1, :], b_src[layer].partition_broadcast(128), w=[t])
    return TTview(t, t.t[:, 0, :]), TTview(t, t.t[:, 1, :])


class TTview:
    def __init__(self, parent, ap):
        self.t = ap
        self.b = parent.b

    def __getitem__(self, k):
        return self.t[k]


def finish(k, nc, st):
    k.P.emit(st, final_wait_ops=k.out_ops)
    st.close()
    return nc


def _bufs(xs):
    out = []
    for x in xs:
        if x is None:
            continue
        out.append(getattr(x, "b", x))
    return out


def ssd_layer(k, c):
    nc = k.nc
    PS = c["PS"]
    x_d = c["x_d"]
    ident_b, rmat_f, rmat_b, lmat_f, lmat_b, ones_f, ones_b = (c[n] for n in
        ("ident_b", "rmat_f", "rmat_b", "lmat_f", "lmat_b", "ones_f", "ones_b"))
    xs_d, bt_d, bT_d, cT_d, z_d, dt_d, a_d = (c[n] for n in ("xs_d", "bt_d", "bT_d", "cT_d", "z_d", "dt_d", "a_d"))
    B_xs, B_bt, B_bT, B_cT, B_z, B_dt, B_a = (c[n] for n in ("B_xs", "B_bt", "B_bT", "B_cT", "B_z", "B_dt", "B_a"))
    st_outer = k.st
    st = contextlib.ExitStack()
    k.st = st

    xT = k.sb("xT", [128, 8, S], BF16)
    xT_b = [Buf(f"xT{t}") for t in range(NT)]
    st1a = contextlib.ExitStack()
    k.st = st1a
    xbf = [k.sb(f"xbf{i}", [128, D], BF16) for i in range(3)]
    for tt in range(NT):
        xb = xbf[tt % 3]
        k.dma("pool", xb[:, :], x_d[tt * 128:(tt + 1) * 128, :], w=[xb], partial=False)
        pb = PS[tt % 2]
        pv = pb[:, :].bitcast(BF16)
        for kc in range(8):
            k.tr(pv[:, kc * 128:(kc + 1) * 128], xb[:, kc * 128:(kc + 1) * 128], ident_b[:, :], r=[xb, ident_b], w=[pb])
        dst = xT.t[:, :, tt * 128:(tt + 1) * 128]
        src = pv.rearrange("p (k t) -> p k t", k=8)
        if tt % 2:
            k.act(dst, src, AF.Copy, r=[pb], w=[xT_b[tt]])
        else:
            k.v("dve", "tensor_copy", r=[pb], w=[xT_b[tt]], out=dst, in_=src)

    w_in_v = c["ssd_w_in"].rearrange("(kc p) c -> p kc c", p=128)
    wz = k.sb("wz", [128, 8, 2048], BF16)
    wz_b = [Buf(f"wz{i}") for i in range(4)]
    for cb in range(4):
        k.dma("pool", wz[:, :, cb * 512:(cb + 1) * 512], w_in_v[:, :, cb * 512:(cb + 1) * 512], w=[wz_b[cb]])
    wdt = k.sb("wdt", [128, 8, 32], BF16)
    k.dma("pool", wdt[:, :, :], w_in_v[:, :, 6144:6176], w=[wdt])
    dtraw = k.sb("dtraw", [128, NT, 32], F32)
    zs = [k.sb(f"zs{i}", [128, 2048], BF16) for i in range(2)]
    n_ps = 0
    for tt in range(NT):
        zt = zs[tt % 2]
        for cb in range(4):
            pb = PS[6 + n_ps % 2]
            n_ps += 1
            for kc in range(8):
                k.mm(pb[:, :], xT.t[:, kc, tt * 128:(tt + 1) * 128], wz[:, kc, cb * 512:(cb + 1) * 512], kc == 0, kc == 7,
                     r=[xT_b[tt], wz_b[cb]], w=[pb])
            k.act(zt[:, cb * 512:(cb + 1) * 512], pb[:, :], AF.Silu, r=[pb], w=[zt], partial=True)
        k.dma("sp", z_d[tt * 128:(tt + 1) * 128, :], zt[:, :], r=[zt], w=[B_z])
        pb = PS[6 + n_ps % 2]
        n_ps += 1
        for kc in range(8):
            k.mm(pb[:, 0:32], xT.t[:, kc, tt * 128:(tt + 1) * 128], wdt[:, kc, :], kc == 0, kc == 7, r=[xT_b[tt], wdt], w=[pb])
        k.v("dve", "tensor_copy", r=[pb], w=[dtraw], partial=True, out=dtraw[:, tt, :], in_=pb[:, 0:32])

    sm32 = k.sb("sm32", [128, 3, 32], F32)
    k.dma("sp", sm32[:, 0, :], c["ssd_dt_bias"].partition_broadcast(128), w=[sm32])
    k.dma("sp", sm32[:, 1, :], c["ssd_a_log"].partition_broadcast(128), w=[sm32])
    e1 = k.sb("e1", [128, NT, 32], F32)
    dtv = k.sb("dtv", [128, NT, 32], F32)
    av = k.sb("av", [128, NT, 32], F32)
    k.v("dve", "tensor_tensor", r=[dtraw, sm32], w=[dtraw], out=dtraw[:, :, :], in0=dtraw[:, :, :],
        in1=bc(sm32[:, 0, :], [[0, NT], [1, 32]]), op=ALU.add)
    k.act(e1[:, :, :], dtraw[:, :, :], AF.Exp, r=[dtraw], w=[e1])
    k.act(dtv[:, :, :], e1[:, :, :], AF.Ln, r=[e1], w=[dtv], bias=1.0)
    k.act(sm32[:, 2, :], sm32[:, 1, :], AF.Exp, r=[sm32], w=[sm32])
    k.v("dve", "scalar_tensor_tensor", r=[dtv, sm32], w=[av], out=av[:, :, :], in0=dtv[:, :, :], scalar=-1.0,
        in1=bc(sm32[:, 2, :], [[0, NT], [1, 32]]), op0=ALU.mult, op1=ALU.mult)
    k.dma("sp", dt_d.rearrange("(t p) h -> p t h", p=128), dtv[:, :, :], r=[dtv], w=[B_dt])
    k.dma("sp", a_d.rearrange("(t p) h -> p t h", p=128), av[:, :, :], r=[av], w=[B_a])

    st1a.close()
    k.P.fence()
    k.st = st
    convw = k.sb("convw", [128, 128], F32)
    convb = k.sb("convb", [128, 32], F32)
    k.dma("sp", convw[:, :], c["ssd_conv_w"], w=[convw])
    k.dma("sp", convb[:, :], c["ssd_conv_b"], w=[convb])
    diagw = k.sb("diagw", [128, 128, 128], BF16)
    k.v("dve", "tensor_tensor", r=[ident_b, convw], w=[diagw], out=diagw[:, :, :], in0=bc(ident_b[:, :], [[0, 128], [1, 128]]),
        in1=bc(convw[:, :], [[1, 128], [0, 128]]), op=ALU.mult)
    wch = [k.sb(f"wch{i}", [128, 8, 128], BF16) for i in range(3)]
    xpre = [k.sb(f"xpre{i}", [128, 3 + S], BF16) for i in range(2)]
    xact = [k.sb(f"xact{i}", [128, S], BF16) for i in range(2)]
    stg = [k.sb(f"stg{i}", [128, 8, 128], BF16) for i in range(3)]
    for xp in xpre:
        k.v("dve", "memset", w=[xp], ap=xp[:, 0:3], constant=0.0)
    xs_v = xs_d.rearrange("(t p) c -> p t c", p=128)
    bt_v = bt_d.rearrange("(t p) c -> p t c", p=128)
    n_ps = 0
    n_cv = 0
    n_tr = 0

    def conv_block(cc, tb, xp, xa):
        nonlocal n_cv
        cv = PS[6 + n_cv % 2]
        n_cv += 1
        for tap in range(4):
            k.mm(cv[:, :], diagw[:, cc * 4 + tap, :], xp[:, tb * 512 + tap:tb * 512 + tap + 512], tap == 0, tap == 3, r=[diagw, xp], w=[cv])
        k.act(xa[:, tb * 512:(tb + 1) * 512], cv[:, :], AF.Silu, r=[cv, convb], w=[xa], partial=True, bias=convb[:, cc:cc + 1])

    for cc in range(32):
        wc = wch[cc % 3]
        k.dma("pool", wc[:, :, :], w_in_v[:, :, 2048 + cc * 128:2048 + (cc + 1) * 128], w=[wc], partial=False)
        xp = xpre[cc % 2]
        xa = xact[cc % 2]
        for tb in range(8):
            pb = PS[2 + n_ps % 2]
            n_ps += 1
            for kc in range(8):
                k.mm(pb[:, :], wc[:, kc, :], xT.t[:, kc, tb * 512:(tb + 1) * 512], kc == 0, kc == 7,
                     r=[wc] + xT_b[4 * tb:4 * tb + 4], w=[pb])
            if tb > 0:
                conv_block(cc, tb - 1, xp, xa)
            k.act(xp[:, 3 + tb * 512:3 + (tb + 1) * 512], pb[:, :], AF.Copy, r=[pb], w=[xp], partial=True)
        conv_block(cc, 7, xp, xa)
        if cc < 24:
            dv, Bd, col = (xs_v, B_xs, cc * 128) if cc < 16 else (bt_v, B_bt, (cc - 16) * 128)
            for g8 in range(4):
                pb = PS[4 + n_tr % 2]
                sg = stg[n_tr % 3]
                n_tr += 1
                pv = pb[:, :].bitcast(BF16)
                for j in range(8):
                    t = g8 * 8 + j
                    k.tr(pv[:, j * 128:(j + 1) * 128], xa[:, t * 128:(t + 1) * 128], ident_b[:, :], r=[xa, ident_b], w=[pb])
                k.v("dve", "tensor_copy", r=[pb], w=[sg], out=sg[:, :, :], in_=pv.rearrange("p (j c) -> p j c", j=8))
                k.dma("sp", dv[:, g8 * 8:(g8 + 1) * 8, col:col + 128], sg[:, :, :], r=[sg], w=[Bd])
        if 16 <= cc < 24:
            k.dma("sp", bT_d[(cc - 16) * 128:(cc - 15) * 128, :], xa[:, :], r=[xa], w=[B_bT])
        if cc >= 24:
            k.dma("sp", cT_d[(cc - 24) * 128:(cc - 23) * 128, :], xa[:, :], r=[xa], w=[B_cT])
    st.close()
    k.P.fence()

    st = contextlib.ExitStack()
    k.st = st
    h1_d = c["h1_d"]
    B_h1 = c["B_h"][0]
    eps_ln, eps_rms = c["eps_ln"], c["eps_rms"]
    wout = k.sb("wout", [128, 16, D], BF16)
    k.dma("pool", wout[:, :, :], c["ssd_w_out"].rearrange("(kc p) d -> p kc d", p=128), w=[wout], partial=False)
    normw = k.sb("normw", [128, 2048], F32)
    k.dma("sp", normw[:, :], c["ssd_norm_w"].partition_broadcast(128), w=[normw])
    dsk = k.sb("dsk", [128, 32], F32)
    k.dma("sp", dsk[:, :], c["ssd_d"].partition_broadcast(128), w=[dsk])
    diagD = k.sb("diagD", [128, 32, 128], BF16)
    identb3 = bc(ident_b[:, :], [[0, 32], [1, 128]])
    k.v("dve", "tensor_tensor", r=[ident_b, dsk], w=[diagD], out=diagD[:, :, :], in0=identb3,
        in1=bc(dsk[:, :], [[1, 32], [0, 128]]), op=ALU.mult)
    state = k.sb("state", [128, 2048], F32)
    state_bf = k.sb("state_bf", [128, 2048], BF16)
    st_b = [Buf(f"st{g}") for g in range(8)]
    stbf_b = [Buf(f"stbf{g}") for g in range(8)]
    k.v("dve", "memset", w=st_b, ap=state[:, :], constant=0.0)
    k.v("dve", "memset", w=stbf_b, ap=state_bf[:, :], constant=0.0)

    NB = 3
    xs_t = [k.sb(f"xs_t{i}", [128, 2048], BF16) for i in range(NB)]
    bt_t = [k.sb(f"bt_t{i}", [128, 1024], BF16) for i in range(NB)]
    bT_t = [k.sb(f"bT_t{i}", [128, 8, 128], BF16) for i in range(NB)]
    cT_t = [k.sb(f"cT_t{i}", [128, 8, 128], BF16) for i in range(NB)]
    a_t = [k.sb(f"a_t{i}", [128, 32], F32) for i in range(NB)]
    dt_t = [k.sb(f"dt_t{i}", [128, 32], F32) for i in range(NB)]
    z_t = [k.sb(f"z_t{i}", [128, 2048], BF16) for i in range(NB)]
    x_t = [k.sb(f"x_t{i}", [128, D], F32) for i in range(NB)]
    exps2 = [k.sb(f"exps{i}", [128, 96], F32) for i in range(2)]
    a_bf2 = [k.sb(f"a_bf{i}", [128, 32], BF16) for i in range(2)]
    dtte2 = [k.sb(f"dtte{i}", [128, 32], F32) for i in range(2)]
    aR2 = [k.sb(f"aR{i}", [128, 32, 128], BF16) for i in range(2)]
    dec = [k.sb(f"dec{i}", [128, 8, 128], BF16) for i in range(2)]
    smk = [k.sb(f"smk{i}", [128, 2, 128], BF16) for i in range(2)]
    MT = [k.sb(f"MT{i}", [128, 8, 128], BF16) for i in range(2)]
    xdt2 = [k.sb(f"xdt{i}", [128, 2048], BF16) for i in range(2)]
    xw2 = [k.sb(f"xw{i}", [128, 2048], BF16) for i in range(2)]
    tmpq = [k.sb(f"tmpq{i}", [128, 512], F32) for i in range(1)] * 2
    stq = [k.sb(f"stq{i}", [128, 512], F32) for i in range(1)] * 2
    ycomb2 = [k.sb(f"ycomb{i}", [128, 2048], F32) for i in range(2)]
    ssq = k.sb("ssq", [128, 8], F32)
    junk = k.sb("junk", [128, 256], F32)
    lnv8 = k.sb("lnv8", [128, 8], F32)
    rstd8 = k.sb("rstd8", [128, 8], F32)
    yn = k.sb("yn", [128, 2048], BF16)
    ynT = k.sb("ynT", [128, 16, 128], BF16)
    res = k.sb("res", [128, D], F32)
    hout = [k.sb(f"hout{i}", [128, D], F32) for i in range(1)] * 2
    lntmp = (k.sb("ln_stats", [128, 12], F32), k.sb("ln_mv", [128, 2], F32), k.sb("ln_lnv", [128, 1], F32),
             k.sb("ln_rstd", [128, 1], F32))
    bT_v = bT_d.rearrange("(g n) t -> n g t", n=128)
    cT_v = cT_d.rearrange("(g n) t -> n g t", n=128)
    g_bc, b_bc = load_ln(k, c["ln_mix_g"], c["ln_mix_b"], 0)
    MISC, SEG0, SEG1, YB, YOB, STB, OPB, TRB = PS

    hT_r = None

    def loads(tt):
        i = tt % NB
        sl = slice(tt * 128, (tt + 1) * 128)
        k.dma("sp", a_t[i][:, :], a_d[sl, :], r=[B_a], w=[a_t[i]], partial=False)
        k.dma("sp", dt_t[i][:, :], dt_d[sl, :], r=[B_dt], w=[dt_t[i]], partial=False)
        k.dma("sp", xs_t[i][:, :], xs_d[sl, :], r=[B_xs], w=[xs_t[i]], partial=False)
        k.dma("sp", bT_t[i][:, :, :], bT_v[:, :, sl], r=[B_bT], w=[bT_t[i]], partial=False)
        k.dma("sp", cT_t[i][:, :, :], cT_v[:, :, sl], r=[B_cT], w=[cT_t[i]], partial=False)
        k.dma("sp", bt_t[i][:, :], bt_d[sl, :], r=[B_bt], w=[bt_t[i]], partial=False)
        k.dma("sp", z_t[i][:, :], z_d[sl, :], r=[B_z], w=[z_t[i]], partial=False)
        k.dma("sp", x_t[i][:, :], x_d[sl, :], w=[x_t[i]], partial=False)

    def prologue(tt):
        i = tt % NB
        j = tt % 2
        xs_, a_, dt_ = xs_t[i], a_t[i], dt_t[i]
        exps, a_bf, dtte, aR, xdt, xw = exps2[j], a_bf2[j], dtte2[j], aR2[j], xdt2[j], xw2[j]
        k.mm(MISC[:, 0:32], rmat_f[:, :], a_[:, :], True, True, r=[rmat_f, a_], w=[MISC])
        k.mm(MISC[:, 32:64], lmat_f[:, :], a_[:, :], True, True, r=[lmat_f, a_], w=[MISC])
        k.mm(MISC[:, 64:96], ones_f[:, :], a_[:, :], True, True, r=[ones_f, a_], w=[MISC])
        k.act(exps[:, :], MISC[:, 0:96], AF.Exp, r=[MISC], w=[exps])
        k.v("dve", "tensor_tensor", r=[dt_, exps], w=[dtte], out=dtte[:, :], in0=dt_[:, :], in1=exps[:, 32:64], op=ALU.mult)
        k.v("dve", "tensor_copy", r=[a_], w=[a_bf], out=a_bf[:, :], in_=a_[:, :])
        k.v("dve", "tensor_tensor", r=[a_bf, rmat_b], w=[aR], out=aR[:, :, :], in0=bc(a_bf[:, :], [[1, 32], [0, 128]]),
            in1=bc(rmat_b[:, :], [[0, 32], [1, 128]]), op=ALU.mult)
        xs3 = xs_[:, :].rearrange("p (h d) -> p h d", h=32)
        k.v("pool", "tensor_tensor", r=[xs_, dt_], w=[xdt], out=xdt[:, :].rearrange("p (h d) -> p h d", h=32), in0=xs3,
            in1=bc(dt_[:, :], [[1, 32], [0, 64]]), op=ALU.mult)
        k.v("pool", "tensor_tensor", r=[xs_, dtte], w=[xw], out=xw[:, :].rearrange("p (h d) -> p h d", h=32), in0=xs3,
            in1=bc(dtte[:, :], [[1, 32], [0, 64]]), op=ALU.mult)

    def quarter(tt, q):
        i = tt % NB
        j = tt % 2
        xs_, bt_, bT_, cT_ = xs_t[i], bt_t[i], bT_t[i], cT_t[i]
        exps, aR, xdt, xw, ycomb = exps2[j], aR2[j], xdt2[j], xw2[j], ycomb2[j]
        SEG = (SEG0, SEG1)
        for half in range(2):
            k.mm(SEG[half][:, :], lmat_b[:, :], aR[:, q * 8 + half * 4:q * 8 + half * 4 + 4, :], True, True,
                 r=[lmat_b, aR], w=[SEG[half]])
        for gi in range(2):
            g = q * 2 + gi
            k.mm(MISC[:, 128 + gi * 128:256 + gi * 128], bT_[:, g, :], cT_[:, g, :], True, True, r=[bT_, cT_], w=[MISC])
        dq = dec[q % 2]
        for half in range(2):
            k.act(dq[:, half * 4:half * 4 + 4, :], SEG[half][:, :].rearrange("p (h l) -> p h l", h=4), AF.Exp,
                  r=[SEG[half]], w=[dq], partial=True)
        sq_ = smk[q % 2]
        k.v("dve", "tensor_tensor", r=[MISC, rmat_f], w=[sq_], out=sq_[:, :, :],
            in0=MISC[:, 128:384].rearrange("p (g l) -> p g l", g=2), in1=bc(rmat_f[:, :], [[0, 2], [1, 128]]), op=ALU.mult)
        mq = MT[q % 2]
        k.v("dve", "tensor_tensor", r=[dq, sq_], w=[mq], out=mq[:, :, :].rearrange("p (g r) l -> p g r l", g=2),
            in0=dq[:, :, :].rearrange("p (g r) l -> p g r l", g=2),
            in1=bc(sq_[:, :, :], [[128, 2], [0, 4], [1, 128]]), op=ALU.mult)
        for gi in range(2):
            g = q * 2 + gi
            k.mm(YOB[:, gi * 256:(gi + 1) * 256], cT_[:, g, :], state_bf[:, g * 256:(g + 1) * 256], True, True,
                 r=[cT_, stbf_b[g]], w=[YOB])
        for gi in range(2):
            g = q * 2 + gi
            k.mm(STB[:, gi * 256:(gi + 1) * 256], bt_[:, g * 128:(g + 1) * 128], xw[:, g * 256:(g + 1) * 256], True, True,
                 r=[bt_, xw], w=[STB])
        for hq in range(8):
            h = q * 8 + hq
            k.mm(YB[:, hq * 64:(hq + 1) * 64], diagD[:, h, :], xs_[:, h * 64:(h + 1) * 64], True, False,
                 r=[diagD, xs_], w=[YB])
            k.mm(YB[:, hq * 64:(hq + 1) * 64], mq[:, hq, :], xdt[:, h * 64:(h + 1) * 64], False, True,
                 r=[mq, xdt], w=[YB])
        sq2 = stq[q % 2]
        sl = slice(q * 512, (q + 1) * 512)
        gb = [st_b[q * 2], st_b[q * 2 + 1]]
        k.v("dve", "tensor_tensor", r=gb + [exps], w=[sq2], out=sq2[:, :].rearrange("p (h d) -> p h d", h=8),
            in0=state[:, sl].rearrange("p (h d) -> p h d", h=8), in1=bc(exps[:, 64 + q * 8:64 + q * 8 + 8], [[1, 8], [0, 64]]),
            op=ALU.mult)
        k.v("dve", "tensor_tensor", r=[STB, sq2], w=gb, out=state[:, sl], in0=STB[:, :], in1=sq2[:, :], op=ALU.add)
        k.act(state_bf[:, sl], state[:, sl], AF.Copy, r=gb, w=[stbf_b[q * 2], stbf_b[q * 2 + 1]])
        tq = tmpq[q % 2]
        k.v("dve", "tensor_tensor", r=[YOB, exps], w=[tq], out=tq[:, :].rearrange("p (h d) -> p h d", h=8),
            in0=YOB[:, :].rearrange("p (h d) -> p h d", h=8), in1=bc(exps[:, q * 8:q * 8 + 8], [[1, 8], [0, 64]]), op=ALU.mult)
        k.v("dve", "tensor_tensor", r=[YB, tq], w=[ycomb], partial=True, out=ycomb[:, q * 512:(q + 1) * 512], in0=YB[:, :],
            in1=tq[:, :], op=ALU.add)

    def epi(tt, part):
        i = tt % NB
        ycomb = ycomb2[tt % 2]
        z_, x_ = z_t[i], x_t[i]
        if part == 0:
            k.v("dve", "tensor_tensor", r=[ycomb, z_], w=[ycomb], out=ycomb[:, :], in0=ycomb[:, :], in1=z_[:, :], op=ALU.mult)
            for g in range(8):
                k.act(junk[:, :], ycomb[:, g * 256:(g + 1) * 256], AF.Square, r=[ycomb], w=[junk, ssq], partial=True,
                      accum_out=ssq[:, g:g + 1])
            k.act(lnv8[:, :], ssq[:, :], AF.Ln, r=[ssq, eps_rms], w=[lnv8], scale=1.0 / 256.0, bias=eps_rms[:, 0:1])
            k.act(rstd8[:, :], lnv8[:, :], AF.Exp, r=[lnv8], w=[rstd8], scale=-0.5)
        elif part == 1:
            k.v("pool", "tensor_tensor", r=[ycomb, rstd8], w=[ycomb], out=ycomb[:, :].rearrange("p (g d) -> p g d", g=8),
                in0=ycomb[:, :].rearrange("p (g d) -> p g d", g=8), in1=bc(rstd8[:, :], [[1, 8], [0, 256]]), op=ALU.mult)
            k.v("pool", "tensor_tensor", r=[ycomb, normw], w=[yn], out=yn[:, :], in0=ycomb[:, :], in1=normw[:, :], op=ALU.mult)
            for g8 in range(2):
                pv = TRB[:, :].bitcast(BF16)
                for j in range(8):
                    kc = g8 * 8 + j
                    k.tr(pv[:, j * 128:(j + 1) * 128], yn[:, kc * 128:(kc + 1) * 128], ident_b[:, :], r=[yn, ident_b], w=[TRB])
                k.act(ynT[:, g8 * 8:(g8 + 1) * 8, :], pv.rearrange("p (j c) -> p j c", j=8), AF.Copy, r=[TRB], w=[ynT], partial=True)
        elif part == 2:
            for dh in range(2):
                for kc in range(16):
                    k.mm(OPB[:, :], ynT[:, kc, :], wout[:, kc, dh * 512:(dh + 1) * 512], kc == 0, kc == 15, r=[ynT, wout], w=[OPB])
                k.v("dve", "scalar_tensor_tensor", r=[x_, OPB], w=[res], partial=True, out=res[:, dh * 512:(dh + 1) * 512],
                    in0=x_[:, dh * 512:(dh + 1) * 512], scalar=ALPHA, in1=OPB[:, :], op0=ALU.mult, op1=ALU.add)
        else:
            ho = hout[tt % 2]
            layer_norm_tile(k, res, g_bc, b_bc, ho, lntmp, eps_ln, "s", eng2="pool")
            op = k.dma("sp", h1_d[tt * 128:(tt + 1) * 128, :], ho[:, :], r=[ho], w=[B_h1[tt]])
            k.out_ops.append(op)

    loads(0)
    loads(1)
    prologue(0)
    def rstep(tile, step):
        route_step(k, c, 0, tile, hout[0], hT_r, (TRB, TRB), OPB, step)

    FUSE_L0 = False

    for tt in range(NT):
        for q in range(4):
            quarter(tt, q)
            if FUSE_L0 and tt >= 2 and q == 3:
                rstep(tt - 2, 2)
            if tt > 0:
                epi(tt - 1, q)
            if FUSE_L0 and tt >= 2 and q == 0:
                rstep(tt - 2, 0)
            if FUSE_L0 and tt >= 2 and q == 2:
                rstep(tt - 2, 1)
            if q == 1 and tt + 1 < NT:
                prologue(tt + 1)
        if tt + 2 < NT:
            loads(tt + 2)
    epi(NT - 1, 0)
    if FUSE_L0:
        rstep(NT - 2, 0)
    epi(NT - 1, 1)
    epi(NT - 1, 2)
    if FUSE_L0:
        rstep(NT - 2, 1)
        rstep(NT - 2, 2)
    epi(NT - 1, 3)
    if FUSE_L0:
        for step in range(3):
            rstep(NT - 1, step)
    st.close()
    k.P.fence()
    k.st = st_outer


def moe_layer_dense(k, c, layer, hin_d, B_hin, hout_d, B_hout, is_final):
    PS = c["PS"]
    ident_f = c["ident_f"]
    eps_ln = c["eps_ln"]
    st_outer = k.st
    st = contextlib.ExitStack()
    k.st = st
    L = f"m{layer}"
    TH = 16
    xT = k.sb(L + "xT", [128, 8, TH * 128], BF16)
    xT_b = [Buf(f"{L}xT{t}") for t in range(TH)]
    acc = k.sb(L + "acc", [128, TH, D], F32)
    acc_b = [Buf(f"{L}acc{t}") for t in range(TH)]
    rw = k.sb(L + "rw", [128, 8, E], F32)
    k.dma("sp", rw[:, :, :], c["router_w"].rearrange("(kc p) e -> p kc e", p=128), w=[rw])
    rb = k.sb(L + "rb", [128, E], F32)
    k.dma("sp", rb[:, :], c["router_bias"].partition_broadcast(128), w=[rb])
    g_bc, b_bc = load_ln(k, c["ln_ffn_g"], c["ln_ffn_b"], layer)
    hin = [k.sb(f"{L}hin{i}", [128, D], F32) for i in range(2)]
    hTf = k.sb(L + "hTf", [128, 8, 128], F32)
    lg = k.sb(L + "lg", [128, TH, E], F32)
    aff = k.sb(L + "aff", [128, TH, E], F32)
    sel = k.sb(L + "sel", [128, TH, E], F32)
    p6 = k.sb(L + "p6", [128, TH * 4, 6], F32)
    gs = k.sb(L + "gs", [128, TH * 4], F32)
    gmax = k.sb(L + "gmax", [128, TH], F32)
    gm = k.sb(L + "gm", [128, TH * 4], F32)
    pen = k.sb(L + "pen", [128, TH * 4], F32)
    selm = k.sb(L + "selm", [128, TH, E], F32)
    m1 = k.sb(L + "m1", [128, TH], F32)
    mk1 = k.sb(L + "mk1", [128, TH, E], F32)
    selm2 = k.sb(L + "selm2", [128, TH, E], F32)
    mk2 = k.sb(L + "mk2", [128, TH, E], F32)
    wsum = k.sb(L + "wsum", [128, TH], F32)
    gates = k.sb(L + "gates", [128, TH, E], F32)
    wg = [k.sb(f"{L}wg{i}", [128, 8, DFF], BF16) for i in range(2)]
    wu = [k.sb(f"{L}wu{i}", [128, 8, DFF], BF16) for i in range(2)]
    wd = [k.sb(f"{L}wd{i}", [128, 4, D], BF16) for i in range(2)]
    hT = [k.sb(f"{L}hT{i}", [128, 4, 512], BF16) for i in range(2)]
    sg = [k.sb(f"{L}sg{i}", [128, 512], BF16) for i in range(2)]
    res = [k.sb(f"{L}res{i}", [128, D], F32) for i in range(2)]
    lntmp = (k.sb(L + "ln_stats", [128, 12], F32), k.sb(L + "ln_mv", [128, 2], F32), k.sb(L + "ln_lnv", [128, 1], F32),
             k.sb(L + "ln_rstd", [128, 1], F32))
    wgv = c["moe_w_gate"][layer].rearrange("e (kc p) f -> e p kc f", p=128)
    wuv = c["moe_w_up"][layer].rearrange("e (kc p) f -> e p kc f", p=128)
    wdv = c["moe_w_down"][layer].rearrange("e (fc p) d -> e p fc d", p=128)
    n_w = 0
    n_g = 0
    n_d = 0
    n_h = 0
    for hf in range(2):
        for t in range(TH):
            tt = hf * TH + t
            hi = hin[t % 2]
            k.dma("sp", hi[:, :], hin_d[tt * 128:(tt + 1) * 128, :], r=[B_hin[tt]], w=[hi], partial=False)
            for half in range(2):
                pb = PS[half]
                for j in range(4):
                    kc = half * 4 + j
                    k.tr(pb[:, j * 128:(j + 1) * 128], hi[:, kc * 128:(kc + 1) * 128], ident_f[:, :], r=[hi, ident_f], w=[pb])
                k.act(hTf[:, half * 4:half * 4 + 4, :], pb[:, :].rearrange("p (j c) -> p j c", j=4), AF.Copy, r=[pb], w=[hTf],
                      partial=True)
            k.v("dve", "tensor_copy", r=[hTf], w=[xT_b[t]], out=xT.t[:, :, t * 128:(t + 1) * 128], in_=hTf[:, :, :])
            pl = PS[2]
            for kc in range(8):
                k.mm(pl[:, 0:E], hTf[:, kc, :], rw[:, kc, :], kc == 0, kc == 7, r=[hTf, rw], w=[pl])
            k.act(lg[:, t, :], pl[:, 0:E], AF.Exp, r=[pl], w=[lg], partial=True, scale=-1.0)
        V = lambda name, **kw: k.v("dve", name, **kw)
        V("tensor_scalar", r=[lg], w=[lg], out=lg[:, :, :], in0=lg[:, :, :], scalar1=1.0, scalar2=None, op0=ALU.add)
        V("reciprocal", r=[lg], w=[aff], out=aff[:, :, :], in_=lg[:, :, :])
        V("tensor_tensor", r=[aff, rb], w=[sel], out=sel[:, :, :], in0=aff[:, :, :], in1=bc(rb[:, :], [[0, TH], [1, E]]), op=ALU.add)
        s4 = sel[:, :, :].rearrange("p t (g i) -> p (t g) i", g=4)
        V("tensor_tensor", r=[sel], w=[p6], partial=True, out=p6[:, :, 0:3], in0=s4[:, :, 0:3], in1=s4[:, :, 1:4], op=ALU.add)
        V("tensor_tensor", r=[sel], w=[p6], partial=True, out=p6[:, :, 3:5], in0=s4[:, :, 0:2], in1=s4[:, :, 2:4], op=ALU.add)
        V("tensor_tensor", r=[sel], w=[p6], partial=True, out=p6[:, :, 5:6], in0=s4[:, :, 0:1], in1=s4[:, :, 3:4], op=ALU.add)
        V("tensor_reduce", r=[p6], w=[gs], out=gs[:, :], in_=p6[:, :, :], axis=AX.X, op=ALU.max)
        V("tensor_reduce", r=[gs], w=[gmax], out=gmax[:, :], in_=gs[:, :].rearrange("p (t g) -> p t g", g=4), axis=AX.X, op=ALU.max)
        V("tensor_tensor", r=[gs, gmax], w=[gm], out=gm[:, :].rearrange("p (t g) -> p t g", g=4),
          in0=gs[:, :].rearrange("p (t g) -> p t g", g=4), in1=bc(gmax[:, :], [[1, TH], [0, 4]]), op=ALU.is_ge)
        V("tensor_scalar", r=[gm], w=[pen], out=pen[:, :], in0=gm[:, :], scalar1=-1.0, scalar2=1.0e4, op0=ALU.add, op1=ALU.mult)
        sm4 = selm[:, :, :].rearrange("p t (g i) -> p (t g) i", g=4)
        V("tensor_tensor", r=[sel, gm], w=[selm], out=sm4, in0=s4, in1=bc(gm[:, :], [[1, TH * 4], [0, 4]]), op=ALU.mult)
        V("tensor_tensor", r=[selm, pen], w=[selm], out=sm4, in0=sm4, in1=bc(pen[:, :], [[1, TH * 4], [0, 4]]), op=ALU.add)
        V("tensor_reduce", r=[selm], w=[m1], out=m1[:, :], in_=selm[:, :, :], axis=AX.X, op=ALU.max)
        V("tensor_tensor", r=[selm, m1], w=[mk1], out=mk1[:, :, :], in0=selm[:, :, :], in1=bc(m1[:, :], [[1, TH], [0, E]]), op=ALU.is_ge)
        V("scalar_tensor_tensor", r=[mk1, selm], w=[selm2], out=selm2[:, :, :], in0=mk1[:, :, :], scalar=-1.0e4, in1=selm[:, :, :],
          op0=ALU.mult, op1=ALU.add)
        V("tensor_reduce", r=[selm2], w=[m1], out=m1[:, :], in_=selm2[:, :, :], axis=AX.X, op=ALU.max)
        V("tensor_tensor", r=[selm2, m1], w=[mk2], out=mk2[:, :, :], in0=selm2[:, :, :], in1=bc(m1[:, :], [[1, TH], [0, E]]), op=ALU.is_ge)
        V("tensor_tensor", r=[mk1, mk2], w=[mk1], out=mk1[:, :, :], in0=mk1[:, :, :], in1=mk2[:, :, :], op=ALU.add)
        V("tensor_tensor", r=[mk1, aff], w=[mk1], out=mk1[:, :, :], in0=mk1[:, :, :], in1=aff[:, :, :], op=ALU.mult)
        V("tensor_reduce", r=[mk1], w=[wsum], out=wsum[:, :], in_=mk1[:, :, :], axis=AX.X, op=ALU.add)
        V("reciprocal", r=[wsum], w=[wsum], out=wsum[:, :], in_=wsum[:, :])
        V("tensor_tensor", r=[mk1, wsum], w=[gates], out=gates[:, :, :], in0=mk1[:, :, :], in1=bc(wsum[:, :], [[1, TH], [0, E]]), op=ALU.mult)
        for e in range(E):
            i = n_w % 2
            n_w += 1
            k.dma("pool", wg[i][:, :, :], wgv[e], w=[wg[i]], partial=False)
            k.dma("pool", wu[i][:, :, :], wuv[e], w=[wu[i]], partial=False)
            k.dma("pool", wd[i][:, :, :], wdv[e], w=[wd[i]], partial=False)
            for tb in range(4):
                hTt = hT[n_h % 2]
                n_h += 1
                xr = xT_b[tb * 4:tb * 4 + 4]
                for fc in range(4):
                    G = PS[n_g % 2]
                    U = PS[2 + n_g % 2]
                    s_ = sg[n_g % 2]
                    n_g += 1
                    for kc in range(8):
                        k.mm(G[:, :], wg[i][:, kc, fc * 128:(fc + 1) * 128], xT.t[:, kc, tb * 512:(tb + 1) * 512], kc == 0, kc == 7,
                             r=[wg[i]] + xr, w=[G])
                    for kc in range(8):
                        k.mm(U[:, :], wu[i][:, kc, fc * 128:(fc + 1) * 128], xT.t[:, kc, tb * 512:(tb + 1) * 512], kc == 0, kc == 7,
                             r=[wu[i]] + xr, w=[U])
                    k.act(s_[:, :], G[:, :], AF.Silu, r=[G], w=[s_])
                    k.v("dve", "tensor_tensor", r=[s_, U], w=[hTt], partial=True, out=hTt[:, fc, :], in0=s_[:, :], in1=U[:, :], op=ALU.mult)
                for tl in range(4):
                    t = tb * 4 + tl
                    for dh in range(2):
                        Dp = PS[4 + n_d % 4]
                        n_d += 1
                        for fc in range(4):
                            k.mm(Dp[:, :], hTt[:, fc, tl * 128:(tl + 1) * 128], wd[i][:, fc, dh * 512:(dh + 1) * 512], fc == 0, fc == 3,
                                 r=[hTt, wd[i]], w=[Dp])
                        gsc = gates[:, t, e:e + 1]
                        if e == 0:
                            k.v("dve", "tensor_scalar", r=[Dp, gates], w=[acc_b[t]], partial=True, out=acc[:, t, dh * 512:(dh + 1) * 512],
                                in0=Dp[:, :], scalar1=gsc, scalar2=None, op0=ALU.mult)
                        else:
                            k.v("dve", "scalar_tensor_tensor", r=[Dp, gates, acc_b[t]], w=[acc_b[t]], partial=True,
                                out=acc[:, t, dh * 512:(dh + 1) * 512], in0=Dp[:, :], scalar=gsc, in1=acc[:, t, dh * 512:(dh + 1) * 512],
                                op0=ALU.mult, op1=ALU.add)
        for t in range(TH):
            tt = hf * TH + t
            hi = hin[t % 2]
            k.dma("sp", hi[:, :], hin_d[tt * 128:(tt + 1) * 128, :], r=[B_hin[tt]], w=[hi], partial=False)
            r_ = res[t % 2]
            k.v("dve", "scalar_tensor_tensor", r=[hi, acc_b[t]], w=[r_], out=r_[:, :], in0=hi[:, :], scalar=ALPHA, in1=acc[:, t, :],
                op0=ALU.mult, op1=ALU.add)
            layer_norm_tile(k, r_, g_bc, b_bc, r_, lntmp, eps_ln, L)
            op = k.dma("sp", hout_d[tt * 128:(tt + 1) * 128, :], r_[:, :], r=[r_], w=[B_hout[tt]] if B_hout is not None else [])
            k.out_ops.append(op)
    st.close()
    k.P.fence()
    k.st = st_outer


def mla_layer(k, c):
    PS = c["PS"]
    ident_b, ones_f, eps_ln, eps_rms = c["ident_b"], c["ones_f"], c["eps_ln"], c["eps_rms"]
    DBK = c["DBK"]
    h2_d, h3_d, attnT_d = c["h2_d"], c["h3_d"], c["attnT_d"]
    B_h2, B_h3, B_attn = c["B_h"][1], c["B_h"][2], c["B_attn"]
    st_outer = k.st
    if c["lg_all"][1] is None:
        c["lg_all"][1] = k.sb("lg_all1", [128, NT, E], F32)
    stA = contextlib.ExitStack()
    k.st = stA
    cT = k.sb("cT", [128, 5, S], BF16)
    cT_b = [Buf(f"cT{t}") for t in range(NT)]
    krT = k.sb("krT", [128, S], BF16)
    krT_b = [Buf(f"krT{t}") for t in range(8)]
    st = contextlib.ExitStack()
    k.st = st
    wdn = k.sb("wdn", [128, 8, 640], BF16)
    k.dma("pool", wdn[:, :, :], c["mla_w_down"].rearrange("(kc p) n -> p kc n", p=128)[:, :, 0:640], w=[wdn], partial=False)
    wkr = k.sb("wkr", [128, 8, 192], BF16)
    k.dma("pool", wkr[:, :, :], c["mla_w_kr"].rearrange("(kc p) n -> p kc n", p=128), w=[wkr], partial=False)
    qkn = k.sb("qkn", [128, 640], F32)
    k.dma("sp", qkn[:, 0:384], c["mla_q_norm"].partition_broadcast(128), w=[qkn])
    k.dma("sp", qkn[:, 384:640], c["mla_kv_norm"].partition_broadcast(128), w=[qkn])
    hin = [k.sb(f"a1hin{i}", [128, D], F32) for i in range(2)]
    hbf = [k.sb(f"a1hbf{i}", [128, D], BF16) for i in range(2)]
    hT = [k.sb(f"a1hT{i}", [128, 8, 512], BF16) for i in range(2)]
    cq = k.sb("a1cq", [128, 640], F32)
    cqn = k.sb("a1cqn", [128, 640], BF16)
    junk = k.sb("a1junk", [128, 384], F32)
    ss2 = k.sb("a1ss2", [128, 2], F32)
    ln2 = k.sb("a1ln2", [128, 2], F32)
    rs2 = k.sb("a1rs2", [128, 2], F32)
    rope = [k.sb(f"a1rope{i}", [128, 2, 512], F32) for i in range(2)]
    rt1 = k.sb("a1rt1", [128, 512], F32)
    rt2 = k.sb("a1rt2", [128, 512], F32)
    rope_v = c["c_rope"]
    cq2 = [cq, k.sb("a1cq_b", [128, 640], F32)]
    hT_bufs_all = {}

    def a1_front(tt):
        tb, tl = tt // 4, tt % 4
        hTb = hT[tb % 2]
        if tl == 0:
            hT_bufs_all[tb] = [Buf(f"hTb{tb}_{i}") for i in range(4)]
        hT_bufs = hT_bufs_all[tb]
        hi = hin[tt % 2]
        hb = hbf[tt % 2]
        k.dma("sp", hi[:, :], h2_d[tt * 128:(tt + 1) * 128, :], r=[B_h2[tt]], w=[hi], partial=False)
        k.v("dve", "tensor_copy", r=[hi], w=[hb], out=hb[:, :], in_=hi[:, :])
        pb = PS[tt % 2]
        pv = pb[:, :].bitcast(BF16)
        for kc in range(8):
            k.tr(pv[:, kc * 128:(kc + 1) * 128], hb[:, kc * 128:(kc + 1) * 128], ident_b[:, :], r=[hb, ident_b], w=[pb])
        k.act(hTb[:, :, tl * 128:(tl + 1) * 128], pv.rearrange("p (k t) -> p k t", k=8), AF.Copy, r=[pb, hT_bufs[tl]], w=[hT_bufs[tl]])
        P0, P1 = PS[2 + tt % 2], PS[4 + tt % 2]
        for kc in range(8):
            k.mm(P0[:, :], hTb[:, kc, tl * 128:(tl + 1) * 128], wdn[:, kc, 0:512], kc == 0, kc == 7, r=[hT_bufs[tl], wdn], w=[P0])
        for kc in range(8):
            k.mm(P1[:, 0:128], hTb[:, kc, tl * 128:(tl + 1) * 128], wdn[:, kc, 512:640], kc == 0, kc == 7, r=[hT_bufs[tl], wdn], w=[P1])
        cq_ = cq2[tt % 2]
        k.act(cq_[:, 0:512], P0[:, :], AF.Copy, r=[P0], w=[cq_], partial=True)
        k.act(cq_[:, 512:640], P1[:, 0:128], AF.Copy, r=[P1], w=[cq_], partial=True)

    def a1_back(tt):
        cq_ = cq2[tt % 2]
        k.act(junk[:, 0:384], cq_[:, 0:384], AF.Square, r=[cq_], w=[junk, ss2], partial=True, accum_out=ss2[:, 0:1])
        k.act(junk[:, 0:256], cq_[:, 384:640], AF.Square, r=[cq_], w=[junk, ss2], partial=True, accum_out=ss2[:, 1:2])
        k.act(ln2[:, 0:1], ss2[:, 0:1], AF.Ln, r=[ss2, eps_rms], w=[ln2], partial=True, scale=1.0 / 384.0, bias=eps_rms[:, 0:1])
        k.act(ln2[:, 1:2], ss2[:, 1:2], AF.Ln, r=[ss2, eps_rms], w=[ln2], partial=True, scale=1.0 / 256.0, bias=eps_rms[:, 0:1])
        k.act(rs2[:, :], ln2[:, :], AF.Exp, r=[ln2], w=[rs2], scale=-0.5)
        k.v("dve", "scalar_tensor_tensor", r=[cq_, rs2, qkn], w=[cqn], partial=True, out=cqn[:, 0:384], in0=cq_[:, 0:384], scalar=rs2[:, 0:1],
            in1=qkn[:, 0:384], op0=ALU.mult, op1=ALU.mult)
        k.v("dve", "scalar_tensor_tensor", r=[cq_, rs2, qkn], w=[cqn], partial=True, out=cqn[:, 384:640], in0=cq_[:, 384:640], scalar=rs2[:, 1:2],
            in1=qkn[:, 384:640], op0=ALU.mult, op1=ALU.mult)
        pb2 = PS[tt % 2]
        pv2 = pb2[:, :].bitcast(BF16)
        for j in range(5):
            k.tr(pv2[:, j * 128:(j + 1) * 128], cqn[:, j * 128:(j + 1) * 128], ident_b[:, :], r=[cqn, ident_b], w=[pb2])
        k.act(cT.t[:, :, tt * 128:(tt + 1) * 128], pv2[:, 0:640].rearrange("p (j t) -> p j t", j=5), AF.Copy, r=[pb2], w=[cT_b[tt]])

    def a1_krope(tb):
        hTb = hT[tb % 2]
        hT_bufs = hT_bufs_all[tb]
        rp = rope[tb % 2]
        k.dma("sp", rp[:, 0, :], rope_v[0][:, tb * 512:(tb + 1) * 512], w=[rp])
        k.dma("sp", rp[:, 1, :], rope_v[1][:, tb * 512:(tb + 1) * 512], w=[rp])
        KA, KB = PS[6], PS[7]
        for kc in range(8):
            k.mm(KA[0:96, :], wkr[:, kc, 0:96], hTb[:, kc, :], kc == 0, kc == 7, r=hT_bufs + [wkr], w=[KA])
        for kc in range(8):
            k.mm(KB[0:96, :], wkr[:, kc, 96:192], hTb[:, kc, :], kc == 0, kc == 7, r=hT_bufs + [wkr], w=[KB])
        k.v("dve", "tensor_tensor", r=[KA, rp], w=[rt1], out=rt1[64:96, :], in0=KA[64:96, :], in1=rp[64:96, 0, :], op=ALU.mult)
        k.v("dve", "tensor_tensor", r=[KB, rp], w=[rt2], out=rt2[64:96, :], in0=KB[64:96, :], in1=rp[64:96, 1, :], op=ALU.mult)
        k.v("dve", "tensor_tensor", r=[rt1, rt2], w=[krT_b[tb]], out=krT[64:96, tb * 512:(tb + 1) * 512], in0=rt1[64:96, :], in1=rt2[64:96, :],
            op=ALU.add)

    a1_front(0)
    for tt in range(NT):
        if tt + 1 < NT:
            a1_front(tt + 1)
        a1_back(tt)
        if tt % 4 == 3:
            a1_krope(tt // 4)
    st.close()
    k.P.fence()
    st = contextlib.ExitStack()
    k.st = st
    HG = 4
    qT = k.sb("qT", [128, HG, S], BF16)
    kT = k.sb("kT", [128, HG, S], BF16)
    Vt = k.sb("Vt", [128, NT, HG, 65], BF16)
    qT_b = [[Buf(f"qT{h}_{b}") for b in range(8)] for h in range(HG)]
    kT_b = [[Buf(f"kT{h}_{b}") for b in range(8)] for h in range(HG)]
    V_b = [Buf(f"V{t}") for t in range(NT)]
    k.v("dve", "memset", w=V_b, ap=Vt[:, :, :, :], constant=1.0)
    wq = [k.sb(f"wq{i}", [128, 3, HG * 96], BF16) for i in range(2)]
    wqs = [k.sb(f"wqs{i}", [128, 3, HG * 96], BF16) for i in range(2)]
    wkn = [k.sb(f"wkn{i}", [128, 2, HG * 64], BF16) for i in range(2)]
    wv = [k.sb(f"wv{i}", [128, 2, HG * 64], BF16) for i in range(2)]
    amask_f = k.sb("amask_f", [128, 4, 512], F32)
    amask = k.sb("amask", [128, 4, 512], BF16)
    k.dma("sp", amask_f[:, :, :], c["c_amask"], w=[amask_f], partial=False)
    k.v("dve", "tensor_copy", r=[amask_f], w=[amask], out=amask[:, :, :], in_=amask_f[:, :, :])
    rope = [k.sb(f"a2rope{i}", [128, 2, 512], F32) for i in range(2)]
    rt1s = [k.sb(f"a2rt1{i}", [128, 512], F32) for i in range(2)]
    rt2s = [k.sb(f"a2rt2{i}", [128, 512], F32) for i in range(2)]
    pt = [k.sb(f"pt{i}", [128, 2, 512], BF16) for i in range(4)]
    rinv2 = [k.sb(f"rinv{i}", [128, 512], F32) for i in range(2)]
    bcs2 = [k.sb(f"bcs{i}", [128, 512], F32) for i in range(2)]
    rn_d = c["rn_d"]
    rn_b = [Buf(f"rn{i}") for i in range(128)]
    at = [k.sb(f"at{i}", [128, 512], BF16) for i in range(2)]
    DB = [k.ps_pair(i) for i in range(4)] if False else None
    wq_v = c["mla_w_uq"].rearrange("(kc p) n -> p kc n", p=128)
    wqs_v = c["mla_w_uq_sw"].rearrange("(kc p) n -> p kc n", p=128)
    wkn_v = c["mla_w_kn"].rearrange("(kc p) n -> p kc n", p=128)
    wv_v = c["mla_w_v"].rearrange("(kc p) n -> p kc n", p=128)
    SCALE = 1.0 / math.sqrt(96.0)
    n_st = 0
    n_ot = 0
    n_at = 0
    for hg in range(MLA_H // HG):
        i = hg % 2
        k.dma("pool", wq[i][:, :, :], wq_v[:, :, hg * HG * 96:(hg + 1) * HG * 96], w=[wq[i]], partial=False)
        k.dma("pool", wqs[i][:, :, :], wqs_v[:, :, hg * HG * 96:(hg + 1) * HG * 96], w=[wqs[i]], partial=False)
        k.dma("pool", wkn[i][:, :, :], wkn_v[:, :, hg * HG * 64:(hg + 1) * HG * 64], w=[wkn[i]], partial=False)
        k.dma("pool", wv[i][:, :, :], wv_v[:, :, hg * HG * 64:(hg + 1) * HG * 64], w=[wv[i]], partial=False)
        for tb in range(8):
            blk = slice(tb * 512, (tb + 1) * 512)
            cr = cT_b[tb * 4:tb * 4 + 4]
            rp = rope[tb % 2]
            k.dma("sp", rp[:, 0, :], rope_v[0][:, blk], w=[rp])
            k.dma("sp", rp[:, 1, :], rope_v[1][:, blk], w=[rp])
            for h in range(HG):
                n_pj = (tb * HG + h) % 2
                QA, QB = PS[n_pj * 2], PS[n_pj * 2 + 1]
                for kc in range(3):
                    k.mm(QA[0:96, :], wq[i][:, kc, h * 96:(h + 1) * 96], cT.t[:, kc, blk], kc == 0, kc == 2, r=[wq[i]] + cr, w=[QA])
                for kc in range(3):
                    k.mm(QB[0:96, :], wqs[i][:, kc, h * 96:(h + 1) * 96], cT.t[:, kc, blk], kc == 0, kc == 2, r=[wqs[i]] + cr, w=[QB])
                KN = PS[6 + n_pj]
                for kc in range(2):
                    k.mm(KN[0:64, :], wkn[i][:, kc, h * 64:(h + 1) * 64], cT.t[:, 3 + kc, blk], kc == 0, kc == 1, r=[wkn[i]] + cr, w=[KN])
                qb = qT_b[h][tb]
                rt1, rt2 = rt1s[n_pj], rt2s[n_pj]
                k.act(qT.t[0:64, h, blk], QA[0:64, :], AF.Copy, r=[QA], w=[qb], partial=True)
                k.v("dve", "tensor_tensor", r=[QA, rp], w=[rt1], out=rt1[64:96, :], in0=QA[64:96, :], in1=rp[64:96, 0, :], op=ALU.mult)
                k.v("dve", "tensor_tensor", r=[QB, rp], w=[rt2], out=rt2[64:96, :], in0=QB[64:96, :], in1=rp[64:96, 1, :], op=ALU.mult)
                k.v("dve", "tensor_tensor", r=[rt1, rt2], w=[qb], partial=True, out=qT.t[64:96, h, blk], in0=rt1[64:96, :], in1=rt2[64:96, :],
                    op=ALU.add)
                kb = kT_b[h][tb]
                k.act(kT.t[0:64, h, blk], KN[0:64, :], AF.Copy, r=[KN], w=[kb], partial=True)
                k.v("pool", "tensor_copy", r=[krT_b[tb]], w=[kb], partial=True, out=kT.t[64:96, h, blk], in_=krT[64:96, blk])
            for tl in range(4):
                tt = tb * 4 + tl
                VP = PS[4 + tl % 2]
                for kc in range(2):
                    k.mm(VP[:, 0:HG * 64], cT.t[:, 3 + kc, tt * 128:(tt + 1) * 128], wv[i][:, kc, :], kc == 0, kc == 1, r=[wv[i], cT_b[tt]], w=[VP])
                k.act(Vt.t[:, tt, :, 0:64], VP[:, 0:HG * 64].rearrange("p (h d) -> p h d", h=HG), AF.Copy, r=[VP], w=[V_b[tt]])
        pairs = []
        for h in range(HG):
            for Qb in range(8):
                nk = 4 * Qb + 4
                for jp in range(nk // 2):
                    pairs.append((h, Qb, jp, nk))
        OTs = {}

        def emit_qk(p):
            nonlocal n_st, n_ot
            h, Qb, jp, nk = p
            if jp == 0:
                OTs[(h, Qb)] = PS[4 + n_ot % 2]
                n_ot += 1
            j0 = jp * 2
            diag = j0 >= 4 * Qb
            q0 = (j0 - 4 * Qb) * 128 if diag else 0
            qs0 = Qb * 512
            pr = n_st % 3
            SA, SB = ((PS[0], PS[1]), (PS[2], PS[3]), (PS[6], PS[7]))[pr]
            ptt = pt[n_st % 4]
            n_st += 1
            for u, SP_ in enumerate((SA, SB)):
                j = j0 + u
                k.mm(SP_[:, q0:512], kT.t[0:96, h, j * 128:(j + 1) * 128], qT.t[0:96, h, qs0 + q0:qs0 + 512], True, True,
                     r=[kT_b[h][j // 4], qT_b[h][Qb]], w=[SP_])
            return (p, q0, diag, SA, SB, ptt, pr)

        def emit_exp(st_):
            p, q0, diag, SA, SB, ptt, pr = st_
            h, Qb, jp, nk = p
            dbk = DBK[pr]
            k.act(ptt[:, :, q0:512], dbk[:, :].rearrange("p (u q) -> p u q", u=2)[:, :, q0:512], AF.Exp, r=[SA, SB], w=[ptt],
                  scale=SCALE)
            if diag:
                jj = jp * 2 - 4 * Qb
                k.v("dve", "tensor_tensor", r=[ptt, amask], w=[ptt], out=ptt[:, :, q0:512], in0=ptt[:, :, q0:512],
                    in1=amask[:, jj:jj + 2, q0:512], op=ALU.mult)

        def emit_pv(st_):
            p, q0, diag, SA, SB, ptt, pr = st_
            h, Qb, jp, nk = p
            OT = OTs[(h, Qb)]
            for u in range(2):
                j = jp * 2 + u
                k.mm(OT[0:65, q0:512], Vt.t[:, j, h, 0:65], ptt[:, u, q0:512], j == 0, j == nk - 1, r=[V_b[j], ptt], w=[OT])

        def emit_norm(p):
            nonlocal n_at
            h, Qb, jp, nk = p
            habs = hg * HG + h
            qs0 = Qb * 512
            OT = OTs[(h, Qb)]
            slot = (habs % 16) * 8 + Qb
            ri = rinv2[n_at % 2]
            bcs = bcs2[n_at % 2]
            a_ = at[n_at % 2]
            n_at += 1
            k.v("dve", "reciprocal", r=[OT], w=[ri], out=ri[64:65, :], in_=OT[64:65, :])
            k.dma("sp", rn_d[slot:slot + 1, :], ri[64:65, :], r=[ri], w=[rn_b[slot]], partial=False)
            k.dma("sp", bcs[0:64, :], rn_d[slot].partition_broadcast(64), r=[rn_b[slot]], w=[bcs], partial=False)
            k.v("dve", "tensor_tensor", r=[OT, bcs], w=[a_], out=a_[0:64, :], in0=OT[0:64, :], in1=bcs[0:64, :], op=ALU.mult)
            k.dma("sp", attnT_d[habs * 64:(habs + 1) * 64, qs0:qs0 + 512], a_[0:64, :], r=[a_], w=[B_attn])

        pend = []
        for p in pairs:
            cur = emit_qk(p)
            emit_exp(cur)
            pend.append(cur)
            if len(pend) > 2:
                d_ = pend.pop(0)
                emit_pv(d_)
                pp = d_[0]
                if pp[2] == pp[3] // 2 - 1:
                    emit_norm(pp)
        while pend:
            d_ = pend.pop(0)
            emit_pv(d_)
            pp = d_[0]
            if pp[2] == pp[3] // 2 - 1:
                emit_norm(pp)
    st.close()
    stA.close()
    k.P.fence()
    st = contextlib.ExitStack()
    k.st = st
    wo = k.sb("wo", [128, 8, D], BF16)
    k.dma("pool", wo[:, :, :], c["mla_w_out"].rearrange("(kc p) d -> p kc d", p=128), w=[wo], partial=False)
    g_bc, b_bc = load_ln(k, c["ln_mix_g"], c["ln_mix_b"], 1)
    NB3 = 4
    aT = [k.sb(f"aT{i}", [128, 8, 128], BF16) for i in range(NB3)]
    hin = [k.sb(f"a3hin{i}", [128, D], F32) for i in range(NB3)]
    res = [k.sb(f"a3res{i}", [128, D], F32) for i in range(2)]
    lntmp = (k.sb("a3ln_stats", [128, 12], F32), k.sb("a3ln_mv", [128, 2], F32), k.sb("a3ln_lnv", [128, 1], F32),
             k.sb("a3ln_rstd", [128, 1], F32))
    at_v = attnT_d.rearrange("(kc p) t -> p kc t", p=128)
    hT_r3 = [k.sb(f"a3hTr{i}", [128, 8, 128], F32) for i in range(2)]

    def a3_issue(tt):
        k.dma("sp", aT[tt % NB3][:, :, :], at_v[:, :, tt * 128:(tt + 1) * 128], r=[B_attn], w=[aT[tt % NB3]], partial=False)
        k.dma("sp", hin[tt % NB3][:, :], h2_d[tt * 128:(tt + 1) * 128, :], r=[B_h2[tt]], w=[hin[tt % NB3]], partial=False)

    a3_issue(0)
    a3_issue(1)
    for tt in range(NT):
        if tt + 2 < NT:
            a3_issue(tt + 2)
        a_ = aT[tt % NB3]
        hi = hin[tt % NB3]
        r_ = res[tt % 2]
        for dh in range(2):
            OP = PS[4 + dh]
            for kc in range(8):
                k.mm(OP[:, :], a_[:, kc, :], wo[:, kc, dh * 512:(dh + 1) * 512], kc == 0, kc == 7, r=[a_, wo], w=[OP])
            k.v("dve", "scalar_tensor_tensor", r=[hi, OP], w=[r_], partial=True, out=r_[:, dh * 512:(dh + 1) * 512],
                in0=hi[:, dh * 512:(dh + 1) * 512], scalar=ALPHA, in1=OP[:, :], op0=ALU.mult, op1=ALU.add)
        if tt > 0:
            route_tile(k, c, 1, tt - 1, res[(tt - 1) % 2], hT_r3[(tt - 1) % 2], (PS[0], PS[1]), PS[6 + (tt - 1) % 2])
        layer_norm_tile(k, r_, g_bc, b_bc, r_, lntmp, eps_ln, "a3", eng2="pool")
        op = k.dma("sp", h3_d[tt * 128:(tt + 1) * 128, :], r_[:, :], r=[r_], w=[B_h3[tt]])
        k.out_ops.append(op)
    route_tile(k, c, 1, NT - 1, res[(NT - 1) % 2], hT_r3[(NT - 1) % 2], (PS[0], PS[1]), PS[6 + (NT - 1) % 2])
    st.close()
    k.P.fence()
    k.st = st_outer


I32 = mybir.dt.int32
IOA = bass.IndirectOffsetOnAxis
N_ITEMS = 32


def moe_layer(k, c, layer, hin_d, B_hin, hout_d, B_hout, is_final):
    PS = c["PS"]
    ident_f, ident_b, ones_f, eps_ln = c["ident_f"], c["ident_b"], c["ones_f"], c["eps_ln"]
    xsl_d, ysl_d, B_xsl, B_ysl = c["xsl_d"], c["ysl_d"], c["B_xsl"], c["B_ysl"]
    st_outer = k.st
    stR = contextlib.ExitStack()
    k.st = stR
    L = f"s{layer}"
    TH = NT
    g1 = k.sb(L + "g1", [128, TH], F32)
    g2 = k.sb(L + "g2", [128, TH], F32)
    pos1_i = k.sb(L + "pos1i", [128, TH], I32)
    pos2_i = k.sb(L + "pos2i", [128, TH], I32)
    widx = k.sb(L + "widx", [128, 2, N_ITEMS], I32)
    wg = [k.sb(f"{L}wg{i}", [128, 8, DFF], BF16) for i in range(2)]
    wu = [k.sb(f"{L}wu{i}", [128, 8, DFF], BF16) for i in range(2)]
    wd = [k.sb(f"{L}wd{i}", [128, 4, D], BF16) for i in range(2)]
    wsrc = (c["moe_w_gate"], c["moe_w_up"], c["moe_w_down"])

    def item_loads_w(i):
        b = i % 2
        for src, dst in zip(wsrc, (wg[b], wu[b], wd[b])):
            nh = dst.t.shape[1] // 2
            for hh in range(2):
                def gw(e, src=src, dst=dst, hh=hh, nh=nh, i=i):
                    if getattr(k, "bc_val", None) is None:
                        r_ = e.alloc_register("moe_bc")
                        e.reg_mov(r_, 2 * E * 128 * 2 - 1)
                        k.bc_val = e.snap(r_)
                    return e.indirect_dma_start(
                        out=dst[:, hh * nh:(hh + 1) * nh, :].rearrange("p a b -> p (a b)"), out_offset=None, in_=src,
                        in_offset=IOA(ap=widx[:, hh, i:i + 1], axis=0), bounds_check=k.bc_val, oob_is_err=False)
                k.P.add("pool", gw, _bufs([widx]), _bufs([dst]), dma=True, partial=(hh == 1))
    st = contextlib.ExitStack()
    k.st = st
    rw = k.sb(L + "rw", [128, 8, E], F32)
    k.dma("sp", rw[:, :, :], c["router_w"].rearrange("(kc p) e -> p kc e", p=128), w=[rw])
    rb = k.sb(L + "rb", [128, E], F32)
    k.dma("sp", rb[:, :], c["router_bias"].partition_broadcast(128), w=[rb])
    umat = k.sb(L + "umat", [128, 128], F32)
    k.dma("sp", umat[:, :], c["c_umat"], w=[umat])
    misc = k.sb(L + "misc", [128, 48], F32)
    k.dma("sp", misc[:, :], c["c_misc"], w=[misc])
    hin = [k.sb(f"{L}hin{i}", [128, D], F32) for i in range(3)]
    hTf = [k.sb(f"{L}hTf{i}", [128, 8, 128], F32) for i in range(2)]
    fused = bool(c.get("lg_fused", {}).get(layer))
    lg = c["lg_all"][layer] if fused else k.sb(L + "lg", [128, TH, E], F32)
    for tt in range(0 if fused else TH):
        hi = hin[tt % 3]
        hT_ = hTf[tt % 2]
        k.dma("sp", hi[:, :], hin_d[tt * 128:(tt + 1) * 128, :], r=[B_hin[tt]], w=[hi], partial=False)
        for half in range(2):
            pb = PS[(tt % 2) * 2 + half]
            for j in range(4):
                kc = half * 4 + j
                k.tr(pb[:, j * 128:(j + 1) * 128], hi[:, kc * 128:(kc + 1) * 128], ident_f[:, :], r=[hi, ident_f], w=[pb])
            if half == 0:
                k.act(hT_[:, 0:4, :], pb[:, :].rearrange("p (j c) -> p j c", j=4), AF.Copy, r=[pb], w=[hT_], partial=True)
            else:
                k.v("dve", "tensor_copy", r=[pb], w=[hT_], partial=True, out=hT_[:, 4:8, :], in_=pb[:, :].rearrange("p (j c) -> p j c", j=4))
        pl = PS[4 + tt % 2]
        for kc in range(8):
            k.mm(pl[:, 0:E], hT_[:, kc, :], rw[:, kc, :], kc == 0, kc == 7, r=[hT_, rw], w=[pl])
        k.act(lg[:, tt, :], pl[:, 0:E], AF.Exp, r=[pl], w=[lg], partial=True, scale=-1.0)
    T3 = lambda nm: k.sb(L + nm, [128, TH, E], F32)
    aff, sel, selm, mk1, selm2, mk2, w12, posall = (T3(n) for n in ("aff", "sel", "selm", "mk1", "selm2", "mk2", "w12", "posall"))
    p6 = k.sb(L + "p6", [128, TH * 4, 6], F32)
    gs = k.sb(L + "gs", [128, TH * 4], F32)
    gmax = k.sb(L + "gmax", [128, TH], F32)
    gm = k.sb(L + "gm", [128, TH * 4], F32)
    pen = k.sb(L + "pen", [128, TH * 4], F32)
    m1 = k.sb(L + "m1", [128, TH], F32)
    wsum = k.sb(L + "wsum", [128, TH], F32)
    V = lambda name, **kw: k.v("dve", name, **kw)
    V("tensor_scalar", r=[lg], w=[lg], out=lg[:, :, :], in0=lg[:, :, :], scalar1=1.0, scalar2=None, op0=ALU.add)
    V("reciprocal", r=[lg], w=[aff], out=aff[:, :, :], in_=lg[:, :, :])
    V("tensor_tensor", r=[aff, rb], w=[sel], out=sel[:, :, :], in0=aff[:, :, :], in1=bc(rb[:, :], [[0, TH], [1, E]]), op=ALU.add)
    s4 = sel[:, :, :].rearrange("p t (g i) -> p (t g) i", g=4)
    V("tensor_tensor", r=[sel], w=[p6], partial=True, out=p6[:, :, 0:3], in0=s4[:, :, 0:3], in1=s4[:, :, 1:4], op=ALU.add)
    V("tensor_tensor", r=[sel], w=[p6], partial=True, out=p6[:, :, 3:5], in0=s4[:, :, 0:2], in1=s4[:, :, 2:4], op=ALU.add)
    V("tensor_tensor", r=[sel], w=[p6], partial=True, out=p6[:, :, 5:6], in0=s4[:, :, 0:1], in1=s4[:, :, 3:4], op=ALU.add)
    V("tensor_reduce", r=[p6], w=[gs], out=gs[:, :], in_=p6[:, :, :], axis=AX.X, op=ALU.max)
    V("tensor_reduce", r=[gs], w=[gmax], out=gmax[:, :], in_=gs[:, :].rearrange("p (t g) -> p t g", g=4), axis=AX.X, op=ALU.max)
    V("tensor_tensor", r=[gs, gmax], w=[gm], out=gm[:, :].rearrange("p (t g) -> p t g", g=4),
      in0=gs[:, :].rearrange("p (t g) -> p t g", g=4), in1=bc(gmax[:, :], [[1, TH], [0, 4]]), op=ALU.is_ge)
    V("tensor_scalar", r=[gm], w=[pen], out=pen[:, :], in0=gm[:, :], scalar1=-1.0, scalar2=1.0e4, op0=ALU.add, op1=ALU.mult)
    sm4 = selm[:, :, :].rearrange("p t (g i) -> p (t g) i", g=4)
    V("tensor_tensor", r=[sel, gm], w=[selm], out=sm4, in0=s4, in1=bc(gm[:, :], [[1, TH * 4], [0, 4]]), op=ALU.mult)
    V("tensor_tensor", r=[selm, pen], w=[selm], out=sm4, in0=sm4, in1=bc(pen[:, :], [[1, TH * 4], [0, 4]]), op=ALU.add)
    V("tensor_reduce", r=[selm], w=[m1], out=m1[:, :], in_=selm[:, :, :], axis=AX.X, op=ALU.max)
    V("tensor_tensor", r=[selm, m1], w=[mk1], out=mk1[:, :, :], in0=selm[:, :, :], in1=bc(m1[:, :], [[1, TH], [0, E]]), op=ALU.is_ge)
    V("scalar_tensor_tensor", r=[mk1, selm], w=[selm2], out=selm2[:, :, :], in0=mk1[:, :, :], scalar=-1.0e4, in1=selm[:, :, :],
      op0=ALU.mult, op1=ALU.add)
    V("tensor_reduce", r=[selm2], w=[m1], out=m1[:, :], in_=selm2[:, :, :], axis=AX.X, op=ALU.max)
    V("tensor_tensor", r=[selm2, m1], w=[mk2], out=mk2[:, :, :], in0=selm2[:, :, :], in1=bc(m1[:, :], [[1, TH], [0, E]]), op=ALU.is_ge)
    V("tensor_tensor", r=[mk1, aff], w=[w12], out=w12[:, :, :], in0=mk1[:, :, :], in1=aff[:, :, :], op=ALU.mult)
    V("tensor_reduce", r=[w12], w=[g1], out=g1[:, :], in_=w12[:, :, :], axis=AX.X, op=ALU.add)
    V("tensor_tensor", r=[mk2, aff], w=[w12], out=w12[:, :, :], in0=mk2[:, :, :], in1=aff[:, :, :], op=ALU.mult)
    V("tensor_reduce", r=[w12], w=[g2], out=g2[:, :], in_=w12[:, :, :], axis=AX.X, op=ALU.add)
    V("tensor_tensor", r=[g1, g2], w=[wsum], out=wsum[:, :], in0=g1[:, :], in1=g2[:, :], op=ALU.add)
    V("reciprocal", r=[wsum], w=[wsum], out=wsum[:, :], in_=wsum[:, :])
    V("tensor_tensor", r=[g1, wsum], w=[g1], out=g1[:, :], in0=g1[:, :], in1=wsum[:, :], op=ALU.mult)
    V("tensor_tensor", r=[g2, wsum], w=[g2], out=g2[:, :], in0=g2[:, :], in1=wsum[:, :], op=ALU.mult)
    mk = w12
    V("tensor_tensor", r=[mk1, mk2], w=[mk], out=mk[:, :, :], in0=mk1[:, :, :], in1=mk2[:, :, :], op=ALU.add)
    PRE, TOT = PS[6], PS[7]
    mk2d = mk[:, :, :].rearrange("p t e -> p (t e)")
    k.mm(PRE[:, :], umat[:, :], mk2d, True, True, r=[umat, mk], w=[PRE])
    k.mm(TOT[:, :], ones_f[:, :], mk2d, True, True, r=[ones_f, mk], w=[TOT])
    tot = sel
    base = selm
    V("tensor_copy", r=[TOT], w=[tot], out=tot[:, :, :], in_=TOT[:, :].rearrange("p (t e) -> p t e", e=E))
    base_b = [Buf(f"{L}base{t}") for t in range(TH)]
    V("memset", w=[base_b[0]], ap=base[:, 0, :], constant=0.0)
    for t in range(1, TH):
        V("tensor_tensor", r=[base_b[t - 1], tot], w=[base_b[t]], out=base[:, t, :], in0=base[:, t - 1, :], in1=tot[:, t - 1, :], op=ALU.add)
    sm16 = k.sb(L + "sm16", [128, 6, E], F32)
    cmp8 = k.sb(L + "cmp8", [128, E, 8], F32)
    V("tensor_tensor", r=[base_b[TH - 1], tot], w=[sm16], out=sm16[:, 0, :], in0=base[:, TH - 1, :], in1=tot[:, TH - 1, :], op=ALU.add)
    V("tensor_tensor", r=[sm16, misc], w=[cmp8], out=cmp8[:, :, :], in0=bc(sm16[:, 0, :], [[1, E], [0, 8]]),
      in1=bc(misc[:, 0:8], [[0, E], [1, 8]]), op=ALU.is_gt)
    V("tensor_reduce", r=[cmp8], w=[sm16], out=sm16[:, 1, :], in_=cmp8[:, :, :], axis=AX.X, op=ALU.add)
    V("tensor_scalar", r=[sm16], w=[sm16], out=sm16[:, 2, :], in0=sm16[:, 1, :], scalar1=512.0, scalar2=None, op0=ALU.mult)
    V("memset", w=[sm16], ap=sm16[:, 3, 0:1], constant=0.0)
    for e in range(1, E):
        V("tensor_tensor", r=[sm16], w=[sm16], out=sm16[:, 3, e:e + 1], in0=sm16[:, 3, e - 1:e], in1=sm16[:, 2, e - 1:e], op=ALU.add)
    V("tensor_tensor", r=[sm16], w=[sm16], out=sm16[:, 4, :], in0=sm16[:, 3, :], in1=sm16[:, 2, :], op=ALU.add)
    V("tensor_tensor", r=[PRE] + base_b, w=[posall], out=posall[:, :, :], in0=PRE[:, :].rearrange("p (t e) -> p t e", e=E), in1=base[:, :, :],
      op=ALU.add)
    V("tensor_tensor", r=[posall, sm16], w=[posall], out=posall[:, :, :], in0=posall[:, :, :], in1=bc(sm16[:, 3, :], [[0, TH], [1, E]]),
      op=ALU.add)
    posf = k.sb(L + "posf", [128, 2, TH], F32)
    V("tensor_tensor", r=[mk1, posall], w=[mk1], out=mk1[:, :, :], in0=mk1[:, :, :], in1=posall[:, :, :], op=ALU.mult)
    V("tensor_reduce", r=[mk1], w=[posf], out=posf[:, 0, :], in_=mk1[:, :, :], axis=AX.X, op=ALU.add)
    V("tensor_tensor", r=[mk2, posall], w=[mk2], out=mk2[:, :, :], in0=mk2[:, :, :], in1=posall[:, :, :], op=ALU.mult)
    V("tensor_reduce", r=[mk2], w=[posf], out=posf[:, 1, :], in_=mk2[:, :, :], axis=AX.X, op=ALU.add)
    V("tensor_copy", r=[posf], w=[pos1_i], out=pos1_i[:, :], in_=posf[:, 0, :])
    V("tensor_copy", r=[posf], w=[pos2_i], out=pos2_i[:, :], in_=posf[:, 1, :])
    icmp = k.sb(L + "icmp", [128, N_ITEMS, E], F32)
    ei = k.sb(L + "ei", [128, N_ITEMS], F32)
    V("tensor_tensor", r=[sm16, misc], w=[icmp], out=icmp[:, :, :], in0=bc(sm16[:, 4, :], [[0, N_ITEMS], [1, E]]),
      in1=bc(misc[:, 8:40], [[1, N_ITEMS], [0, E]]), op=ALU.is_le)
    V("tensor_reduce", r=[icmp], w=[ei], out=ei[:, :], in_=icmp[:, :, :], axis=AX.X, op=ALU.add)
    oob = k.sb(L + "oob", [128, N_ITEMS], F32)
    V("tensor_scalar", r=[ei], w=[oob], out=oob[:, :], in0=ei[:, :], scalar1=float(E), scalar2=4.0e6, op0=ALU.is_ge, op1=ALU.mult)
    V("tensor_scalar", r=[ei], w=[ei], out=ei[:, :], in0=ei[:, :], scalar1=15.0, scalar2=float(layer * E), op0=ALU.min, op1=ALU.add)
    V("tensor_scalar", r=[ei], w=[ei], out=ei[:, :], in0=ei[:, :], scalar1=256.0, scalar2=None, op0=ALU.mult)
    V("tensor_tensor", r=[ei, oob], w=[ei], out=ei[:, :], in0=ei[:, :], in1=oob[:, :], op=ALU.add)
    V("scalar_tensor_tensor", r=[misc, ei], w=[ei], out=ei[:, :], in0=bc(misc[:, 40:41], [[0, N_ITEMS]]), scalar=2.0, in1=ei[:, :],
      op0=ALU.mult, op1=ALU.add)
    V("tensor_copy", r=[ei], w=[widx], partial=True, out=widx[:, 0, :], in_=ei[:, :])
    V("tensor_scalar", r=[ei], w=[ei], out=ei[:, :], in0=ei[:, :], scalar1=1.0, scalar2=None, op0=ALU.add)
    V("tensor_copy", r=[ei], w=[widx], partial=True, out=widx[:, 1, :], in_=ei[:, :])
    stage = c.get("moe_stage", "C")
    if "dbg_outs" in c:
        do = c["dbg_outs"]
        for nm, t_ in (("d_pos1", pos1_i), ("d_pos2", pos2_i), ("d_g1", g1), ("d_g2", g2)):
            k.out_ops.append(k.dma("sp", do[nm], t_[:, :], r=[t_]))
        k.out_ops.append(k.dma("sp", do["d_widx"], widx[:, :, :], r=[widx]))
    if stage == "P":
        st.close(); stR.close(); k.P.fence(); k.st = st_outer
        return
    xb = [k.sb(f"{L}xb{i}", [128, D], BF16) for i in range(3)]
    item_loads_w(0)
    item_loads_w(1)
    for tt in range(TH):
        x_ = xb[tt % 3]
        hi = hin[tt % 3]
        k.dma("sp", hi[:, :], hin_d[tt * 128:(tt + 1) * 128, :], r=[B_hin[tt]], w=[hi], partial=False)
        k.act(x_[:, :], hi[:, :], AF.Copy, r=[hi], w=[x_])
        for pi_ in (pos1_i, pos2_i):
            k.P.add("pool", (lambda e, x_=x_, pi_=pi_, tt=tt: e.indirect_dma_start(
                out=xsl_d, out_offset=IOA(ap=pi_[:, tt:tt + 1], axis=0), in_=x_[:, :], in_offset=None)),
                _bufs([x_, pi_]), [B_xsl], dma=True, partial=True)
    st.close()
    k.P.fence()
    if stage == "S":
        stR.close(); k.st = st_outer
        return
    st = contextlib.ExitStack()
    k.st = st
    xi = [k.sb(f"{L}xi{i}", [128, 4, D], BF16) for i in range(2)]
    xTi = [k.sb(f"{L}xTi{i}", [128, 8, 512], BF16) for i in range(2)]
    hT = [k.sb(f"{L}hT{i}", [128, 4, 512], BF16) for i in range(2)]
    sg = [k.sb(f"{L}sg{i}", [128, 512], BF16) for i in range(2)]
    yi = [k.sb(f"{L}yi{i}", [128, 4, D], F32) for i in range(2)]
    xsl_v = xsl_d.rearrange("(i s p) d -> i p s d", p=128, s=4)
    ysl_v = ysl_d.rearrange("(i s p) d -> i p s d", p=128, s=4)
    n_g = 0
    n_d = 0

    def item_loads(i, with_w=True):
        b = i % 2
        if with_w:
            item_loads_w(i)
        k.dma("sp", xi[b][:, :, :], xsl_v[i], r=[B_xsl], w=[xi[b]], partial=False)

    item_loads(0, with_w=False)
    for i in range(N_ITEMS):
        b = i % 2
        if i + 1 < N_ITEMS:
            item_loads(i + 1, with_w=(i + 1 >= 2))
        xT_ = xTi[b]
        for s_ in range(4):
            pb = PS[6 + s_ % 2]
            pv = pb[:, :].bitcast(BF16)
            for kc in range(8):
                k.tr(pv[:, kc * 128:(kc + 1) * 128], xi[b][:, s_, kc * 128:(kc + 1) * 128], ident_b[:, :], r=[xi[b], ident_b], w=[pb])
            if s_ % 2:
                k.act(xT_[:, :, s_ * 128:(s_ + 1) * 128], pv.rearrange("p (k t) -> p k t", k=8), AF.Copy, r=[pb], w=[xT_], partial=True)
            else:
                k.v("dve", "tensor_copy", r=[pb], w=[xT_], partial=True, out=xT_[:, :, s_ * 128:(s_ + 1) * 128],
                    in_=pv.rearrange("p (k t) -> p k t", k=8))
        hTt = hT[b]
        for fc in range(4):
            G = PS[n_g % 2]
            U = PS[2 + n_g % 2]
            s2 = sg[n_g % 2]
            n_g += 1
            for kc in range(8):
                k.mm(G[:, :], wg[b][:, kc, fc * 128:(fc + 1) * 128], xT_[:, kc, :], kc == 0, kc == 7, r=[wg[b], xT_], w=[G])
            for kc in range(8):
                k.mm(U[:, :], wu[b][:, kc, fc * 128:(fc + 1) * 128], xT_[:, kc, :], kc == 0, kc == 7, r=[wu[b], xT_], w=[U])
            k.act(s2[:, :], G[:, :], AF.Silu, r=[G], w=[s2])
            k.v("dve", "tensor_tensor", r=[s2, U], w=[hTt], partial=True, out=hTt[:, fc, :], in0=s2[:, :], in1=U[:, :], op=ALU.mult)
        y_ = yi[b]
        for tl in range(4):
            for dh in range(2):
                Dp = PS[4 + n_d % 2]
                n_d += 1
                for fc in range(4):
                    k.mm(Dp[:, :], hTt[:, fc, tl * 128:(tl + 1) * 128], wd[b][:, fc, dh * 512:(dh + 1) * 512], fc == 0, fc == 3,
                         r=[hTt, wd[b]], w=[Dp])
                if n_d % 2:
                    k.act(y_[:, tl, dh * 512:(dh + 1) * 512], Dp[:, :], AF.Copy, r=[Dp], w=[y_], partial=True)
                else:
                    k.v("dve", "tensor_copy", r=[Dp], w=[y_], partial=True, out=y_[:, tl, dh * 512:(dh + 1) * 512], in_=Dp[:, :])
        k.dma("sp", ysl_v[i], y_[:, :, :], r=[y_], w=[B_ysl])
    st.close()
    k.P.fence()
    if stage == "X":
        stR.close(); k.st = st_outer
        return
    st = contextlib.ExitStack()
    k.st = st
    g_bc, b_bc = load_ln(k, c["ln_ffn_g"], c["ln_ffn_b"], layer)
    NBC = 4
    hin = [k.sb(f"{L}chin{i}", [128, D], F32) for i in range(NBC)]
    y1 = [k.sb(f"{L}y1{i}", [128, D], F32) for i in range(NBC)]
    y2 = [k.sb(f"{L}y2{i}", [128, D], F32) for i in range(NBC)]
    res = [k.sb(f"{L}res{i}", [128, D], F32) for i in range(2)]
    lntmp = (k.sb(L + "ln_stats", [128, 12], F32), k.sb(L + "ln_mv", [128, 2], F32), k.sb(L + "ln_lnv", [128, 1], F32),
             k.sb(L + "ln_rstd", [128, 1], F32))

    def c_issue(tt):
        b = tt % NBC
        k.dma("sp", hin[b][:, :], hin_d[tt * 128:(tt + 1) * 128, :], r=[B_hin[tt]], w=[hin[b]], partial=False)
        for y_, pi_ in ((y1[b], pos1_i), (y2[b], pos2_i)):
            k.P.add("pool", (lambda e, y_=y_, pi_=pi_, tt=tt: e.indirect_dma_start(
                out=y_[:, :], out_offset=None, in_=ysl_d, in_offset=IOA(ap=pi_[:, tt:tt + 1], axis=0))),
                _bufs([pi_]) + [B_ysl], _bufs([y_]), dma=True, partial=False)

    c_issue(0)
    c_issue(1)
    for tt in range(TH):
        b = tt % NBC
        if tt + 2 < TH:
            c_issue(tt + 2)
        hi = hin[b]
        r_ = res[tt % 2]
        k.v("dve", "tensor_scalar", r=[y1[b], g1], w=[y1[b]], out=y1[b][:, :], in0=y1[b][:, :], scalar1=g1[:, tt:tt + 1], scalar2=None, op0=ALU.mult)
        k.v("dve", "scalar_tensor_tensor", r=[y2[b], g2, y1[b]], w=[y1[b]], out=y1[b][:, :], in0=y2[b][:, :], scalar=g2[:, tt:tt + 1], in1=y1[b][:, :],
            op0=ALU.mult, op1=ALU.add)
        k.v("dve", "scalar_tensor_tensor", r=[hi, y1[b]], w=[r_], out=r_[:, :], in0=hi[:, :], scalar=ALPHA, in1=y1[b][:, :],
            op0=ALU.mult, op1=ALU.add)
        layer_norm_tile(k, r_, g_bc, b_bc, r_, lntmp, eps_ln, L, eng2="dve")
        op = k.dma("sp", hout_d[tt * 128:(tt + 1) * 128, :], r_[:, :], r=[r_], w=[B_hout[tt]] if B_hout is not None else [])
        k.out_ops.append(op)
    st.close()
    stR.close()
    k.P.fence()
    k.st = st_outer


def _consts():
    j = np.arange(128)
    rmat = (j[:, None] <= j[None, :]).astype(np.float32)
    lmat = (j[:, None] > j[None, :]).astype(np.float32)
    inv = (10000.0 ** (-np.arange(0, 32, 2, dtype=np.float32) / 32)).astype(np.float32)
    ang = np.arange(S, dtype=np.float32)[:, None] * inv[None, :]
    cos, sin = np.cos(ang).astype(np.float32), np.sin(ang).astype(np.float32)
    rope = np.zeros((2, 128, S), np.float32)
    rope[0, 64:80] = cos.T
    rope[0, 80:96] = cos.T
    rope[1, 64:80] = -sin.T
    rope[1, 80:96] = sin.T
    kk = np.arange(128)[:, None, None]
    jj = np.arange(4)[None, :, None]
    qq = np.arange(512)[None, None, :]
    amask = ((qq // 64) >= ((jj * 128 + kk) // 64)).astype(np.float32)
    umat = (j[:, None] < j[None, :]).astype(np.float32)
    misc = np.zeros((128, 48), np.float32)
    misc[:, 0:8] = 512.0 * np.arange(8)[None, :]
    misc[:, 8:40] = 512.0 * np.arange(32)[None, :]
    misc[:, 40] = np.arange(128)
    return {"c_ident": np.eye(128, dtype=np.float32), "c_rmat": rmat, "c_lmat": lmat, "c_rope": rope,
            "c_amask": np.ascontiguousarray(amask), "c_umat": umat, "c_misc": misc}


def prep_shared(inp):
    f = lambda a: np.ascontiguousarray(np.asarray(a, dtype=np.float32))
    sh = {}
    sh["ssd_w_in"] = f(inp["ssd_w_in"][0])
    cw = np.asarray(inp["ssd_conv_w"][0], np.float32)
    sh["ssd_conv_w"] = f(cw.reshape(4, 32, 128).transpose(2, 1, 0).reshape(128, 128))
    sh["ssd_conv_b"] = f(np.asarray(inp["ssd_conv_b"][0], np.float32).reshape(32, 128).T)
    for n in ("ssd_dt_bias", "ssd_a_log", "ssd_d", "ssd_norm_w", "ssd_w_out", "mla_w_down", "mla_q_norm", "mla_w_uq",
              "mla_kv_norm", "mla_w_ukv", "mla_w_out"):
        sh[n] = f(inp[n][0])
    wd = sh["mla_w_down"]
    sh["mla_w_kr"] = f(np.concatenate([wd[:, 0:64], wd[:, 640:672], wd[:, 0:64], wd[:, 656:672], wd[:, 640:656]], axis=1))
    wkv = sh.pop("mla_w_ukv").reshape(256, 16, 128)
    sh["mla_w_kn"] = f(wkv[:, :, 0:64].reshape(256, 1024))
    sh["mla_w_v"] = f(wkv[:, :, 64:128].reshape(256, 1024))
    wq = sh["mla_w_uq"].reshape(384, 16, 96)
    sh["mla_w_uq_sw"] = f(np.concatenate([wq[:, :, 0:64], wq[:, :, 80:96], wq[:, :, 64:80]], axis=2).reshape(384, 1536))
    for n in ("router_w", "router_bias", "ln_mix_g", "ln_mix_b", "ln_ffn_g", "ln_ffn_b"):
        sh[n] = f(inp[n])
    for n in ("moe_w_gate", "moe_w_up"):
        w = np.asarray(inp[n], np.float32).reshape(2, E, 8, 128, DFF)
        sh[n] = f(w.transpose(0, 1, 3, 2, 4).reshape(2 * E * 128 * 2, 2048))
    w = np.asarray(inp["moe_w_down"], np.float32).reshape(2, E, 4, 128, D)
    sh["moe_w_down"] = f(w.transpose(0, 1, 3, 2, 4).reshape(2 * E * 128 * 2, 2048))
    sh.update(_consts())
    return sh


_NC_CACHE = {}


def kernel(**inputs):
    sh = prep_shared(inputs)
    x = np.asarray(inputs["x"], np.float32)
    if "nc" not in _NC_CACHE:
        _NC_CACHE["nc"] = build_program()
    nc = _NC_CACHE["nc"]
    in_maps = []
    for b in range(8):
        m = dict(sh)
        m["x"] = np.ascontiguousarray(x[b])
        in_maps.append(m)
    res = run_bass_kernel_spmd(nc, in_maps, core_ids=list(range(8)))
    return np.stack([np.asarray(r["out"], np.float32) for r in res.results], axis=0)
```

```python
import contextlib
import math
import numpy as np
import ml_dtypes
import concourse.bass as bass
import concourse.mybir as mybir
from concourse.bass_utils import run_bass_kernel_spmd

F32 = mybir.dt.float32
BF16 = mybir.dt.bfloat16
AF = mybir.ActivationFunctionType
ALU = mybir.AluOpType
AX = mybir.AxisListType

KEEP_WARM = 0
N_DMA_SEMS = {"sp": 12, "pool": 8, "act": 4}

S = 4096
D = 1024
NT = S // 128
ALPHA = (2.0 * 2) ** 0.25
LN_EPS = 1e-5
RMS_EPS = 1e-6
NH_SSD = 32
E = 16
DFF = 512
MLA_H = 16


class Buf:
    __slots__ = ("name", "gen_w", "gen_r", "prev_w", "prev_r")

    def __init__(self, name=""):
        self.name = name
        self.gen_w = []
        self.gen_r = []
        self.prev_w = []
        self.prev_r = []


class Op:
    __slots__ = ("eng", "fn", "deps", "is_dma", "idx", "needed", "sig", "dma_i")

    def __init__(self, eng, fn, is_dma, idx):
        self.eng = eng
        self.fn = fn
        self.deps = []
        self.is_dma = is_dma
        self.idx = idx
        self.needed = False
        self.sig = None
        self.dma_i = None


class Prog:
    def __init__(self, nc):
        self.nc = nc
        self.ops = []
        self.by_eng = {e: [] for e in ("pe", "act", "dve", "pool", "sp")}
        self.dma_count = {q: 0 for q in N_DMA_SEMS}
        self.fence_deps = []
        self.fence_seen = set()

    def fence(self):
        deps = []
        for e, lst in self.by_eng.items():
            last_c = None
            dmas = []
            for o in reversed(lst):
                if o.is_dma:
                    if len(dmas) < N_DMA_SEMS[e]:
                        dmas.append(o)
                elif last_c is None:
                    last_c = o
                if last_c is not None and (e not in N_DMA_SEMS or len(dmas) >= N_DMA_SEMS[e]):
                    break
            if last_c is not None:
                deps.append(last_c)
            deps.extend(dmas)
        self.fence_deps = deps
        self.fence_seen = set()

    def add(self, eng, fn, reads=(), writes=(), dma=False, partial=False):
        op = Op(eng, fn, dma, len(self.ops))
        deps = {}
        if self.fence_deps and eng not in self.fence_seen:
            self.fence_seen.add(eng)
            for o in self.fence_deps:
                deps[o.idx] = o

        def dep(o):
            if o is op:
                return
            if (not o.is_dma) and (not dma) and o.eng == eng and eng == "pe":
                return
            deps[o.idx] = o

        for b in reads:
            for w in b.gen_w:
                dep(w)
        for b in writes:
            if b.gen_r or not partial or not b.gen_w:
                for r in b.gen_r:
                    dep(r)
                for w in b.gen_w:
                    dep(w)
                b.prev_w, b.prev_r = b.gen_w, b.gen_r
                b.gen_w, b.gen_r = [op], []
            else:
                for r in b.prev_r:
                    dep(r)
                for w in b.prev_w:
                    dep(w)
                b.gen_w.append(op)
        for b in reads:
            if b in writes:
                continue
            if dma:
                b.gen_r.append(op)
            else:
                b.gen_r = [r for r in b.gen_r if r.is_dma or r.eng != eng]
                b.gen_r.append(op)
        for b in writes:
            if not dma and len(b.gen_w) > 1:
                b.gen_w = [w for w in b.gen_w if w.is_dma or w.eng != eng or w is op]
        op.deps = list(deps.values())
        for d in op.deps:
            d.needed = True
        self.ops.append(op)
        self.by_eng[eng].append(op)
        if dma:
            op.dma_i = self.dma_count[eng]
            self.dma_count[eng] += 1
        return op

    def emit(self, st, final_wait_ops=()):
        nc = self.nc
        esem = {e: st.enter_context(nc.semaphore("s_" + e)) for e in self.by_eng}
        dsem = {
            q: [st.enter_context(nc.semaphore(f"d_{q}{i}")) for i in range(n)]
            for q, n in N_DMA_SEMS.items()
        }
        cnt = {e: 0 for e in self.by_eng}
        for op in self.ops:
            if op.is_dma:
                n = N_DMA_SEMS[op.eng]
                j = op.dma_i % n
                op.sig = (dsem[op.eng][j], 16 * (op.dma_i // n + 1))
            elif op.needed:
                cnt[op.eng] += 1
                op.sig = (esem[op.eng], cnt[op.eng])
        self.final_counts = dict(cnt)
        block = st.enter_context(nc.Block())

        def run_engine(ename, eng):
            known = {}

            def wait(sig):
                sem, val = sig
                if known.get(sem.num, 0) >= val:
                    return
                eng.wait_ge(sem, val)
                known[sem.num] = val

            for op in self.by_eng[ename]:
                for d in op.deps:
                    wait(d.sig)
                if op.is_dma:
                    sem, val = op.sig
                    if val > 16:
                        wait((sem, val - 16))
                    ins = op.fn(eng)
                    ins.then_inc(sem, 16)
                else:
                    ins = op.fn(eng)
                    if op.sig is not None:
                        ins.then_inc(op.sig[0], 1)
            if ename == "sp":
                for op in final_wait_ops:
                    wait(op.sig)

        @block.tensor
        def _(e):
            run_engine("pe", e)

        @block.scalar
        def _(e):
            run_engine("act", e)

        @block.vector
        def _(e):
            run_engine("dve", e)

        @block.gpsimd
        def _(e):
            run_engine("pool", e)

        @block.sync
        def _(e):
            run_engine("sp", e)


class TT:
    def __init__(self, t, name):
        self.t = t
        self.b = Buf(name)

    def __getitem__(self, k):
        return self.t[k]


def _bufs(xs):
    out = []
    for x in xs:
        if x is None:
            continue
        out.append(x.b if isinstance(x, TT) else x)
    return out


class K:
    def __init__(self, nc, st):
        self.nc = nc
        self.st = st
        self.P = Prog(nc)
        self.out_ops = []

    def sb(self, name, shape, dt):
        return TT(self.st.enter_context(self.nc.sbuf_tensor(name, shape, dt)), name)

    def ps(self, name, shape, dt):
        return TT(self.st.enter_context(self.nc.psum_tensor(name, shape, dt)), name)

    def dram(self, name, shape, dt, kind="Internal"):
        return self.nc.dram_tensor(name, shape, dt, kind=kind).ap()

    def op(self, eng, fn, r=(), w=(), partial=False):
        return self.P.add(eng, fn, _bufs(r), _bufs(w), dma=False, partial=partial)

    def dma(self, q, out, in_, r=(), w=(), partial=True):
        return self.P.add(q, lambda e: e.dma_start(out=out, in_=in_), _bufs(r), _bufs(w), dma=True, partial=partial)

    def mm(self, out, lhsT, rhs, start, stop, r=(), w=()):
        return self.P.add("pe", lambda e: e.matmul(out, lhsT=lhsT, rhs=rhs, start=start, stop=stop),
                          _bufs(r), _bufs(w), partial=True)

    def tr(self, out, in_, ident, r=(), w=()):
        return self.P.add("pe", lambda e: e.transpose(out=out, in_=in_, identity=ident),
                          _bufs(r), _bufs(w), partial=True)

    def act(self, out, in_, func, r=(), w=(), partial=False, **kw):
        return self.P.add("act", lambda e: e.activation(out=out, in_=in_, func=func, **kw),
                          _bufs(r), _bufs(w), partial=partial)

    def v(self, eng, name, r=(), w=(), partial=False, **kw):
        return self.P.add(eng, lambda e: getattr(e, name)(**kw), _bufs(r), _bufs(w), partial=partial)


def bc(ap, dims):
    a = list(ap.ap)
    return bass.AP(ap.tensor, ap.offset, [list(a[0])] + [list(d) for d in dims])


def layer_norm_tile(k, r, g_bc, b_bc, out, tmp, eps_t, tag, eng2="dve"):
    stats, mv, lnv, rstd = tmp
    for i in range(2):
        k.v("dve", "bn_stats", r=[r], w=[stats], partial=True, out=stats[:, i * 6:(i + 1) * 6], in_=r[:, i * 512:(i + 1) * 512])
    k.v("dve", "bn_aggr", r=[stats], w=[mv], out=mv[:, 0:2], in_=stats[:, 0:12])
    k.act(lnv[:, 0:1], mv[:, 1:2], AF.Ln, r=[mv, eps_t], w=[lnv], bias=eps_t[:, 0:1])
    k.act(rstd[:, 0:1], lnv[:, 0:1], AF.Exp, r=[lnv], w=[rstd], scale=-0.5)
    k.v("dve", "tensor_scalar", r=[r, mv, rstd], w=[r], out=r[:, :], in0=r[:, :], scalar1=mv[:, 0:1], scalar2=rstd[:, 0:1],
        op0=ALU.subtract, op1=ALU.mult)
    k.v(eng2, "tensor_tensor", r=[r, g_bc], w=[r], out=r[:, :], in0=r[:, :], in1=g_bc[:, :], op=ALU.mult)
    k.v(eng2, "tensor_tensor", r=[r, b_bc], w=[out], out=out[:, :], in0=r[:, :], in1=b_bc[:, :], op=ALU.add)


def build_program(stop_after=None, dbg=False):
    nc = bass.Bass("TRN2", target_bir_lowering=False)
    st = contextlib.ExitStack()
    k = K(nc, st)

    def din(name, shape, dt=F32):
        return nc.dram_tensor(name, list(shape), dt, kind="ExternalInput").ap()

    x_d = din("x", [S, D])
    ssd_w_in = din("ssd_w_in", [D, 6176])
    ssd_conv_w = din("ssd_conv_w", [4, 4096])
    ssd_conv_b = din("ssd_conv_b", [4096])
    ssd_dt_bias = din("ssd_dt_bias", [32])
    ssd_a_log = din("ssd_a_log", [32])
    ssd_d = din("ssd_d", [32])
    ssd_norm_w = din("ssd_norm_w", [2048])
    ssd_w_out = din("ssd_w_out", [2048, D])
    mla_w_down = din("mla_w_down", [D, 672])
    mla_w_kr = din("mla_w_kr", [D, 192])
    mla_q_norm = din("mla_q_norm", [384])
    mla_w_uq = din("mla_w_uq", [384, 1536])
    mla_w_uq_sw = din("mla_w_uq_sw", [384, 1536])
    mla_kv_norm = din("mla_kv_norm", [256])
    mla_w_kn = din("mla_w_kn", [256, 1024])
    mla_w_v = din("mla_w_v", [256, 1024])
    mla_w_out = din("mla_w_out", [D, D])
    router_w = din("router_w", [D, E])
    router_bias = din("router_bias", [E])
    moe_w_gate = din("moe_w_gate", [2 * E * 128 * 2, 2048])
    moe_w_up = din("moe_w_up", [2 * E * 128 * 2, 2048])
    moe_w_down = din("moe_w_down", [2 * E * 128 * 2, 2048])
    ln_mix_g = din("ln_mix_g", [2, D])
    ln_mix_b = din("ln_mix_b", [2, D])
    ln_ffn_g = din("ln_ffn_g", [2, D])
    ln_ffn_b = din("ln_ffn_b", [2, D])
    c_ident = din("c_ident", [128, 128])
    c_rmat = din("c_rmat", [128, 128])
    c_lmat = din("c_lmat", [128, 128])
    c_rope = din("c_rope", [2, 128, S])
    c_amask = din("c_amask", [128, 4, 512])
    c_umat = din("c_umat", [128, 128])
    c_misc = din("c_misc", [128, 48])
    out_d = nc.dram_tensor("out", [S, D], F32, kind="ExternalOutput").ap()

    h1_d = k.dram("h1_d", [S, D], F32, kind="ExternalOutput" if dbg else "Internal")
    h2_d = k.dram("h2_d", [S, D], F32, kind="ExternalOutput" if dbg else "Internal")
    h3_d = k.dram("h3_d", [S, D], F32, kind="ExternalOutput" if dbg else "Internal")
    xs_d = k.dram("xs_d", [S, 2048], BF16)
    bt_d = k.dram("bt_d", [S, 1024], BF16)
    bT_d = k.dram("bT_d", [1024, S], BF16)
    cT_d = k.dram("cT_d", [1024, S], BF16)
    z_d = k.dram("z_d", [S, 2048], BF16)
    dt_d = k.dram("dt_d", [S, 32], F32)
    a_d = k.dram("a_d", [S, 32], F32)
    attnT_d = k.dram("attnT_d", [D, S], BF16)
    rn_d = k.dram("rn_d", [128, 512], F32)
    NSLOT = 16384
    xsl_d = k.dram("xsl_d", [NSLOT, D], BF16)
    ysl_d = k.dram("ysl_d", [NSLOT, D], F32)
    B_xsl = Buf("xsl"); B_ysl = Buf("ysl")
    zt = k.sb("zero_t", [128, D], BF16)
    k.v("pool", "memset", w=[zt], ap=zt[:, :], constant=0.0)
    xsl_z = xsl_d.rearrange("(a p) d -> a p d", p=128)
    for a_ in range(NSLOT // 128):
        k.dma("sp", xsl_z[a_], zt[:, :], r=[zt], w=[B_xsl])
    B_h = [[Buf(f"h{i}_{t}") for t in range(NT)] for i in range(4)]
    B_xs = Buf("xs_d"); B_bt = Buf("bt_d"); B_bT = Buf("bT_d"); B_cT = Buf("cT_d")
    B_z = Buf("z_d"); B_dt = Buf("dt_d"); B_a = Buf("a_d"); B_attn = Buf("attn_d")
    B_out = Buf("out")

    ident_f = k.sb("ident_f", [128, 128], F32)
    ident_b = k.sb("ident_b", [128, 128], BF16)
    rmat_f = k.sb("rmat_f", [128, 128], F32)
    rmat_b = k.sb("rmat_b", [128, 128], BF16)
    lmat_f = k.sb("lmat_f", [128, 128], F32)
    lmat_b = k.sb("lmat_b", [128, 128], BF16)
    ones_f = k.sb("ones_f", [128, 128], F32)
    ones_b = k.sb("ones_b", [128, 128], BF16)
    eps_ln = k.sb("eps_ln", [128, 1], F32)
    eps_rms = k.sb("eps_rms", [128, 1], F32)
    k.dma("sp", ident_f[:, :], c_ident, w=[ident_f])
    k.dma("sp", rmat_f[:, :], c_rmat, w=[rmat_f])
    k.dma("sp", lmat_f[:, :], c_lmat, w=[lmat_f])
    k.v("dve", "tensor_copy", r=[ident_f], w=[ident_b], out=ident_b[:, :], in_=ident_f[:, :])
    k.v("dve", "tensor_copy", r=[rmat_f], w=[rmat_b], out=rmat_b[:, :], in_=rmat_f[:, :])
    k.v("dve", "tensor_copy", r=[lmat_f], w=[lmat_b], out=lmat_b[:, :], in_=lmat_f[:, :])
    k.v("dve", "memset", w=[ones_f], ap=ones_f[:, :], constant=1.0)
    k.v("dve", "memset", w=[ones_b], ap=ones_b[:, :], constant=1.0)
    k.v("dve", "memset", w=[eps_ln], ap=eps_ln[:, :], constant=LN_EPS)
    k.v("dve", "memset", w=[eps_rms], ap=eps_rms[:, :], constant=RMS_EPS)

    DBK = [k.ps(f"psd{i}", [128, 1024], F32) for i in range(2)]
    PS = []
    for i in range(4):
        v_ = TTview(DBK[i // 2], DBK[i // 2].t[:, (i % 2) * 512:(i % 2 + 1) * 512])
        v_.b = Buf(f"ps{i}")
        PS.append(v_)
    PS += [k.ps(f"ps{i}", [128, 512], F32) for i in range(4, 6)]
    DBK.append(k.ps("psd2", [128, 1024], F32))
    for i in range(2):
        v_ = TTview(DBK[2], DBK[2].t[:, i * 512:(i + 1) * 512])
        v_.b = Buf(f"ps{6 + i}")
        PS.append(v_)

    rw_g = k.sb("rw_g", [128, 8, E], F32)
    k.dma("sp", rw_g[:, :, :], router_w.rearrange("(kc p) e -> p kc e", p=128), w=[rw_g])
    lg_all = [None, None]
    lg_fused = {}
    ctx = dict(locals())
    if stop_after is not None and stop_after.startswith("moeonly"):
        h1_in = din("h1_in", [S, D])
        ctx["moe_stage"] = stop_after.split(":")[1]
        ctx["dbg_outs"] = {}
        for nm, shp, dt_ in (("d_pos1", [128, NT], mybir.dt.int32), ("d_pos2", [128, NT], mybir.dt.int32),
                             ("d_widx", [128, 2, 32], mybir.dt.int32), ("d_g1", [128, NT], F32), ("d_g2", [128, NT], F32)):
            ctx["dbg_outs"][nm] = nc.dram_tensor(nm, shp, dt_, kind="ExternalOutput").ap()
        moe_layer(k, ctx, 0, h1_in, [Buf() for _ in range(NT)], h2_d, B_h[1], False)
        return finish(k, nc, st)
    ssd_layer(k, ctx)
    if stop_after == "ssd":
        return finish(k, nc, st)
    moe_layer(k, ctx, 0, h1_d, B_h[0], h2_d, B_h[1], False)
    if stop_after == "moe0":
        return finish(k, nc, st)
    mla_layer(k, ctx)
    if stop_after == "mla":
        return finish(k, nc, st)
    moe_layer(k, ctx, 1, h3_d, B_h[2], out_d, None, True)
    return finish(k, nc, st)


def route_step(k, c, layer, tt, src, hT_, tr_banks, lg_bank, step):
    ident_f, rw, lg = c["ident_f"], c["rw_g"], c["lg_all"][layer]
    if step < 2:
        half = step
        pb = tr_banks[half]
        for j in range(4):
            kc = half * 4 + j
            k.tr(pb[:, j * 128:(j + 1) * 128], src[:, kc * 128:(kc + 1) * 128], ident_f[:, :], r=[src, ident_f], w=[pb])
        k.act(hT_[:, half * 4:half * 4 + 4, :], pb[:, :].rearrange("p (j c) -> p j c", j=4), AF.Copy, r=[pb], w=[hT_], partial=True)
    else:
        for kc in range(8):
            k.mm(lg_bank[:, 0:E], hT_[:, kc, :], rw[:, kc, :], kc == 0, kc == 7, r=[hT_, rw], w=[lg_bank])
        k.act(lg[:, tt, :], lg_bank[:, 0:E], AF.Exp, r=[lg_bank], w=[lg], partial=True, scale=-1.0)
        c["lg_fused"][layer] = True


def route_tile(k, c, layer, tt, src, hT_, tr_banks, lg_bank):
    for step in range(3):
        route_step(k, c, layer, tt, src, hT_, tr_banks, lg_bank, step)


def load_ln(k, g_src, b_src, layer):
    t = k.sb(f"lnp_{k.P.dma_count['sp']}", [128, 2, D], F32)
    k.dma("sp", t[:, 0, :], g_src[layer].partition_broadcast(128), w=[t])
    k.dma("sp", t[:, 1, :], b_src[layer].partition_broadcast(128), w=[t])
    return TTview(t, t.t[:, 0, :]), TTview(t, t.t[:, 1, :])


class TTview:
    def __init__(self, parent, ap):
        self.t = ap
        self.b = parent.b

    def __getitem__(self, k):
        return self.t[k]


def finish(k, nc, st):
    k.P.emit(st, final_wait_ops=k.out_ops)
    st.close()
    return nc


def _bufs(xs):
    out = []
    for x in xs:
        if x is None:
            continue
        out.append(getattr(x, "b", x))
    return out


def ssd_layer(k, c):
    nc = k.nc
    PS = c["PS"]
    x_d = c["x_d"]
    ident_b, rmat_f, rmat_b, lmat_f, lmat_b, ones_f, ones_b = (c[n] for n in
        ("ident_b", "rmat_f", "rmat_b", "lmat_f", "lmat_b", "ones_f", "ones_b"))
    xs_d, bt_d, bT_d, cT_d, z_d, dt_d, a_d = (c[n] for n in ("xs_d", "bt_d", "bT_d", "cT_d", "z_d", "dt_d", "a_d"))
    B_xs, B_bt, B_bT, B_cT, B_z, B_dt, B_a = (c[n] for n in ("B_xs", "B_bt", "B_bT", "B_cT", "B_z", "B_dt", "B_a"))
    st_outer = k.st
    st = contextlib.ExitStack()
    k.st = st

    xT = k.sb("xT", [128, 8, S], BF16)
    xT_b = [Buf(f"xT{t}") for t in range(NT)]
    st1a = contextlib.ExitStack()
    k.st = st1a
    xbf = [k.sb(f"xbf{i}", [128, D], BF16) for i in range(3)]
    for tt in range(NT):
        xb = xbf[tt % 3]
        k.dma("pool", xb[:, :], x_d[tt * 128:(tt + 1) * 128, :], w=[xb], partial=False)
        pb = PS[tt % 2]
        pv = pb[:, :].bitcast(BF16)
        for kc in range(8):
            k.tr(pv[:, kc * 128:(kc + 1) * 128], xb[:, kc * 128:(kc + 1) * 128], ident_b[:, :], r=[xb, ident_b], w=[pb])
        dst = xT.t[:, :, tt * 128:(tt + 1) * 128]
        src = pv.rearrange("p (k t) -> p k t", k=8)
        if tt % 2:
            k.act(dst, src, AF.Copy, r=[pb], w=[xT_b[tt]])
        else:
            k.v("dve", "tensor_copy", r=[pb], w=[xT_b[tt]], out=dst, in_=src)

    w_in_v = c["ssd_w_in"].rearrange("(kc p) c -> p kc c", p=128)
    wz = k.sb("wz", [128, 8, 2048], BF16)
    wz_b = [Buf(f"wz{i}") for i in range(4)]
    for cb in range(4):
        k.dma("pool", wz[:, :, cb * 512:(cb + 1) * 512], w_in_v[:, :, cb * 512:(cb + 1) * 512], w=[wz_b[cb]])
    wdt = k.sb("wdt", [128, 8, 32], BF16)
    k.dma("pool", wdt[:, :, :], w_in_v[:, :, 6144:6176], w=[wdt])
    dtraw = k.sb("dtraw", [128, NT, 32], F32)
    zs = [k.sb(f"zs{i}", [128, 2048], BF16) for i in range(2)]
    n_ps = 0
    for tt in range(NT):
        zt = zs[tt % 2]
        for cb in range(4):
            pb = PS[6 + n_ps % 2]
            n_ps += 1
            for kc in range(8):
                k.mm(pb[:, :], xT.t[:, kc, tt * 128:(tt + 1) * 128], wz[:, kc, cb * 512:(cb + 1) * 512], kc == 0, kc == 7,
                     r=[xT_b[tt], wz_b[cb]], w=[pb])
            k.act(zt[:, cb * 512:(cb + 1) * 512], pb[:, :], AF.Silu, r=[pb], w=[zt], partial=True)
        k.dma("sp", z_d[tt * 128:(tt + 1) * 128, :], zt[:, :], r=[zt], w=[B_z])
        pb = PS[6 + n_ps % 2]
        n_ps += 1
        for kc in range(8):
            k.mm(pb[:, 0:32], xT.t[:, kc, tt * 128:(tt + 1) * 128], wdt[:, kc, :], kc == 0, kc == 7, r=[xT_b[tt], wdt], w=[pb])
        k.v("dve", "tensor_copy", r=[pb], w=[dtraw], partial=True, out=dtraw[:, tt, :], in_=pb[:, 0:32])

    sm32 = k.sb("sm32", [128, 3, 32], F32)
    k.dma("sp", sm32[:, 0, :], c["ssd_dt_bias"].partition_broadcast(128), w=[sm32])
    k.dma("sp", sm32[:, 1, :], c["ssd_a_log"].partition_broadcast(128), w=[sm32])
    e1 = k.sb("e1", [128, NT, 32], F32)
    dtv = k.sb("dtv", [128, NT, 32], F32)
    av = k.sb("av", [128, NT, 32], F32)
    k.v("dve", "tensor_tensor", r=[dtraw, sm32], w=[dtraw], out=dtraw[:, :, :], in0=dtraw[:, :, :],
        in1=bc(sm32[:, 0, :], [[0, NT], [1, 32]]), op=ALU.add)
    k.act(e1[:, :, :], dtraw[:, :, :], AF.Exp, r=[dtraw], w=[e1])
    k.act(dtv[:, :, :], e1[:, :, :], AF.Ln, r=[e1], w=[dtv], bias=1.0)
    k.act(sm32[:, 2, :], sm32[:, 1, :], AF.Exp, r=[sm32], w=[sm32])
    k.v("dve", "scalar_tensor_tensor", r=[dtv, sm32], w=[av], out=av[:, :, :], in0=dtv[:, :, :], scalar=-1.0,
        in1=bc(sm32[:, 2, :], [[0, NT], [1, 32]]), op0=ALU.mult, op1=ALU.mult)
    k.dma("sp", dt_d.rearrange("(t p) h -> p t h", p=128), dtv[:, :, :], r=[dtv], w=[B_dt])
    k.dma("sp", a_d.rearrange("(t p) h -> p t h", p=128), av[:, :, :], r=[av], w=[B_a])

    st1a.close()
    k.P.fence()
    k.st = st
    convw = k.sb("convw", [128, 128], F32)
    convb = k.sb("convb", [128, 32], F32)
    k.dma("sp", convw[:, :], c["ssd_conv_w"], w=[convw])
    k.dma("sp", convb[:, :], c["ssd_conv_b"], w=[convb])
    diagw = k.sb("diagw", [128, 128, 128], BF16)
    k.v("dve", "tensor_tensor", r=[ident_b, convw], w=[diagw], out=diagw[:, :, :], in0=bc(ident_b[:, :], [[0, 128], [1, 128]]),
        in1=bc(convw[:, :], [[1, 128], [0, 128]]), op=ALU.mult)
    wch = [k.sb(f"wch{i}", [128, 8, 128], BF16) for i in range(3)]
    xpre = [k.sb(f"xpre{i}", [128, 3 + S], BF16) for i in range(2)]
    xact = [k.sb(f"xact{i}", [128, S], BF16) for i in range(2)]
    stg = [k.sb(f"stg{i}", [128, 8, 128], BF16) for i in range(3)]
    for xp in xpre:
        k.v("dve", "memset", w=[xp], ap=xp[:, 0:3], constant=0.0)
    xs_v = xs_d.rearrange("(t p) c -> p t c", p=128)
    bt_v = bt_d.rearrange("(t p) c -> p t c", p=128)
    n_ps = 0
    n_cv = 0
    n_tr = 0

    def conv_block(cc, tb, xp, xa):
        nonlocal n_cv
        cv = PS[6 + n_cv % 2]
        n_cv += 1
        for tap in range(4):
            k.mm(cv[:, :], diagw[:, cc * 4 + tap, :], xp[:, tb * 512 + tap:tb * 512 + tap + 512], tap == 0, tap == 3, r=[diagw, xp], w=[cv])
        k.act(xa[:, tb * 512:(tb + 1) * 512], cv[:, :], AF.Silu, r=[cv, convb], w=[xa], partial=True, bias=convb[:, cc:cc + 1])

    for cc in range(32):
        wc = wch[cc % 3]
        k.dma("pool", wc[:, :, :], w_in_v[:, :, 2048 + cc * 128:2048 + (cc + 1) * 128], w=[wc], partial=False)
        xp = xpre[cc % 2]
        xa = xact[cc % 2]
        for tb in range(8):
            pb = PS[2 + n_ps % 2]
            n_ps += 1
            for kc in range(8):
                k.mm(pb[:, :], wc[:, kc, :], xT.t[:, kc, tb * 512:(tb + 1) * 512], kc == 0, kc == 7,
                     r=[wc] + xT_b[4 * tb:4 * tb + 4], w=[pb])
            if tb > 0:
                conv_block(cc, tb - 1, xp, xa)
            k.act(xp[:, 3 + tb * 512:3 + (tb + 1) * 512], pb[:, :], AF.Copy, r=[pb], w=[xp], partial=True)
        conv_block(cc, 7, xp, xa)
        if cc < 24:
            dv, Bd, col = (xs_v, B_xs, cc * 128) if cc < 16 else (bt_v, B_bt, (cc - 16) * 128)
            for g8 in range(4):
                pb = PS[4 + n_tr % 2]
                sg = stg[n_tr % 3]
                n_tr += 1
                pv = pb[:, :].bitcast(BF16)
                for j in range(8):
                    t = g8 * 8 + j
                    k.tr(pv[:, j * 128:(j + 1) * 128], xa[:, t * 128:(t + 1) * 128], ident_b[:, :], r=[xa, ident_b], w=[pb])
                k.v("dve", "tensor_copy", r=[pb], w=[sg], out=sg[:, :, :], in_=pv.rearrange("p (j c) -> p j c", j=8))
                k.dma("sp", dv[:, g8 * 8:(g8 + 1) * 8, col:col + 128], sg[:, :, :], r=[sg], w=[Bd])
        if 16 <= cc < 24:
            k.dma("sp", bT_d[(cc - 16) * 128:(cc - 15) * 128, :], xa[:, :], r=[xa], w=[B_bT])
        if cc >= 24:
            k.dma("sp", cT_d[(cc - 24) * 128:(cc - 23) * 128, :], xa[:, :], r=[xa], w=[B_cT])
    st.close()
    k.P.fence()

    st = contextlib.ExitStack()
    k.st = st
    h1_d = c["h1_d"]
    B_h1 = c["B_h"][0]
    eps_ln, eps_rms = c["eps_ln"], c["eps_rms"]
    wout = k.sb("wout", [128, 16, D], BF16)
    k.dma("pool", wout[:, :, :], c["ssd_w_out"].rearrange("(kc p) d -> p kc d", p=128), w=[wout], partial=False)
    normw = k.sb("normw", [128, 2048], F32)
    k.dma("sp", normw[:, :], c["ssd_norm_w"].partition_broadcast(128), w=[normw])
    dsk = k.sb("dsk", [128, 32], F32)
    k.dma("sp", dsk[:, :], c["ssd_d"].partition_broadcast(128), w=[dsk])
    diagD = k.sb("diagD", [128, 32, 128], BF16)
    identb3 = bc(ident_b[:, :], [[0, 32], [1, 128]])
    k.v("dve", "tensor_tensor", r=[ident_b, dsk], w=[diagD], out=diagD[:, :, :], in0=identb3,
        in1=bc(dsk[:, :], [[1, 32], [0, 128]]), op=ALU.mult)
    state = k.sb("state", [128, 2048], F32)
    state_bf = k.sb("state_bf", [128, 2048], BF16)
    st_b = [Buf(f"st{g}") for g in range(8)]
    stbf_b = [Buf(f"stbf{g}") for g in range(8)]
    k.v("dve", "memset", w=st_b, ap=state[:, :], constant=0.0)
    k.v("dve", "memset", w=stbf_b, ap=state_bf[:, :], constant=0.0)

    NB = 3
    xs_t = [k.sb(f"xs_t{i}", [128, 2048], BF16) for i in range(NB)]
    bt_t = [k.sb(f"bt_t{i}", [128, 1024], BF16) for i in range(NB)]
    bT_t = [k.sb(f"bT_t{i}", [128, 8, 128], BF16) for i in range(NB)]
    cT_t = [k.sb(f"cT_t{i}", [128, 8, 128], BF16) for i in range(NB)]
    a_t = [k.sb(f"a_t{i}", [128, 32], F32) for i in range(NB)]
    dt_t = [k.sb(f"dt_t{i}", [128, 32], F32) for i in range(NB)]
    z_t = [k.sb(f"z_t{i}", [128, 2048], BF16) for i in range(NB)]
    x_t = [k.sb(f"x_t{i}", [128, D], F32) for i in range(NB)]
    exps2 = [k.sb(f"exps{i}", [128, 96], F32) for i in range(2)]
    a_bf2 = [k.sb(f"a_bf{i}", [128, 32], BF16) for i in range(2)]
    dtte2 = [k.sb(f"dtte{i}", [128, 32], F32) for i in range(2)]
    aR2 = [k.sb(f"aR{i}", [128, 32, 128], BF16) for i in range(2)]
    dec = [k.sb(f"dec{i}", [128, 8, 128], BF16) for i in range(2)]
    smk = [k.sb(f"smk{i}", [128, 2, 128], BF16) for i in range(2)]
    MT = [k.sb(f"MT{i}", [128, 8, 128], BF16) for i in range(2)]
    xdt2 = [k.sb(f"xdt{i}", [128, 2048], BF16) for i in range(2)]
    xw2 = [k.sb(f"xw{i}", [128, 2048], BF16) for i in range(2)]
    tmpq = [k.sb(f"tmpq{i}", [128, 512], F32) for i in range(1)] * 2
    stq = [k.sb(f"stq{i}", [128, 512], F32) for i in range(1)] * 2
    ycomb2 = [k.sb(f"ycomb{i}", [128, 2048], F32) for i in range(2)]
    ssq = k.sb("ssq", [128, 8], F32)
    junk = k.sb("junk", [128, 256], F32)
    lnv8 = k.sb("lnv8", [128, 8], F32)
    rstd8 = k.sb("rstd8", [128, 8], F32)
    yn = k.sb("yn", [128, 2048], BF16)
    ynT = k.sb("ynT", [128, 16, 128], BF16)
    res = k.sb("res", [128, D], F32)
    hout = [k.sb(f"hout{i}", [128, D], F32) for i in range(1)] * 2
    lntmp = (k.sb("ln_stats", [128, 12], F32), k.sb("ln_mv", [128, 2], F32), k.sb("ln_lnv", [128, 1], F32),
             k.sb("ln_rstd", [128, 1], F32))
    bT_v = bT_d.rearrange("(g n) t -> n g t", n=128)
    cT_v = cT_d.rearrange("(g n) t -> n g t", n=128)
    g_bc, b_bc = load_ln(k, c["ln_mix_g"], c["ln_mix_b"], 0)
    MISC, SEG0, SEG1, YB, YOB, STB, OPB, TRB = PS

    hT_r = None

    def loads(tt):
        i = tt % NB
        sl = slice(tt * 128, (tt + 1) * 128)
        k.dma("sp", a_t[i][:, :], a_d[sl, :], r=[B_a], w=[a_t[i]], partial=False)
        k.dma("sp", dt_t[i][:, :], dt_d[sl, :], r=[B_dt], w=[dt_t[i]], partial=False)
        k.dma("sp", xs_t[i][:, :], xs_d[sl, :], r=[B_xs], w=[xs_t[i]], partial=False)
        k.dma("sp", bT_t[i][:, :, :], bT_v[:, :, sl], r=[B_bT], w=[bT_t[i]], partial=False)
        k.dma("sp", cT_t[i][:, :, :], cT_v[:, :, sl], r=[B_cT], w=[cT_t[i]], partial=False)
        k.dma("sp", bt_t[i][:, :], bt_d[sl, :], r=[B_bt], w=[bt_t[i]], partial=False)
        k.dma("sp", z_t[i][:, :], z_d[sl, :], r=[B_z], w=[z_t[i]], partial=False)
        k.dma("sp", x_t[i][:, :], x_d[sl, :], w=[x_t[i]], partial=False)

    def prologue(tt):
        i = tt % NB
        j = tt % 2
        xs_, a_, dt_ = xs_t[i], a_t[i], dt_t[i]
        exps, a_bf, dtte, aR, xdt, xw = exps2[j], a_bf2[j], dtte2[j], aR2[j], xdt2[j], xw2[j]
        k.mm(MISC[:, 0:32], rmat_f[:, :], a_[:, :], True, True, r=[rmat_f, a_], w=[MISC])
        k.mm(MISC[:, 32:64], lmat_f[:, :], a_[:, :], True, True, r=[lmat_f, a_], w=[MISC])
        k.mm(MISC[:, 64:96], ones_f[:, :], a_[:, :], True, True, r=[ones_f, a_], w=[MISC])
        k.act(exps[:, :], MISC[:, 0:96], AF.Exp, r=[MISC], w=[exps])
        k.v("dve", "tensor_tensor", r=[dt_, exps], w=[dtte], out=dtte[:, :], in0=dt_[:, :], in1=exps[:, 32:64], op=ALU.mult)
        k.v("dve", "tensor_copy", r=[a_], w=[a_bf], out=a_bf[:, :], in_=a_[:, :])
        k.v("dve", "tensor_tensor", r=[a_bf, rmat_b], w=[aR], out=aR[:, :, :], in0=bc(a_bf[:, :], [[1, 32], [0, 128]]),
            in1=bc(rmat_b[:, :], [[0, 32], [1, 128]]), op=ALU.mult)
        xs3 = xs_[:, :].rearrange("p (h d) -> p h d", h=32)
        k.v("pool", "tensor_tensor", r=[xs_, dt_], w=[xdt], out=xdt[:, :].rearrange("p (h d) -> p h d", h=32), in0=xs3,
            in1=bc(dt_[:, :], [[1, 32], [0, 64]]), op=ALU.mult)
        k.v("pool", "tensor_tensor", r=[xs_, dtte], w=[xw], out=xw[:, :].rearrange("p (h d) -> p h d", h=32), in0=xs3,
            in1=bc(dtte[:, :], [[1, 32], [0, 64]]), op=ALU.mult)

    def quarter(tt, q):
        i = tt % NB
        j = tt % 2
        xs_, bt_, bT_, cT_ = xs_t[i], bt_t[i], bT_t[i], cT_t[i]
        exps, aR, xdt, xw, ycomb = exps2[j], aR2[j], xdt2[j], xw2[j], ycomb2[j]
        SEG = (SEG0, SEG1)
        for half in range(2):
            k.mm(SEG[half][:, :], lmat_b[:, :], aR[:, q * 8 + half * 4:q * 8 + half * 4 + 4, :], True, True,
                 r=[lmat_b, aR], w=[SEG[half]])
        for gi in range(2):
            g = q * 2 + gi
            k.mm(MISC[:, 128 + gi * 128:256 + gi * 128], bT_[:, g, :], cT_[:, g, :], True, True, r=[bT_, cT_], w=[MISC])
        dq = dec[q % 2]
        for half in range(2):
            k.act(dq[:, half * 4:half * 4 + 4, :], SEG[half][:, :].rearrange("p (h l) -> p h l", h=4), AF.Exp,
                  r=[SEG[half]], w=[dq], partial=True)
        sq_ = smk[q % 2]
        k.v("dve", "tensor_tensor", r=[MISC, rmat_f], w=[sq_], out=sq_[:, :, :],
            in0=MISC[:, 128:384].rearrange("p (g l) -> p g l", g=2), in1=bc(rmat_f[:, :], [[0, 2], [1, 128]]), op=ALU.mult)
        mq = MT[q % 2]
        k.v("dve", "tensor_tensor", r=[dq, sq_], w=[mq], out=mq[:, :, :].rearrange("p (g r) l -> p g r l", g=2),
            in0=dq[:, :, :].rearrange("p (g r) l -> p g r l", g=2),
            in1=bc(sq_[:, :, :], [[128, 2], [0, 4], [1, 128]]), op=ALU.mult)
        for gi in range(2):
            g = q * 2 + gi
            k.mm(YOB[:, gi * 256:(gi + 1) * 256], cT_[:, g, :], state_bf[:, g * 256:(g + 1) * 256], True, True,
                 r=[cT_, stbf_b[g]], w=[YOB])
        for gi in range(2):
            g = q * 2 + gi
            k.mm(STB[:, gi * 256:(gi + 1) * 256], bt_[:, g * 128:(g + 1) * 128], xw[:, g * 256:(g + 1) * 256], True, True,
                 r=[bt_, xw], w=[STB])
        for hq in range(8):
            h = q * 8 + hq
            k.mm(YB[:, hq * 64:(hq + 1) * 64], diagD[:, h, :], xs_[:, h * 64:(h + 1) * 64], True, False,
                 r=[diagD, xs_], w=[YB])
            k.mm(YB[:, hq * 64:(hq + 1) * 64], mq[:, hq, :], xdt[:, h * 64:(h + 1) * 64], False, True,
                 r=[mq, xdt], w=[YB])
        sq2 = stq[q % 2]
        sl = slice(q * 512, (q + 1) * 512)
        gb = [st_b[q * 2], st_b[q * 2 + 1]]
        k.v("dve", "tensor_tensor", r=gb + [exps], w=[sq2], out=sq2[:, :].rearrange("p (h d) -> p h d", h=8),
            in0=state[:, sl].rearrange("p (h d) -> p h d", h=8), in1=bc(exps[:, 64 + q * 8:64 + q * 8 + 8], [[1, 8], [0, 64]]),
            op=ALU.mult)
        k.v("dve", "tensor_tensor", r=[STB, sq2], w=gb, out=state[:, sl], in0=STB[:, :], in1=sq2[:, :], op=ALU.add)
        k.act(state_bf[:, sl], state[:, sl], AF.Copy, r=gb, w=[stbf_b[q * 2], stbf_b[q * 2 + 1]])
        tq = tmpq[q % 2]
        k.v("dve", "tensor_tensor", r=[YOB, exps], w=[tq], out=tq[:, :].rearrange("p (h d) -> p h d", h=8),
            in0=YOB[:, :].rearrange("p (h d) -> p h d", h=8), in1=bc(exps[:, q * 8:q * 8 + 8], [[1, 8], [0, 64]]), op=ALU.mult)
        k.v("dve", "tensor_tensor", r=[YB, tq], w=[ycomb], partial=True, out=ycomb[:, q * 512:(q + 1) * 512], in0=YB[:, :],
            in1=tq[:, :], op=ALU.add)

    def epi(tt, part):
        i = tt % NB
        ycomb = ycomb2[tt % 2]
        z_, x_ = z_t[i], x_t[i]
        if part == 0:
            k.v("dve", "tensor_tensor", r=[ycomb, z_], w=[ycomb], out=ycomb[:, :], in0=ycomb[:, :], in1=z_[:, :], op=ALU.mult)
            for g in range(8):
                k.act(junk[:, :], ycomb[:, g * 256:(g + 1) * 256], AF.Square, r=[ycomb], w=[junk, ssq], partial=True,
                      accum_out=ssq[:, g:g + 1])
            k.act(lnv8[:, :], ssq[:, :], AF.Ln, r=[ssq, eps_rms], w=[lnv8], scale=1.0 / 256.0, bias=eps_rms[:, 0:1])
            k.act(rstd8[:, :], lnv8[:, :], AF.Exp, r=[lnv8], w=[rstd8], scale=-0.5)
        elif part == 1:
            k.v("pool", "tensor_tensor", r=[ycomb, rstd8], w=[ycomb], out=ycomb[:, :].rearrange("p (g d) -> p g d", g=8),
                in0=ycomb[:, :].rearrange("p (g d) -> p g d", g=8), in1=bc(rstd8[:, :], [[1, 8], [0, 256]]), op=ALU.mult)
            k.v("pool", "tensor_tensor", r=[ycomb, normw], w=[yn], out=yn[:, :], in0=ycomb[:, :], in1=normw[:, :], op=ALU.mult)
            for g8 in range(2):
                pv = TRB[:, :].bitcast(BF16)
                for j in range(8):
                    kc = g8 * 8 + j
                    k.tr(pv[:, j * 128:(j + 1) * 128], yn[:, kc * 128:(kc + 1) * 128], ident_b[:, :], r=[yn, ident_b], w=[TRB])
                k.act(ynT[:, g8 * 8:(g8 + 1) * 8, :], pv.rearrange("p (j c) -> p j c", j=8), AF.Copy, r=[TRB], w=[ynT], partial=True)
        elif part == 2:
            for dh in range(2):
                for kc in range(16):
                    k.mm(OPB[:, :], ynT[:, kc, :], wout[:, kc, dh * 512:(dh + 1) * 512], kc == 0, kc == 15, r=[ynT, wout], w=[OPB])
                k.v("dve", "scalar_tensor_tensor", r=[x_, OPB], w=[res], partial=True, out=res[:, dh * 512:(dh + 1) * 512],
                    in0=x_[:, dh * 512:(dh + 1) * 512], scalar=ALPHA, in1=OPB[:, :], op0=ALU.mult, op1=ALU.add)
        else:
            ho = hout[tt % 2]
            layer_norm_tile(k, res, g_bc, b_bc, ho, lntmp, eps_ln, "s", eng2="pool")
            op = k.dma("sp", h1_d[tt * 128:(tt + 1) * 128, :], ho[:, :], r=[ho], w=[B_h1[tt]])
            k.out_ops.append(op)

    loads(0)
    loads(1)
    prologue(0)
    def rstep(tile, step):
        route_step(k, c, 0, tile, hout[0], hT_r, (TRB, TRB), OPB, step)

    FUSE_L0 = False

    for tt in range(NT):
        for q in range(4):
            quarter(tt, q)
            if FUSE_L0 and tt >= 2 and q == 3:
                rstep(tt - 2, 2)
            if tt > 0:
                epi(tt - 1, q)
            if FUSE_L0 and tt >= 2 and q == 0:
                rstep(tt - 2, 0)
            if FUSE_L0 and tt >= 2 and q == 2:
                rstep(tt - 2, 1)
            if q == 1 and tt + 1 < NT:
                prologue(tt + 1)
        if tt + 2 < NT:
            loads(tt + 2)
    epi(NT - 1, 0)
    if FUSE_L0:
        rstep(NT - 2, 0)
    epi(NT - 1, 1)
    epi(NT - 1, 2)
    if FUSE_L0:
        rstep(NT - 2, 1)
        rstep(NT - 2, 2)
    epi(NT - 1, 3)
    if FUSE_L0:
        for step in range(3):
            rstep(NT - 1, step)
    st.close()
    k.P.fence()
    k.st = st_outer


def moe_layer_dense(k, c, layer, hin_d, B_hin, hout_d, B_hout, is_final):
    PS = c["PS"]
    ident_f = c["ident_f"]
    eps_ln = c["eps_ln"]
    st_outer = k.st
    st = contextlib.ExitStack()
    k.st = st
    L = f"m{layer}"
    TH = 16
    xT = k.sb(L + "xT", [128, 8, TH * 128], BF16)
    xT_b = [Buf(f"{L}xT{t}") for t in range(TH)]
    acc = k.sb(L + "acc", [128, TH, D], F32)
    acc_b = [Buf(f"{L}acc{t}") for t in range(TH)]
    rw = k.sb(L + "rw", [128, 8, E], F32)
    k.dma("sp", rw[:, :, :], c["router_w"].rearrange("(kc p) e -> p kc e", p=128), w=[rw])
    rb = k.sb(L + "rb", [128, E], F32)
    k.dma("sp", rb[:, :], c["router_bias"].partition_broadcast(128), w=[rb])
    g_bc, b_bc = load_ln(k, c["ln_ffn_g"], c["ln_ffn_b"], layer)
    hin = [k.sb(f"{L}hin{i}", [128, D], F32) for i in range(2)]
    hTf = k.sb(L + "hTf", [128, 8, 128], F32)
    lg = k.sb(L + "lg", [128, TH, E], F32)
    aff = k.sb(L + "aff", [128, TH, E], F32)
    sel = k.sb(L + "sel", [128, TH, E], F32)
    p6 = k.sb(L + "p6", [128, TH * 4, 6], F32)
    gs = k.sb(L + "gs", [128, TH * 4], F32)
    gmax = k.sb(L + "gmax", [128, TH], F32)
    gm = k.sb(L + "gm", [128, TH * 4], F32)
    pen = k.sb(L + "pen", [128, TH * 4], F32)
    selm = k.sb(L + "selm", [128, TH, E], F32)
    m1 = k.sb(L + "m1", [128, TH], F32)
    mk1 = k.sb(L + "mk1", [128, TH, E], F32)
    selm2 = k.sb(L + "selm2", [128, TH, E], F32)
    mk2 = k.sb(L + "mk2", [128, TH, E], F32)
    wsum = k.sb(L + "wsum", [128, TH], F32)
    gates = k.sb(L + "gates", [128, TH, E], F32)
    wg = [k.sb(f"{L}wg{i}", [128, 8, DFF], BF16) for i in range(2)]
    wu = [k.sb(f"{L}wu{i}", [128, 8, DFF], BF16) for i in range(2)]
    wd = [k.sb(f"{L}wd{i}", [128, 4, D], BF16) for i in range(2)]
    hT = [k.sb(f"{L}hT{i}", [128, 4, 512], BF16) for i in range(2)]
    sg = [k.sb(f"{L}sg{i}", [128, 512], BF16) for i in range(2)]
    res = [k.sb(f"{L}res{i}", [128, D], F32) for i in range(2)]
    lntmp = (k.sb(L + "ln_stats", [128, 12], F32), k.sb(L + "ln_mv", [128, 2], F32), k.sb(L + "ln_lnv", [128, 1], F32),
             k.sb(L + "ln_rstd", [128, 1], F32))
    wgv = c["moe_w_gate"][layer].rearrange("e (kc p) f -> e p kc f", p=128)
    wuv = c["moe_w_up"][layer].rearrange("e (kc p) f -> e p kc f", p=128)
    wdv = c["moe_w_down"][layer].rearrange("e (fc p) d -> e p fc d", p=128)
    n_w = 0
    n_g = 0
    n_d = 0
    n_h = 0
    for hf in range(2):
        for t in range(TH):
            tt = hf * TH + t
            hi = hin[t % 2]
            k.dma("sp", hi[:, :], hin_d[tt * 128:(tt + 1) * 128, :], r=[B_hin[tt]], w=[hi], partial=False)
            for half in range(2):
                pb = PS[half]
                for j in range(4):
                    kc = half * 4 + j
                    k.tr(pb[:, j * 128:(j + 1) * 128], hi[:, kc * 128:(kc + 1) * 128], ident_f[:, :], r=[hi, ident_f], w=[pb])
                k.act(hTf[:, half * 4:half * 4 + 4, :], pb[:, :].rearrange("p (j c) -> p j c", j=4), AF.Copy, r=[pb], w=[hTf],
                      partial=True)
            k.v("dve", "tensor_copy", r=[hTf], w=[xT_b[t]], out=xT.t[:, :, t * 128:(t + 1) * 128], in_=hTf[:, :, :])
            pl = PS[2]
            for kc in range(8):
                k.mm(pl[:, 0:E], hTf[:, kc, :], rw[:, kc, :], kc == 0, kc == 7, r=[hTf, rw], w=[pl])
            k.act(lg[:, t, :], pl[:, 0:E], AF.Exp, r=[pl], w=[lg], partial=True, scale=-1.0)
        V = lambda name, **kw: k.v("dve", name, **kw)
        V("tensor_scalar", r=[lg], w=[lg], out=lg[:, :, :], in0=lg[:, :, :], scalar1=1.0, scalar2=None, op0=ALU.add)
        V("reciprocal", r=[lg], w=[aff], out=aff[:, :, :], in_=lg[:, :, :])
        V("tensor_tensor", r=[aff, rb], w=[sel], out=sel[:, :, :], in0=aff[:, :, :], in1=bc(rb[:, :], [[0, TH], [1, E]]), op=ALU.add)
        s4 = sel[:, :, :].rearrange("p t (g i) -> p (t g) i", g=4)
        V("tensor_tensor", r=[sel], w=[p6], partial=True, out=p6[:, :, 0:3], in0=s4[:, :, 0:3], in1=s4[:, :, 1:4], op=ALU.add)
        V("tensor_tensor", r=[sel], w=[p6], partial=True, out=p6[:, :, 3:5], in0=s4[:, :, 0:2], in1=s4[:, :, 2:4], op=ALU.add)
        V("tensor_tensor", r=[sel], w=[p6], partial=True, out=p6[:, :, 5:6], in0=s4[:, :, 0:1], in1=s4[:, :, 3:4], op=ALU.add)
        V("tensor_reduce", r=[p6], w=[gs], out=gs[:, :], in_=p6[:, :, :], axis=AX.X, op=ALU.max)
        V("tensor_reduce", r=[gs], w=[gmax], out=gmax[:, :], in_=gs[:, :].rearrange("p (t g) -> p t g", g=4), axis=AX.X, op=ALU.max)
        V("tensor_tensor", r=[gs, gmax], w=[gm], out=gm[:, :].rearrange("p (t g) -> p t g", g=4),
          in0=gs[:, :].rearrange("p (t g) -> p t g", g=4), in1=bc(gmax[:, :], [[1, TH], [0, 4]]), op=ALU.is_ge)
        V("tensor_scalar", r=[gm], w=[pen], out=pen[:, :], in0=gm[:, :], scalar1=-1.0, scalar2=1.0e4, op0=ALU.add, op1=ALU.mult)
        sm4 = selm[:, :, :].rearrange("p t (g i) -> p (t g) i", g=4)
        V("tensor_tensor", r=[sel, gm], w=[selm], out=sm4, in0=s4, in1=bc(gm[:, :], [[1, TH * 4], [0, 4]]), op=ALU.mult)
        V("tensor_tensor", r=[selm, pen], w=[selm], out=sm4, in0=sm4, in1=bc(pen[:, :], [[1, TH * 4], [0, 4]]), op=ALU.add)
        V("tensor_reduce", r=[selm], w=[m1], out=m1[:, :], in_=selm[:, :, :], axis=AX.X, op=ALU.max)
        V("tensor_tensor", r=[selm, m1], w=[mk1], out=mk1[:, :, :], in0=selm[:, :, :], in1=bc(m1[:, :], [[1, TH], [0, E]]), op=ALU.is_ge)
        V("scalar_tensor_tensor", r=[mk1, selm], w=[selm2], out=selm2[:, :, :], in0=mk1[:, :, :], scalar=-1.0e4, in1=selm[:, :, :],
          op0=ALU.mult, op1=ALU.add)
        V("tensor_reduce", r=[selm2], w=[m1], out=m1[:, :], in_=selm2[:, :, :], axis=AX.X, op=ALU.max)
        V("tensor_tensor", r=[selm2, m1], w=[mk2], out=mk2[:, :, :], in0=selm2[:, :, :], in1=bc(m1[:, :], [[1, TH], [0, E]]), op=ALU.is_ge)
        V("tensor_tensor", r=[mk1, mk2], w=[mk1], out=mk1[:, :, :], in0=mk1[:, :, :], in1=mk2[:, :, :], op=ALU.add)
        V("tensor_tensor", r=[mk1, aff], w=[mk1], out=mk1[:, :, :], in0=mk1[:, :, :], in1=aff[:, :, :], op=ALU.mult)
        V("tensor_reduce", r=[mk1], w=[wsum], out=wsum[:, :], in_=mk1[:, :, :], axis=AX.X, op=ALU.add)
        V("reciprocal", r=[wsum], w=[wsum], out=wsum[:, :], in_=wsum[:, :])
        V("tensor_tensor", r=[mk1, wsum], w=[gates], out=gates[:, :, :], in0=mk1[:, :, :], in1=bc(wsum[:, :], [[1, TH], [0, E]]), op=ALU.mult)
        for e in range(E):
            i = n_w % 2
            n_w += 1
            k.dma("pool", wg[i][:, :, :], wgv[e], w=[wg[i]], partial=False)
            k.dma("pool", wu[i][:, :, :], wuv[e], w=[wu[i]], partial=False)
            k.dma("pool", wd[i][:, :, :], wdv[e], w=[wd[i]], partial=False)
            for tb in range(4):
                hTt = hT[n_h % 2]
                n_h += 1
                xr = xT_b[tb * 4:tb * 4 + 4]
                for fc in range(4):
                    G = PS[n_g % 2]
                    U = PS[2 + n_g % 2]
                    s_ = sg[n_g % 2]
                    n_g += 1
                    for kc in range(8):
                        k.mm(G[:, :], wg[i][:, kc, fc * 128:(fc + 1) * 128], xT.t[:, kc, tb * 512:(tb + 1) * 512], kc == 0, kc == 7,
                             r=[wg[i]] + xr, w=[G])
                    for kc in range(8):
                        k.mm(U[:, :], wu[i][:, kc, fc * 128:(fc + 1) * 128], xT.t[:, kc, tb * 512:(tb + 1) * 512], kc == 0, kc == 7,
                             r=[wu[i]] + xr, w=[U])
                    k.act(s_[:, :], G[:, :], AF.Silu, r=[G], w=[s_])
                    k.v("dve", "tensor_tensor", r=[s_, U], w=[hTt], partial=True, out=hTt[:, fc, :], in0=s_[:, :], in1=U[:, :], op=ALU.mult)
                for tl in range(4):
                    t = tb * 4 + tl
                    for dh in range(2):
                        Dp = PS[4 + n_d % 4]
                        n_d += 1
                        for fc in range(4):
                            k.mm(Dp[:, :], hTt[:, fc, tl * 128:(tl + 1) * 128], wd[i][:, fc, dh * 512:(dh + 1) * 512], fc == 0, fc == 3,
                                 r=[hTt, wd[i]], w=[Dp])
                        gsc = gates[:, t, e:e + 1]
                        if e == 0:
                            k.v("dve", "tensor_scalar", r=[Dp, gates], w=[acc_b[t]], partial=True, out=acc[:, t, dh * 512:(dh + 1) * 512],
                                in0=Dp[:, :], scalar1=gsc, scalar2=None, op0=ALU.mult)
                        else:
                            k.v("dve", "scalar_tensor_tensor", r=[Dp, gates, acc_b[t]], w=[acc_b[t]], partial=True,
                                out=acc[:, t, dh * 512:(dh + 1) * 512], in0=Dp[:, :], scalar=gsc, in1=acc[:, t, dh * 512:(dh + 1) * 512],
                                op0=ALU.mult, op1=ALU.add)
        for t in range(TH):
            tt = hf * TH + t
            hi = hin[t % 2]
            k.dma("sp", hi[:, :], hin_d[tt * 128:(tt + 1) * 128, :], r=[B_hin[tt]], w=[hi], partial=False)
            r_ = res[t % 2]
            k.v("dve", "scalar_tensor_tensor", r=[hi, acc_b[t]], w=[r_], out=r_[:, :], in0=hi[:, :], scalar=ALPHA, in1=acc[:, t, :],
                op0=ALU.mult, op1=ALU.add)
            layer_norm_tile(k, r_, g_bc, b_bc, r_, lntmp, eps_ln, L)
            op = k.dma("sp", hout_d[tt * 128:(tt + 1) * 128, :], r_[:, :], r=[r_], w=[B_hout[tt]] if B_hout is not None else [])
            k.out_ops.append(op)
    st.close()
    k.P.fence()
    k.st = st_outer


def mla_layer(k, c):
    PS = c["PS"]
    ident_b, ones_f, eps_ln, eps_rms = c["ident_b"], c["ones_f"], c["eps_ln"], c["eps_rms"]
    DBK = c["DBK"]
    h2_d, h3_d, attnT_d = c["h2_d"], c["h3_d"], c["attnT_d"]
    B_h2, B_h3, B_attn = c["B_h"][1], c["B_h"][2], c["B_attn"]
    st_outer = k.st
    if c["lg_all"][1] is None:
        c["lg_all"][1] = k.sb("lg_all1", [128, NT, E], F32)
    stA = contextlib.ExitStack()
    k.st = stA
    cT = k.sb("cT", [128, 5, S], BF16)
    cT_b = [Buf(f"cT{t}") for t in range(NT)]
    krT = k.sb("krT", [128, S], BF16)
    krT_b = [Buf(f"krT{t}") for t in range(8)]
    st = contextlib.ExitStack()
    k.st = st
    wdn = k.sb("wdn", [128, 8, 640], BF16)
    k.dma("pool", wdn[:, :, :], c["mla_w_down"].rearrange("(kc p) n -> p kc n", p=128)[:, :, 0:640], w=[wdn], partial=False)
    wkr = k.sb("wkr", [128, 8, 192], BF16)
    k.dma("pool", wkr[:, :, :], c["mla_w_kr"].rearrange("(kc p) n -> p kc n", p=128), w=[wkr], partial=False)
    qkn = k.sb("qkn", [128, 640], F32)
    k.dma("sp", qkn[:, 0:384], c["mla_q_norm"].partition_broadcast(128), w=[qkn])
    k.dma("sp", qkn[:, 384:640], c["mla_kv_norm"].partition_broadcast(128), w=[qkn])
    hin = [k.sb(f"a1hin{i}", [128, D], F32) for i in range(2)]
    hbf = [k.sb(f"a1hbf{i}", [128, D], BF16) for i in range(2)]
    hT = [k.sb(f"a1hT{i}", [128, 8, 512], BF16) for i in range(2)]
    cq = k.sb("a1cq", [128, 640], F32)
    cqn = k.sb("a1cqn", [128, 640], BF16)
    junk = k.sb("a1junk", [128, 384], F32)
    ss2 = k.sb("a1ss2", [128, 2], F32)
    ln2 = k.sb("a1ln2", [128, 2], F32)
    rs2 = k.sb("a1rs2", [128, 2], F32)
    rope = [k.sb(f"a1rope{i}", [128, 2, 512], F32) for i in range(2)]
    rt1 = k.sb("a1rt1", [128, 512], F32)
    rt2 = k.sb("a1rt2", [128, 512], F32)
    rope_v = c["c_rope"]
    cq2 = [cq, k.sb("a1cq_b", [128, 640], F32)]
    hT_bufs_all = {}

    def a1_front(tt):
        tb, tl = tt // 4, tt % 4
        hTb = hT[tb % 2]
        if tl == 0:
            hT_bufs_all[tb] = [Buf(f"hTb{tb}_{i}") for i in range(4)]
        hT_bufs = hT_bufs_all[tb]
        hi = hin[tt % 2]
        hb = hbf[tt % 2]
        k.dma("sp", hi[:, :], h2_d[tt * 128:(tt + 1) * 128, :], r=[B_h2[tt]], w=[hi], partial=False)
        k.v("dve", "tensor_copy", r=[hi], w=[hb], out=hb[:, :], in_=hi[:, :])
        pb = PS[tt % 2]
        pv = pb[:, :].bitcast(BF16)
        for kc in range(8):
            k.tr(pv[:, kc * 128:(kc + 1) * 128], hb[:, kc * 128:(kc + 1) * 128], ident_b[:, :], r=[hb, ident_b], w=[pb])
        k.act(hTb[:, :, tl * 128:(tl + 1) * 128], pv.rearrange("p (k t) -> p k t", k=8), AF.Copy, r=[pb, hT_bufs[tl]], w=[hT_bufs[tl]])
        P0, P1 = PS[2 + tt % 2], PS[4 + tt % 2]
        for kc in range(8):
            k.mm(P0[:, :], hTb[:, kc, tl * 128:(tl + 1) * 128], wdn[:, kc, 0:512], kc == 0, kc == 7, r=[hT_bufs[tl], wdn], w=[P0])
        for kc in range(8):
            k.mm(P1[:, 0:128], hTb[:, kc, tl * 128:(tl + 1) * 128], wdn[:, kc, 512:640], kc == 0, kc == 7, r=[hT_bufs[tl], wdn], w=[P1])
        cq_ = cq2[tt % 2]
        k.act(cq_[:, 0:512], P0[:, :], AF.Copy, r=[P0], w=[cq_], partial=True)
        k.act(cq_[:, 512:640], P1[:, 0:128], AF.Copy, r=[P1], w=[cq_], partial=True)

    def a1_back(tt):
        cq_ = cq2[tt % 2]
        k.act(junk[:, 0:384], cq_[:, 0:384], AF.Square, r=[cq_], w=[junk, ss2], partial=True, accum_out=ss2[:, 0:1])
        k.act(junk[:, 0:256], cq_[:, 384:640], AF.Square, r=[cq_], w=[junk, ss2], partial=True, accum_out=ss2[:, 1:2])
        k.act(ln2[:, 0:1], ss2[:, 0:1], AF.Ln, r=[ss2, eps_rms], w=[ln2], partial=True, scale=1.0 / 384.0, bias=eps_rms[:, 0:1])
        k.act(ln2[:, 1:2], ss2[:, 1:2], AF.Ln, r=[ss2, eps_rms], w=[ln2], partial=True, scale=1.0 / 256.0, bias=eps_rms[:, 0:1])
        k.act(rs2[:, :], ln2[:, :], AF.Exp, r=[ln2], w=[rs2], scale=-0.5)
        k.v("dve", "scalar_tensor_tensor", r=[cq_, rs2, qkn], w=[cqn], partial=True, out=cqn[:, 0:384], in0=cq_[:, 0:384], scalar=rs2[:, 0:1],
            in1=qkn[:, 0:384], op0=ALU.mult, op1=ALU.mult)
        k.v("dve", "scalar_tensor_tensor", r=[cq_, rs2, qkn], w=[cqn], partial=True, out=cqn[:, 384:640], in0=cq_[:, 384:640], scalar=rs2[:, 1:2],
            in1=qkn[:, 384:640], op0=ALU.mult, op1=ALU.mult)
        pb2 = PS[tt % 2]
        pv2 = pb2[:, :].bitcast(BF16)
        for j in range(5):
            k.tr(pv2[:, j * 128:(j + 1) * 128], cqn[:, j * 128:(j + 1) * 128], ident_b[:, :], r=[cqn, ident_b], w=[pb2])
        k.act(cT.t[:, :, tt * 128:(tt + 1) * 128], pv2[:, 0:640].rearrange("p (j t) -> p j t", j=5), AF.Copy, r=[pb2], w=[cT_b[tt]])

    def a1_krope(tb):
        hTb = hT[tb % 2]
        hT_bufs = hT_bufs_all[tb]
        rp = rope[tb % 2]
        k.dma("sp", rp[:, 0, :], rope_v[0][:, tb * 512:(tb + 1) * 512], w=[rp])
        k.dma("sp", rp[:, 1, :], rope_v[1][:, tb * 512:(tb + 1) * 512], w=[rp])
        KA, KB = PS[6], PS[7]
        for kc in range(8):
            k.mm(KA[0:96, :], wkr[:, kc, 0:96], hTb[:, kc, :], kc == 0, kc == 7, r=hT_bufs + [wkr], w=[KA])
        for kc in range(8):
            k.mm(KB[0:96, :], wkr[:, kc, 96:192], hTb[:, kc, :], kc == 0, kc == 7, r=hT_bufs + [wkr], w=[KB])
        k.v("dve", "tensor_tensor", r=[KA, rp], w=[rt1], out=rt1[64:96, :], in0=KA[64:96, :], in1=rp[64:96, 0, :], op=ALU.mult)
        k.v("dve", "tensor_tensor", r=[KB, rp], w=[rt2], out=rt2[64:96, :], in0=KB[64:96, :], in1=rp[64:96, 1, :], op=ALU.mult)
        k.v("dve", "tensor_tensor", r=[rt1, rt2], w=[krT_b[tb]], out=krT[64:96, tb * 512:(tb + 1) * 512], in0=rt1[64:96, :], in1=rt2[64:96, :],
            op=ALU.add)

    a1_front(0)
    for tt in range(NT):
        if tt + 1 < NT:
            a1_front(tt + 1)
        a1_back(tt)
        if tt % 4 == 3:
            a1_krope(tt // 4)
    st.close()
    k.P.fence()
    st = contextlib.ExitStack()
    k.st = st
    HG = 4
    qT = k.sb("qT", [128, HG, S], BF16)
    kT = k.sb("kT", [128, HG, S], BF16)
    Vt = k.sb("Vt", [128, NT, HG, 65], BF16)
    qT_b = [[Buf(f"qT{h}_{b}") for b in range(8)] for h in range(HG)]
    kT_b = [[Buf(f"kT{h}_{b}") for b in range(8)] for h in range(HG)]
    V_b = [Buf(f"V{t}") for t in range(NT)]
    k.v("dve", "memset", w=V_b, ap=Vt[:, :, :, :], constant=1.0)
    wq = [k.sb(f"wq{i}", [128, 3, HG * 96], BF16) for i in range(2)]
    wqs = [k.sb(f"wqs{i}", [128, 3, HG * 96], BF16) for i in range(2)]
    wkn = [k.sb(f"wkn{i}", [128, 2, HG * 64], BF16) for i in range(2)]
    wv = [k.sb(f"wv{i}", [128, 2, HG * 64], BF16) for i in range(2)]
    amask_f = k.sb("amask_f", [128, 4, 512], F32)
    amask = k.sb("amask", [128, 4, 512], BF16)
    k.dma("sp", amask_f[:, :, :], c["c_amask"], w=[amask_f], partial=False)
    k.v("dve", "tensor_copy", r=[amask_f], w=[amask], out=amask[:, :, :], in_=amask_f[:, :, :])
    rope = [k.sb(f"a2rope{i}", [128, 2, 512], F32) for i in range(2)]
    rt1s = [k.sb(f"a2rt1{i}", [128, 512], F32) for i in range(2)]
    rt2s = [k.sb(f"a2rt2{i}", [128, 512], F32) for i in range(2)]
    pt = [k.sb(f"pt{i}", [128, 2, 512], BF16) for i in range(4)]
    rinv2 = [k.sb(f"rinv{i}", [128, 512], F32) for i in range(2)]
    bcs2 = [k.sb(f"bcs{i}", [128, 512], F32) for i in range(2)]
    rn_d = c["rn_d"]
    rn_b = [Buf(f"rn{i}") for i in range(128)]
    at = [k.sb(f"at{i}", [128, 512], BF16) for i in range(2)]
    DB = [k.ps_pair(i) for i in range(4)] if False else None
    wq_v = c["mla_w_uq"].rearrange("(kc p) n -> p kc n", p=128)
    wqs_v = c["mla_w_uq_sw"].rearrange("(kc p) n -> p kc n", p=128)
    wkn_v = c["mla_w_kn"].rearrange("(kc p) n -> p kc n", p=128)
    wv_v = c["mla_w_v"].rearrange("(kc p) n -> p kc n", p=128)
    SCALE = 1.0 / math.sqrt(96.0)
    n_st = 0
    n_ot = 0
    n_at = 0
    for hg in range(MLA_H // HG):
        i = hg % 2
        k.dma("pool", wq[i][:, :, :], wq_v[:, :, hg * HG * 96:(hg + 1) * HG * 96], w=[wq[i]], partial=False)
        k.dma("pool", wqs[i][:, :, :], wqs_v[:, :, hg * HG * 96:(hg + 1) * HG * 96], w=[wqs[i]], partial=False)
        k.dma("pool", wkn[i][:, :, :], wkn_v[:, :, hg * HG * 64:(hg + 1) * HG * 64], w=[wkn[i]], partial=False)
        k.dma("pool", wv[i][:, :, :], wv_v[:, :, hg * HG * 64:(hg + 1) * HG * 64], w=[wv[i]], partial=False)
        for tb in range(8):
            blk = slice(tb * 512, (tb + 1) * 512)
            cr = cT_b[tb * 4:tb * 4 + 4]
            rp = rope[tb % 2]
            k.dma("sp", rp[:, 0, :], rope_v[0][:, blk], w=[rp])
            k.dma("sp", rp[:, 1, :], rope_v[1][:, blk], w=[rp])
            for h in range(HG):
                n_pj = (tb * HG + h) % 2
                QA, QB = PS[n_pj * 2], PS[n_pj * 2 + 1]
                for kc in range(3):
                    k.mm(QA[0:96, :], wq[i][:, kc, h * 96:(h + 1) * 96], cT.t[:, kc, blk], kc == 0, kc == 2, r=[wq[i]] + cr, w=[QA])
                for kc in range(3):
                    k.mm(QB[0:96, :], wqs[i][:, kc, h * 96:(h + 1) * 96], cT.t[:, kc, blk], kc == 0, kc == 2, r=[wqs[i]] + cr, w=[QB])
                KN = PS[6 + n_pj]
                for kc in range(2):
                    k.mm(KN[0:64, :], wkn[i][:, kc, h * 64:(h + 1) * 64], cT.t[:, 3 + kc, blk], kc == 0, kc == 1, r=[wkn[i]] + cr, w=[KN])
                qb = qT_b[h][tb]
                rt1, rt2 = rt1s[n_pj], rt2s[n_pj]
                k.act(qT.t[0:64, h, blk], QA[0:64, :], AF.Copy, r=[QA], w=[qb], partial=True)
                k.v("dve", "tensor_tensor", r=[QA, rp], w=[rt1], out=rt1[64:96, :], in0=QA[64:96, :], in1=rp[64:96, 0, :], op=ALU.mult)
                k.v("dve", "tensor_tensor", r=[QB, rp], w=[rt2], out=rt2[64:96, :], in0=QB[64:96, :], in1=rp[64:96, 1, :], op=ALU.mult)
                k.v("dve", "tensor_tensor", r=[rt1, rt2], w=[qb], partial=True, out=qT.t[64:96, h, blk], in0=rt1[64:96, :], in1=rt2[64:96, :],
                    op=ALU.add)
                kb = kT_b[h][tb]
                k.act(kT.t[0:64, h, blk], KN[0:64, :], AF.Copy, r=[KN], w=[kb], partial=True)
                k.v("pool", "tensor_copy", r=[krT_b[tb]], w=[kb], partial=True, out=kT.t[64:96, h, blk], in_=krT[64:96, blk])
            for tl in range(4):
                tt = tb * 4 + tl
                VP = PS[4 + tl % 2]
                for kc in range(2):
                    k.mm(VP[:, 0:HG * 64], cT.t[:, 3 + kc, tt * 128:(tt + 1) * 128], wv[i][:, kc, :], kc == 0, kc == 1, r=[wv[i], cT_b[tt]], w=[VP])
                k.act(Vt.t[:, tt, :, 0:64], VP[:, 0:HG * 64].rearrange("p (h d) -> p h d", h=HG), AF.Copy, r=[VP], w=[V_b[tt]])
        pairs = []
        for h in range(HG):
            for Qb in range(8):
                nk = 4 * Qb + 4
                for jp in range(nk // 2):
                    pairs.append((h, Qb, jp, nk))
        OTs = {}

        def emit_qk(p):
            nonlocal n_st, n_ot
            h, Qb, jp, nk = p
            if jp == 0:
                OTs[(h, Qb)] = PS[4 + n_ot % 2]
                n_ot += 1
            j0 = jp * 2
            diag = j0 >= 4 * Qb
            q0 = (j0 - 4 * Qb) * 128 if diag else 0
            qs0 = Qb * 512
            pr = n_st % 3
            SA, SB = ((PS[0], PS[1]), (PS[2], PS[3]), (PS[6], PS[7]))[pr]
            ptt = pt[n_st % 4]
            n_st += 1
            for u, SP_ in enumerate((SA, SB)):
                j = j0 + u
                k.mm(SP_[:, q0:512], kT.t[0:96, h, j * 128:(j + 1) * 128], qT.t[0:96, h, qs0 + q0:qs0 + 512], True, True,
                     r=[kT_b[h][j // 4], qT_b[h][Qb]], w=[SP_])
            return (p, q0, diag, SA, SB, ptt, pr)

        def emit_exp(st_):
            p, q0, diag, SA, SB, ptt, pr = st_
            h, Qb, jp, nk = p
            dbk = DBK[pr]
            k.act(ptt[:, :, q0:512], dbk[:, :].rearrange("p (u q) -> p u q", u=2)[:, :, q0:512], AF.Exp, r=[SA, SB], w=[ptt],
                  scale=SCALE)
            if diag:
                jj = jp * 2 - 4 * Qb
                k.v("dve", "tensor_tensor", r=[ptt, amask], w=[ptt], out=ptt[:, :, q0:512], in0=ptt[:, :, q0:512],
                    in1=amask[:, jj:jj + 2, q0:512], op=ALU.mult)

        def emit_pv(st_):
            p, q0, diag, SA, SB, ptt, pr = st_
            h, Qb, jp, nk = p
            OT = OTs[(h, Qb)]
            for u in range(2):
                j = jp * 2 + u
                k.mm(OT[0:65, q0:512], Vt.t[:, j, h, 0:65], ptt[:, u, q0:512], j == 0, j == nk - 1, r=[V_b[j], ptt], w=[OT])

        def emit_norm(p):
            nonlocal n_at
            h, Qb, jp, nk = p
            habs = hg * HG + h
            qs0 = Qb * 512
            OT = OTs[(h, Qb)]
            slot = (habs % 16) * 8 + Qb
            ri = rinv2[n_at % 2]
            bcs = bcs2[n_at % 2]
            a_ = at[n_at % 2]
            n_at += 1
            k.v("dve", "reciprocal", r=[OT], w=[ri], out=ri[64:65, :], in_=OT[64:65, :])
            k.dma("sp", rn_d[slot:slot + 1, :], ri[64:65, :], r=[ri], w=[rn_b[slot]], partial=False)
            k.dma("sp", bcs[0:64, :], rn_d[slot].partition_broadcast(64), r=[rn_b[slot]], w=[bcs], partial=False)
            k.v("dve", "tensor_tensor", r=[OT, bcs], w=[a_], out=a_[0:64, :], in0=OT[0:64, :], in1=bcs[0:64, :], op=ALU.mult)
            k.dma("sp", attnT_d[habs * 64:(habs + 1) * 64, qs0:qs0 + 512], a_[0:64, :], r=[a_], w=[B_attn])

        pend = []
        for p in pairs:
            cur = emit_qk(p)
            emit_exp(cur)
            pend.append(cur)
            if len(pend) > 2:
                d_ = pend.pop(0)
                emit_pv(d_)
                pp = d_[0]
                if pp[2] == pp[3] // 2 - 1:
                    emit_norm(pp)
        while pend:
            d_ = pend.pop(0)
            emit_pv(d_)
            pp = d_[0]
            if pp[2] == pp[3] // 2 - 1:
                emit_norm(pp)
    st.close()
    stA.close()
    k.P.fence()
    st = contextlib.ExitStack()
    k.st = st
    wo = k.sb("wo", [128, 8, D], BF16)
    k.dma("pool", wo[:, :, :], c["mla_w_out"].rearrange("(kc p) d -> p kc d", p=128), w=[wo], partial=False)
    g_bc, b_bc = load_ln(k, c["ln_mix_g"], c["ln_mix_b"], 1)
    NB3 = 4
    aT = [k.sb(f"aT{i}", [128, 8, 128], BF16) for i in range(NB3)]
    hin = [k.sb(f"a3hin{i}", [128, D], F32) for i in range(NB3)]
    res = [k.sb(f"a3res{i}", [128, D], F32) for i in range(2)]
    lntmp = (k.sb("a3ln_stats", [128, 12], F32), k.sb("a3ln_mv", [128, 2], F32), k.sb("a3ln_lnv", [128, 1], F32),
             k.sb("a3ln_rstd", [128, 1], F32))
    at_v = attnT_d.rearrange("(kc p) t -> p kc t", p=128)
    hT_r3 = [k.sb(f"a3hTr{i}", [128, 8, 128], F32) for i in range(2)]

    def a3_issue(tt):
        k.dma("sp", aT[tt % NB3][:, :, :], at_v[:, :, tt * 128:(tt + 1) * 128], r=[B_attn], w=[aT[tt % NB3]], partial=False)
        k.dma("sp", hin[tt % NB3][:, :], h2_d[tt * 128:(tt + 1) * 128, :], r=[B_h2[tt]], w=[hin[tt % NB3]], partial=False)

    a3_issue(0)
    a3_issue(1)
    for tt in range(NT):
        if tt + 2 < NT:
            a3_issue(tt + 2)
        a_ = aT[tt % NB3]
        hi = hin[tt % NB3]
        r_ = res[tt % 2]
        for dh in range(2):
            OP = PS[4 + dh]
            for kc in range(8):
                k.mm(OP[:, :], a_[:, kc, :], wo[:, kc, dh * 512:(dh + 1) * 512], kc == 0, kc == 7, r=[a_, wo], w=[OP])
            k.v("dve", "scalar_tensor_tensor", r=[hi, OP], w=[r_], partial=True, out=r_[:, dh * 512:(dh + 1) * 512],
                in0=hi[:, dh * 512:(dh + 1) * 512], scalar=ALPHA, in1=OP[:, :], op0=ALU.mult, op1=ALU.add)
        if tt > 0:
            route_tile(k, c, 1, tt - 1, res[(tt - 1) % 2], hT_r3[(tt - 1) % 2], (PS[0], PS[1]), PS[6 + (tt - 1) % 2])
        layer_norm_tile(k, r_, g_bc, b_bc, r_, lntmp, eps_ln, "a3", eng2="pool")
        op = k.dma("sp", h3_d[tt * 128:(tt + 1) * 128, :], r_[:, :], r=[r_], w=[B_h3[tt]])
        k.out_ops.append(op)
    route_tile(k, c, 1, NT - 1, res[(NT - 1) % 2], hT_r3[(NT - 1) % 2], (PS[0], PS[1]), PS[6 + (NT - 1) % 2])
    st.close()
    k.P.fence()
    k.st = st_outer


I32 = mybir.dt.int32
IOA = bass.IndirectOffsetOnAxis
N_ITEMS = 32


def moe_layer(k, c, layer, hin_d, B_hin, hout_d, B_hout, is_final):
    PS = c["PS"]
    ident_f, ident_b, ones_f, eps_ln = c["ident_f"], c["ident_b"], c["ones_f"], c["eps_ln"]
    xsl_d, ysl_d, B_xsl, B_ysl = c["xsl_d"], c["ysl_d"], c["B_xsl"], c["B_ysl"]
    st_outer = k.st
    stR = contextlib.ExitStack()
    k.st = stR
    L = f"s{layer}"
    TH = NT
    g1 = k.sb(L + "g1", [128, TH], F32)
    g2 = k.sb(L + "g2", [128, TH], F32)
    pos1_i = k.sb(L + "pos1i", [128, TH], I32)
    pos2_i = k.sb(L + "pos2i", [128, TH], I32)
    widx = k.sb(L + "widx", [128, 2, N_ITEMS], I32)
    NWB = 3
    wg = [k.sb(f"{L}wg{i}", [128, 8, DFF], BF16) for i in range(NWB)]
    wu = [k.sb(f"{L}wu{i}", [128, 8, DFF], BF16) for i in range(NWB)]
    wd = [k.sb(f"{L}wd{i}", [128, 4, D], BF16) for i in range(NWB)]
    wsrc = (c["moe_w_gate"], c["moe_w_up"], c["moe_w_down"])

    def item_loads_w(i, parts=(0, 1, 2)):
        b = i % NWB
        for pi_, (src, dst) in enumerate(zip(wsrc, (wg[b], wu[b], wd[b]))):
            if pi_ not in parts:
                continue
            nh = dst.t.shape[1] // 2
            for hh in range(2):
                def gw(e, src=src, dst=dst, hh=hh, nh=nh, i=i):
                    if getattr(k, "bc_val", None) is None:
                        r_ = e.alloc_register("moe_bc")
                        e.reg_mov(r_, 2 * E * 128 * 2 - 1)
                        k.bc_val = e.snap(r_)
                    return e.indirect_dma_start(
                        out=dst[:, hh * nh:(hh + 1) * nh, :].rearrange("p a b -> p (a b)"), out_offset=None, in_=src,
                        in_offset=IOA(ap=widx[:, hh, i:i + 1], axis=0), bounds_check=k.bc_val, oob_is_err=False)
                k.P.add("pool", gw, _bufs([widx]), _bufs([dst]), dma=True, partial=(hh == 1))
    st = contextlib.ExitStack()
    k.st = st
    rw = k.sb(L + "rw", [128, 8, E], F32)
    k.dma("sp", rw[:, :, :], c["router_w"].rearrange("(kc p) e -> p kc e", p=128), w=[rw])
    rb = k.sb(L + "rb", [128, E], F32)
    k.dma("sp", rb[:, :], c["router_bias"].partition_broadcast(128), w=[rb])
    umat = k.sb(L + "umat", [128, 128], F32)
    k.dma("sp", umat[:, :], c["c_umat"], w=[umat])
    misc = k.sb(L + "misc", [128, 48], F32)
    k.dma("sp", misc[:, :], c["c_misc"], w=[misc])
    hin = [k.sb(f"{L}hin{i}", [128, D], F32) for i in range(3)]
    hTf = [k.sb(f"{L}hTf{i}", [128, 8, 128], F32) for i in range(2)]
    fused = bool(c.get("lg_fused", {}).get(layer))
    lg = c["lg_all"][layer] if fused else k.sb(L + "lg", [128, TH, E], F32)
    for tt in range(0 if fused else TH):
        hi = hin[tt % 3]
        hT_ = hTf[tt % 2]
        k.dma("sp", hi[:, :], hin_d[tt * 128:(tt + 1) * 128, :], r=[B_hin[tt]], w=[hi], partial=False)
        for half in range(2):
            pb = PS[(tt % 2) * 2 + half]
            for j in range(4):
                kc = half * 4 + j
                k.tr(pb[:, j * 128:(j + 1) * 128], hi[:, kc * 128:(kc + 1) * 128], ident_f[:, :], r=[hi, ident_f], w=[pb])
            if half == 0:
                k.act(hT_[:, 0:4, :], pb[:, :].rearrange("p (j c) -> p j c", j=4), AF.Copy, r=[pb], w=[hT_], partial=True)
            else:
                k.v("dve", "tensor_copy", r=[pb], w=[hT_], partial=True, out=hT_[:, 4:8, :], in_=pb[:, :].rearrange("p (j c) -> p j c", j=4))
        pl = PS[4 + tt % 2]
        for kc in range(8):
            k.mm(pl[:, 0:E], hT_[:, kc, :], rw[:, kc, :], kc == 0, kc == 7, r=[hT_, rw], w=[pl])
        k.act(lg[:, tt, :], pl[:, 0:E], AF.Exp, r=[pl], w=[lg], partial=True, scale=-1.0)
    T3 = lambda nm: k.sb(L + nm, [128, TH, E], F32)
    aff, sel, selm, mk1, selm2, mk2, w12, posall = (T3(n) for n in ("aff", "sel", "selm", "mk1", "selm2", "mk2", "w12", "posall"))
    p6 = k.sb(L + "p6", [128, TH * 4, 6], F32)
    gs = k.sb(L + "gs", [128, TH * 4], F32)
    gmax = k.sb(L + "gmax", [128, TH], F32)
    gm = k.sb(L + "gm", [128, TH * 4], F32)
    pen = k.sb(L + "pen", [128, TH * 4], F32)
    m1 = k.sb(L + "m1", [128, TH], F32)
    wsum = k.sb(L + "wsum", [128, TH], F32)
    V = lambda name, **kw: k.v("dve", name, **kw)
    V("tensor_scalar", r=[lg], w=[lg], out=lg[:, :, :], in0=lg[:, :, :], scalar1=1.0, scalar2=None, op0=ALU.add)
    V("reciprocal", r=[lg], w=[aff], out=aff[:, :, :], in_=lg[:, :, :])
    V("tensor_tensor", r=[aff, rb], w=[sel], out=sel[:, :, :], in0=aff[:, :, :], in1=bc(rb[:, :], [[0, TH], [1, E]]), op=ALU.add)
    s4 = sel[:, :, :].rearrange("p t (g i) -> p (t g) i", g=4)
    V("tensor_tensor", r=[sel], w=[p6], partial=True, out=p6[:, :, 0:3], in0=s4[:, :, 0:3], in1=s4[:, :, 1:4], op=ALU.add)
    V("tensor_tensor", r=[sel], w=[p6], partial=True, out=p6[:, :, 3:5], in0=s4[:, :, 0:2], in1=s4[:, :, 2:4], op=ALU.add)
    V("tensor_tensor", r=[sel], w=[p6], partial=True, out=p6[:, :, 5:6], in0=s4[:, :, 0:1], in1=s4[:, :, 3:4], op=ALU.add)
    V("tensor_reduce", r=[p6], w=[gs], out=gs[:, :], in_=p6[:, :, :], axis=AX.X, op=ALU.max)
    V("tensor_reduce", r=[gs], w=[gmax], out=gmax[:, :], in_=gs[:, :].rearrange("p (t g) -> p t g", g=4), axis=AX.X, op=ALU.max)
    V("tensor_tensor", r=[gs, gmax], w=[gm], out=gm[:, :].rearrange("p (t g) -> p t g", g=4),
      in0=gs[:, :].rearrange("p (t g) -> p t g", g=4), in1=bc(gmax[:, :], [[1, TH], [0, 4]]), op=ALU.is_ge)
    V("tensor_scalar", r=[gm], w=[pen], out=pen[:, :], in0=gm[:, :], scalar1=-1.0, scalar2=1.0e4, op0=ALU.add, op1=ALU.mult)
    sm4 = selm[:, :, :].rearrange("p t (g i) -> p (t g) i", g=4)
    V("tensor_tensor", r=[sel, gm], w=[selm], out=sm4, in0=s4, in1=bc(gm[:, :], [[1, TH * 4], [0, 4]]), op=ALU.mult)
    V("tensor_tensor", r=[selm, pen], w=[selm], out=sm4, in0=sm4, in1=bc(pen[:, :], [[1, TH * 4], [0, 4]]), op=ALU.add)
    V("tensor_reduce", r=[selm], w=[m1], out=m1[:, :], in_=selm[:, :, :], axis=AX.X, op=ALU.max)
    V("tensor_tensor", r=[selm, m1], w=[mk1], out=mk1[:, :, :], in0=selm[:, :, :], in1=bc(m1[:, :], [[1, TH], [0, E]]), op=ALU.is_ge)
    V("scalar_tensor_tensor", r=[mk1, selm], w=[selm2], out=selm2[:, :, :], in0=mk1[:, :, :], scalar=-1.0e4, in1=selm[:, :, :],
      op0=ALU.mult, op1=ALU.add)
    V("tensor_reduce", r=[selm2], w=[m1], out=m1[:, :], in_=selm2[:, :, :], axis=AX.X, op=ALU.max)
    V("tensor_tensor", r=[selm2, m1], w=[mk2], out=mk2[:, :, :], in0=selm2[:, :, :], in1=bc(m1[:, :], [[1, TH], [0, E]]), op=ALU.is_ge)
    V("tensor_tensor", r=[mk1, aff], w=[w12], out=w12[:, :, :], in0=mk1[:, :, :], in1=aff[:, :, :], op=ALU.mult)
    V("tensor_reduce", r=[w12], w=[g1], out=g1[:, :], in_=w12[:, :, :], axis=AX.X, op=ALU.add)
    V("tensor_tensor", r=[mk2, aff], w=[w12], out=w12[:, :, :], in0=mk2[:, :, :], in1=aff[:, :, :], op=ALU.mult)
    V("tensor_reduce", r=[w12], w=[g2], out=g2[:, :], in_=w12[:, :, :], axis=AX.X, op=ALU.add)
    V("tensor_tensor", r=[g1, g2], w=[wsum], out=wsum[:, :], in0=g1[:, :], in1=g2[:, :], op=ALU.add)
    V("reciprocal", r=[wsum], w=[wsum], out=wsum[:, :], in_=wsum[:, :])
    V("tensor_tensor", r=[g1, wsum], w=[g1], out=g1[:, :], in0=g1[:, :], in1=wsum[:, :], op=ALU.mult)
    V("tensor_tensor", r=[g2, wsum], w=[g2], out=g2[:, :], in0=g2[:, :], in1=wsum[:, :], op=ALU.mult)
    mk = w12
    V("tensor_tensor", r=[mk1, mk2], w=[mk], out=mk[:, :, :], in0=mk1[:, :, :], in1=mk2[:, :, :], op=ALU.add)
    PRE, TOT = PS[6], PS[7]
    mk2d = mk[:, :, :].rearrange("p t e -> p (t e)")
    k.mm(PRE[:, :], umat[:, :], mk2d, True, True, r=[umat, mk], w=[PRE])
    k.mm(TOT[:, :], ones_f[:, :], mk2d, True, True, r=[ones_f, mk], w=[TOT])
    tot = sel
    base = selm
    V("tensor_copy", r=[TOT], w=[tot], out=tot[:, :, :], in_=TOT[:, :].rearrange("p (t e) -> p t e", e=E))
    base_b = [Buf(f"{L}base{t}") for t in range(TH)]
    V("memset", w=[base_b[0]], ap=base[:, 0, :], constant=0.0)
    for t in range(1, TH):
        V("tensor_tensor", r=[base_b[t - 1], tot], w=[base_b[t]], out=base[:, t, :], in0=base[:, t - 1, :], in1=tot[:, t - 1, :], op=ALU.add)
    sm16 = k.sb(L + "sm16", [128, 6, E], F32)
    cmp8 = k.sb(L + "cmp8", [128, E, 8], F32)
    V("tensor_tensor", r=[base_b[TH - 1], tot], w=[sm16], out=sm16[:, 0, :], in0=base[:, TH - 1, :], in1=tot[:, TH - 1, :], op=ALU.add)
    V("tensor_tensor", r=[sm16, misc], w=[cmp8], out=cmp8[:, :, :], in0=bc(sm16[:, 0, :], [[1, E], [0, 8]]),
      in1=bc(misc[:, 0:8], [[0, E], [1, 8]]), op=ALU.is_gt)
    V("tensor_reduce", r=[cmp8], w=[sm16], out=sm16[:, 1, :], in_=cmp8[:, :, :], axis=AX.X, op=ALU.add)
    V("tensor_scalar", r=[sm16], w=[sm16], out=sm16[:, 2, :], in0=sm16[:, 1, :], scalar1=512.0, scalar2=None, op0=ALU.mult)
    V("memset", w=[sm16], ap=sm16[:, 3, 0:1], constant=0.0)
    for e in range(1, E):
        V("tensor_tensor", r=[sm16], w=[sm16], out=sm16[:, 3, e:e + 1], in0=sm16[:, 3, e - 1:e], in1=sm16[:, 2, e - 1:e], op=ALU.add)
    V("tensor_tensor", r=[sm16], w=[sm16], out=sm16[:, 4, :], in0=sm16[:, 3, :], in1=sm16[:, 2, :], op=ALU.add)
    V("tensor_tensor", r=[PRE] + base_b, w=[posall], out=posall[:, :, :], in0=PRE[:, :].rearrange("p (t e) -> p t e", e=E), in1=base[:, :, :],
      op=ALU.add)
    V("tensor_tensor", r=[posall, sm16], w=[posall], out=posall[:, :, :], in0=posall[:, :, :], in1=bc(sm16[:, 3, :], [[0, TH], [1, E]]),
      op=ALU.add)
    posf = k.sb(L + "posf", [128, 2, TH], F32)
    V("tensor_tensor", r=[mk1, posall], w=[mk1], out=mk1[:, :, :], in0=mk1[:, :, :], in1=posall[:, :, :], op=ALU.mult)
    V("tensor_reduce", r=[mk1], w=[posf], out=posf[:, 0, :], in_=mk1[:, :, :], axis=AX.X, op=ALU.add)
    V("tensor_tensor", r=[mk2, posall], w=[mk2], out=mk2[:, :, :], in0=mk2[:, :, :], in1=posall[:, :, :], op=ALU.mult)
    V("tensor_reduce", r=[mk2], w=[posf], out=posf[:, 1, :], in_=mk2[:, :, :], axis=AX.X, op=ALU.add)
    V("tensor_copy", r=[posf], w=[pos1_i], out=pos1_i[:, :], in_=posf[:, 0, :])
    V("tensor_copy", r=[posf], w=[pos2_i], out=pos2_i[:, :], in_=posf[:, 1, :])
    icmp = k.sb(L + "icmp", [128, N_ITEMS, E], F32)
    ei = k.sb(L + "ei", [128, N_ITEMS], F32)
    V("tensor_tensor", r=[sm16, misc], w=[icmp], out=icmp[:, :, :], in0=bc(sm16[:, 4, :], [[0, N_ITEMS], [1, E]]),
      in1=bc(misc[:, 8:40], [[1, N_ITEMS], [0, E]]), op=ALU.is_le)
    V("tensor_reduce", r=[icmp], w=[ei], out=ei[:, :], in_=icmp[:, :, :], axis=AX.X, op=ALU.add)
    oob = k.sb(L + "oob", [128, N_ITEMS], F32)
    V("tensor_scalar", r=[ei], w=[oob], out=oob[:, :], in0=ei[:, :], scalar1=float(E), scalar2=4.0e6, op0=ALU.is_ge, op1=ALU.mult)
    V("tensor_scalar", r=[ei], w=[ei], out=ei[:, :], in0=ei[:, :], scalar1=15.0, scalar2=float(layer * E), op0=ALU.min, op1=ALU.add)
    V("tensor_scalar", r=[ei], w=[ei], out=ei[:, :], in0=ei[:, :], scalar1=256.0, scalar2=None, op0=ALU.mult)
    V("tensor_tensor", r=[ei, oob], w=[ei], out=ei[:, :], in0=ei[:, :], in1=oob[:, :], op=ALU.add)
    V("scalar_tensor_tensor", r=[misc, ei], w=[ei], out=ei[:, :], in0=bc(misc[:, 40:41], [[0, N_ITEMS]]), scalar=2.0, in1=ei[:, :],
      op0=ALU.mult, op1=ALU.add)
    V("tensor_copy", r=[ei], w=[widx], partial=True, out=widx[:, 0, :], in_=ei[:, :])
    V("tensor_scalar", r=[ei], w=[ei], out=ei[:, :], in0=ei[:, :], scalar1=1.0, scalar2=None, op0=ALU.add)
    V("tensor_copy", r=[ei], w=[widx], partial=True, out=widx[:, 1, :], in_=ei[:, :])
    stage = c.get("moe_stage", "C")
    if "dbg_outs" in c:
        do = c["dbg_outs"]
        for nm, t_ in (("d_pos1", pos1_i), ("d_pos2", pos2_i), ("d_g1", g1), ("d_g2", g2)):
            k.out_ops.append(k.dma("sp", do[nm], t_[:, :], r=[t_]))
        k.out_ops.append(k.dma("sp", do["d_widx"], widx[:, :, :], r=[widx]))
    if stage == "P":
        st.close(); stR.close(); k.P.fence(); k.st = st_outer
        return
    xb = [k.sb(f"{L}xb{i}", [128, D], BF16) for i in range(3)]
    item_loads_w(0)
    item_loads_w(1)
    for tt in range(TH):
        x_ = xb[tt % 3]
        hi = hin[tt % 3]
        k.dma("sp", hi[:, :], hin_d[tt * 128:(tt + 1) * 128, :], r=[B_hin[tt]], w=[hi], partial=False)
        k.act(x_[:, :], hi[:, :], AF.Copy, r=[hi], w=[x_])
        for pi_ in (pos1_i, pos2_i):
            k.P.add("pool", (lambda e, x_=x_, pi_=pi_, tt=tt: e.indirect_dma_start(
                out=xsl_d, out_offset=IOA(ap=pi_[:, tt:tt + 1], axis=0), in_=x_[:, :], in_offset=None)),
                _bufs([x_, pi_]), [B_xsl], dma=True, partial=True)
    st.close()
    k.P.fence()
    if stage == "S":
        stR.close(); k.st = st_outer
        return
    st = contextlib.ExitStack()
    k.st = st
    xi = [k.sb(f"{L}xi{i}", [128, 4, D], BF16) for i in range(2)]
    xTi = [k.sb(f"{L}xTi{i}", [128, 8, 512], BF16) for i in range(2)]
    hT = [k.sb(f"{L}hT{i}", [128, 4, 512], BF16) for i in range(2)]
    sg = [k.sb(f"{L}sg{i}", [128, 512], BF16) for i in range(2)]
    yi = [k.sb(f"{L}yi{i}", [128, 4, D], F32) for i in range(2)]
    xsl_v = xsl_d.rearrange("(i s p) d -> i p s d", p=128, s=4)
    ysl_v = ysl_d.rearrange("(i s p) d -> i p s d", p=128, s=4)
    n_g = 0
    n_d = 0

    xi = xi + [k.sb(f"{L}xi2", [128, 4, D], BF16)]

    def x_load(i):
        k.dma("sp", xi[i % 3][:, :, :], xsl_v[i], r=[B_xsl], w=[xi[i % 3]], partial=False)

    def stage_T(i):
        xT_ = xTi[i % 2]
        x_ = xi[i % 3]
        for s_ in range(4):
            pb = PS[6 + s_ % 2]
            pv = pb[:, :].bitcast(BF16)
            for kc in range(8):
                k.tr(pv[:, kc * 128:(kc + 1) * 128], x_[:, s_, kc * 128:(kc + 1) * 128], ident_b[:, :], r=[x_, ident_b], w=[pb])
            if s_ % 2:
                k.act(xT_[:, :, s_ * 128:(s_ + 1) * 128], pv.rearrange("p (k t) -> p k t", k=8), AF.Copy, r=[pb], w=[xT_], partial=True)
            else:
                k.v("dve", "tensor_copy", r=[pb], w=[xT_], partial=True, out=xT_[:, :, s_ * 128:(s_ + 1) * 128],
                    in_=pv.rearrange("p (k t) -> p k t", k=8))

    def stage_GU(i):
        nonlocal n_g
        b = i % 2
        wb = i % NWB
        xT_ = xTi[b]
        hTt = hT[b]
        for fc in range(4):
            G = PS[n_g % 2]
            U = PS[2 + n_g % 2]
            s2 = sg[n_g % 2]
            n_g += 1
            for kc in range(8):
                k.mm(G[:, :], wg[wb][:, kc, fc * 128:(fc + 1) * 128], xT_[:, kc, :], kc == 0, kc == 7, r=[wg[wb], xT_], w=[G])
            for kc in range(8):
                k.mm(U[:, :], wu[wb][:, kc, fc * 128:(fc + 1) * 128], xT_[:, kc, :], kc == 0, kc == 7, r=[wu[wb], xT_], w=[U])
            k.act(s2[:, :], G[:, :], AF.Silu, r=[G], w=[s2])
            k.v("dve", "tensor_tensor", r=[s2, U], w=[hTt], partial=True, out=hTt[:, fc, :], in0=s2[:, :], in1=U[:, :], op=ALU.mult)

    def stage_D(i):
        nonlocal n_d
        b = i % 2
        wb = i % NWB
        hTt = hT[b]
        y_ = yi[b]
        for tl in range(4):
            for dh in range(2):
                Dp = PS[4 + n_d % 2]
                n_d += 1
                for fc in range(4):
                    k.mm(Dp[:, :], hTt[:, fc, tl * 128:(tl + 1) * 128], wd[wb][:, fc, dh * 512:(dh + 1) * 512], fc == 0, fc == 3,
                         r=[hTt, wd[wb]], w=[Dp])
                if n_d % 2:
                    k.act(y_[:, tl, dh * 512:(dh + 1) * 512], Dp[:, :], AF.Copy, r=[Dp], w=[y_], partial=True)
                else:
                    k.v("dve", "tensor_copy", r=[Dp], w=[y_], partial=True, out=y_[:, tl, dh * 512:(dh + 1) * 512], in_=Dp[:, :])
        k.dma("sp", ysl_v[i], y_[:, :, :], r=[y_], w=[B_ysl])

    for i in range(3):
        x_load(i)
    stage_T(0)
    stage_T(1)
    stage_GU(0)
    for i in range(N_ITEMS):
        if i + 2 < N_ITEMS:
            item_loads_w(i + 2, parts=(0, 1))
        if 2 <= i + 1 < N_ITEMS:
            item_loads_w(i + 1, parts=(2,))
        if i + 3 < N_ITEMS:
            x_load(i + 3)
        if i + 2 < N_ITEMS:
            stage_T(i + 2)
        if i + 1 < N_ITEMS:
            stage_GU(i + 1)
        stage_D(i)
    st.close()
    k.P.fence()
    if stage == "X":
        stR.close(); k.st = st_outer
        return
    st = contextlib.ExitStack()
    k.st = st
    g_bc, b_bc = load_ln(k, c["ln_ffn_g"], c["ln_ffn_b"], layer)
    NBC = 4
    hin = [k.sb(f"{L}chin{i}", [128, D], F32) for i in range(NBC)]
    y1 = [k.sb(f"{L}y1{i}", [128, D], F32) for i in range(NBC)]
    y2 = [k.sb(f"{L}y2{i}", [128, D], F32) for i in range(NBC)]
    res = [k.sb(f"{L}res{i}", [128, D], F32) for i in range(2)]
    lntmp = (k.sb(L + "ln_stats", [128, 12], F32), k.sb(L + "ln_mv", [128, 2], F32), k.sb(L + "ln_lnv", [128, 1], F32),
             k.sb(L + "ln_rstd", [128, 1], F32))

    def c_issue(tt):
        b = tt % NBC
        k.dma("sp", hin[b][:, :], hin_d[tt * 128:(tt + 1) * 128, :], r=[B_hin[tt]], w=[hin[b]], partial=False)
        for y_, pi_ in ((y1[b], pos1_i), (y2[b], pos2_i)):
            k.P.add("pool", (lambda e, y_=y_, pi_=pi_, tt=tt: e.indirect_dma_start(
                out=y_[:, :], out_offset=None, in_=ysl_d, in_offset=IOA(ap=pi_[:, tt:tt + 1], axis=0))),
                _bufs([pi_]) + [B_ysl], _bufs([y_]), dma=True, partial=False)

    c_issue(0)
    c_issue(1)
    for tt in range(TH):
        b = tt % NBC
        if tt + 2 < TH:
            c_issue(tt + 2)
        hi = hin[b]
        r_ = res[tt % 2]
        k.v("dve", "tensor_scalar", r=[y1[b], g1], w=[y1[b]], out=y1[b][:, :], in0=y1[b][:, :], scalar1=g1[:, tt:tt + 1], scalar2=None, op0=ALU.mult)
        k.v("dve", "scalar_tensor_tensor", r=[y2[b], g2, y1[b]], w=[y1[b]], out=y1[b][:, :], in0=y2[b][:, :], scalar=g2[:, tt:tt + 1], in1=y1[b][:, :],
            op0=ALU.mult, op1=ALU.add)
        k.v("dve", "scalar_tensor_tensor", r=[hi, y1[b]], w=[r_], out=r_[:, :], in0=hi[:, :], scalar=ALPHA, in1=y1[b][:, :],
            op0=ALU.mult, op1=ALU.add)
        layer_norm_tile(k, r_, g_bc, b_bc, r_, lntmp, eps_ln, L, eng2="dve")
        op = k.dma("sp", hout_d[tt * 128:(tt + 1) * 128, :], r_[:, :], r=[r_], w=[B_hout[tt]] if B_hout is not None else [])
        k.out_ops.append(op)
    st.close()
    stR.close()
    k.P.fence()
    k.st = st_outer


def _consts():
    j = np.arange(128)
    rmat = (j[:, None] <= j[None, :]).astype(np.float32)
    lmat = (j[:, None] > j[None, :]).astype(np.float32)
    inv = (10000.0 ** (-np.arange(0, 32, 2, dtype=np.float32) / 32)).astype(np.float32)
    ang = np.arange(S, dtype=np.float32)[:, None] * inv[None, :]
    cos, sin = np.cos(ang).astype(np.float32), np.sin(ang).astype(np.float32)
    rope = np.zeros((2, 128, S), np.float32)
    rope[0, 64:80] = cos.T
    rope[0, 80:96] = cos.T
    rope[1, 64:80] = -sin.T
    rope[1, 80:96] = sin.T
    kk = np.arange(128)[:, None, None]
    jj = np.arange(4)[None, :, None]
    qq = np.arange(512)[None, None, :]
    amask = ((qq // 64) >= ((jj * 128 + kk) // 64)).astype(np.float32)
    umat = (j[:, None] < j[None, :]).astype(np.float32)
    misc = np.zeros((128, 48), np.float32)
    misc[:, 0:8] = 512.0 * np.arange(8)[None, :]
    misc[:, 8:40] = 512.0 * np.arange(32)[None, :]
    misc[:, 40] = np.arange(128)
    return {"c_ident": np.eye(128, dtype=np.float32), "c_rmat": rmat, "c_lmat": lmat, "c_rope": rope,
            "c_amask": np.ascontiguousarray(amask), "c_umat": umat, "c_misc": misc}


def prep_shared(inp):
    f = lambda a: np.ascontiguousarray(np.asarray(a, dtype=np.float32))
    sh = {}
    sh["ssd_w_in"] = f(inp["ssd_w_in"][0])
    cw = np.asarray(inp["ssd_conv_w"][0], np.float32)
    sh["ssd_conv_w"] = f(cw.reshape(4, 32, 128).transpose(2, 1, 0).reshape(128, 128))
    sh["ssd_conv_b"] = f(np.asarray(inp["ssd_conv_b"][0], np.float32).reshape(32, 128).T)
    for n in ("ssd_dt_bias", "ssd_a_log", "ssd_d", "ssd_norm_w", "ssd_w_out", "mla_w_down", "mla_q_norm", "mla_w_uq",
              "mla_kv_norm", "mla_w_ukv", "mla_w_out"):
        sh[n] = f(inp[n][0])
    wd = sh["mla_w_down"]
    sh["mla_w_kr"] = f(np.concatenate([wd[:, 0:64], wd[:, 640:672], wd[:, 0:64], wd[:, 656:672], wd[:, 640:656]], axis=1))
    wkv = sh.pop("mla_w_ukv").reshape(256, 16, 128)
    sh["mla_w_kn"] = f(wkv[:, :, 0:64].reshape(256, 1024))
    sh["mla_w_v"] = f(wkv[:, :, 64:128].reshape(256, 1024))
    wq = sh["mla_w_uq"].reshape(384, 16, 96)
    sh["mla_w_uq_sw"] = f(np.concatenate([wq[:, :, 0:64], wq[:, :, 80:96], wq[:, :, 64:80]], axis=2).reshape(384, 1536))
    for n in ("router_w", "router_bias", "ln_mix_g", "ln_mix_b", "ln_ffn_g", "ln_ffn_b"):
        sh[n] = f(inp[n])
    for n in ("moe_w_gate", "moe_w_up"):
        w = np.asarray(inp[n], np.float32).reshape(2, E, 8, 128, DFF)
        sh[n] = f(w.transpose(0, 1, 3, 2, 4).reshape(2 * E * 128 * 2, 2048))
    w = np.asarray(inp["moe_w_down"], np.float32).reshape(2, E, 4, 128, D)
    sh["moe_w_down"] = f(w.transpose(0, 1, 3, 2, 4).reshape(2 * E * 128 * 2, 2048))
    sh.update(_consts())
    return sh


_NC_CACHE = {}


def kernel(**inputs):
    sh = prep_shared(inputs)
    x = np.asarray(inputs["x"], np.float32)
    if "nc" not in _NC_CACHE:
        _NC_CACHE["nc"] = build_program()
    nc = _NC_CACHE["nc"]
    in_maps = []
    for b in range(8):
        m = dict(sh)
        m["x"] = np.ascontiguousarray(x[b])
        in_maps.append(m)
    res = run_bass_kernel_spmd(nc, in_maps, core_ids=list(range(8)))
    return np.stack([np.asarray(r["out"], np.float32) for r in res.results], axis=0)
```
